# Optimizing a Trainium2 kernel written in Bass

```python
import jax, jax.numpy as jnp
from jax import lax
import numpy as np

D_MODEL = 2048
BATCH = 4
SEQ = 4096
DEPTH = 1

SB_HEADS = 8
SB_HEAD_DIM = 128
SB_WIDTH = SB_HEADS * SB_HEAD_DIM
MLA_HEADS = 8
MLA_NOPE_DIM = 128
MLA_ROPE_DIM = 64
MLA_V_DIM = 128
MLA_QK_DIM = MLA_NOPE_DIM + MLA_ROPE_DIM
Q_LORA_RANK = 512
KV_LORA_RANK = 512
MLA_WIDTH = MLA_HEADS * MLA_V_DIM
ROPE_THETA = 10000.0
MIX_WIDTH = SB_WIDTH + MLA_WIDTH
IN_SPLITS = [SB_WIDTH, 2 * SB_WIDTH, 3 * SB_WIDTH,
             3 * SB_WIDTH + Q_LORA_RANK, 3 * SB_WIDTH + Q_LORA_RANK + KV_LORA_RANK]
IN_WIDTH = 3 * SB_WIDTH + Q_LORA_RANK + KV_LORA_RANK + MLA_ROPE_DIM
Q_BLOCK = 128
N_EXPERTS = 32
TOP_K = 4
D_EXPERT = D_MODEL
SWIGLU_ALPHA = 1.702
SWIGLU_LIMIT = 7.0
ROW_BLOCK = 128
NORM_EPS = 1e-6
N_MOD = 6

kernel_name = "hybrid_sb_mla_moe_adaln_layer"


def rmsnorm(x, gain):
    x32 = x.astype(jnp.float32)
    y = x32 * lax.rsqrt(jnp.mean(x32 * x32, axis=-1, keepdims=True) + NORM_EPS)
    return (y * gain.astype(jnp.float32)).astype(x.dtype)


def modulate(h, shift, scale):
    return h * (1.0 + scale[:, None, :]) + shift[:, None, :]


def rope_tables(positions, rot_dim):
    half = rot_dim // 2
    inv_freq = 1.0 / (ROPE_THETA ** (jnp.arange(half, dtype=jnp.float32) * 2.0 / rot_dim))
    ang = positions.astype(jnp.float32)[..., None] * inv_freq
    return jnp.cos(ang)[:, :, None, :], jnp.sin(ang)[:, :, None, :]


def apply_rope(x, cos, sin):
    x32 = x.astype(jnp.float32)
    x1, x2 = jnp.split(x32, 2, axis=-1)
    return jnp.concatenate([x1 * cos - x2 * sin, x2 * cos + x1 * sin], axis=-1).astype(x.dtype)


def to_blocks(t):
    b, s = t.shape[:2]
    return t.reshape((b, s // Q_BLOCK, Q_BLOCK) + t.shape[2:]).swapaxes(0, 1)


def from_blocks(t):
    nb, b = t.shape[:2]
    return t.swapaxes(0, 1).reshape((b, nb * Q_BLOCK) + t.shape[3:])


def stick_breaking_attention(q, k, v):
    s_len, dh = q.shape[1], q.shape[-1]
    k32 = k.astype(jnp.float32)
    key_idx = jnp.arange(s_len)
    n_blocks = s_len // Q_BLOCK

    def block(args):
        q_blk, i = args
        z = jnp.einsum("bqhd,bkhd->bhqk", q_blk.astype(jnp.float32), k32) * (dh ** -0.5)
        q_idx = i * Q_BLOCK + jnp.arange(Q_BLOCK)
        mask = key_idx[None, :] < q_idx[:, None]
        log_keep = jnp.where(mask, jax.nn.log_sigmoid(-z), 0.0)
        shifted = jnp.concatenate([log_keep[..., 1:], jnp.zeros_like(log_keep[..., :1])], axis=-1)
        later = lax.cumsum(shifted, axis=3, reverse=True)
        a = jnp.where(mask, jnp.exp(jax.nn.log_sigmoid(z) + later), 0.0)
        return jnp.einsum("bhqk,bkhd->bqhd", a.astype(v.dtype), v)

    out = lax.map(block, (to_blocks(q), jnp.arange(n_blocks)))
    return from_blocks(out)


def mla_attention(q_nope, q_rope, k_nope, k_rope, v):
    s_len = q_nope.shape[1]
    kn32 = k_nope.astype(jnp.float32)
    kr32 = k_rope.astype(jnp.float32)
    key_idx = jnp.arange(s_len)
    n_blocks = s_len // Q_BLOCK
    scale = MLA_QK_DIM ** -0.5

    def block(args):
        qn, qr, i = args
        sc = (jnp.einsum("bqhd,bkhd->bhqk", qn.astype(jnp.float32), kn32)
              + jnp.einsum("bqhr,bkr->bhqk", qr.astype(jnp.float32), kr32)) * scale
        q_idx = i * Q_BLOCK + jnp.arange(Q_BLOCK)
        mask = key_idx[None, :] <= q_idx[:, None]
        p = jax.nn.softmax(jnp.where(mask, sc, -jnp.inf), axis=-1)
        return jnp.einsum("bhqk,bkhd->bqhd", p.astype(v.dtype), v)

    out = lax.map(block, (to_blocks(q_nope), to_blocks(q_rope), jnp.arange(n_blocks)))
    return from_blocks(out)


def clamped_swiglu(u):
    glu = jnp.minimum(u[..., ::2], SWIGLU_LIMIT)
    lin = jnp.clip(u[..., 1::2], -SWIGLU_LIMIT, SWIGLU_LIMIT)
    return glu * jax.nn.sigmoid(SWIGLU_ALPHA * glu) * (lin + 1.0)


def moe_ffn(h, w_router, b_router, w1, b1, w2, b2):
    b, s, d = h.shape
    n = b * s
    hf = h.reshape(n, d)
    logits = (hf @ w_router + b_router).astype(jnp.float32)
    top_val, top_idx = lax.top_k(logits, TOP_K)
    top_w = jax.nn.softmax(top_val, axis=-1)
    n_assign = n * TOP_K
    e_flat = top_idx.reshape(-1)
    w_flat = top_w.reshape(-1)
    tok_flat = jnp.arange(n_assign, dtype=jnp.int32) // TOP_K
    order = jnp.argsort(e_flat)
    e_sorted = e_flat[order]
    counts = jnp.bincount(e_flat, length=N_EXPERTS)
    starts = jnp.cumsum(counts) - counts
    padded = (counts + ROW_BLOCK - 1) // ROW_BLOCK * ROW_BLOCK
    padded_ends = jnp.cumsum(padded)
    padded_starts = padded_ends - padded
    dest = padded_starts[e_sorted] + jnp.arange(n_assign, dtype=jnp.int32) - starts[e_sorted]
    n_rows = n_assign + N_EXPERTS * ROW_BLOCK
    n_row_blocks = n_rows // ROW_BLOCK
    row_tok = jnp.zeros((n_rows,), jnp.int32).at[dest].set(tok_flat[order])
    row_w = jnp.zeros((n_rows,), jnp.float32).at[dest].set(w_flat[order])
    block_start = jnp.arange(n_row_blocks, dtype=jnp.int32) * ROW_BLOCK
    block_e = jnp.minimum(jnp.searchsorted(padded_ends, block_start, side="right"), N_EXPERTS - 1)
    xs = hf[row_tok].reshape(n_row_blocks, ROW_BLOCK, d)

    def expert_block(args):
        x_blk, e = args
        u = x_blk @ w1[e] + b1[e]
        return clamped_swiglu(u) @ w2[e] + b2[e]

    ys = lax.map(expert_block, (xs, block_e)).reshape(n_rows, d)
    out = jnp.zeros((n, d), jnp.float32).at[row_tok].add(row_w[:, None] * ys.astype(jnp.float32))
    return out.astype(h.dtype).reshape(b, s, d)


def setup_inputs(seed: int = 0) -> dict:
    key = jax.random.key(seed)
    ks = jax.random.split(key, 24)
    f32 = jnp.float32

    def nrm(k, shape, fan_in):
        return jax.random.normal(k, shape, f32) * (fan_in ** -0.5)

    def gain(k, shape):
        return 1.0 + 0.05 * jax.random.normal(k, shape, f32)

    def small(k, shape, s=0.02):
        return s * jax.random.normal(k, shape, f32)

    offsets = jax.random.randint(ks[2], (BATCH, 1), 0, 1024, dtype=jnp.int32)
    positions = offsets + jnp.arange(SEQ, dtype=jnp.int32)[None, :]
    return {
        "x": jax.random.normal(ks[0], (BATCH, SEQ, D_MODEL), f32),
        "c": jax.random.normal(ks[1], (BATCH, D_MODEL), f32),
        "positions": positions,
        "g_attn": gain(ks[3], (DEPTH, D_MODEL)),
        "w_mod": nrm(ks[4], (DEPTH, D_MODEL, N_MOD * D_MODEL), D_MODEL),
        "b_mod": small(ks[5], (DEPTH, N_MOD * D_MODEL)),
        "w_in": nrm(ks[6], (DEPTH, D_MODEL, IN_WIDTH), D_MODEL),
        "g_q_lat": gain(ks[7], (DEPTH, Q_LORA_RANK)),
        "w_q_up": nrm(ks[8], (DEPTH, Q_LORA_RANK, MLA_HEADS * MLA_QK_DIM), Q_LORA_RANK),
        "g_kv_lat": gain(ks[9], (DEPTH, KV_LORA_RANK)),
        "w_kv_up": nrm(ks[10], (DEPTH, KV_LORA_RANK, MLA_HEADS * (MLA_NOPE_DIM + MLA_V_DIM)), KV_LORA_RANK),
        "w_out": nrm(ks[11], (DEPTH, MIX_WIDTH, D_MODEL), MIX_WIDTH),
        "g_ffn": gain(ks[12], (DEPTH, D_MODEL)),
        "w_router": nrm(ks[13], (DEPTH, D_MODEL, N_EXPERTS), D_MODEL),
        "b_router": small(ks[14], (DEPTH, N_EXPERTS), 0.01),
        "w1": nrm(ks[15], (DEPTH, N_EXPERTS, D_MODEL, 2 * D_EXPERT), D_MODEL),
        "b1": small(ks[16], (DEPTH, N_EXPERTS, 2 * D_EXPERT)),
        "w2": nrm(ks[17], (DEPTH, N_EXPERTS, D_EXPERT, D_MODEL), D_EXPERT),
        "b2": small(ks[18], (DEPTH, N_EXPERTS, D_MODEL)),
        "g_final": gain(ks[19], (D_MODEL,)),
    }


def reference(x, c, positions, g_attn, w_mod, b_mod, w_in, g_q_lat, w_q_up, g_kv_lat, w_kv_up,
              w_out, g_ffn, w_router, b_router, w1, b1, w2, b2, g_final):
    b, s, _ = x.shape
    cos, sin = rope_tables(positions, MLA_ROPE_DIM)
    c_act = jax.nn.silu(c)
    for l in range(DEPTH):
        mod = c_act @ w_mod[l] + b_mod[l]
        shift1, scale1, gate1, shift2, scale2, gate2 = jnp.split(mod, N_MOD, axis=-1)

        h = modulate(rmsnorm(x, g_attn[l]), shift1, scale1)
        proj = h @ w_in[l]
        sb_q, sb_k, sb_v, q_lat, kv_lat, k_rope = jnp.split(proj, IN_SPLITS, axis=-1)
        sb_out = stick_breaking_attention(
            sb_q.reshape(b, s, SB_HEADS, SB_HEAD_DIM),
            sb_k.reshape(b, s, SB_HEADS, SB_HEAD_DIM),
            sb_v.reshape(b, s, SB_HEADS, SB_HEAD_DIM))
        q = (rmsnorm(q_lat, g_q_lat[l]) @ w_q_up[l]).reshape(b, s, MLA_HEADS, MLA_QK_DIM)
        q_nope, q_rope = q[..., :MLA_NOPE_DIM], q[..., MLA_NOPE_DIM:]
        kv = (rmsnorm(kv_lat, g_kv_lat[l]) @ w_kv_up[l]).reshape(b, s, MLA_HEADS, MLA_NOPE_DIM + MLA_V_DIM)
        k_nope, v = kv[..., :MLA_NOPE_DIM], kv[..., MLA_NOPE_DIM:]
        q_rope = apply_rope(q_rope, cos, sin)
        k_rope = apply_rope(k_rope[:, :, None, :], cos, sin)[:, :, 0, :]
        mla_out = mla_attention(q_nope, q_rope, k_nope, k_rope, v)
        mixed = jnp.concatenate([sb_out.reshape(b, s, SB_WIDTH), mla_out.reshape(b, s, MLA_WIDTH)], axis=-1)
        x = x + gate1[:, None, :] * (mixed @ w_out[l])

        h2 = modulate(rmsnorm(x, g_ffn[l]), shift2, scale2)
        x = x + gate2[:, None, :] * moe_ffn(h2, w_router[l], b_router[l], w1[l], b1[l], w2[l], b2[l])
    return rmsnorm(x, g_final)
```

```python
import contextlib
import numpy as np
import concourse.bass as bass
import concourse.mybir as mybir
from concourse.bass_utils import run_bass_kernel_spmd

F32 = mybir.dt.float32
BF16 = mybir.dt.bfloat16
I32 = mybir.dt.int32
AF = mybir.ActivationFunctionType
ALU = mybir.AluOpType
AX = mybir.AxisListType

D = 2048
S_ALL = 4096
T_OWN = 2048
NE = 32
CAPR = 2048
RB = 128
NROWS = 4 * T_OWN + NE * RB
NBLK = NROWS // RB
EPS = 1e-6
PI = float(np.pi)


class Buf:
    __slots__ = ("name", "w", "r", "dsem")

    def __init__(self, name):
        self.name = name
        self.w = {}
        self.r = {}
        self.dsem = None


class Sync:
    ENG = ("pe", "act", "dve", "pool", "sp")

    def __init__(self, nc):
        self.nc = nc
        self.e = {"pe": nc.tensor, "act": nc.scalar, "dve": nc.vector, "pool": nc.gpsimd, "sp": nc.sync}
        self.sems = {}
        self.cnt = {}
        for n in self.ENG:
            self.sems[n] = nc.alloc_semaphore("es_" + n)
            self.cnt[n] = 0
        self.seen = {n: {} for n in self.ENG}
        self.free_d = []
        self.nd = 0

    def _wait(self, eng, deps):
        for k, v in deps.items():
            if v <= 0 or (k == eng and eng == "pe"):
                continue
            if self.seen[eng].get(k, 0) >= v:
                continue
            self.e[eng].wait_ge(self.sems[k], v)
            self.seen[eng][k] = v

    @staticmethod
    def _merge(d, s):
        for k, v in s.items():
            if d.get(k, 0) < v:
                d[k] = v

    def _deps(self, reads, writes):
        d = {}
        for b in reads:
            self._merge(d, b.w)
        for b in writes:
            self._merge(d, b.w)
            self._merge(d, b.r)
        return d

    def op(self, eng, fn, reads=(), writes=()):
        self._wait(eng, self._deps(reads, writes))
        ins = fn()
        self.cnt[eng] += 1
        ins.then_inc(self.sems[eng], 1)
        me = {eng: self.cnt[eng]}
        for b in reads:
            self._merge(b.r, me)
        for b in writes:
            b.w = dict(me)
            b.r = {}
        return ins

    def _dsem(self, buf):
        if buf.dsem is None:
            if self.free_d:
                buf.dsem = self.free_d.pop()
            else:
                self.nd += 1
                buf.dsem = "d%d" % self.nd
                self.sems[buf.dsem] = self.nc.alloc_semaphore(buf.dsem)
                self.cnt[buf.dsem] = 0
        return buf.dsem

    def release(self, bufs):
        for b in bufs:
            if b.dsem is not None:
                self.free_d.append(b.dsem)
                b.dsem = None

    def dma(self, sb, items, reads=(), writes=()):
        key = self._dsem(sb)
        deps = self._deps(reads, writes)
        if self.cnt[key] > 0:
            deps[key] = max(deps.get(key, 0), self.cnt[key])
        for q, fn in items:
            self._wait(q, deps)
        for q, fn in items:
            ins = fn()
            ins.then_inc(self.sems[key], 16)
            self.cnt[key] += 16
        me = {key: self.cnt[key]}
        for b in reads:
            self._merge(b.r, me)
        for b in writes:
            b.w = dict(me)
            b.r = {}

    @contextlib.contextmanager
    def guard(self, regs, thr):
        before = dict(self.cnt)
        seen0 = {k: dict(v) for k, v in self.seen.items()}
        with self.nc.If_cmp(regs, thr, "IS_GT"):
            yield
        after = dict(self.cnt)
        with self.nc.Else():
            for k, v in after.items():
                d = v - before.get(k, 0)
                if d > 0:
                    eng = k if k in self.ENG else "sp"
                    if before.get(k, 0) > 0:
                        self.e[eng].wait_ge(self.sems[k], before[k])
                    self.e[eng].sem_inc(self.sems[k], d)
        self.seen = seen0

    def barrier(self, engines=None):
        allv = {k: v for k, v in self.cnt.items() if v > 0}
        for en in (engines or self.ENG):
            self._wait(en, allv)


class Ring:
    def __init__(self, items):
        self.items = items
        self.i = 0

    def next(self):
        it = self.items[self.i % len(self.items)]
        self.i += 1
        return it


class Ctx:
    def __init__(self, nc, S, es, tag):
        self.nc, self.S, self.es, self.tag = nc, S, es, tag
        self.bufs = []
        self.n = 0

    def sb(self, name, shape, dt):
        self.n += 1
        t = self.es.enter_context(self.nc.sbuf_tensor("%s_%s%d" % (self.tag, name, self.n), shape, dt))
        b = Buf(name)
        self.bufs.append(b)
        return t, b

    def ps(self, name, shape, dt):
        self.n += 1
        t = self.es.enter_context(self.nc.psum_tensor("%s_%s%d" % (self.tag, name, self.n), shape, dt))
        b = Buf(name)
        self.bufs.append(b)
        return t, b

    def ring_sb(self, name, shape, dt, n):
        return Ring([self.sb(name, shape, dt) for _ in range(n)])

    def ring_ps(self, name, shape, dt, n):
        return Ring([self.ps(name, shape, dt) for _ in range(n)])


@contextlib.contextmanager
def phase(nc, S, tag):
    with contextlib.ExitStack() as es:
        c = Ctx(nc, S, es, tag)
        yield c
        S.barrier()
        S.release(c.bufs)


def build_program(stage=99, dbg=False):
    nc = bass.Bass("TRN2", target_bir_lowering=False)
    S = Sync(nc)

    def din(name, shape, dt=F32):
        return nc.dram_tensor(name, list(shape), dt, kind="ExternalInput").ap()

    def dscr(name, shape, dt):
        return nc.dram_tensor(name, list(shape), dt, kind="Internal").ap()

    x_all = din("x_all", [S_ALL, D])
    x_own = din("x_own", [T_OWN, D])
    pos_all = din("pos_all", [1, S_ALL], I32)
    pos_own = din("pos_own", [1, T_OWN], I32)
    cT = din("cT", [128, 16])
    vecs = din("vecs", [128, 16 * 2 + 96])
    rows = din("rows", [4, D])
    glat = din("glat", [128, 8])
    ropec = din("ropec", [128, 4])
    masks = din("masks", [2, 4, 128, 128])
    consts = din("consts", [8, 128, 128])
    w_mod = din("w_mod", [D, 6 * D])
    w_in = din("w_in", [D, 4160])
    w_kr = din("w_kr", [D, 128])
    w_qn = din("w_qn", [512, 1024])
    w_qra = din("w_qra", [512, 512])
    w_qrs = din("w_qrs", [512, 512])
    w_kn = din("w_kn", [512, 1024])
    w_v = din("w_v", [512, 1024])
    w_out = din("w_out", [D, D])
    w_router = din("w_router", [128, 16, NE])
    w1g = din("w1g", [NE, D, D])
    w1l = din("w1l", [NE, D, D])
    b1g = din("b1g", [NE, 128, 16])
    b1l = din("b1l", [NE, 128, 16])
    b1g_r = din("b1g_r", [NE, 1, D])
    b1l_r = din("b1l_r", [NE, 1, D])
    w2 = din("w2", [NE, D, D])
    b2 = din("b2", [NE, 1, D])
    out = nc.dram_tensor("out", [T_OWN, D], F32, kind="ExternalOutput").ap()
    dbg_out = nc.dram_tensor("dbg", [T_OWN, D], F32, kind="ExternalOutput").ap() if dbg else None

    mod_d = dscr("mod_d", [96, 128], F32)
    hT_all = dscr("hT_all", [D, S_ALL], BF16)
    hT_own = dscr("hT_own", [D, T_OWN], BF16)
    QT_sb = dscr("QT_sb", [1024, T_OWN], BF16)
    KT_sb = dscr("KT_sb", [1024, S_ALL], BF16)
    V_sb = dscr("V_sb", [S_ALL, 1024], BF16)
    qlnT = dscr("qlnT", [512, T_OWN], BF16)
    kvlnT = dscr("kvlnT", [512, S_ALL], BF16)
    KrT = dscr("KrT", [64, S_ALL], BF16)
    QnT = dscr("QnT", [1024, T_OWN], BF16)
    QrT = dscr("QrT", [512, T_OWN], BF16)
    KnT = dscr("KnT", [1024, S_ALL], BF16)
    V_ml = dscr("V_ml", [S_ALL, 1024], BF16)
    mixT = dscr("mixT", [D, T_OWN], BF16)
    x1_d = dscr("x1_d", [T_OWN, D], F32)
    xs_d = dscr("xs_d", [NROWS, D], BF16)
    ys_p = [dscr("ys_d%d" % q, [NROWS, 512], F32) for q in range(4)]
    cnt_d = dscr("cnt_d", [1, NE], I32)
    Bd = {}

    def db(name):
        if name not in Bd:
            Bd[name] = Buf(name)
        return Bd[name]

    qi = [0]

    def q2():
        qi[0] += 1
        return "sp"

    with contextlib.ExitStack() as glob:
        G = Ctx(nc, S, glob, "g")
        cst, b_cst = G.sb("cst", [128, 8, 128], F32)
        cstb, b_cstb = G.sb("cstb", [128, 8, 128], BF16)
        S.dma(b_cst, [("sp", lambda: nc.sync.dma_start(out=cst[:], in_=consts.rearrange("c p n -> p c n")))], writes=[b_cst])
        S.op("dve", lambda: nc.vector.tensor_copy(cstb[:], cst[:]), reads=[b_cst], writes=[b_cstb])
        ident_f = cst[:, 0, :]
        ident_b = cstb[:, 0, :]
        negtri_b = cstb[:, 1, :]
        ones_b = cstb[:, 2, :]
        tri_b = cstb[:, 3, :]
        negones33_b = cstb[:, 4, 0:33]
        bc33_b = cstb[0:33, 5, :]
        vec, b_vec = G.sb("vec", [128, 128], F32)
        S.dma(b_vec, [("sp", lambda: nc.sync.dma_start(out=vec[:], in_=vecs))], writes=[b_vec])
        gl, b_gl = G.sb("gl", [128, 8], F32)
        S.dma(b_gl, [("sp", lambda: nc.sync.dma_start(out=gl[:], in_=glat))], writes=[b_gl])
        rc, b_rc = G.sb("rc", [128, 4], F32)
        S.dma(b_rc, [("sp", lambda: nc.sync.dma_start(out=rc[:], in_=ropec))], writes=[b_rc])
        epsT, b_eps = G.sb("eps", [128, 1], F32)
        S.op("dve", lambda: nc.vector.memset(epsT[:], EPS), writes=[b_eps])
        modT, b_modT = G.sb("modT", [128, 96], F32)
        gm, b_gm = G.sb("gm", [128, 32], F32)

        with phase(nc, S, "M") as P:
            ct, b_ct = P.sb("ct", [128, 16], F32)
            S.dma(b_ct, [("sp", lambda: nc.sync.dma_start(out=ct[:], in_=cT))], writes=[b_ct])
            ca, b_ca = P.sb("ca", [128, 16], F32)
            S.op("act", lambda: nc.scalar.activation(ca[:], ct[:], AF.Silu), reads=[b_ct], writes=[b_ca])
            wr = P.ring_sb("wm", [128, 16, 512], F32, 2)
            pm, b_pm = P.ps("pm", [128, 96], F32)
            wv = w_mod.rearrange("(k p) n -> p k n", p=128)
            for g in range(24):
                wt, b_wt = wr.next()
                S.dma(b_wt, [(q2(), lambda wt=wt, g=g: nc.sync.dma_start(out=wt[:], in_=wv[:, :, g * 512:(g + 1) * 512]))], writes=[b_wt])
                for c in range(4):
                    j = g * 4 + c
                    for k in range(16):
                        S.op("pe", lambda wt=wt, c=c, k=k, j=j: nc.tensor.matmul(
                            pm[:, j:j + 1], wt[:, k, c * 128:(c + 1) * 128], ca[:, k:k + 1], start=(k == 0), stop=(k == 15)),
                            reads=[b_wt, b_ca], writes=[b_pm])
            S.op("dve", lambda: nc.vector.tensor_tensor(modT[:], pm[:], vec[:, 32:128], ALU.add), reads=[b_pm, b_vec], writes=[b_modT])
            S.op("dve", lambda: nc.vector.scalar_tensor_tensor(gm[:, 0:16], modT[:, 16:32], 1.0, vec[:, 0:16], ALU.add, ALU.mult),
                 reads=[b_modT, b_vec], writes=[b_gm])
            S.op("dve", lambda: nc.vector.scalar_tensor_tensor(gm[:, 16:32], modT[:, 64:80], 1.0, vec[:, 16:32], ALU.add, ALU.mult),
                 reads=[b_modT, b_vec], writes=[b_gm])
            pt, b_pt = P.ps("pt", [128, 128], F32)
            S.op("pe", lambda: nc.tensor.transpose(pt[0:96, :], modT[:, 0:96], ident_f), reads=[b_modT, b_cst], writes=[b_pt])
            mt, b_mt = P.sb("mt", [96, 128], F32)
            S.op("dve", lambda: nc.vector.tensor_copy(mt[:], pt[0:96, :]), reads=[b_pt], writes=[b_mt])
            S.dma(b_mt, [("sp", lambda: nc.sync.dma_start(out=mod_d, in_=mt[:]))], reads=[b_mt], writes=[db("mod_d")])

        mod_flat = mod_d.rearrange("j p -> (j p)").rearrange("(o n) -> o n", o=1)

        def bc_row(ap_row, n):
            return ap_row.to_broadcast([128, n])

        def norm_to_hT(P, src, dst, ntile, dstname, gcol, shcol):
            xr = P.ring_sb("x", [128, 4, D], F32, 2)
            xnr = P.ring_sb("xn", [128, 4, D], BF16, 2)
            sq, b_sq = P.sb("sq", [128, D], BF16)
            ssr = P.ring_sb("ss", [128, 4], F32, 2)
            ptr = P.ring_ps("ptr", [128, 2048], BF16, 2)
            hr = P.ring_sb("h", [128, 16, 512], BF16, 2)
            sv = src.rearrange("(n j p) d -> n p j d", p=128, j=4)
            dv = dst.rearrange("(k p) t -> p k t", p=128)
            for n in range(ntile):
                xt, b_x = xr.next()
                S.dma(b_x, [("sp", lambda xt=xt, n=n: nc.sync.dma_start(out=xt[:], in_=sv[n]))], writes=[b_x])
                ss, b_ss = ssr.next()
                xn, b_xn = xnr.next()
                ht, b_h = hr.next()
                for j in range(4):
                    S.op("act", lambda xt=xt, ss=ss, j=j: nc.scalar.activation(sq[:], xt[:, j, :], AF.Square, accum_out=ss[:, j:j + 1]),
                         reads=[b_x], writes=[b_sq, b_ss])
                S.op("act", lambda ss=ss: nc.scalar.activation(ss[:], ss[:], AF.Sqrt, bias=epsT[:, 0:1], scale=1.0 / D),
                     reads=[b_ss, b_eps], writes=[b_ss])
                S.op("dve", lambda ss=ss: nc.vector.reciprocal(ss[:], ss[:]), reads=[b_ss], writes=[b_ss])
                for j in range(4):
                    S.op("dve", lambda xt=xt, xn=xn, ss=ss, j=j: nc.vector.tensor_scalar(
                        xn[:, j, :], xt[:, j, :], ss[:, j:j + 1], None, op0=ALU.mult), reads=[b_x, b_ss], writes=[b_xn])
                for j in range(4):
                    pt_, b_p = ptr.next()
                    for k in range(16):
                        S.op("pe", lambda pt_=pt_, xn=xn, j=j, k=k: nc.tensor.transpose(
                            pt_[:, k * 128:(k + 1) * 128], xn[:, j, k * 128:(k + 1) * 128], ident_b), reads=[b_xn, b_cstb], writes=[b_p])
                    for k in range(16):
                        eng = "dve" if k % 2 == 0 else "pool"
                        if eng == "pool":
                            eng = "dve"
                        S.op(eng, lambda pt_=pt_, ht=ht, j=j, k=k: nc.vector.tensor_scalar(
                            ht[:, k, j * 128:(j + 1) * 128], pt_[:, k * 128:(k + 1) * 128],
                            gm[:, gcol + k:gcol + k + 1], modT[:, shcol + k:shcol + k + 1], op0=ALU.mult, op1=ALU.add),
                            reads=[b_p, b_gm, b_modT], writes=[b_h])
                S.dma(b_h, [("sp", lambda ht=ht, n=n: nc.sync.dma_start(out=dv[:, :, n * 512:(n + 1) * 512], in_=ht[:]))],
                      reads=[b_h], writes=[db(dstname)])

        if stage >= 1:
            with phase(nc, S, "A") as P:
                norm_to_hT(P, x_all, hT_all, 8, "hT_all", 0, 0)
            with phase(nc, S, "A2") as P:
                norm_to_hT(P, x_own, hT_own, 4, "hT_own", 0, 0)

        def load_w(P, ring, Wap, KC, c0, ncols):
            wt, b_w = ring.next()
            wv_ = Wap.rearrange("(k p) n -> p k n", p=128)
            S.dma(b_w, [("pool", lambda: nc.gpsimd.dma_start(out=wt[:, 0:KC, 0:ncols], in_=wv_[:, :, c0:c0 + ncols]))], writes=[b_w])
            return wt, b_w

        def load_a(ring, Aap, aname, KC, t0):
            at, b_a = ring.next()
            av = Aap.rearrange("(k p) t -> p k t", p=128)
            S.dma(b_a, [("sp", lambda: nc.sync.dma_start(out=at[:, 0:KC, :], in_=av[:, :, t0:t0 + 512]))], reads=[db(aname)], writes=[b_a])
            return at, b_a

        def gemm_fm(P, Wap, c0, N, Aap, aname, T, KC, dst, dname, scale, rings):
            wr_, ar_, pr_, sr_ = rings
            dv = dst.rearrange("(c p) t -> p c t", p=128)
            for g in range(N // 512):
                wt, b_w = load_w(P, wr_, Wap, KC, c0 + g * 512, 512)
                for tt in range(T // 512):
                    at, b_a = load_a(ar_, Aap, aname, KC, tt * 512)
                    st, b_s = sr_.next()
                    for c in range(4):
                        ps_, b_p = pr_.next()
                        for k in range(KC):
                            S.op("pe", lambda ps_=ps_, wt=wt, at=at, c=c, k=k: nc.tensor.matmul(
                                ps_[:], wt[:, k, c * 128:(c + 1) * 128], at[:, k, :], start=(k == 0), stop=(k == KC - 1)),
                                reads=[b_w, b_a], writes=[b_p])
                        S.op("act", lambda ps_=ps_, st=st, c=c: nc.scalar.activation(st[:, c, :], ps_[:], AF.Copy, scale=scale),
                             reads=[b_p], writes=[b_s])
                    S.dma(b_s, [("sp", lambda st=st, g=g, tt=tt: nc.sync.dma_start(
                        out=dv[:, g * 4:(g + 1) * 4, tt * 512:(tt + 1) * 512], in_=st[:]))], reads=[b_s], writes=[db(dname)])

        def gemm_tm(P, Wap, c0, N, Aap, aname, T, KC, dst, dname, rings):
            wr_, ar_, pr_, sr_ = rings
            dv = dst.rearrange("(n j p) c -> n p j c", p=128, j=4)
            for g in range(N // 512):
                wt, b_w = load_w(P, wr_, Wap, KC, c0 + g * 512, 512)
                for tt in range(T // 512):
                    at, b_a = load_a(ar_, Aap, aname, KC, tt * 512)
                    st, b_s = sr_.next()
                    for j in range(4):
                        ps_, b_p = pr_.next()
                        for k in range(KC):
                            S.op("pe", lambda ps_=ps_, wt=wt, at=at, j=j, k=k: nc.tensor.matmul(
                                ps_[:], at[:, k, j * 128:(j + 1) * 128], wt[:, k, :], start=(k == 0), stop=(k == KC - 1)),
                                reads=[b_w, b_a], writes=[b_p])
                        S.op("act", lambda ps_=ps_, st=st, j=j: nc.scalar.activation(st[:, j, :], ps_[:], AF.Copy),
                             reads=[b_p], writes=[b_s])
                    S.dma(b_s, [("sp", lambda st=st, g=g, tt=tt: nc.sync.dma_start(
                        out=dv[tt][:, :, g * 512:(g + 1) * 512], in_=st[:]))], reads=[b_s], writes=[db(dname)])

        def latent_norm(P, c0, Aap, aname, T, gcol, dst, dname, rings):
            wr_, ar_, pr_, sr_ = rings
            dv = dst.rearrange("(c p) t -> p c t", p=128)
            l32, b_l = P.sb("l32", [128, 4, 512], F32)
            lsq, b_q = P.sb("lsq", [128, 4, 512], BF16)
            rs, b_rs = P.sb("rs", [128, 512], F32)
            wt, b_w = load_w(P, wr_, w_in, 16, c0, 512)
            for tt in range(T // 512):
                at, b_a = load_a(ar_, Aap, aname, 16, tt * 512)
                st, b_s = sr_.next()
                for c in range(4):
                    ps_, b_p = pr_.next()
                    for k in range(16):
                        S.op("pe", lambda ps_=ps_, at=at, c=c, k=k: nc.tensor.matmul(
                            ps_[:], wt[:, k, c * 128:(c + 1) * 128], at[:, k, :], start=(k == 0), stop=(k == 15)),
                            reads=[b_w, b_a], writes=[b_p])
                    S.op("act", lambda ps_=ps_, c=c: nc.scalar.activation(l32[:, c, :], ps_[:], AF.Copy), reads=[b_p], writes=[b_l])
                    S.op("dve", lambda c=c: nc.vector.tensor_tensor(lsq[:, c, :], l32[:, c, :], l32[:, c, :], ALU.mult), reads=[b_l], writes=[b_q])
                ps_, b_p = pr_.next()
                for c in range(4):
                    S.op("pe", lambda ps_=ps_, c=c: nc.tensor.matmul(ps_[:], ones_b, lsq[:, c, :], start=(c == 0), stop=(c == 3)),
                         reads=[b_q, b_cstb], writes=[b_p])
                S.op("act", lambda ps_=ps_: nc.scalar.activation(rs[:], ps_[:], AF.Sqrt, bias=epsT[:, 0:1], scale=1.0 / 512),
                     reads=[b_p, b_eps], writes=[b_rs])
                S.op("dve", lambda: nc.vector.reciprocal(rs[:], rs[:]), reads=[b_rs], writes=[b_rs])
                for c in range(4):
                    S.op("dve", lambda st=st, c=c: nc.vector.scalar_tensor_tensor(
                        st[:, c, :], l32[:, c, :], gl[:, gcol + c:gcol + c + 1], rs[:], ALU.mult, ALU.mult),
                        reads=[b_l, b_gl, b_rs], writes=[b_s])
                S.dma(b_s, [("sp", lambda st=st, tt=tt: nc.sync.dma_start(out=dv[:, :, tt * 512:(tt + 1) * 512], in_=st[:]))],
                      reads=[b_s], writes=[db(dname)])

        def rope_tables(P, pos_ap, T, scale, name):
            pi_, b_pi = P.sb("posi", [128, T], I32)
            S.dma(b_pi, [("sp", lambda: nc.sync.dma_start(out=pi_[:], in_=pos_ap.to_broadcast([128, T])))], writes=[b_pi])
            ang, b_an = P.sb("ang", [128, T], F32)
            S.op("dve", lambda: nc.vector.tensor_copy(ang[:], pi_[:]), reads=[b_pi], writes=[b_an])
            C, b_C = P.sb("C" + name, [128, T], F32)
            Sg, b_S = P.sb("S" + name, [128, T], F32)
            tmp, b_t = P.sb("rtmp", [128, T], F32)
            ni, b_ni = P.sb("rni", [128, T], I32)
            S.op("dve", lambda: nc.vector.tensor_scalar(ang[:], ang[:], rc[:, 0:1], None, op0=ALU.mult), reads=[b_an, b_rc], writes=[b_an])
            for dst_, b_d, offs in ((Sg, b_S, 0.0), (C, b_C, 0.5 * PI)):
                S.op("dve", lambda offs=offs: nc.vector.tensor_scalar(tmp[:], ang[:], offs, 1.0 / (2 * PI), op0=ALU.add, op1=ALU.mult), reads=[b_an], writes=[b_t])
                S.op("dve", lambda: nc.vector.tensor_copy(ni[:], tmp[:]), reads=[b_t], writes=[b_ni])
                S.op("dve", lambda: nc.vector.tensor_copy(tmp[:], ni[:]), reads=[b_ni], writes=[b_t])
                S.op("dve", lambda: nc.vector.scalar_tensor_tensor(tmp[:], tmp[:], -2 * PI, ang[:], ALU.mult, ALU.add), reads=[b_t, b_an], writes=[b_t])
                if offs != 0.0:
                    S.op("dve", lambda offs=offs: nc.vector.tensor_scalar(tmp[:], tmp[:], offs, None, op0=ALU.add), reads=[b_t], writes=[b_t])
                S.op("dve", lambda dst_=dst_: nc.vector.tensor_scalar(dst_[:], tmp[:], PI, 2 * PI, op0=ALU.is_gt, op1=ALU.mult), reads=[b_t], writes=[b_d])
                S.op("dve", lambda dst_=dst_: nc.vector.tensor_tensor(tmp[:], tmp[:], dst_[:], ALU.subtract), reads=[b_t, b_d], writes=[b_t])
                S.op("dve", lambda dst_=dst_: nc.vector.tensor_scalar(dst_[:], tmp[:], -PI, 2 * PI, op0=ALU.is_lt, op1=ALU.mult), reads=[b_t], writes=[b_d])
                S.op("dve", lambda dst_=dst_: nc.vector.tensor_tensor(tmp[:], tmp[:], dst_[:], ALU.add), reads=[b_t, b_d], writes=[b_t])
                S.op("act", lambda dst_=dst_: nc.scalar.activation(dst_[:], tmp[:], AF.Sin), reads=[b_t], writes=[b_d])
            S.op("dve", lambda: nc.vector.tensor_scalar(Sg[:], Sg[:], rc[:, 1:2], float(scale), op0=ALU.mult, op1=ALU.mult),
                 reads=[b_S, b_rc], writes=[b_S])
            if scale != 1.0:
                S.op("dve", lambda: nc.vector.tensor_scalar(C[:], C[:], float(scale), None, op0=ALU.mult), reads=[b_C], writes=[b_C])
            return (C, b_C), (Sg, b_S)

        def rope_proj(P, Wa, ca0, Ws, cs0, nh, Aap, aname, T, KC, CS, dst, dname, rings):
            wr_, ar_, pr_, sr_ = rings
            (C, b_C), (Sg, b_S) = CS
            wa, b_wa = load_w(P, wr_, Wa, KC, ca0, nh * 64)
            ws, b_ws = load_w(P, wr_, Ws, KC, cs0, nh * 64)
            t1, b_t1 = P.sb("rt1", [64, 512], F32)
            t2, b_t2 = P.sb("rt2", [64, 512], F32)
            for tt in range(T // 512):
                at, b_a = load_a(ar_, Aap, aname, KC, tt * 512)
                for h in range(nh):
                    pa, b_pa = pr_.next()
                    pb, b_pb = pr_.next()
                    for k in range(KC):
                        S.op("pe", lambda pa=pa, at=at, h=h, k=k: nc.tensor.matmul(
                            pa[0:64, :], wa[:, k, h * 64:(h + 1) * 64], at[:, k, :], start=(k == 0), stop=(k == KC - 1)),
                            reads=[b_wa, b_a], writes=[b_pa])
                    for k in range(KC):
                        S.op("pe", lambda pb=pb, at=at, h=h, k=k: nc.tensor.matmul(
                            pb[0:64, :], ws[:, k, h * 64:(h + 1) * 64], at[:, k, :], start=(k == 0), stop=(k == KC - 1)),
                            reads=[b_ws, b_a], writes=[b_pb])
                    st, b_s = sr_.next()
                    S.op("dve", lambda pa=pa, tt=tt: nc.vector.tensor_tensor(t1[:], pa[0:64, :], C[0:64, tt * 512:(tt + 1) * 512], ALU.mult),
                         reads=[b_pa, b_C], writes=[b_t1])
                    S.op("dve", lambda pb=pb, tt=tt: nc.vector.tensor_tensor(t2[:], pb[0:64, :], Sg[0:64, tt * 512:(tt + 1) * 512], ALU.mult),
                         reads=[b_pb, b_S], writes=[b_t2])
                    S.op("dve", lambda st=st: nc.vector.tensor_tensor(st[0:64, 0, :], t1[:], t2[:], ALU.add), reads=[b_t1, b_t2], writes=[b_s])
                    S.dma(b_s, [("sp", lambda st=st, h=h, tt=tt: nc.sync.dma_start(
                        out=dst[h * 64:(h + 1) * 64, tt * 512:(tt + 1) * 512], in_=st[0:64, 0, :]))], reads=[b_s], writes=[db(dname)])

        if stage >= 2:
            with phase(nc, S, "B") as P:
                rings = (P.ring_sb("w", [128, 16, 512], BF16, 2), P.ring_sb("a", [128, 16, 512], BF16, 2),
                         P.ring_ps("p", [128, 512], F32, 6), P.ring_sb("st", [128, 4, 512], BF16, 2))
                gemm_fm(P, w_in, 0, 1024, hT_own, "hT_own", T_OWN, 16, QT_sb, "QT_sb", 128 ** -0.5, rings)
                gemm_fm(P, w_in, 1024, 1024, hT_all, "hT_all", S_ALL, 16, KT_sb, "KT_sb", 1.0, rings)
                gemm_tm(P, w_in, 2048, 1024, hT_all, "hT_all", S_ALL, 16, V_sb, "V_sb", rings)
                latent_norm(P, 3072, hT_own, "hT_own", T_OWN, 0, qlnT, "qlnT", rings)
                latent_norm(P, 3584, hT_all, "hT_all", S_ALL, 4, kvlnT, "kvlnT", rings)
            with phase(nc, S, "B2") as P:
                rings = (P.ring_sb("w", [128, 16, 512], BF16, 2), P.ring_sb("a", [128, 16, 512], BF16, 2),
                         P.ring_ps("p", [128, 512], F32, 6), P.ring_sb("st", [128, 4, 512], BF16, 2))
                CSk = rope_tables(P, pos_all, S_ALL, 1.0, "k")
                rope_proj(P, w_kr, 0, w_kr, 64, 1, hT_all, "hT_all", S_ALL, 16, CSk, KrT, "KrT", rings)
            with phase(nc, S, "B3") as P:
                rings = (P.ring_sb("w", [128, 16, 512], BF16, 2), P.ring_sb("a", [128, 16, 512], BF16, 2),
                         P.ring_ps("p", [128, 512], F32, 6), P.ring_sb("st", [128, 4, 512], BF16, 2))
                CSq = rope_tables(P, pos_own, T_OWN, 192 ** -0.5, "q")
                rope_proj(P, w_qra, 0, w_qrs, 0, 8, qlnT, "qlnT", T_OWN, 4, CSq, QrT, "QrT", rings)
                gemm_fm(P, w_qn, 0, 1024, qlnT, "qlnT", T_OWN, 4, QnT, "QnT", 192 ** -0.5, rings)
                gemm_fm(P, w_kn, 0, 1024, kvlnT, "kvlnT", S_ALL, 4, KnT, "KnT", 1.0, rings)
                gemm_tm(P, w_v, 0, 1024, kvlnT, "kvlnT", S_ALL, 4, V_ml, "V_ml", rings)

        def attention(P, is_sb):
            mk, b_mk = P.sb("mk", [128, 4, 128], F32)
            S.dma(b_mk, [("sp", lambda: nc.sync.dma_start(out=mk[:], in_=masks[0 if is_sb else 1].rearrange("z p n -> p z n")))], writes=[b_mk])
            mkb, b_mkb = P.sb("mkb", [128, 4, 128], BF16)
            S.op("dve", lambda: nc.vector.tensor_copy(mkb[:], mk[:]), reads=[b_mk], writes=[b_mkb])
            ktr = P.ring_sb("kt", [128, S_ALL], BF16, 2)
            vr = P.ring_sb("v", [128, 32, 128], BF16, 2)
            qr = P.ring_sb("q", [128, T_OWN], BF16, 2)
            if not is_sb:
                krt, b_krt = P.sb("krt", [64, S_ALL], BF16)
                S.dma(b_krt, [("sp", lambda: nc.sync.dma_start(out=krt[:], in_=KrT))], reads=[db("KrT")], writes=[b_krt])
                qrr = P.ring_sb("qr", [64, T_OWN], BF16, 2)
            pA = P.ring_ps("pA", [128, 512], F32, 2)
            pB = P.ring_ps("pB", [128, 512], F32, 2)
            pO = P.ring_ps("pO", [128, 512], F32, 2)
            if is_sb:
                pC, b_pC = P.ps("pC", [128, 512], F32)
                e32r = P.ring_sb("e32", [128, 512], F32, 2)
                spr = P.ring_sb("sp", [128, 512], BF16, 2)
                Tt, b_T = P.sb("T", [33, 512], F32)
                THL, b_THL = P.sb("THL", [33, 512], BF16)
            else:
                pD = P.ring_ps("pD", [128, 512], F32, 2)
                rdn, b_rdn = P.sb("rdn", [128, 512], F32)
            ar = P.ring_sb("a", [128, 512], BF16, 3)
            mor = P.ring_sb("mo", [128, 512], BF16, 2)
            zer, b_zer = P.sb("zer", [128, 128], BF16)
            S.op("dve", lambda: nc.vector.memset(zer[:], 0.0), writes=[b_zer])
            KT = KT_sb if is_sb else KnT
            QT = QT_sb if is_sb else QnT
            VV = V_sb if is_sb else V_ml
            kn, qn, vn = ("KT_sb", "QT_sb", "V_sb") if is_sb else ("KnT", "QnT", "V_ml")
            for h in range(8):
                kt, b_kt = ktr.next()
                S.dma(b_kt, [("sp", lambda kt=kt, h=h: nc.sync.dma_start(out=kt[:], in_=KT[h * 128:(h + 1) * 128, :]))], reads=[db(kn)], writes=[b_kt])
                vt, b_vt = vr.next()
                S.dma(b_vt, [("sp", lambda vt=vt, h=h: nc.sync.dma_start(
                    out=vt[:], in_=VV.rearrange("(kb p) c -> p kb c", p=128)[:, :, h * 128:(h + 1) * 128]))], reads=[db(vn)], writes=[b_vt])
                qt, b_qt = qr.next()
                S.dma(b_qt, [("sp", lambda qt=qt, h=h: nc.sync.dma_start(out=qt[:], in_=QT[h * 128:(h + 1) * 128, :]))], reads=[db(qn)], writes=[b_qt])
                if not is_sb:
                    qrt, b_qrt = qrr.next()
                    S.dma(b_qrt, [("sp", lambda qrt=qrt, h=h: nc.sync.dma_start(out=qrt[:], in_=QrT[h * 64:(h + 1) * 64, :]))], reads=[db("QrT")], writes=[b_qrt])
                for m in range(4):
                    q0 = m * 512
                    po, b_po = pO.next()
                    S.op("pe", lambda po=po, qt=qt, q0=q0: nc.tensor.matmul(po[:], zer[:], qt[:, q0:q0 + 512], start=True, stop=False),
                         reads=[b_zer, b_qt], writes=[b_po])
                    if is_sb:
                        S.op("dve", lambda: nc.vector.memset(Tt[:], 0.0), writes=[b_T])
                        S.op("dve", lambda: nc.vector.memset(THL[:], 0.0), writes=[b_THL])
                    else:
                        pd, b_pd = pD.next()
                        S.op("pe", lambda pd=pd, qt=qt, q0=q0: nc.tensor.matmul(pd[:], zer[:], qt[:, q0:q0 + 512], start=True, stop=False),
                             reads=[b_zer, b_qt], writes=[b_pd])
                    kmax = 8 * m + 7
                    for kb in range(kmax, -1, -1):
                        last = (kb == 0)
                        smin = 0
                        while 8 * m + 1 + 2 * smin < kb:
                            smin += 1
                        c0 = smin * 128
                        cs = slice(c0, 512)
                        qs = slice(q0 + c0, q0 + 512)
                        ks = slice(kb * 128, (kb + 1) * 128)
                        bslot = None
                        if kb >= 8 * m:
                            r = kb - 8 * m
                            bslot, zone = r // 2, (r // 2 % 2) * 2 + (r % 2)
                        pa, b_pa = pA.next()
                        at, b_at = ar.next()
                        if is_sb:
                            S.op("pe", lambda pa=pa, kt=kt, qt=qt, ks=ks, qs=qs, cs=cs: nc.tensor.matmul(pa[:, cs], kt[:, ks], qt[:, qs], start=True, stop=True),
                                 reads=[b_kt, b_qt], writes=[b_pa])
                            e32, b_e = e32r.next()
                            spm, b_sp = spr.next()
                            S.op("act", lambda pa=pa, e32=e32, cs=cs: nc.scalar.activation(e32[:, cs], pa[:, cs], AF.Exp), reads=[b_pa], writes=[b_e])
                            S.op("act", lambda e32=e32, spm=spm, cs=cs: nc.scalar.activation(spm[:, cs], e32[:, cs], AF.Ln, bias=1.0), reads=[b_e], writes=[b_sp])
                            if bslot is not None:
                                bs = slice(bslot * 128, (bslot + 1) * 128)
                                S.op("pool", lambda spm=spm, bs=bs, zone=zone: nc.gpsimd.tensor_tensor(spm[:, bs], spm[:, bs], mkb[:, zone, :], ALU.mult),
                                     reads=[b_sp, b_mkb], writes=[b_sp])
                            pb, b_pb = pB.next()
                            S.op("pe", lambda pb=pb, kt=kt, qt=qt, ks=ks, qs=qs, cs=cs: nc.tensor.matmul(pb[:, cs], kt[:, ks], qt[:, qs], start=True, stop=False),
                                 reads=[b_kt, b_qt], writes=[b_pb])
                            S.op("pe", lambda pb=pb, spm=spm, cs=cs: nc.tensor.matmul(pb[:, cs], negtri_b, spm[:, cs], start=False, stop=False),
                                 reads=[b_sp, b_cstb], writes=[b_pb])
                            S.op("pe", lambda pb=pb, cs=cs: nc.tensor.matmul(pb[:, cs], bc33_b, THL[0:33, cs], start=False, stop=True),
                                 reads=[b_THL, b_cstb], writes=[b_pb])
                            S.op("act", lambda pb=pb, at=at, cs=cs: nc.scalar.activation(at[:, cs], pb[:, cs], AF.Exp), reads=[b_pb], writes=[b_at])
                            if not last:
                                S.op("pe", lambda spm=spm, cs=cs: nc.tensor.matmul(pC[0:33, cs], negones33_b, spm[:, cs], start=True, stop=True),
                                     reads=[b_sp, b_cstb], writes=[b_pC])
                                S.op("dve", lambda cs=cs: nc.vector.tensor_tensor(Tt[:, cs], Tt[:, cs], pC[0:33, cs], ALU.add), reads=[b_pC, b_T], writes=[b_T])
                                S.op("dve", lambda cs=cs: nc.vector.tensor_copy(THL[:, cs], Tt[:, cs]), reads=[b_T], writes=[b_THL])
                                S.op("dve", lambda cs=cs: nc.vector.tensor_tensor(THL[32:33, cs], Tt[32:33, cs], THL[32:33, cs], ALU.subtract),
                                     reads=[b_T, b_THL], writes=[b_THL])
                        else:
                            S.op("pe", lambda pa=pa, kt=kt, qt=qt, ks=ks, qs=qs, cs=cs: nc.tensor.matmul(pa[:, cs], kt[:, ks], qt[:, qs], start=True, stop=False),
                                 reads=[b_kt, b_qt], writes=[b_pa])
                            S.op("pe", lambda pa=pa, qrt=qrt, ks=ks, qs=qs, cs=cs: nc.tensor.matmul(pa[:, cs], krt[:, ks], qrt[:, qs], start=False, stop=True),
                                 reads=[b_krt, b_qrt], writes=[b_pa])
                            S.op("act", lambda pa=pa, at=at, cs=cs: nc.scalar.activation(at[:, cs], pa[:, cs], AF.Exp), reads=[b_pa], writes=[b_at])
                        if bslot is not None:
                            bs = slice(bslot * 128, (bslot + 1) * 128)
                            S.op("pool", lambda at=at, bs=bs, zone=zone: nc.gpsimd.tensor_tensor(at[:, bs], at[:, bs], mkb[:, zone, :], ALU.mult),
                                 reads=[b_at, b_mkb], writes=[b_at])
                        S.op("pe", lambda po=po, vt=vt, at=at, kb=kb, cs=cs, last=last: nc.tensor.matmul(po[:, cs], vt[:, kb, :], at[:, cs], start=False, stop=last),
                             reads=[b_vt, b_at], writes=[b_po])
                        if not is_sb:
                            S.op("pe", lambda pd=pd, at=at, cs=cs, last=last: nc.tensor.matmul(pd[:, cs], ones_b, at[:, cs], start=False, stop=last),
                                 reads=[b_cstb, b_at], writes=[b_pd])
                    mo, b_mo = mor.next()
                    if is_sb:
                        S.op("act", lambda po=po, mo=mo: nc.scalar.activation(mo[:], po[:], AF.Copy), reads=[b_po], writes=[b_mo])
                    else:
                        S.op("dve", lambda pd=pd: nc.vector.reciprocal(rdn[:], pd[:]), reads=[b_pd], writes=[b_rdn])
                        S.op("dve", lambda po=po, mo=mo: nc.vector.tensor_tensor(mo[:], po[:], rdn[:], ALU.mult), reads=[b_po, b_rdn], writes=[b_mo])
                    r0 = (0 if is_sb else 1024) + h * 128
                    S.dma(b_mo, [("sp", lambda mo=mo, r0=r0, q0=q0: nc.sync.dma_start(out=mixT[r0:r0 + 128, q0:q0 + 512], in_=mo[:]))],
                          reads=[b_mo], writes=[db("mixT")])

        if stage >= 3:
            with phase(nc, S, "C1") as P:
                attention(P, True)
            with phase(nc, S, "C2") as P:
                attention(P, False)

        if stage >= 4:
            with phase(nc, S, "D") as P:
                g1, b_g1 = P.sb("g1", [128, D], F32)
                S.dma(b_g1, [("sp", lambda: nc.sync.dma_start(out=g1[:], in_=bc_row(mod_flat[:, 32 * 128:48 * 128], D)))], reads=[db("mod_d")], writes=[b_g1])
                wr_ = P.ring_sb("w", [128, 16, 512], BF16, 2)
                ar_ = P.ring_sb("a", [128, 16, 512], BF16, 2)
                pr_ = P.ring_ps("p", [128, 512], F32, 4)
                xor_ = P.ring_sb("xo", [128, 512], F32, 3)
                tr_ = P.ring_sb("t", [128, 512], F32, 3)
                for g in range(4):
                    wt, b_w = load_w(P, wr_, w_out, 16, g * 512, 512)
                    for tt in range(4):
                        at, b_a = load_a(ar_, mixT, "mixT", 16, tt * 512)
                        for j in range(4):
                            ps_, b_p = pr_.next()
                            for k in range(16):
                                S.op("pe", lambda ps_=ps_, wt=wt, at=at, j=j, k=k: nc.tensor.matmul(
                                    ps_[:], at[:, k, j * 128:(j + 1) * 128], wt[:, k, :], start=(k == 0), stop=(k == 15)),
                                    reads=[b_w, b_a], writes=[b_p])
                            rws = slice((tt * 4 + j) * 128, (tt * 4 + j + 1) * 128)
                            cls = slice(g * 512, (g + 1) * 512)
                            xo, b_xo = xor_.next()
                            S.dma(b_xo, [("sp", lambda xo=xo, rws=rws, cls=cls: nc.sync.dma_start(out=xo[:], in_=x_own[rws, cls]))], writes=[b_xo])
                            t_, b_t = tr_.next()
                            S.op("dve", lambda ps_=ps_, t_=t_, cls=cls: nc.vector.tensor_tensor(t_[:], ps_[:], g1[:, cls], ALU.mult), reads=[b_p, b_g1], writes=[b_t])
                            S.op("pool", lambda t_=t_, xo=xo: nc.gpsimd.tensor_tensor(t_[:], t_[:], xo[:], ALU.add), reads=[b_t, b_xo], writes=[b_t])
                            S.dma(b_t, [("sp", lambda t_=t_, rws=rws, cls=cls: nc.sync.dma_start(out=x1_d[rws, cls], in_=t_[:]))], reads=[b_t], writes=[db("x1_d")])


        SMAX = T_OWN // 128
        dest_i, b_di = G.sb("dest_i", [128, 64], I32)
        w4, b_w4 = G.sb("w4", [128, 16, 4], F32)
        cnt_i, b_cni = G.sb("cnt_i", [128, NE], I32)
        idx_i, b_idx = G.sb("idx_i", [128, NE * SMAX], I32)
        if stage >= 5:
            with phase(nc, S, "R") as P:
                gm2, b_gm2 = P.sb("gm2", [128, D], F32)
                sh2, b_sh2 = P.sb("sh2", [128, D], F32)
                tmpD, b_tD = P.sb("tmpD", [128, D], F32)
                S.dma(b_gm2, [("sp", lambda: nc.sync.dma_start(out=gm2[:], in_=bc_row(mod_flat[:, 64 * 128:80 * 128], D)))], reads=[db("mod_d")], writes=[b_gm2])
                S.dma(b_tD, [("sp", lambda: nc.sync.dma_start(out=tmpD[:], in_=bc_row(rows[2:3, :], D)))], writes=[b_tD])
                S.dma(b_sh2, [("sp", lambda: nc.sync.dma_start(out=sh2[:], in_=bc_row(mod_flat[:, 48 * 128:64 * 128], D)))], reads=[db("mod_d")], writes=[b_sh2])
                S.op("dve", lambda: nc.vector.scalar_tensor_tensor(gm2[:], gm2[:], 1.0, tmpD[:], ALU.add, ALU.mult), reads=[b_gm2, b_tD], writes=[b_gm2])
                brt, b_brt = P.sb("brt", [128, NE], F32)
                S.dma(b_brt, [("sp", lambda: nc.sync.dma_start(out=brt[:], in_=bc_row(rows[1:2, 0:NE], NE)))], writes=[b_brt])
                wrs, b_wrs = P.sb("wrs", [128, 16, NE], F32)
                S.dma(b_wrs, [("sp", lambda: nc.sync.dma_start(out=wrs[:], in_=w_router))], writes=[b_wrs])
                h2b, b_h2b = P.sb("h2b", [128, 16 * D], BF16)
                maskf, b_mf = P.sb("maskf", [128, 16, NE], F32)
                wgt, b_wg = P.sb("wgt", [128, 16, NE], F32)
                posf, b_pf = P.sb("posf", [128, 16, NE], F32)
                cntb, b_cn = P.sb("cntb", [128, NE], F32)
                S.op("dve", lambda: nc.vector.memset(cntb[:], 0.0), writes=[b_cn])
                x1r = P.ring_sb("x1", [128, D], F32, 2)
                h2r = P.ring_sb("h2", [128, D], F32, 2)
                h2T, b_h2T = P.sb("h2T", [128, 16, 128], F32)
                sq, b_sq = P.sb("sq", [128, D], BF16)
                sm = P.ring_sb("sm", [128, 64], F32, 2)
                ex, b_ex = P.sb("ex", [128, NE], F32)
                mb, b_mb = P.sb("mb", [128, NE], BF16)
                ptr = P.ring_ps("pt", [128, 512], F32, 2)
                pl, b_pl = P.ps("pl", [128, NE], F32)
                pp, b_pp = P.ps("pp", [128, NE], F32)
                pc, b_pc = P.ps("pc", [128, NE], F32)
                for i in range(16):
                    xb, b_xb = x1r.next()
                    S.dma(b_xb, [("sp", lambda xb=xb, i=i: nc.sync.dma_start(out=xb[:], in_=x1_d[i * 128:(i + 1) * 128, :]))], reads=[db("x1_d")], writes=[b_xb])
                    s_, b_s = sm.next()
                    S.op("act", lambda xb=xb, s_=s_: nc.scalar.activation(sq[:], xb[:], AF.Square, accum_out=s_[:, 0:1]), reads=[b_xb], writes=[b_sq, b_s])
                    S.op("act", lambda s_=s_: nc.scalar.activation(s_[:, 0:1], s_[:, 0:1], AF.Sqrt, bias=epsT[:, 0:1], scale=1.0 / D), reads=[b_s, b_eps], writes=[b_s])
                    S.op("dve", lambda s_=s_: nc.vector.reciprocal(s_[:, 0:1], s_[:, 0:1]), reads=[b_s], writes=[b_s])
                    h2, b_h2 = h2r.next()
                    S.op("dve", lambda xb=xb, h2=h2, s_=s_: nc.vector.scalar_tensor_tensor(h2[:], xb[:], s_[:, 0:1], gm2[:], ALU.mult, ALU.mult), reads=[b_xb, b_s, b_gm2], writes=[b_h2])
                    S.op("pool", lambda h2=h2: nc.gpsimd.tensor_tensor(h2[:], h2[:], sh2[:], ALU.add), reads=[b_h2, b_sh2], writes=[b_h2])
                    S.op("act", lambda h2=h2, i=i: nc.scalar.activation(h2b[:, i * D:(i + 1) * D], h2[:], AF.Copy), reads=[b_h2], writes=[b_h2b])
                    for g in range(4):
                        pt_, b_p = ptr.next()
                        for c in range(4):
                            k = g * 4 + c
                            S.op("pe", lambda pt_=pt_, h2=h2, c=c, k=k: nc.tensor.transpose(pt_[:, c * 128:(c + 1) * 128], h2[:, k * 128:(k + 1) * 128], ident_f),
                                 reads=[b_h2, b_cst], writes=[b_p])
                        S.op("dve", lambda pt_=pt_, g=g: nc.vector.tensor_copy(h2T[:, g * 4:(g + 1) * 4, :], pt_[:].rearrange("p (c n) -> p c n", c=4)), reads=[b_p], writes=[b_h2T])
                    for k in range(16):
                        S.op("pe", lambda k=k: nc.tensor.matmul(pl[:], h2T[:, k, :], wrs[:, k, :], start=(k == 0), stop=(k == 15)), reads=[b_h2T, b_wrs], writes=[b_pl])
                    lg = s_[:, 32:64]
                    S.op("dve", lambda lg=lg: nc.vector.tensor_tensor(lg, pl[:], brt[:], ALU.add), reads=[b_pl, b_brt], writes=[b_s])
                    S.op("dve", lambda s_=s_, lg=lg: nc.vector.max(out=s_[:, 8:16], in_=lg), reads=[b_s], writes=[b_s])
                    S.op("dve", lambda s_=s_: nc.vector.tensor_scalar(s_[:, 16:17], s_[:, 8:9], -1.0, None, op0=ALU.mult), reads=[b_s], writes=[b_s])
                    S.op("dve", lambda s_=s_, lg=lg, i=i: nc.vector.tensor_scalar(maskf[:, i, :], lg, s_[:, 11:12], None, op0=ALU.is_ge), reads=[b_s], writes=[b_mf])
                    S.op("act", lambda s_=s_, lg=lg: nc.scalar.activation(ex[:], lg, AF.Exp, bias=s_[:, 16:17], scale=1.0), reads=[b_s], writes=[b_ex])
                    S.op("dve", lambda i=i: nc.vector.tensor_tensor(ex[:], ex[:], maskf[:, i, :], ALU.mult), reads=[b_ex, b_mf], writes=[b_ex])
                    S.op("dve", lambda s_=s_: nc.vector.reduce_sum(s_[:, 17:18], ex[:], AX.X), reads=[b_ex], writes=[b_s])
                    S.op("dve", lambda s_=s_: nc.vector.reciprocal(s_[:, 17:18], s_[:, 17:18]), reads=[b_s], writes=[b_s])
                    S.op("dve", lambda s_=s_, i=i: nc.vector.tensor_scalar(wgt[:, i, :], ex[:], s_[:, 17:18], None, op0=ALU.mult), reads=[b_ex, b_s], writes=[b_wg])
                    S.op("dve", lambda i=i: nc.vector.tensor_copy(mb[:], maskf[:, i, :]), reads=[b_mf], writes=[b_mb])
                    S.op("pe", lambda: nc.tensor.matmul(pp[:], tri_b, mb[:], start=True, stop=True), reads=[b_mb, b_cstb], writes=[b_pp])
                    S.op("pe", lambda: nc.tensor.matmul(pc[:], ones_b, mb[:], start=True, stop=True), reads=[b_mb, b_cstb], writes=[b_pc])
                    S.op("dve", lambda i=i: nc.vector.tensor_tensor(posf[:, i, :], pp[:], cntb[:], ALU.add), reads=[b_pp, b_cn], writes=[b_pf])
                    S.op("dve", lambda: nc.vector.tensor_tensor(cntb[:], cntb[:], pc[:], ALU.add), reads=[b_pc, b_cn], writes=[b_cn])
                S.op("dve", lambda: nc.vector.tensor_copy(cnt_i[:], cntb[:]), reads=[b_cn], writes=[b_cni])
                q_, b_q = P.sb("q_", [128, NE], F32)
                nf, b_nf = P.sb("nf", [128, NE], F32)
                nI, b_nI = P.sb("nI", [128, NE], I32)
                inc, b_inc = P.sb("inc", [128, NE], F32)
                inc2, b_inc2 = P.sb("inc2", [128, NE], F32)
                base, b_base = P.sb("base", [128, NE], F32)
                S.op("dve", lambda: nc.vector.tensor_scalar(q_[:], cntb[:], float(RB - 1), 1.0 / RB, op0=ALU.add, op1=ALU.mult), reads=[b_cn], writes=[b_q])
                S.op("dve", lambda: nc.vector.tensor_copy(nI[:], q_[:]), reads=[b_q], writes=[b_nI])
                S.op("dve", lambda: nc.vector.tensor_copy(nf[:], nI[:]), reads=[b_nI], writes=[b_nf])
                S.op("dve", lambda: nc.vector.tensor_tensor(inc[:], nf[:], q_[:], ALU.is_gt), reads=[b_nf, b_q], writes=[b_inc])
                S.op("dve", lambda: nc.vector.tensor_tensor(nf[:], nf[:], inc[:], ALU.subtract), reads=[b_nf, b_inc], writes=[b_nf])
                S.op("dve", lambda: nc.vector.tensor_scalar(nf[:], nf[:], float(RB), None, op0=ALU.mult), reads=[b_nf], writes=[b_nf])
                S.op("dve", lambda: nc.vector.tensor_copy(inc[:], nf[:]), reads=[b_nf], writes=[b_inc])
                cur, b_cur, oth, b_oth = inc, b_inc, inc2, b_inc2
                for sh in (1, 2, 4, 8, 16):
                    S.op("dve", lambda cur=cur, oth=oth, sh=sh: nc.vector.tensor_copy(oth[:, 0:sh], cur[:, 0:sh]), reads=[b_cur], writes=[b_oth])
                    S.op("dve", lambda cur=cur, oth=oth, sh=sh: nc.vector.tensor_tensor(oth[:, sh:NE], cur[:, sh:NE], cur[:, 0:NE - sh], ALU.add), reads=[b_cur, b_oth], writes=[b_oth])
                    cur, b_cur, oth, b_oth = oth, b_oth, cur, b_cur
                ends, b_ends = cur, b_cur
                S.op("dve", lambda: nc.vector.tensor_tensor(base[:], ends[:], nf[:], ALU.subtract), reads=[b_ends, b_nf], writes=[b_base])
                idxf, b_idxf = P.sb("idxf", [128, NE * SMAX], F32)
                for e in range(NE):
                    S.op("dve", lambda e=e: nc.vector.tensor_scalar(idxf[:, e * SMAX:(e + 1) * SMAX], cst[:, 7, 0:SMAX], base[:, e:e + 1], None, op0=ALU.add),
                         reads=[b_cst, b_base], writes=[b_idxf])
                S.op("dve", lambda: nc.vector.tensor_scalar(idxf[:], idxf[:], float(NROWS - 1), None, op0=ALU.min), reads=[b_idxf], writes=[b_idxf])
                S.op("dve", lambda: nc.vector.tensor_copy(idx_i[:], idxf[:]), reads=[b_idxf], writes=[b_idx])
                dm, b_dm = P.sb("dm", [128, NE], F32)
                eq, b_eq = P.sb("eq", [128, NE], F32)
                d8r = P.ring_sb("d8", [128, 16], F32, 2)
                for i in range(16):
                    d8, b_d8 = d8r.next()
                    S.op("dve", lambda i=i: nc.vector.tensor_tensor(dm[:], posf[:, i, :], base[:], ALU.add), reads=[b_pf, b_base], writes=[b_dm])
                    S.op("dve", lambda i=i: nc.vector.scalar_tensor_tensor(dm[:], dm[:], 1.0, maskf[:, i, :], ALU.add, ALU.mult), reads=[b_dm, b_mf], writes=[b_dm])
                    S.op("dve", lambda d8=d8: nc.vector.max(out=d8[:, 0:8], in_=dm[:]), reads=[b_dm], writes=[b_d8])
                    S.op("dve", lambda d8=d8: nc.vector.tensor_scalar(d8[:, 8:12], d8[:, 0:4], -1.0, None, op0=ALU.add), reads=[b_d8], writes=[b_d8])
                    S.op("dve", lambda d8=d8, i=i: nc.vector.tensor_copy(dest_i[:, i * 4:i * 4 + 4], d8[:, 8:12]), reads=[b_d8], writes=[b_di])
                    for k in range(4):
                        S.op("dve", lambda d8=d8, k=k: nc.vector.tensor_scalar(eq[:], dm[:], d8[:, k:k + 1], None, op0=ALU.is_equal), reads=[b_dm, b_d8], writes=[b_eq])
                        S.op("dve", lambda i=i: nc.vector.tensor_tensor(eq[:], eq[:], wgt[:, i, :], ALU.mult), reads=[b_eq, b_wg], writes=[b_eq])
                        S.op("dve", lambda i=i, k=k: nc.vector.reduce_sum(w4[:, i, k:k + 1], eq[:], AX.X), reads=[b_eq], writes=[b_w4])
                    for k in range(4):
                        S.dma(b_h2b, [("pool", lambda i=i, k=k: nc.gpsimd.indirect_dma_start(
                            out=xs_d, out_offset=bass.IndirectOffsetOnAxis(ap=dest_i[:, i * 4 + k:i * 4 + k + 1], axis=0), in_=h2b[:, i * D:(i + 1) * D], in_offset=None))],
                            reads=[b_h2b, b_di], writes=[db("xs_d")])

        if stage >= 6:
            with phase(nc, S, "E") as P:
                regs = nc.alloc_registers("cntreg", engines=mybir.ALL_ENGINES)
                engname = {mybir.EngineType.Pool: "pool", mybir.EngineType.Activation: "act", mybir.EngineType.PE: "pe",
                           mybir.EngineType.DVE: "dve", mybir.EngineType.SP: "sp"}
                xsT, b_xsT = P.sb("xsT", [128, 16, T_OWN], BF16)
                actT, b_act = P.sb("actT", [128, 16, T_OWN], BF16)
                xrr = P.ring_sb("xr", [128, D], BF16, 2)
                w1r = P.ring_sb("w1", [128, 16, 256], BF16, 2)
                w2r = P.ring_sb("w2", [128, 16, 512], BF16, 1)
                b1r = P.ring_sb("b1", [128, 32], F32, 2)
                b2r = P.ring_sb("b2", [128, D], F32, 1)
                ptb = P.ring_ps("ptb", [128, 1024], BF16, 2)
                pg = P.ring_ps("pg", [128, 512], F32, 2)
                bbr = P.ring_sb("bb", [128, 512], F32, 2)
                glr = P.ring_sb("gl", [128, 512], F32, 2)
                abr = P.ring_sb("ab", [128, 256], BF16, 2)
                py = P.ring_ps("py", [128, 512], F32, 2)
                sg, b_sg = P.sb("sg", [128, 256], F32)
                yr = P.ring_sb("y", [128, 512], F32, 3)
                import os as _os
                for e in range(int(_os.environ.get('K_NEXP', NE))):
                    for reg in regs:
                        S.op(engname[reg.engine], lambda reg=reg, e=e: nc.reg_load(reg, cnt_i[0:1, e:e + 1]), reads=[b_cni])
                    b1t, b_b1 = b1r.next()
                    S.dma(b_b1, [("sp", lambda b1t=b1t, e=e: nc.sync.dma_start(out=b1t[:, 0:16], in_=b1g[e])),
                                 ("sp", lambda b1t=b1t, e=e: nc.sync.dma_start(out=b1t[:, 16:32], in_=b1l[e]))], writes=[b_b1])
                    b2t, b_b2 = b2r.next()
                    S.dma(b_b2, [("sp", lambda b2t=b2t, e=e: nc.sync.dma_start(out=b2t[:], in_=b2[e].to_broadcast([128, D])))], writes=[b_b2])
                    ESECT = int(_os.environ.get('K_ESECT', 7))
                    for s_i in range(SMAX if ESECT & 1 else 0):
                        with S.guard(regs, s_i * 128):
                            xr_, b_xr = xrr.next()
                            col = e * SMAX + s_i
                            S.dma(b_xr, [("pool", lambda xr_=xr_, col=col: nc.gpsimd.indirect_dma_start(
                                out=xr_[:], out_offset=None, in_=xs_d, in_offset=bass.IndirectOffsetOnAxis(ap=idx_i[:, col:col + 1], axis=0)))],
                                reads=[db("xs_d"), b_idx], writes=[b_xr])
                            for g in range(2):
                                pt_, b_p = ptb.next()
                                for c in range(8):
                                    k = g * 8 + c
                                    S.op("pe", lambda pt_=pt_, xr_=xr_, c=c, k=k: nc.tensor.transpose(pt_[:, c * 128:(c + 1) * 128], xr_[:, k * 128:(k + 1) * 128], ident_b),
                                         reads=[b_xr, b_cstb], writes=[b_p])
                                S.op("act", lambda pt_=pt_, g=g, s_i=s_i: nc.scalar.activation(
                                    xsT[:, g * 8:(g + 1) * 8, s_i * 128:(s_i + 1) * 128], pt_[:].rearrange("p (c n) -> p c n", c=8), AF.Copy), reads=[b_p], writes=[b_xsT])
                    for pc_ in range(8 if ESECT & 2 else 0):
                        wg_, b_wg_ = w1r.next()
                        wl_, b_wl_ = w1r.next()
                        cs_ = slice(pc_ * 256, (pc_ + 1) * 256)
                        S.dma(b_wg_, [("pool", lambda wg_=wg_, e=e, cs_=cs_: nc.gpsimd.dma_start(out=wg_[:], in_=w1g[e].rearrange("(k p) n -> p k n", p=128)[:, :, cs_]))], writes=[b_wg_])
                        S.dma(b_wl_, [("pool", lambda wl_=wl_, e=e, cs_=cs_: nc.gpsimd.dma_start(out=wl_[:], in_=w1l[e].rearrange("(k p) n -> p k n", p=128)[:, :, cs_]))], writes=[b_wl_])
                        bb_, b_bb = bbr.next()
                        S.dma(b_bb, [("sp", lambda bb_=bb_, e=e, cs_=cs_: nc.sync.dma_start(out=bb_[:, 0:256], in_=b1g_r[e][:, cs_].to_broadcast([128, 256]))),
                                     ("sp", lambda bb_=bb_, e=e, cs_=cs_: nc.sync.dma_start(out=bb_[:, 256:512], in_=b1l_r[e][:, cs_].to_broadcast([128, 256])))], writes=[b_bb])
                        for s_i in range(SMAX):
                            rs_ = slice(s_i * 128, (s_i + 1) * 128)
                            with S.guard(regs, s_i * 128):
                                pg_, b_pg = pg.next()
                                for k in range(16):
                                    S.op("pe", lambda pg_=pg_, wg_=wg_, k=k, rs_=rs_: nc.tensor.matmul(pg_[:, 0:256], xsT[:, k, rs_], wg_[:, k, :], start=(k == 0), stop=(k == 15)),
                                         reads=[b_wg_, b_xsT], writes=[b_pg])
                                for k in range(16):
                                    S.op("pe", lambda pg_=pg_, wl_=wl_, k=k, rs_=rs_: nc.tensor.matmul(pg_[:, 256:512], xsT[:, k, rs_], wl_[:, k, :], start=(k == 0), stop=(k == 15)),
                                         reads=[b_wl_, b_xsT], writes=[b_pg])
                                gl_, b_gl_ = glr.next()
                                S.op("dve", lambda pg_=pg_, gl_=gl_, bb_=bb_: nc.vector.tensor_tensor(gl_[:], pg_[:], bb_[:], ALU.add), reads=[b_pg, b_bb], writes=[b_gl_])
                                S.op("pool", lambda gl_=gl_: nc.gpsimd.tensor_scalar(gl_[:, 0:256], gl_[:, 0:256], 7.0, None, op0=ALU.min), reads=[b_gl_], writes=[b_gl_])
                                S.op("act", lambda gl_=gl_: nc.scalar.activation(sg[:], gl_[:, 0:256], AF.Sigmoid, scale=1.702), reads=[b_gl_], writes=[b_sg])
                                S.op("pool", lambda gl_=gl_: nc.gpsimd.tensor_scalar(gl_[:, 256:512], gl_[:, 256:512], 7.0, -7.0, op0=ALU.min, op1=ALU.max), reads=[b_gl_], writes=[b_gl_])
                                S.op("pool", lambda gl_=gl_: nc.gpsimd.tensor_tensor(sg[:], sg[:], gl_[:, 0:256], ALU.mult), reads=[b_gl_, b_sg], writes=[b_sg])
                                ab_, b_ab = abr.next()
                                S.op("dve", lambda gl_=gl_, ab_=ab_: nc.vector.scalar_tensor_tensor(ab_[:], gl_[:, 256:512], 1.0, sg[:], ALU.add, ALU.mult), reads=[b_gl_, b_sg], writes=[b_ab])
                                pt_, b_p = ptb.next()
                                for c in range(2):
                                    S.op("pe", lambda pt_=pt_, ab_=ab_, c=c: nc.tensor.transpose(pt_[:, c * 128:(c + 1) * 128], ab_[:, c * 128:(c + 1) * 128], ident_b),
                                         reads=[b_ab, b_cstb], writes=[b_p])
                                S.op("act", lambda pt_=pt_, pc_=pc_, rs_=rs_: nc.scalar.activation(
                                    actT[:, pc_ * 2:pc_ * 2 + 2, rs_], pt_[:, 0:256].rearrange("p (c n) -> p c n", c=2), AF.Copy), reads=[b_p], writes=[b_act])
                    for np_ in range(4 if ESECT & 4 else 0):
                        w2t, b_w2 = w2r.next()
                        cs_ = slice(np_ * 512, (np_ + 1) * 512)
                        S.dma(b_w2, [("pool", lambda w2t=w2t, e=e, cs_=cs_: nc.gpsimd.dma_start(out=w2t[:], in_=w2[e].rearrange("(k p) n -> p k n", p=128)[:, :, cs_]))], writes=[b_w2])
                        for s_i in range(SMAX):
                            rs_ = slice(s_i * 128, (s_i + 1) * 128)
                            with S.guard(regs, s_i * 128):
                                py_, b_py = py.next()
                                for k in range(16):
                                    S.op("pe", lambda py_=py_, w2t=w2t, rs_=rs_, k=k: nc.tensor.matmul(py_[:], actT[:, k, rs_], w2t[:, k, :], start=(k == 0), stop=(k == 15)),
                                         reads=[b_act, b_w2], writes=[b_py])
                                y_, b_y = yr.next()
                                S.op("dve", lambda py_=py_, y_=y_, b2t=b2t, cs_=cs_: nc.vector.tensor_tensor(y_[:], py_[:], b2t[:, cs_], ALU.add), reads=[b_py, b_b2], writes=[b_y])
                                col = e * SMAX + s_i
                                S.dma(b_y, [("pool", lambda y_=y_, col=col, np_=np_: nc.gpsimd.indirect_dma_start(
                                    out=ys_p[np_], out_offset=bass.IndirectOffsetOnAxis(ap=idx_i[:, col:col + 1], axis=0), in_=y_[:], in_offset=None))],
                                    reads=[b_y, b_idx], writes=[db("ys_d")])

        if stage >= 7:
            with phase(nc, S, "F") as P:
                g2, b_g2 = P.sb("g2", [128, D], F32)
                gf, b_gf = P.sb("gf", [128, D], F32)
                S.dma(b_g2, [("sp", lambda: nc.sync.dma_start(out=g2[:], in_=bc_row(mod_flat[:, 80 * 128:96 * 128], D)))], reads=[db("mod_d")], writes=[b_g2])
                S.dma(b_gf, [("sp", lambda: nc.sync.dma_start(out=gf[:], in_=bc_row(rows[0:1, :], D)))], writes=[b_gf])
                gr = P.ring_sb("ga", [128, D], F32, 4)
                accr = P.ring_sb("acc", [128, D], F32, 2)
                x1r = P.ring_sb("x1", [128, D], F32, 2)
                sq, b_sq = P.sb("sq", [128, D], BF16)
                ssr = P.ring_sb("ss", [128, 1], F32, 2)
                for i in range(16):
                    xb, b_xb = x1r.next()
                    S.dma(b_xb, [("sp", lambda xb=xb, i=i: nc.sync.dma_start(out=xb[:], in_=x1_d[i * 128:(i + 1) * 128, :]))], reads=[db("x1_d")], writes=[b_xb])
                    acc, b_acc = accr.next()
                    for k in range(4):
                        ga, b_ga = gr.next()
                        S.dma(b_ga, [("pool", lambda ga=ga, i=i, k=k, q=q: nc.gpsimd.indirect_dma_start(
                            out=ga[:, q * 512:(q + 1) * 512], out_offset=None, in_=ys_p[q], in_offset=bass.IndirectOffsetOnAxis(ap=dest_i[:, i * 4 + k:i * 4 + k + 1], axis=0)))
                            for q in range(4)], reads=[db("ys_d"), b_di], writes=[b_ga])
                        if k == 0:
                            S.op("dve", lambda ga=ga, acc=acc, i=i, k=k: nc.vector.tensor_scalar(acc[:], ga[:], w4[:, i, k:k + 1], None, op0=ALU.mult), reads=[b_ga, b_w4], writes=[b_acc])
                        else:
                            S.op("dve", lambda ga=ga, acc=acc, i=i, k=k: nc.vector.scalar_tensor_tensor(acc[:], ga[:], w4[:, i, k:k + 1], acc[:], ALU.mult, ALU.add), reads=[b_ga, b_w4, b_acc], writes=[b_acc])
                    S.op("pool", lambda acc=acc: nc.gpsimd.tensor_tensor(acc[:], acc[:], g2[:], ALU.mult), reads=[b_acc, b_g2], writes=[b_acc])
                    S.op("dve", lambda acc=acc, xb=xb: nc.vector.tensor_tensor(acc[:], acc[:], xb[:], ALU.add), reads=[b_acc, b_xb], writes=[b_acc])
                    ss, b_ss = ssr.next()
                    S.op("act", lambda acc=acc, ss=ss: nc.scalar.activation(sq[:], acc[:], AF.Square, accum_out=ss[:, 0:1]), reads=[b_acc], writes=[b_sq, b_ss])
                    S.op("act", lambda ss=ss: nc.scalar.activation(ss[:], ss[:], AF.Sqrt, bias=epsT[:, 0:1], scale=1.0 / D), reads=[b_ss, b_eps], writes=[b_ss])
                    S.op("dve", lambda ss=ss: nc.vector.reciprocal(ss[:], ss[:]), reads=[b_ss], writes=[b_ss])
                    S.op("dve", lambda acc=acc, ss=ss: nc.vector.scalar_tensor_tensor(acc[:], acc[:], ss[:, 0:1], gf[:], ALU.mult, ALU.mult), reads=[b_acc, b_ss, b_gf], writes=[b_acc])
                    S.dma(b_acc, [("sp", lambda acc=acc, i=i: nc.sync.dma_start(out=out[i * 128:(i + 1) * 128, :], in_=acc[:]))], reads=[b_acc], writes=[db("out")])

        if dbg and stage <= 4:
            with phase(nc, S, "DBG") as P:
                t_, b_t = P.sb("dbg", [128, 16, D], F32) if False else (None, None)
                r = P.ring_sb("r", [128, D], F32, 2)
                for i in range(16):
                    t_, b_t = r.next()
                    S.dma(b_t, [("sp", lambda t_=t_, i=i: nc.sync.dma_start(out=t_[:], in_=x1_d[i * 128:(i + 1) * 128, :]))], reads=[db("x1_d")], writes=[b_t])
                    S.dma(b_t, [("sp", lambda t_=t_, i=i: nc.sync.dma_start(out=dbg_out[i * 128:(i + 1) * 128, :], in_=t_[:]))], reads=[b_t], writes=[db("dbg")])

        S.barrier()
    return nc


def own_blocks(p):
    blks = []
    for m in range(8):
        blks += [4 * m + (0 if p == 0 else 1), 4 * m + (3 if p == 0 else 2)]
    return blks


def host_prepare(inp):
    f32 = np.float32
    x = np.asarray(inp["x"], f32)
    c = np.asarray(inp["c"], f32)
    pos = np.asarray(inp["positions"], np.int32)
    g_attn = np.asarray(inp["g_attn"], f32)[0]
    g_ffn = np.asarray(inp["g_ffn"], f32)[0]
    b_mod = np.asarray(inp["b_mod"], f32)[0]
    w_in = np.asarray(inp["w_in"], f32)[0]
    w_q_up = np.asarray(inp["w_q_up"], f32)[0].reshape(512, 8, 192)
    w_kv_up = np.asarray(inp["w_kv_up"], f32)[0].reshape(512, 8, 256)
    fm = lambda v: np.ascontiguousarray(v.reshape(-1, 128).T)
    vecs = np.concatenate([fm(g_attn), fm(g_ffn), fm(b_mod)], axis=1).astype(f32)
    rows = np.zeros((4, D), f32)
    rows[0] = np.asarray(inp["g_final"], f32)
    rows[1, :NE] = np.asarray(inp["b_router"], f32)[0]
    rows[2] = g_ffn
    glat = np.concatenate([fm(np.asarray(inp["g_q_lat"], f32)[0]), fm(np.asarray(inp["g_kv_lat"], f32)[0])], axis=1).astype(f32)
    half = 32
    invf = (1.0 / (np.float32(10000.0) ** (np.arange(half, dtype=f32) * f32(2.0) / f32(64)))).astype(f32)
    ropec = np.zeros((128, 4), f32)
    for p_ in range(128):
        i = p_ % 64
        ropec[p_, 0] = invf[i % 32]
        ropec[p_, 1] = -1.0 if i < 32 else 1.0
    ropec[:, 2] = np.pi
    ropec[:, 3] = 1.5 * np.pi
    kk = np.arange(128)[:, None]
    qq = np.arange(128)[None, :]
    ident = np.eye(128, dtype=f32)
    negtri = -(kk >= qq).astype(f32)
    ones = np.ones((128, 128), f32)
    tri_s = (kk < qq).astype(f32)
    negones33 = np.zeros((128, 128), f32)
    negones33[:, :33] = -1.0
    bc33 = np.zeros((128, 128), f32)
    bc33[0, :] = 1.0
    bc33[32, :] = 1.0
    blkstart = np.tile((np.arange(128, dtype=f32) * RB)[None, :], (128, 1))
    off16 = (np.arange(128, dtype=f32)[None, :] % 16) * 128 + np.arange(128, dtype=f32)[:, None]
    consts = np.stack([ident, negtri, ones, tri_s, negones33, bc33, blkstart, off16]).astype(f32)
    TRI_SB = (kk < qq).astype(f32)
    TRI_ML = (kk <= qq).astype(f32)
    ONE = np.ones((128, 128), f32)
    ZERO = np.zeros((128, 128), f32)
    kr = w_in[:, 4096:4160]
    w_kr = np.ascontiguousarray(np.concatenate([kr, kr[:, 32:], kr[:, :32]], axis=1))
    w_qn = np.ascontiguousarray(w_q_up[:, :, :128].reshape(512, 1024))
    qr_ = w_q_up[:, :, 128:]
    w_qra = np.ascontiguousarray(qr_.reshape(512, 512))
    w_qrs = np.ascontiguousarray(np.concatenate([qr_[:, :, 32:], qr_[:, :, :32]], axis=2).reshape(512, 512))
    w_kn = np.ascontiguousarray(w_kv_up[:, :, :128].reshape(512, 1024))
    w_v = np.ascontiguousarray(w_kv_up[:, :, 128:].reshape(512, 1024))
    w1 = np.asarray(inp["w1"], f32)[0]
    b1 = np.asarray(inp["b1"], f32)[0]
    shared = dict(
        vecs=vecs, rows=rows, glat=glat, ropec=ropec, consts=consts,
        w_mod=np.asarray(inp["w_mod"], f32)[0], w_in=w_in, w_kr=w_kr, w_qn=w_qn, w_qra=w_qra, w_qrs=w_qrs,
        w_kn=w_kn, w_v=w_v, w_out=np.asarray(inp["w_out"], f32)[0],
        w_router=np.ascontiguousarray(np.asarray(inp["w_router"], f32)[0].reshape(16, 128, NE).transpose(1, 0, 2)),
        w1g=np.ascontiguousarray(w1[:, :, 0::2]), w1l=np.ascontiguousarray(w1[:, :, 1::2]),
        b1g=np.ascontiguousarray(b1[:, 0::2].reshape(NE, 16, 128).transpose(0, 2, 1)),
        b1l=np.ascontiguousarray(b1[:, 1::2].reshape(NE, 16, 128).transpose(0, 2, 1)),
        b1g_r=np.ascontiguousarray(b1[:, 0::2].reshape(NE, 1, D)), b1l_r=np.ascontiguousarray(b1[:, 1::2].reshape(NE, 1, D)),
        w2=np.asarray(inp["w2"], f32)[0], b2=np.ascontiguousarray(np.asarray(inp["b2"], f32)[0].reshape(NE, 1, D)),
    )
    in_maps = []
    for core in range(8):
        b, p = core // 2, core % 2
        blks = own_blocks(p)
        rowsel = np.concatenate([np.arange(k * 128, (k + 1) * 128) for k in blks])
        if p == 0:
            zs = [(TRI_SB, TRI_ML), (ZERO, ZERO), (ONE, ONE), (TRI_SB, TRI_ML)]
        else:
            zs = [(ONE, ONE), (TRI_SB, TRI_ML), (TRI_SB, TRI_ML), (ZERO, ZERO)]
        masks = np.stack([np.stack([z[0] for z in zs]), np.stack([z[1] for z in zs])]).astype(f32)
        m = dict(shared)
        m.update(
            x_all=np.ascontiguousarray(x[b]), x_own=np.ascontiguousarray(x[b][rowsel]),
            pos_all=np.ascontiguousarray(pos[b][None, :]), pos_own=np.ascontiguousarray(pos[b][rowsel][None, :]),
            cT=fm(c[b]), masks=masks,
        )
        in_maps.append(m)
    return in_maps


_CACHE = {}


def kernel(**inp):
    in_maps = host_prepare(inp)
    if "nc" not in _CACHE:
        _CACHE["nc"] = build_program()
    res = run_bass_kernel_spmd(_CACHE["nc"], in_maps, core_ids=list(range(8)))
    outp = np.zeros((4, S_ALL, D), np.float32)
    for core in range(8):
        b, p = core // 2, core % 2
        o = res.results[core]["out"]
        for i, k in enumerate(own_blocks(p)):
            outp[b, k * 128:(k + 1) * 128] = o[i * 128:(i + 1) * 128]
    return outp
```

```python
import contextlib
import numpy as np
import concourse.bass as bass
import concourse.mybir as mybir
from concourse.bass_utils import run_bass_kernel_spmd

F32 = mybir.dt.float32
BF16 = mybir.dt.bfloat16
I32 = mybir.dt.int32
AF = mybir.ActivationFunctionType
ALU = mybir.AluOpType
AX = mybir.AxisListType

D = 2048
S_ALL = 4096
T_OWN = 2048
NE = 32
CAPR = 2048
RB = 128
NROWS = 4 * T_OWN + NE * RB
NBLK = NROWS // RB
EPS = 1e-6
PI = float(np.pi)


class Buf:
    __slots__ = ("name", "w", "r", "dsem")

    def __init__(self, name):
        self.name = name
        self.w = {}
        self.r = {}
        self.dsem = None


class Sync:
    ENG = ("pe", "act", "dve", "pool", "sp")

    def __init__(self, nc):
        self.nc = nc
        self.e = {"pe": nc.tensor, "act": nc.scalar, "dve": nc.vector, "pool": nc.gpsimd, "sp": nc.sync}
        self.sems = {}
        self.cnt = {}
        for n in self.ENG:
            self.sems[n] = nc.alloc_semaphore("es_" + n)
            self.cnt[n] = 0
        self.seen = {n: {} for n in self.ENG}
        self.free_d = []
        self.nd = 0

    def _wait(self, eng, deps):
        for k, v in deps.items():
            if v <= 0 or (k == eng and eng == "pe"):
                continue
            if self.seen[eng].get(k, 0) >= v:
                continue
            self.e[eng].wait_ge(self.sems[k], v)
            self.seen[eng][k] = v

    @staticmethod
    def _merge(d, s):
        for k, v in s.items():
            if d.get(k, 0) < v:
                d[k] = v

    def _deps(self, reads, writes):
        d = {}
        for b in reads:
            self._merge(d, b.w)
        for b in writes:
            self._merge(d, b.w)
            self._merge(d, b.r)
        return d

    def op(self, eng, fn, reads=(), writes=()):
        self._wait(eng, self._deps(reads, writes))
        ins = fn()
        self.cnt[eng] += 1
        ins.then_inc(self.sems[eng], 1)
        me = {eng: self.cnt[eng]}
        for b in reads:
            self._merge(b.r, me)
        for b in writes:
            b.w = dict(me)
            b.r = {}
        return ins

    def _dsem(self, buf):
        if buf.dsem is None:
            if self.free_d:
                buf.dsem = self.free_d.pop()
            else:
                self.nd += 1
                buf.dsem = "d%d" % self.nd
                self.sems[buf.dsem] = self.nc.alloc_semaphore(buf.dsem)
                self.cnt[buf.dsem] = 0
        return buf.dsem

    def release(self, bufs):
        for b in bufs:
            if b.dsem is not None:
                self.free_d.append(b.dsem)
                b.dsem = None

    def dma(self, sb, items, reads=(), writes=()):
        key = self._dsem(sb)
        deps = self._deps(reads, writes)
        if self.cnt[key] > 0:
            deps[key] = max(deps.get(key, 0), self.cnt[key])
        for q, fn in items:
            self._wait(q, deps)
        for q, fn in items:
            ins = fn()
            ins.then_inc(self.sems[key], 16)
            self.cnt[key] += 16
        me = {key: self.cnt[key]}
        for b in reads:
            self._merge(b.r, me)
        for b in writes:
            b.w = dict(me)
            b.r = {}

    @contextlib.contextmanager
    def guard(self, regs, thr):
        before = dict(self.cnt)
        seen0 = {k: dict(v) for k, v in self.seen.items()}
        with self.nc.If_cmp(regs, thr, "IS_GT"):
            yield
        after = dict(self.cnt)
        with self.nc.Else():
            for k, v in after.items():
                d = v - before.get(k, 0)
                if d > 0:
                    eng = k if k in self.ENG else "sp"
                    if before.get(k, 0) > 0:
                        self.e[eng].wait_ge(self.sems[k], before[k])
                    self.e[eng].sem_inc(self.sems[k], d)
        self.seen = seen0

    def barrier(self, engines=None):
        allv = {k: v for k, v in self.cnt.items() if v > 0}
        for en in (engines or self.ENG):
            self._wait(en, allv)


class Ring:
    def __init__(self, items):
        self.items = items
        self.i = 0

    def next(self):
        it = self.items[self.i % len(self.items)]
        self.i += 1
        return it


class Ctx:
    def __init__(self, nc, S, es, tag):
        self.nc, self.S, self.es, self.tag = nc, S, es, tag
        self.bufs = []
        self.n = 0

    def sb(self, name, shape, dt):
        self.n += 1
        t = self.es.enter_context(self.nc.sbuf_tensor("%s_%s%d" % (self.tag, name, self.n), shape, dt))
        b = Buf(name)
        self.bufs.append(b)
        return t, b

    def ps(self, name, shape, dt):
        self.n += 1
        t = self.es.enter_context(self.nc.psum_tensor("%s_%s%d" % (self.tag, name, self.n), shape, dt))
        b = Buf(name)
        self.bufs.append(b)
        return t, b

    def ring_sb(self, name, shape, dt, n):
        return Ring([self.sb(name, shape, dt) for _ in range(n)])

    def ring_ps(self, name, shape, dt, n):
        return Ring([self.ps(name, shape, dt) for _ in range(n)])


@contextlib.contextmanager
def phase(nc, S, tag):
    with contextlib.ExitStack() as es:
        c = Ctx(nc, S, es, tag)
        yield c
        S.barrier()
        S.release(c.bufs)


def build_program(stage=99, dbg=False):
    nc = bass.Bass("TRN2", target_bir_lowering=False)
    S = Sync(nc)

    def din(name, shape, dt=F32):
        return nc.dram_tensor(name, list(shape), dt, kind="ExternalInput").ap()

    def dscr(name, shape, dt):
        return nc.dram_tensor(name, list(shape), dt, kind="Internal").ap()

    x_all = din("x_all", [S_ALL, D])
    x_own = din("x_own", [T_OWN, D])
    pos_all = din("pos_all", [1, S_ALL], I32)
    pos_own = din("pos_own", [1, T_OWN], I32)
    cT = din("cT", [128, 16])
    vecs = din("vecs", [128, 16 * 2 + 96])
    rows = din("rows", [4, D])
    glat = din("glat", [128, 8])
    ropec = din("ropec", [128, 4])
    masks = din("masks", [2, 4, 128, 128])
    consts = din("consts", [8, 128, 128])
    w_mod = din("w_mod", [D, 6 * D])
    w_in = din("w_in", [D, 4160])
    w_kr = din("w_kr", [D, 128])
    w_qn = din("w_qn", [512, 1024])
    w_qra = din("w_qra", [512, 512])
    w_qrs = din("w_qrs", [512, 512])
    w_kn = din("w_kn", [512, 1024])
    w_v = din("w_v", [512, 1024])
    w_out = din("w_out", [D, D])
    w_router = din("w_router", [128, 16, NE])
    w1g = din("w1g", [NE, D, D])
    w1l = din("w1l", [NE, D, D])
    b1g = din("b1g", [NE, 128, 16])
    b1l = din("b1l", [NE, 128, 16])
    b1g_r = din("b1g_r", [NE, 1, D])
    b1l_r = din("b1l_r", [NE, 1, D])
    w2 = din("w2", [NE, D, D])
    b2 = din("b2", [NE, 1, D])
    out = nc.dram_tensor("out", [T_OWN, D], F32, kind="ExternalOutput").ap()
    dbg_out = nc.dram_tensor("dbg", [T_OWN, D], F32, kind="ExternalOutput").ap() if dbg else None

    mod_d = dscr("mod_d", [96, 128], F32)
    hT_all = dscr("hT_all", [D, S_ALL], BF16)
    hT_own = dscr("hT_own", [D, T_OWN], BF16)
    QT_sb = dscr("QT_sb", [1024, T_OWN], BF16)
    KT_sb = dscr("KT_sb", [1024, S_ALL], BF16)
    V_sb = dscr("V_sb", [S_ALL, 1024], BF16)
    qlnT = dscr("qlnT", [512, T_OWN], BF16)
    kvlnT = dscr("kvlnT", [512, S_ALL], BF16)
    KrT = dscr("KrT", [64, S_ALL], BF16)
    QnT = dscr("QnT", [1024, T_OWN], BF16)
    QrT = dscr("QrT", [512, T_OWN], BF16)
    KnT = dscr("KnT", [1024, S_ALL], BF16)
    V_ml = dscr("V_ml", [S_ALL, 1024], BF16)
    mixT = dscr("mixT", [D, T_OWN], BF16)
    x1_d = dscr("x1_d", [T_OWN, D], F32)
    xs_d = dscr("xs_d", [NROWS, D], BF16)
    ys_p = [dscr("ys_d%d" % q, [NROWS, 512], F32) for q in range(4)]
    cnt_d = dscr("cnt_d", [1, NE], I32)
    Bd = {}

    def db(name):
        if name not in Bd:
            Bd[name] = Buf(name)
        return Bd[name]

    qi = [0]

    def q2():
        qi[0] += 1
        return "sp"

    with contextlib.ExitStack() as glob:
        G = Ctx(nc, S, glob, "g")
        cst, b_cst = G.sb("cst", [128, 8, 128], F32)
        cstb, b_cstb = G.sb("cstb", [128, 8, 128], BF16)
        S.dma(b_cst, [("sp", lambda: nc.sync.dma_start(out=cst[:], in_=consts.rearrange("c p n -> p c n")))], writes=[b_cst])
        S.op("dve", lambda: nc.vector.tensor_copy(cstb[:], cst[:]), reads=[b_cst], writes=[b_cstb])
        ident_f = cst[:, 0, :]
        ident_b = cstb[:, 0, :]
        negtri_b = cstb[:, 1, :]
        ones_b = cstb[:, 2, :]
        tri_b = cstb[:, 3, :]
        negones33_b = cstb[:, 4, 0:33]
        bc33_b = cstb[0:33, 5, :]
        vec, b_vec = G.sb("vec", [128, 128], F32)
        S.dma(b_vec, [("sp", lambda: nc.sync.dma_start(out=vec[:], in_=vecs))], writes=[b_vec])
        gl, b_gl = G.sb("gl", [128, 8], F32)
        S.dma(b_gl, [("sp", lambda: nc.sync.dma_start(out=gl[:], in_=glat))], writes=[b_gl])
        rc, b_rc = G.sb("rc", [128, 4], F32)
        S.dma(b_rc, [("sp", lambda: nc.sync.dma_start(out=rc[:], in_=ropec))], writes=[b_rc])
        epsT, b_eps = G.sb("eps", [128, 1], F32)
        S.op("dve", lambda: nc.vector.memset(epsT[:], EPS), writes=[b_eps])
        modT, b_modT = G.sb("modT", [128, 96], F32)
        gm, b_gm = G.sb("gm", [128, 32], F32)

        with phase(nc, S, "M") as P:
            ct, b_ct = P.sb("ct", [128, 16], F32)
            S.dma(b_ct, [("sp", lambda: nc.sync.dma_start(out=ct[:], in_=cT))], writes=[b_ct])
            ca, b_ca = P.sb("ca", [128, 16], F32)
            S.op("act", lambda: nc.scalar.activation(ca[:], ct[:], AF.Silu), reads=[b_ct], writes=[b_ca])
            wr = P.ring_sb("wm", [128, 16, 512], F32, 2)
            pm, b_pm = P.ps("pm", [128, 96], F32)
            wv = w_mod.rearrange("(k p) n -> p k n", p=128)
            for g in range(24):
                wt, b_wt = wr.next()
                S.dma(b_wt, [(q2(), lambda wt=wt, g=g: nc.sync.dma_start(out=wt[:], in_=wv[:, :, g * 512:(g + 1) * 512]))], writes=[b_wt])
                for c in range(4):
                    j = g * 4 + c
                    for k in range(16):
                        S.op("pe", lambda wt=wt, c=c, k=k, j=j: nc.tensor.matmul(
                            pm[:, j:j + 1], wt[:, k, c * 128:(c + 1) * 128], ca[:, k:k + 1], start=(k == 0), stop=(k == 15)),
                            reads=[b_wt, b_ca], writes=[b_pm])
            S.op("dve", lambda: nc.vector.tensor_tensor(modT[:], pm[:], vec[:, 32:128], ALU.add), reads=[b_pm, b_vec], writes=[b_modT])
            S.op("dve", lambda: nc.vector.scalar_tensor_tensor(gm[:, 0:16], modT[:, 16:32], 1.0, vec[:, 0:16], ALU.add, ALU.mult),
                 reads=[b_modT, b_vec], writes=[b_gm])
            S.op("dve", lambda: nc.vector.scalar_tensor_tensor(gm[:, 16:32], modT[:, 64:80], 1.0, vec[:, 16:32], ALU.add, ALU.mult),
                 reads=[b_modT, b_vec], writes=[b_gm])
            pt, b_pt = P.ps("pt", [128, 128], F32)
            S.op("pe", lambda: nc.tensor.transpose(pt[0:96, :], modT[:, 0:96], ident_f), reads=[b_modT, b_cst], writes=[b_pt])
            mt, b_mt = P.sb("mt", [96, 128], F32)
            S.op("dve", lambda: nc.vector.tensor_copy(mt[:], pt[0:96, :]), reads=[b_pt], writes=[b_mt])
            S.dma(b_mt, [("sp", lambda: nc.sync.dma_start(out=mod_d, in_=mt[:]))], reads=[b_mt], writes=[db("mod_d")])

        mod_flat = mod_d.rearrange("j p -> (j p)").rearrange("(o n) -> o n", o=1)

        def bc_row(ap_row, n):
            return ap_row.to_broadcast([128, n])

        def norm_to_hT(P, src, dst, ntile, dstname, gcol, shcol):
            xr = P.ring_sb("x", [128, 4, D], F32, 2)
            xnr = P.ring_sb("xn", [128, 4, D], BF16, 2)
            sq, b_sq = P.sb("sq", [128, D], BF16)
            ssr = P.ring_sb("ss", [128, 4], F32, 2)
            ptr = P.ring_ps("ptr", [128, 2048], BF16, 2)
            hr = P.ring_sb("h", [128, 16, 512], BF16, 2)
            sv = src.rearrange("(n j p) d -> n p j d", p=128, j=4)
            dv = dst.rearrange("(k p) t -> p k t", p=128)
            for n in range(ntile):
                xt, b_x = xr.next()
                S.dma(b_x, [("sp", lambda xt=xt, n=n: nc.sync.dma_start(out=xt[:], in_=sv[n]))], writes=[b_x])
                ss, b_ss = ssr.next()
                xn, b_xn = xnr.next()
                ht, b_h = hr.next()
                for j in range(4):
                    S.op("act", lambda xt=xt, ss=ss, j=j: nc.scalar.activation(sq[:], xt[:, j, :], AF.Square, accum_out=ss[:, j:j + 1]),
                         reads=[b_x], writes=[b_sq, b_ss])
                S.op("act", lambda ss=ss: nc.scalar.activation(ss[:], ss[:], AF.Sqrt, bias=epsT[:, 0:1], scale=1.0 / D),
                     reads=[b_ss, b_eps], writes=[b_ss])
                S.op("dve", lambda ss=ss: nc.vector.reciprocal(ss[:], ss[:]), reads=[b_ss], writes=[b_ss])
                for j in range(4):
                    S.op("dve", lambda xt=xt, xn=xn, ss=ss, j=j: nc.vector.tensor_scalar(
                        xn[:, j, :], xt[:, j, :], ss[:, j:j + 1], None, op0=ALU.mult), reads=[b_x, b_ss], writes=[b_xn])
                for j in range(4):
                    pt_, b_p = ptr.next()
                    for k in range(16):
                        S.op("pe", lambda pt_=pt_, xn=xn, j=j, k=k: nc.tensor.transpose(
                            pt_[:, k * 128:(k + 1) * 128], xn[:, j, k * 128:(k + 1) * 128], ident_b), reads=[b_xn, b_cstb], writes=[b_p])
                    for k in range(16):
                        eng = "dve" if k % 2 == 0 else "pool"
                        if eng == "pool":
                            eng = "dve"
                        S.op(eng, lambda pt_=pt_, ht=ht, j=j, k=k: nc.vector.tensor_scalar(
                            ht[:, k, j * 128:(j + 1) * 128], pt_[:, k * 128:(k + 1) * 128],
                            gm[:, gcol + k:gcol + k + 1], modT[:, shcol + k:shcol + k + 1], op0=ALU.mult, op1=ALU.add),
                            reads=[b_p, b_gm, b_modT], writes=[b_h])
                S.dma(b_h, [("sp", lambda ht=ht, n=n: nc.sync.dma_start(out=dv[:, :, n * 512:(n + 1) * 512], in_=ht[:]))],
                      reads=[b_h], writes=[db(dstname)])

        if stage >= 1:
            with phase(nc, S, "A") as P:
                norm_to_hT(P, x_all, hT_all, 8, "hT_all", 0, 0)
            with phase(nc, S, "A2") as P:
                norm_to_hT(P, x_own, hT_own, 4, "hT_own", 0, 0)

        def load_w(P, ring, Wap, KC, c0, ncols):
            wt, b_w = ring.next()
            wv_ = Wap.rearrange("(k p) n -> p k n", p=128)
            S.dma(b_w, [("pool", lambda: nc.gpsimd.dma_start(out=wt[:, 0:KC, 0:ncols], in_=wv_[:, :, c0:c0 + ncols]))], writes=[b_w])
            return wt, b_w

        def load_a(ring, Aap, aname, KC, t0):
            at, b_a = ring.next()
            av = Aap.rearrange("(k p) t -> p k t", p=128)
            S.dma(b_a, [("sp", lambda: nc.sync.dma_start(out=at[:, 0:KC, :], in_=av[:, :, t0:t0 + 512]))], reads=[db(aname)], writes=[b_a])
            return at, b_a

        def gemm_fm(P, Wap, c0, N, Aap, aname, T, KC, dst, dname, scale, rings):
            wr_, ar_, pr_, sr_ = rings
            dv = dst.rearrange("(c p) t -> p c t", p=128)
            for g in range(N // 512):
                wt, b_w = load_w(P, wr_, Wap, KC, c0 + g * 512, 512)
                for tt in range(T // 512):
                    at, b_a = load_a(ar_, Aap, aname, KC, tt * 512)
                    st, b_s = sr_.next()
                    for c in range(4):
                        ps_, b_p = pr_.next()
                        for k in range(KC):
                            S.op("pe", lambda ps_=ps_, wt=wt, at=at, c=c, k=k: nc.tensor.matmul(
                                ps_[:], wt[:, k, c * 128:(c + 1) * 128], at[:, k, :], start=(k == 0), stop=(k == KC - 1)),
                                reads=[b_w, b_a], writes=[b_p])
                        S.op("act", lambda ps_=ps_, st=st, c=c: nc.scalar.activation(st[:, c, :], ps_[:], AF.Copy, scale=scale),
                             reads=[b_p], writes=[b_s])
                    S.dma(b_s, [("sp", lambda st=st, g=g, tt=tt: nc.sync.dma_start(
                        out=dv[:, g * 4:(g + 1) * 4, tt * 512:(tt + 1) * 512], in_=st[:]))], reads=[b_s], writes=[db(dname)])

        def gemm_tm(P, Wap, c0, N, Aap, aname, T, KC, dst, dname, rings):
            wr_, ar_, pr_, sr_ = rings
            dv = dst.rearrange("(n j p) c -> n p j c", p=128, j=4)
            for g in range(N // 512):
                wt, b_w = load_w(P, wr_, Wap, KC, c0 + g * 512, 512)
                for tt in range(T // 512):
                    at, b_a = load_a(ar_, Aap, aname, KC, tt * 512)
                    st, b_s = sr_.next()
                    for j in range(4):
                        ps_, b_p = pr_.next()
                        for k in range(KC):
                            S.op("pe", lambda ps_=ps_, wt=wt, at=at, j=j, k=k: nc.tensor.matmul(
                                ps_[:], at[:, k, j * 128:(j + 1) * 128], wt[:, k, :], start=(k == 0), stop=(k == KC - 1)),
                                reads=[b_w, b_a], writes=[b_p])
                        S.op("act", lambda ps_=ps_, st=st, j=j: nc.scalar.activation(st[:, j, :], ps_[:], AF.Copy),
                             reads=[b_p], writes=[b_s])
                    S.dma(b_s, [("sp", lambda st=st, g=g, tt=tt: nc.sync.dma_start(
                        out=dv[tt][:, :, g * 512:(g + 1) * 512], in_=st[:]))], reads=[b_s], writes=[db(dname)])

        def latent_norm(P, c0, Aap, aname, T, gcol, dst, dname, rings):
            wr_, ar_, pr_, sr_ = rings
            dv = dst.rearrange("(c p) t -> p c t", p=128)
            l32, b_l = P.sb("l32", [128, 4, 512], F32)
            lsq, b_q = P.sb("lsq", [128, 4, 512], BF16)
            rs, b_rs = P.sb("rs", [128, 512], F32)
            wt, b_w = load_w(P, wr_, w_in, 16, c0, 512)
            for tt in range(T // 512):
                at, b_a = load_a(ar_, Aap, aname, 16, tt * 512)
                st, b_s = sr_.next()
                for c in range(4):
                    ps_, b_p = pr_.next()
                    for k in range(16):
                        S.op("pe", lambda ps_=ps_, at=at, c=c, k=k: nc.tensor.matmul(
                            ps_[:], wt[:, k, c * 128:(c + 1) * 128], at[:, k, :], start=(k == 0), stop=(k == 15)),
                            reads=[b_w, b_a], writes=[b_p])
                    S.op("act", lambda ps_=ps_, c=c: nc.scalar.activation(l32[:, c, :], ps_[:], AF.Copy), reads=[b_p], writes=[b_l])
                    S.op("dve", lambda c=c: nc.vector.tensor_tensor(lsq[:, c, :], l32[:, c, :], l32[:, c, :], ALU.mult), reads=[b_l], writes=[b_q])
                ps_, b_p = pr_.next()
                for c in range(4):
                    S.op("pe", lambda ps_=ps_, c=c: nc.tensor.matmul(ps_[:], ones_b, lsq[:, c, :], start=(c == 0), stop=(c == 3)),
                         reads=[b_q, b_cstb], writes=[b_p])
                S.op("act", lambda ps_=ps_: nc.scalar.activation(rs[:], ps_[:], AF.Sqrt, bias=epsT[:, 0:1], scale=1.0 / 512),
                     reads=[b_p, b_eps], writes=[b_rs])
                S.op("dve", lambda: nc.vector.reciprocal(rs[:], rs[:]), reads=[b_rs], writes=[b_rs])
                for c in range(4):
                    S.op("dve", lambda st=st, c=c: nc.vector.scalar_tensor_tensor(
                        st[:, c, :], l32[:, c, :], gl[:, gcol + c:gcol + c + 1], rs[:], ALU.mult, ALU.mult),
                        reads=[b_l, b_gl, b_rs], writes=[b_s])
                S.dma(b_s, [("sp", lambda st=st, tt=tt: nc.sync.dma_start(out=dv[:, :, tt * 512:(tt + 1) * 512], in_=st[:]))],
                      reads=[b_s], writes=[db(dname)])

        def rope_tables(P, pos_ap, T, scale, name):
            pi_, b_pi = P.sb("posi", [128, T], I32)
            S.dma(b_pi, [("sp", lambda: nc.sync.dma_start(out=pi_[:], in_=pos_ap.to_broadcast([128, T])))], writes=[b_pi])
            ang, b_an = P.sb("ang", [128, T], F32)
            S.op("dve", lambda: nc.vector.tensor_copy(ang[:], pi_[:]), reads=[b_pi], writes=[b_an])
            C, b_C = P.sb("C" + name, [128, T], F32)
            Sg, b_S = P.sb("S" + name, [128, T], F32)
            tmp, b_t = P.sb("rtmp", [128, T], F32)
            ni, b_ni = P.sb("rni", [128, T], I32)
            S.op("dve", lambda: nc.vector.tensor_scalar(ang[:], ang[:], rc[:, 0:1], None, op0=ALU.mult), reads=[b_an, b_rc], writes=[b_an])
            for dst_, b_d, offs in ((Sg, b_S, 0.0), (C, b_C, 0.5 * PI)):
                S.op("dve", lambda offs=offs: nc.vector.tensor_scalar(tmp[:], ang[:], offs, 1.0 / (2 * PI), op0=ALU.add, op1=ALU.mult), reads=[b_an], writes=[b_t])
                S.op("dve", lambda: nc.vector.tensor_copy(ni[:], tmp[:]), reads=[b_t], writes=[b_ni])
                S.op("dve", lambda: nc.vector.tensor_copy(tmp[:], ni[:]), reads=[b_ni], writes=[b_t])
                S.op("dve", lambda: nc.vector.scalar_tensor_tensor(tmp[:], tmp[:], -2 * PI, ang[:], ALU.mult, ALU.add), reads=[b_t, b_an], writes=[b_t])
                if offs != 0.0:
                    S.op("dve", lambda offs=offs: nc.vector.tensor_scalar(tmp[:], tmp[:], offs, None, op0=ALU.add), reads=[b_t], writes=[b_t])
                S.op("dve", lambda dst_=dst_: nc.vector.tensor_scalar(dst_[:], tmp[:], PI, 2 * PI, op0=ALU.is_gt, op1=ALU.mult), reads=[b_t], writes=[b_d])
                S.op("dve", lambda dst_=dst_: nc.vector.tensor_tensor(tmp[:], tmp[:], dst_[:], ALU.subtract), reads=[b_t, b_d], writes=[b_t])
                S.op("dve", lambda dst_=dst_: nc.vector.tensor_scalar(dst_[:], tmp[:], -PI, 2 * PI, op0=ALU.is_lt, op1=ALU.mult), reads=[b_t], writes=[b_d])
                S.op("dve", lambda dst_=dst_: nc.vector.tensor_tensor(tmp[:], tmp[:], dst_[:], ALU.add), reads=[b_t, b_d], writes=[b_t])
                S.op("act", lambda dst_=dst_: nc.scalar.activation(dst_[:], tmp[:], AF.Sin), reads=[b_t], writes=[b_d])
            S.op("dve", lambda: nc.vector.tensor_scalar(Sg[:], Sg[:], rc[:, 1:2], float(scale), op0=ALU.mult, op1=ALU.mult),
                 reads=[b_S, b_rc], writes=[b_S])
            if scale != 1.0:
                S.op("dve", lambda: nc.vector.tensor_scalar(C[:], C[:], float(scale), None, op0=ALU.mult), reads=[b_C], writes=[b_C])
            return (C, b_C), (Sg, b_S)

        def rope_proj(P, Wa, ca0, Ws, cs0, nh, Aap, aname, T, KC, CS, dst, dname, rings):
            wr_, ar_, pr_, sr_ = rings
            (C, b_C), (Sg, b_S) = CS
            wa, b_wa = load_w(P, wr_, Wa, KC, ca0, nh * 64)
            ws, b_ws = load_w(P, wr_, Ws, KC, cs0, nh * 64)
            t1, b_t1 = P.sb("rt1", [64, 512], F32)
            t2, b_t2 = P.sb("rt2", [64, 512], F32)
            for tt in range(T // 512):
                at, b_a = load_a(ar_, Aap, aname, KC, tt * 512)
                for h in range(nh):
                    pa, b_pa = pr_.next()
                    pb, b_pb = pr_.next()
                    for k in range(KC):
                        S.op("pe", lambda pa=pa, at=at, h=h, k=k: nc.tensor.matmul(
                            pa[0:64, :], wa[:, k, h * 64:(h + 1) * 64], at[:, k, :], start=(k == 0), stop=(k == KC - 1)),
                            reads=[b_wa, b_a], writes=[b_pa])
                    for k in range(KC):
                        S.op("pe", lambda pb=pb, at=at, h=h, k=k: nc.tensor.matmul(
                            pb[0:64, :], ws[:, k, h * 64:(h + 1) * 64], at[:, k, :], start=(k == 0), stop=(k == KC - 1)),
                            reads=[b_ws, b_a], writes=[b_pb])
                    st, b_s = sr_.next()
                    S.op("dve", lambda pa=pa, tt=tt: nc.vector.tensor_tensor(t1[:], pa[0:64, :], C[0:64, tt * 512:(tt + 1) * 512], ALU.mult),
                         reads=[b_pa, b_C], writes=[b_t1])
                    S.op("dve", lambda pb=pb, tt=tt: nc.vector.tensor_tensor(t2[:], pb[0:64, :], Sg[0:64, tt * 512:(tt + 1) * 512], ALU.mult),
                         reads=[b_pb, b_S], writes=[b_t2])
                    S.op("dve", lambda st=st: nc.vector.tensor_tensor(st[0:64, 0, :], t1[:], t2[:], ALU.add), reads=[b_t1, b_t2], writes=[b_s])
                    S.dma(b_s, [("sp", lambda st=st, h=h, tt=tt: nc.sync.dma_start(
                        out=dst[h * 64:(h + 1) * 64, tt * 512:(tt + 1) * 512], in_=st[0:64, 0, :]))], reads=[b_s], writes=[db(dname)])

        if stage >= 2:
            with phase(nc, S, "B") as P:
                rings = (P.ring_sb("w", [128, 16, 512], BF16, 2), P.ring_sb("a", [128, 16, 512], BF16, 2),
                         P.ring_ps("p", [128, 512], F32, 6), P.ring_sb("st", [128, 4, 512], BF16, 2))
                gemm_fm(P, w_in, 0, 1024, hT_own, "hT_own", T_OWN, 16, QT_sb, "QT_sb", 128 ** -0.5, rings)
                gemm_fm(P, w_in, 1024, 1024, hT_all, "hT_all", S_ALL, 16, KT_sb, "KT_sb", 1.0, rings)
                gemm_tm(P, w_in, 2048, 1024, hT_all, "hT_all", S_ALL, 16, V_sb, "V_sb", rings)
                latent_norm(P, 3072, hT_own, "hT_own", T_OWN, 0, qlnT, "qlnT", rings)
                latent_norm(P, 3584, hT_all, "hT_all", S_ALL, 4, kvlnT, "kvlnT", rings)
            with phase(nc, S, "B2") as P:
                rings = (P.ring_sb("w", [128, 16, 512], BF16, 2), P.ring_sb("a", [128, 16, 512], BF16, 2),
                         P.ring_ps("p", [128, 512], F32, 6), P.ring_sb("st", [128, 4, 512], BF16, 2))
                CSk = rope_tables(P, pos_all, S_ALL, 1.0, "k")
                rope_proj(P, w_kr, 0, w_kr, 64, 1, hT_all, "hT_all", S_ALL, 16, CSk, KrT, "KrT", rings)
            with phase(nc, S, "B3") as P:
                rings = (P.ring_sb("w", [128, 16, 512], BF16, 2), P.ring_sb("a", [128, 16, 512], BF16, 2),
                         P.ring_ps("p", [128, 512], F32, 6), P.ring_sb("st", [128, 4, 512], BF16, 2))
                CSq = rope_tables(P, pos_own, T_OWN, 192 ** -0.5, "q")
                rope_proj(P, w_qra, 0, w_qrs, 0, 8, qlnT, "qlnT", T_OWN, 4, CSq, QrT, "QrT", rings)
                gemm_fm(P, w_qn, 0, 1024, qlnT, "qlnT", T_OWN, 4, QnT, "QnT", 192 ** -0.5, rings)
                gemm_fm(P, w_kn, 0, 1024, kvlnT, "kvlnT", S_ALL, 4, KnT, "KnT", 1.0, rings)
                gemm_tm(P, w_v, 0, 1024, kvlnT, "kvlnT", S_ALL, 4, V_ml, "V_ml", rings)

        def attention(P, is_sb):
            mk, b_mk = P.sb("mk", [128, 4, 128], F32)
            S.dma(b_mk, [("sp", lambda: nc.sync.dma_start(out=mk[:], in_=masks[0 if is_sb else 1].rearrange("z p n -> p z n")))], writes=[b_mk])
            mkb, b_mkb = P.sb("mkb", [128, 4, 128], BF16)
            S.op("dve", lambda: nc.vector.tensor_copy(mkb[:], mk[:]), reads=[b_mk], writes=[b_mkb])
            ktr = P.ring_sb("kt", [128, S_ALL], BF16, 2)
            vr = P.ring_sb("v", [128, 32, 128], BF16, 2)
            qr = P.ring_sb("q", [128, T_OWN], BF16, 2)
            if not is_sb:
                krt, b_krt = P.sb("krt", [64, S_ALL], BF16)
                S.dma(b_krt, [("sp", lambda: nc.sync.dma_start(out=krt[:], in_=KrT))], reads=[db("KrT")], writes=[b_krt])
                qrr = P.ring_sb("qr", [64, T_OWN], BF16, 2)
            pA = P.ring_ps("pA", [128, 512], F32, 2)
            pB = P.ring_ps("pB", [128, 512], F32, 2)
            pO = P.ring_ps("pO", [128, 512], F32, 2)
            if is_sb:
                pC, b_pC = P.ps("pC", [128, 512], F32)
                e32r = P.ring_sb("e32", [128, 512], F32, 2)
                spr = P.ring_sb("sp", [128, 512], BF16, 2)
                Tt, b_T = P.sb("T", [33, 512], F32)
                THL, b_THL = P.sb("THL", [33, 512], BF16)
            else:
                pD = P.ring_ps("pD", [128, 512], F32, 2)
                rdn, b_rdn = P.sb("rdn", [128, 512], F32)
            ar = P.ring_sb("a", [128, 512], BF16, 3)
            mor = P.ring_sb("mo", [128, 512], BF16, 2)
            zer, b_zer = P.sb("zer", [128, 128], BF16)
            S.op("dve", lambda: nc.vector.memset(zer[:], 0.0), writes=[b_zer])
            KT = KT_sb if is_sb else KnT
            QT = QT_sb if is_sb else QnT
            VV = V_sb if is_sb else V_ml
            kn, qn, vn = ("KT_sb", "QT_sb", "V_sb") if is_sb else ("KnT", "QnT", "V_ml")
            for h in range(8):
                kt, b_kt = ktr.next()
                S.dma(b_kt, [("sp", lambda kt=kt, h=h: nc.sync.dma_start(out=kt[:], in_=KT[h * 128:(h + 1) * 128, :]))], reads=[db(kn)], writes=[b_kt])
                vt, b_vt = vr.next()
                S.dma(b_vt, [("sp", lambda vt=vt, h=h: nc.sync.dma_start(
                    out=vt[:], in_=VV.rearrange("(kb p) c -> p kb c", p=128)[:, :, h * 128:(h + 1) * 128]))], reads=[db(vn)], writes=[b_vt])
                qt, b_qt = qr.next()
                S.dma(b_qt, [("sp", lambda qt=qt, h=h: nc.sync.dma_start(out=qt[:], in_=QT[h * 128:(h + 1) * 128, :]))], reads=[db(qn)], writes=[b_qt])
                if not is_sb:
                    qrt, b_qrt = qrr.next()
                    S.dma(b_qrt, [("sp", lambda qrt=qrt, h=h: nc.sync.dma_start(out=qrt[:], in_=QrT[h * 64:(h + 1) * 64, :]))], reads=[db("QrT")], writes=[b_qrt])
                for m in range(4):
                    q0 = m * 512
                    po, b_po = pO.next()
                    S.op("pe", lambda po=po, qt=qt, q0=q0: nc.tensor.matmul(po[:], zer[:], qt[:, q0:q0 + 512], start=True, stop=False),
                         reads=[b_zer, b_qt], writes=[b_po])
                    if is_sb:
                        S.op("dve", lambda: nc.vector.memset(Tt[:], 0.0), writes=[b_T])
                        S.op("dve", lambda: nc.vector.memset(THL[:], 0.0), writes=[b_THL])
                    else:
                        pd, b_pd = pD.next()
                        S.op("pe", lambda pd=pd, qt=qt, q0=q0: nc.tensor.matmul(pd[:], zer[:], qt[:, q0:q0 + 512], start=True, stop=False),
                             reads=[b_zer, b_qt], writes=[b_pd])
                    kmax = 8 * m + 7
                    for kb in range(kmax, -1, -1):
                        last = (kb == 0)
                        smin = 0
                        while 8 * m + 1 + 2 * smin < kb:
                            smin += 1
                        c0 = smin * 128
                        cs = slice(c0, 512)
                        qs = slice(q0 + c0, q0 + 512)
                        ks = slice(kb * 128, (kb + 1) * 128)
                        bslot = None
                        if kb >= 8 * m:
                            r = kb - 8 * m
                            bslot, zone = r // 2, (r // 2 % 2) * 2 + (r % 2)
                        pa, b_pa = pA.next()
                        at, b_at = ar.next()
                        if is_sb:
                            S.op("pe", lambda pa=pa, kt=kt, qt=qt, ks=ks, qs=qs, cs=cs: nc.tensor.matmul(pa[:, cs], kt[:, ks], qt[:, qs], start=True, stop=True),
                                 reads=[b_kt, b_qt], writes=[b_pa])
                            e32, b_e = e32r.next()
                            spm, b_sp = spr.next()
                            S.op("act", lambda pa=pa, e32=e32, cs=cs: nc.scalar.activation(e32[:, cs], pa[:, cs], AF.Exp), reads=[b_pa], writes=[b_e])
                            S.op("act", lambda e32=e32, spm=spm, cs=cs: nc.scalar.activation(spm[:, cs], e32[:, cs], AF.Ln, bias=1.0), reads=[b_e], writes=[b_sp])
                            if bslot is not None:
                                bs = slice(bslot * 128, (bslot + 1) * 128)
                                S.op("pool", lambda spm=spm, bs=bs, zone=zone: nc.gpsimd.tensor_tensor(spm[:, bs], spm[:, bs], mkb[:, zone, :], ALU.mult),
                                     reads=[b_sp, b_mkb], writes=[b_sp])
                            pb, b_pb = pB.next()
                            S.op("pe", lambda pb=pb, kt=kt, qt=qt, ks=ks, qs=qs, cs=cs: nc.tensor.matmul(pb[:, cs], kt[:, ks], qt[:, qs], start=True, stop=False),
                                 reads=[b_kt, b_qt], writes=[b_pb])
                            S.op("pe", lambda pb=pb, spm=spm, cs=cs: nc.tensor.matmul(pb[:, cs], negtri_b, spm[:, cs], start=False, stop=False),
                                 reads=[b_sp, b_cstb], writes=[b_pb])
                            S.op("pe", lambda pb=pb, cs=cs: nc.tensor.matmul(pb[:, cs], bc33_b, THL[0:33, cs], start=False, stop=True),
                                 reads=[b_THL, b_cstb], writes=[b_pb])
                            S.op("act", lambda pb=pb, at=at, cs=cs: nc.scalar.activation(at[:, cs], pb[:, cs], AF.Exp), reads=[b_pb], writes=[b_at])
                            if not last:
                                S.op("pe", lambda spm=spm, cs=cs: nc.tensor.matmul(pC[0:33, cs], negones33_b, spm[:, cs], start=True, stop=True),
                                     reads=[b_sp, b_cstb], writes=[b_pC])
                                S.op("dve", lambda cs=cs: nc.vector.tensor_tensor(Tt[:, cs], Tt[:, cs], pC[0:33, cs], ALU.add), reads=[b_pC, b_T], writes=[b_T])
                                S.op("dve", lambda cs=cs: nc.vector.tensor_copy(THL[:, cs], Tt[:, cs]), reads=[b_T], writes=[b_THL])
                                S.op("dve", lambda cs=cs: nc.vector.tensor_tensor(THL[32:33, cs], Tt[32:33, cs], THL[32:33, cs], ALU.subtract),
                                     reads=[b_T, b_THL], writes=[b_THL])
                        else:
                            S.op("pe", lambda pa=pa, kt=kt, qt=qt, ks=ks, qs=qs, cs=cs: nc.tensor.matmul(pa[:, cs], kt[:, ks], qt[:, qs], start=True, stop=False),
                                 reads=[b_kt, b_qt], writes=[b_pa])
                            S.op("pe", lambda pa=pa, qrt=qrt, ks=ks, qs=qs, cs=cs: nc.tensor.matmul(pa[:, cs], krt[:, ks], qrt[:, qs], start=False, stop=True),
                                 reads=[b_krt, b_qrt], writes=[b_pa])
                            S.op("act", lambda pa=pa, at=at, cs=cs: nc.scalar.activation(at[:, cs], pa[:, cs], AF.Exp), reads=[b_pa], writes=[b_at])
                        if bslot is not None:
                            bs = slice(bslot * 128, (bslot + 1) * 128)
                            S.op("pool", lambda at=at, bs=bs, zone=zone: nc.gpsimd.tensor_tensor(at[:, bs], at[:, bs], mkb[:, zone, :], ALU.mult),
                                 reads=[b_at, b_mkb], writes=[b_at])
                        S.op("pe", lambda po=po, vt=vt, at=at, kb=kb, cs=cs, last=last: nc.tensor.matmul(po[:, cs], vt[:, kb, :], at[:, cs], start=False, stop=last),
                             reads=[b_vt, b_at], writes=[b_po])
                        if not is_sb:
                            S.op("pe", lambda pd=pd, at=at, cs=cs, last=last: nc.tensor.matmul(pd[:, cs], ones_b, at[:, cs], start=False, stop=last),
                                 reads=[b_cstb, b_at], writes=[b_pd])
                    mo, b_mo = mor.next()
                    if is_sb:
                        S.op("act", lambda po=po, mo=mo: nc.scalar.activation(mo[:], po[:], AF.Copy), reads=[b_po], writes=[b_mo])
                    else:
                        S.op("dve", lambda pd=pd: nc.vector.reciprocal(rdn[:], pd[:]), reads=[b_pd], writes=[b_rdn])
                        S.op("dve", lambda po=po, mo=mo: nc.vector.tensor_tensor(mo[:], po[:], rdn[:], ALU.mult), reads=[b_po, b_rdn], writes=[b_mo])
                    r0 = (0 if is_sb else 1024) + h * 128
                    S.dma(b_mo, [("sp", lambda mo=mo, r0=r0, q0=q0: nc.sync.dma_start(out=mixT[r0:r0 + 128, q0:q0 + 512], in_=mo[:]))],
                          reads=[b_mo], writes=[db("mixT")])

        if stage >= 3:
            with phase(nc, S, "C1") as P:
                attention(P, True)
            with phase(nc, S, "C2") as P:
                attention(P, False)

        if stage >= 4:
            with phase(nc, S, "D") as P:
                g1, b_g1 = P.sb("g1", [128, D], F32)
                S.dma(b_g1, [("sp", lambda: nc.sync.dma_start(out=g1[:], in_=bc_row(mod_flat[:, 32 * 128:48 * 128], D)))], reads=[db("mod_d")], writes=[b_g1])
                wr_ = P.ring_sb("w", [128, 16, 512], BF16, 2)
                ar_ = P.ring_sb("a", [128, 16, 512], BF16, 2)
                pr_ = P.ring_ps("p", [128, 512], F32, 4)
                xor_ = P.ring_sb("xo", [128, 512], F32, 3)
                tr_ = P.ring_sb("t", [128, 512], F32, 3)
                for g in range(4):
                    wt, b_w = load_w(P, wr_, w_out, 16, g * 512, 512)
                    for tt in range(4):
                        at, b_a = load_a(ar_, mixT, "mixT", 16, tt * 512)
                        for j in range(4):
                            ps_, b_p = pr_.next()
                            for k in range(16):
                                S.op("pe", lambda ps_=ps_, wt=wt, at=at, j=j, k=k: nc.tensor.matmul(
                                    ps_[:], at[:, k, j * 128:(j + 1) * 128], wt[:, k, :], start=(k == 0), stop=(k == 15)),
                                    reads=[b_w, b_a], writes=[b_p])
                            rws = slice((tt * 4 + j) * 128, (tt * 4 + j + 1) * 128)
                            cls = slice(g * 512, (g + 1) * 512)
                            xo, b_xo = xor_.next()
                            S.dma(b_xo, [("sp", lambda xo=xo, rws=rws, cls=cls: nc.sync.dma_start(out=xo[:], in_=x_own[rws, cls]))], writes=[b_xo])
                            t_, b_t = tr_.next()
                            S.op("dve", lambda ps_=ps_, t_=t_, cls=cls: nc.vector.tensor_tensor(t_[:], ps_[:], g1[:, cls], ALU.mult), reads=[b_p, b_g1], writes=[b_t])
                            S.op("pool", lambda t_=t_, xo=xo: nc.gpsimd.tensor_tensor(t_[:], t_[:], xo[:], ALU.add), reads=[b_t, b_xo], writes=[b_t])
                            S.dma(b_t, [("sp", lambda t_=t_, rws=rws, cls=cls: nc.sync.dma_start(out=x1_d[rws, cls], in_=t_[:]))], reads=[b_t], writes=[db("x1_d")])


        SMAX = T_OWN // 128
        dest_i, b_di = G.sb("dest_i", [128, 64], I32)
        w4, b_w4 = G.sb("w4", [128, 16, 4], F32)
        cnt_i, b_cni = G.sb("cnt_i", [128, NE], I32)
        idx_i, b_idx = G.sb("idx_i", [128, NE * SMAX], I32)
        if stage >= 5:
            with phase(nc, S, "R") as P:
                gm2, b_gm2 = P.sb("gm2", [128, D], F32)
                sh2, b_sh2 = P.sb("sh2", [128, D], F32)
                tmpD, b_tD = P.sb("tmpD", [128, D], F32)
                S.dma(b_gm2, [("sp", lambda: nc.sync.dma_start(out=gm2[:], in_=bc_row(mod_flat[:, 64 * 128:80 * 128], D)))], reads=[db("mod_d")], writes=[b_gm2])
                S.dma(b_tD, [("sp", lambda: nc.sync.dma_start(out=tmpD[:], in_=bc_row(rows[2:3, :], D)))], writes=[b_tD])
                S.dma(b_sh2, [("sp", lambda: nc.sync.dma_start(out=sh2[:], in_=bc_row(mod_flat[:, 48 * 128:64 * 128], D)))], reads=[db("mod_d")], writes=[b_sh2])
                S.op("dve", lambda: nc.vector.scalar_tensor_tensor(gm2[:], gm2[:], 1.0, tmpD[:], ALU.add, ALU.mult), reads=[b_gm2, b_tD], writes=[b_gm2])
                brt, b_brt = P.sb("brt", [128, NE], F32)
                S.dma(b_brt, [("sp", lambda: nc.sync.dma_start(out=brt[:], in_=bc_row(rows[1:2, 0:NE], NE)))], writes=[b_brt])
                wrs, b_wrs = P.sb("wrs", [128, 16, NE], F32)
                S.dma(b_wrs, [("sp", lambda: nc.sync.dma_start(out=wrs[:], in_=w_router))], writes=[b_wrs])
                h2b, b_h2b = P.sb("h2b", [128, 16 * D], BF16)
                maskf, b_mf = P.sb("maskf", [128, 16, NE], F32)
                wgt, b_wg = P.sb("wgt", [128, 16, NE], F32)
                posf, b_pf = P.sb("posf", [128, 16, NE], F32)
                cntb, b_cn = P.sb("cntb", [128, NE], F32)
                S.op("dve", lambda: nc.vector.memset(cntb[:], 0.0), writes=[b_cn])
                x1r = P.ring_sb("x1", [128, D], F32, 2)
                h2r = P.ring_sb("h2", [128, D], F32, 2)
                h2T, b_h2T = P.sb("h2T", [128, 16, 128], F32)
                sq, b_sq = P.sb("sq", [128, D], BF16)
                sm = P.ring_sb("sm", [128, 64], F32, 2)
                ex, b_ex = P.sb("ex", [128, NE], F32)
                mb, b_mb = P.sb("mb", [128, NE], BF16)
                ptr = P.ring_ps("pt", [128, 512], F32, 2)
                pl, b_pl = P.ps("pl", [128, NE], F32)
                pp, b_pp = P.ps("pp", [128, NE], F32)
                pc, b_pc = P.ps("pc", [128, NE], F32)
                for i in range(16):
                    xb, b_xb = x1r.next()
                    S.dma(b_xb, [("sp", lambda xb=xb, i=i: nc.sync.dma_start(out=xb[:], in_=x1_d[i * 128:(i + 1) * 128, :]))], reads=[db("x1_d")], writes=[b_xb])
                    s_, b_s = sm.next()
                    S.op("act", lambda xb=xb, s_=s_: nc.scalar.activation(sq[:], xb[:], AF.Square, accum_out=s_[:, 0:1]), reads=[b_xb], writes=[b_sq, b_s])
                    S.op("act", lambda s_=s_: nc.scalar.activation(s_[:, 0:1], s_[:, 0:1], AF.Sqrt, bias=epsT[:, 0:1], scale=1.0 / D), reads=[b_s, b_eps], writes=[b_s])
                    S.op("dve", lambda s_=s_: nc.vector.reciprocal(s_[:, 0:1], s_[:, 0:1]), reads=[b_s], writes=[b_s])
                    h2, b_h2 = h2r.next()
                    S.op("dve", lambda xb=xb, h2=h2, s_=s_: nc.vector.scalar_tensor_tensor(h2[:], xb[:], s_[:, 0:1], gm2[:], ALU.mult, ALU.mult), reads=[b_xb, b_s, b_gm2], writes=[b_h2])
                    S.op("pool", lambda h2=h2: nc.gpsimd.tensor_tensor(h2[:], h2[:], sh2[:], ALU.add), reads=[b_h2, b_sh2], writes=[b_h2])
                    S.op("act", lambda h2=h2, i=i: nc.scalar.activation(h2b[:, i * D:(i + 1) * D], h2[:], AF.Copy), reads=[b_h2], writes=[b_h2b])
                    for g in range(4):
                        pt_, b_p = ptr.next()
                        for c in range(4):
                            k = g * 4 + c
                            S.op("pe", lambda pt_=pt_, h2=h2, c=c, k=k: nc.tensor.transpose(pt_[:, c * 128:(c + 1) * 128], h2[:, k * 128:(k + 1) * 128], ident_f),
                                 reads=[b_h2, b_cst], writes=[b_p])
                        S.op("dve", lambda pt_=pt_, g=g: nc.vector.tensor_copy(h2T[:, g * 4:(g + 1) * 4, :], pt_[:].rearrange("p (c n) -> p c n", c=4)), reads=[b_p], writes=[b_h2T])
                    for k in range(16):
                        S.op("pe", lambda k=k: nc.tensor.matmul(pl[:], h2T[:, k, :], wrs[:, k, :], start=(k == 0), stop=(k == 15)), reads=[b_h2T, b_wrs], writes=[b_pl])
                    lg = s_[:, 32:64]
                    S.op("dve", lambda lg=lg: nc.vector.tensor_tensor(lg, pl[:], brt[:], ALU.add), reads=[b_pl, b_brt], writes=[b_s])
                    S.op("dve", lambda s_=s_, lg=lg: nc.vector.max(out=s_[:, 8:16], in_=lg), reads=[b_s], writes=[b_s])
                    S.op("dve", lambda s_=s_: nc.vector.tensor_scalar(s_[:, 16:17], s_[:, 8:9], -1.0, None, op0=ALU.mult), reads=[b_s], writes=[b_s])
                    S.op("dve", lambda s_=s_, lg=lg, i=i: nc.vector.tensor_scalar(maskf[:, i, :], lg, s_[:, 11:12], None, op0=ALU.is_ge), reads=[b_s], writes=[b_mf])
                    S.op("act", lambda s_=s_, lg=lg: nc.scalar.activation(ex[:], lg, AF.Exp, bias=s_[:, 16:17], scale=1.0), reads=[b_s], writes=[b_ex])
                    S.op("dve", lambda i=i: nc.vector.tensor_tensor(ex[:], ex[:], maskf[:, i, :], ALU.mult), reads=[b_ex, b_mf], writes=[b_ex])
                    S.op("dve", lambda s_=s_: nc.vector.reduce_sum(s_[:, 17:18], ex[:], AX.X), reads=[b_ex], writes=[b_s])
                    S.op("dve", lambda s_=s_: nc.vector.reciprocal(s_[:, 17:18], s_[:, 17:18]), reads=[b_s], writes=[b_s])
                    S.op("dve", lambda s_=s_, i=i: nc.vector.tensor_scalar(wgt[:, i, :], ex[:], s_[:, 17:18], None, op0=ALU.mult), reads=[b_ex, b_s], writes=[b_wg])
                    S.op("dve", lambda i=i: nc.vector.tensor_copy(mb[:], maskf[:, i, :]), reads=[b_mf], writes=[b_mb])
                    S.op("pe", lambda: nc.tensor.matmul(pp[:], tri_b, mb[:], start=True, stop=True), reads=[b_mb, b_cstb], writes=[b_pp])
                    S.op("pe", lambda: nc.tensor.matmul(pc[:], ones_b, mb[:], start=True, stop=True), reads=[b_mb, b_cstb], writes=[b_pc])
                    S.op("dve", lambda i=i: nc.vector.tensor_tensor(posf[:, i, :], pp[:], cntb[:], ALU.add), reads=[b_pp, b_cn], writes=[b_pf])
                    S.op("dve", lambda: nc.vector.tensor_tensor(cntb[:], cntb[:], pc[:], ALU.add), reads=[b_pc, b_cn], writes=[b_cn])
                S.op("dve", lambda: nc.vector.tensor_copy(cnt_i[:], cntb[:]), reads=[b_cn], writes=[b_cni])
                q_, b_q = P.sb("q_", [128, NE], F32)
                nf, b_nf = P.sb("nf", [128, NE], F32)
                nI, b_nI = P.sb("nI", [128, NE], I32)
                inc, b_inc = P.sb("inc", [128, NE], F32)
                inc2, b_inc2 = P.sb("inc2", [128, NE], F32)
                base, b_base = P.sb("base", [128, NE], F32)
                S.op("dve", lambda: nc.vector.tensor_scalar(q_[:], cntb[:], float(RB - 1), 1.0 / RB, op0=ALU.add, op1=ALU.mult), reads=[b_cn], writes=[b_q])
                S.op("dve", lambda: nc.vector.tensor_copy(nI[:], q_[:]), reads=[b_q], writes=[b_nI])
                S.op("dve", lambda: nc.vector.tensor_copy(nf[:], nI[:]), reads=[b_nI], writes=[b_nf])
                S.op("dve", lambda: nc.vector.tensor_tensor(inc[:], nf[:], q_[:], ALU.is_gt), reads=[b_nf, b_q], writes=[b_inc])
                S.op("dve", lambda: nc.vector.tensor_tensor(nf[:], nf[:], inc[:], ALU.subtract), reads=[b_nf, b_inc], writes=[b_nf])
                S.op("dve", lambda: nc.vector.tensor_scalar(nf[:], nf[:], float(RB), None, op0=ALU.mult), reads=[b_nf], writes=[b_nf])
                S.op("dve", lambda: nc.vector.tensor_copy(inc[:], nf[:]), reads=[b_nf], writes=[b_inc])
                cur, b_cur, oth, b_oth = inc, b_inc, inc2, b_inc2
                for sh in (1, 2, 4, 8, 16):
                    S.op("dve", lambda cur=cur, oth=oth, sh=sh: nc.vector.tensor_copy(oth[:, 0:sh], cur[:, 0:sh]), reads=[b_cur], writes=[b_oth])
                    S.op("dve", lambda cur=cur, oth=oth, sh=sh: nc.vector.tensor_tensor(oth[:, sh:NE], cur[:, sh:NE], cur[:, 0:NE - sh], ALU.add), reads=[b_cur, b_oth], writes=[b_oth])
                    cur, b_cur, oth, b_oth = oth, b_oth, cur, b_cur
                ends, b_ends = cur, b_cur
                S.op("dve", lambda: nc.vector.tensor_tensor(base[:], ends[:], nf[:], ALU.subtract), reads=[b_ends, b_nf], writes=[b_base])
                idxf, b_idxf = P.sb("idxf", [128, NE * SMAX], F32)
                for e in range(NE):
                    S.op("dve", lambda e=e: nc.vector.tensor_scalar(idxf[:, e * SMAX:(e + 1) * SMAX], cst[:, 7, 0:SMAX], base[:, e:e + 1], None, op0=ALU.add),
                         reads=[b_cst, b_base], writes=[b_idxf])
                S.op("dve", lambda: nc.vector.tensor_scalar(idxf[:], idxf[:], float(NROWS - 1), None, op0=ALU.min), reads=[b_idxf], writes=[b_idxf])
                S.op("dve", lambda: nc.vector.tensor_copy(idx_i[:], idxf[:]), reads=[b_idxf], writes=[b_idx])
                dm, b_dm = P.sb("dm", [128, NE], F32)
                eq, b_eq = P.sb("eq", [128, NE], F32)
                d8r = P.ring_sb("d8", [128, 16], F32, 2)
                for i in range(16):
                    d8, b_d8 = d8r.next()
                    S.op("dve", lambda i=i: nc.vector.tensor_tensor(dm[:], posf[:, i, :], base[:], ALU.add), reads=[b_pf, b_base], writes=[b_dm])
                    S.op("dve", lambda i=i: nc.vector.scalar_tensor_tensor(dm[:], dm[:], 1.0, maskf[:, i, :], ALU.add, ALU.mult), reads=[b_dm, b_mf], writes=[b_dm])
                    S.op("dve", lambda d8=d8: nc.vector.max(out=d8[:, 0:8], in_=dm[:]), reads=[b_dm], writes=[b_d8])
                    S.op("dve", lambda d8=d8: nc.vector.tensor_scalar(d8[:, 8:12], d8[:, 0:4], -1.0, None, op0=ALU.add), reads=[b_d8], writes=[b_d8])
                    S.op("dve", lambda d8=d8, i=i: nc.vector.tensor_copy(dest_i[:, i * 4:i * 4 + 4], d8[:, 8:12]), reads=[b_d8], writes=[b_di])
                    for k in range(4):
                        S.op("dve", lambda d8=d8, k=k: nc.vector.tensor_scalar(eq[:], dm[:], d8[:, k:k + 1], None, op0=ALU.is_equal), reads=[b_dm, b_d8], writes=[b_eq])
                        S.op("dve", lambda i=i: nc.vector.tensor_tensor(eq[:], eq[:], wgt[:, i, :], ALU.mult), reads=[b_eq, b_wg], writes=[b_eq])
                        S.op("dve", lambda i=i, k=k: nc.vector.reduce_sum(w4[:, i, k:k + 1], eq[:], AX.X), reads=[b_eq], writes=[b_w4])
                    for k in range(4):
                        S.dma(b_h2b, [("pool", lambda i=i, k=k: nc.gpsimd.indirect_dma_start(
                            out=xs_d, out_offset=bass.IndirectOffsetOnAxis(ap=dest_i[:, i * 4 + k:i * 4 + k + 1], axis=0), in_=h2b[:, i * D:(i + 1) * D], in_offset=None))],
                            reads=[b_h2b, b_di], writes=[db("xs_d")])

        if stage >= 6:
            with phase(nc, S, "E") as P:
                regs = nc.alloc_registers("cntreg", engines=mybir.ALL_ENGINES)
                engname = {mybir.EngineType.Pool: "pool", mybir.EngineType.Activation: "act", mybir.EngineType.PE: "pe",
                           mybir.EngineType.DVE: "dve", mybir.EngineType.SP: "sp"}
                xsT, b_xsT = P.sb("xsT", [128, 16, 1024], BF16)
                actT, b_act = P.sb("actT", [128, 16, 1024], BF16)
                xrr = P.ring_sb("xr", [128, D], BF16, 2)
                w1r = P.ring_sb("w1", [128, 16, 256], BF16, 6)
                w2r = P.ring_sb("w2", [128, 16, 512], BF16, 2)
                b1r = P.ring_sb("b1", [128, 32], F32, 2)
                b2r = P.ring_sb("b2", [128, D], F32, 2)
                ptb = P.ring_ps("ptb", [128, 1024], BF16, 2)
                pg = P.ring_ps("pg", [128, 512], F32, 2)
                bbr = P.ring_sb("bb", [128, 512], F32, 3)
                glr = P.ring_sb("gl", [128, 512], F32, 2)
                abr = P.ring_sb("ab", [128, 256], BF16, 2)
                py = P.ring_ps("py", [128, 512], F32, 2)
                sg, b_sg = P.sb("sg", [128, 256], F32)
                yr = P.ring_sb("y", [128, 512], F32, 3)
                import os as _os
                for e in range(int(_os.environ.get('K_NEXP', NE))):
                    for reg in regs:
                        S.op(engname[reg.engine], lambda reg=reg, e=e: nc.reg_load(reg, cnt_i[0:1, e:e + 1]), reads=[b_cni])
                    b1t, b_b1 = b1r.next()
                    S.dma(b_b1, [("sp", lambda b1t=b1t, e=e: nc.sync.dma_start(out=b1t[:, 0:16], in_=b1g[e])),
                                 ("sp", lambda b1t=b1t, e=e: nc.sync.dma_start(out=b1t[:, 16:32], in_=b1l[e]))], writes=[b_b1])
                    b2t, b_b2 = b2r.next()
                    S.dma(b_b2, [("sp", lambda b2t=b2t, e=e: nc.sync.dma_start(out=b2t[:], in_=b2[e].to_broadcast([128, D])))], writes=[b_b2])
                    ESECT = 7
                    for ps in range(2):
                      with (S.guard(regs, 1024) if ps else contextlib.nullcontext()):
                            for s_i in range(ps * 8, ps * 8 + 8):
                                with S.guard(regs, s_i * 128):
                                    xr_, b_xr = xrr.next()
                                    col = e * SMAX + s_i
                                    S.dma(b_xr, [("pool", lambda xr_=xr_, col=col: nc.gpsimd.indirect_dma_start(
                                        out=xr_[:], out_offset=None, in_=xs_d, in_offset=bass.IndirectOffsetOnAxis(ap=idx_i[:, col:col + 1], axis=0)))],
                                        reads=[db("xs_d"), b_idx], writes=[b_xr])
                                    for g in range(2):
                                        pt_, b_p = ptb.next()
                                        for c in range(8):
                                            k = g * 8 + c
                                            S.op("pe", lambda pt_=pt_, xr_=xr_, c=c, k=k: nc.tensor.transpose(pt_[:, c * 128:(c + 1) * 128], xr_[:, k * 128:(k + 1) * 128], ident_b),
                                                 reads=[b_xr, b_cstb], writes=[b_p])
                                        S.op("act", lambda pt_=pt_, g=g, s_i=s_i: nc.scalar.activation(
                                            xsT[:, g * 8:(g + 1) * 8, (s_i - ps * 8) * 128:(s_i - ps * 8 + 1) * 128], pt_[:].rearrange("p (c n) -> p c n", c=8), AF.Copy), reads=[b_p], writes=[b_xsT])
                            for pc_ in range(8 if ESECT & 2 else 0):
                                wg_, b_wg_ = w1r.next()
                                wl_, b_wl_ = w1r.next()
                                cs_ = slice(pc_ * 256, (pc_ + 1) * 256)
                                S.dma(b_wg_, [("pool", lambda wg_=wg_, e=e, cs_=cs_: nc.gpsimd.dma_start(out=wg_[:], in_=w1g[e].rearrange("(k p) n -> p k n", p=128)[:, :, cs_]))], writes=[b_wg_])
                                S.dma(b_wl_, [("pool", lambda wl_=wl_, e=e, cs_=cs_: nc.gpsimd.dma_start(out=wl_[:], in_=w1l[e].rearrange("(k p) n -> p k n", p=128)[:, :, cs_]))], writes=[b_wl_])
                                bb_, b_bb = bbr.next()
                                S.dma(b_bb, [("sp", lambda bb_=bb_, e=e, cs_=cs_: nc.sync.dma_start(out=bb_[:, 0:256], in_=b1g_r[e][:, cs_].to_broadcast([128, 256]))),
                                             ("sp", lambda bb_=bb_, e=e, cs_=cs_: nc.sync.dma_start(out=bb_[:, 256:512], in_=b1l_r[e][:, cs_].to_broadcast([128, 256])))], writes=[b_bb])
                                for s_i in range(ps * 8, ps * 8 + 8):
                                    rs_ = slice((s_i - ps * 8) * 128, (s_i - ps * 8 + 1) * 128)
                                    with S.guard(regs, s_i * 128):
                                        pg_, b_pg = pg.next()
                                        for k in range(16):
                                            S.op("pe", lambda pg_=pg_, wg_=wg_, k=k, rs_=rs_: nc.tensor.matmul(pg_[:, 0:256], xsT[:, k, rs_], wg_[:, k, :], start=(k == 0), stop=(k == 15)),
                                                 reads=[b_wg_, b_xsT], writes=[b_pg])
                                        for k in range(16):
                                            S.op("pe", lambda pg_=pg_, wl_=wl_, k=k, rs_=rs_: nc.tensor.matmul(pg_[:, 256:512], xsT[:, k, rs_], wl_[:, k, :], start=(k == 0), stop=(k == 15)),
                                                 reads=[b_wl_, b_xsT], writes=[b_pg])
                                        gl_, b_gl_ = glr.next()
                                        S.op("dve", lambda pg_=pg_, gl_=gl_, bb_=bb_: nc.vector.tensor_tensor(gl_[:], pg_[:], bb_[:], ALU.add), reads=[b_pg, b_bb], writes=[b_gl_])
                                        S.op("dve", lambda gl_=gl_: nc.vector.tensor_scalar(gl_[:], gl_[:], 7.0, None, op0=ALU.min), reads=[b_gl_], writes=[b_gl_])
                                        S.op("act", lambda gl_=gl_: nc.scalar.activation(sg[:], gl_[:, 0:256], AF.Sigmoid, scale=1.702), reads=[b_gl_], writes=[b_sg])
                                        S.op("dve", lambda gl_=gl_: nc.vector.tensor_scalar(gl_[:, 256:512], gl_[:, 256:512], -7.0, None, op0=ALU.max), reads=[b_gl_], writes=[b_gl_])
                                        S.op("dve", lambda gl_=gl_: nc.vector.tensor_tensor(sg[:], sg[:], gl_[:, 0:256], ALU.mult), reads=[b_gl_, b_sg], writes=[b_sg])
                                        ab_, b_ab = abr.next()
                                        S.op("dve", lambda gl_=gl_, ab_=ab_: nc.vector.scalar_tensor_tensor(ab_[:], gl_[:, 256:512], 1.0, sg[:], ALU.add, ALU.mult), reads=[b_gl_, b_sg], writes=[b_ab])
                                        pt_, b_p = ptb.next()
                                        for c in range(2):
                                            S.op("pe", lambda pt_=pt_, ab_=ab_, c=c: nc.tensor.transpose(pt_[:, c * 128:(c + 1) * 128], ab_[:, c * 128:(c + 1) * 128], ident_b),
                                                 reads=[b_ab, b_cstb], writes=[b_p])
                                        S.op("act", lambda pt_=pt_, pc_=pc_, rs_=rs_: nc.scalar.activation(
                                            actT[:, pc_ * 2:pc_ * 2 + 2, rs_], pt_[:, 0:256].rearrange("p (c n) -> p c n", c=2), AF.Copy), reads=[b_p], writes=[b_act])
                            for np_ in range(4 if ESECT & 4 else 0):
                                w2t, b_w2 = w2r.next()
                                cs_ = slice(np_ * 512, (np_ + 1) * 512)
                                S.dma(b_w2, [("pool", lambda w2t=w2t, e=e, cs_=cs_: nc.gpsimd.dma_start(out=w2t[:], in_=w2[e].rearrange("(k p) n -> p k n", p=128)[:, :, cs_]))], writes=[b_w2])
                                for s_i in range(ps * 8, ps * 8 + 8):
                                    rs_ = slice((s_i - ps * 8) * 128, (s_i - ps * 8 + 1) * 128)
                                    with S.guard(regs, s_i * 128):
                                        py_, b_py = py.next()
                                        for k in range(16):
                                            S.op("pe", lambda py_=py_, w2t=w2t, rs_=rs_, k=k: nc.tensor.matmul(py_[:], actT[:, k, rs_], w2t[:, k, :], start=(k == 0), stop=(k == 15)),
                                                 reads=[b_act, b_w2], writes=[b_py])
                                        y_, b_y = yr.next()
                                        S.op("dve", lambda py_=py_, y_=y_, b2t=b2t, cs_=cs_: nc.vector.tensor_tensor(y_[:], py_[:], b2t[:, cs_], ALU.add), reads=[b_py, b_b2], writes=[b_y])
                                        col = e * SMAX + s_i
                                        S.dma(b_y, [("pool", lambda y_=y_, col=col, np_=np_: nc.gpsimd.indirect_dma_start(
                                            out=ys_p[np_], out_offset=bass.IndirectOffsetOnAxis(ap=idx_i[:, col:col + 1], axis=0), in_=y_[:], in_offset=None))],
                                            reads=[b_y, b_idx], writes=[db("ys_d")])


        if stage >= 7:
            with phase(nc, S, "F") as P:
                g2, b_g2 = P.sb("g2", [128, D], F32)
                gf, b_gf = P.sb("gf", [128, D], F32)
                S.dma(b_g2, [("sp", lambda: nc.sync.dma_start(out=g2[:], in_=bc_row(mod_flat[:, 80 * 128:96 * 128], D)))], reads=[db("mod_d")], writes=[b_g2])
                S.dma(b_gf, [("sp", lambda: nc.sync.dma_start(out=gf[:], in_=bc_row(rows[0:1, :], D)))], writes=[b_gf])
                gr = P.ring_sb("ga", [128, D], F32, 4)
                accr = P.ring_sb("acc", [128, D], F32, 2)
                x1r = P.ring_sb("x1", [128, D], F32, 2)
                sq, b_sq = P.sb("sq", [128, D], BF16)
                ssr = P.ring_sb("ss", [128, 1], F32, 2)
                for i in range(16):
                    xb, b_xb = x1r.next()
                    S.dma(b_xb, [("sp", lambda xb=xb, i=i: nc.sync.dma_start(out=xb[:], in_=x1_d[i * 128:(i + 1) * 128, :]))], reads=[db("x1_d")], writes=[b_xb])
                    acc, b_acc = accr.next()
                    for k in range(4):
                        ga, b_ga = gr.next()
                        S.dma(b_ga, [("pool", lambda ga=ga, i=i, k=k, q=q: nc.gpsimd.indirect_dma_start(
                            out=ga[:, q * 512:(q + 1) * 512], out_offset=None, in_=ys_p[q], in_offset=bass.IndirectOffsetOnAxis(ap=dest_i[:, i * 4 + k:i * 4 + k + 1], axis=0)))
                            for q in range(4)], reads=[db("ys_d"), b_di], writes=[b_ga])
                        if k == 0:
                            S.op("dve", lambda ga=ga, acc=acc, i=i, k=k: nc.vector.tensor_scalar(acc[:], ga[:], w4[:, i, k:k + 1], None, op0=ALU.mult), reads=[b_ga, b_w4], writes=[b_acc])
                        else:
                            S.op("dve", lambda ga=ga, acc=acc, i=i, k=k: nc.vector.scalar_tensor_tensor(acc[:], ga[:], w4[:, i, k:k + 1], acc[:], ALU.mult, ALU.add), reads=[b_ga, b_w4, b_acc], writes=[b_acc])
                    S.op("pool", lambda acc=acc: nc.gpsimd.tensor_tensor(acc[:], acc[:], g2[:], ALU.mult), reads=[b_acc, b_g2], writes=[b_acc])
                    S.op("dve", lambda acc=acc, xb=xb: nc.vector.tensor_tensor(acc[:], acc[:], xb[:], ALU.add), reads=[b_acc, b_xb], writes=[b_acc])
                    ss, b_ss = ssr.next()
                    S.op("act", lambda acc=acc, ss=ss: nc.scalar.activation(sq[:], acc[:], AF.Square, accum_out=ss[:, 0:1]), reads=[b_acc], writes=[b_sq, b_ss])
                    S.op("act", lambda ss=ss: nc.scalar.activation(ss[:], ss[:], AF.Sqrt, bias=epsT[:, 0:1], scale=1.0 / D), reads=[b_ss, b_eps], writes=[b_ss])
                    S.op("dve", lambda ss=ss: nc.vector.reciprocal(ss[:], ss[:]), reads=[b_ss], writes=[b_ss])
                    S.op("dve", lambda acc=acc, ss=ss: nc.vector.scalar_tensor_tensor(acc[:], acc[:], ss[:, 0:1], gf[:], ALU.mult, ALU.mult), reads=[b_acc, b_ss, b_gf], writes=[b_acc])
                    S.dma(b_acc, [("sp", lambda acc=acc, i=i: nc.sync.dma_start(out=out[i * 128:(i + 1) * 128, :], in_=acc[:]))], reads=[b_acc], writes=[db("out")])

        if dbg and stage <= 4:
            with phase(nc, S, "DBG") as P:
                t_, b_t = P.sb("dbg", [128, 16, D], F32) if False else (None, None)
                r = P.ring_sb("r", [128, D], F32, 2)
                for i in range(16):
                    t_, b_t = r.next()
                    S.dma(b_t, [("sp", lambda t_=t_, i=i: nc.sync.dma_start(out=t_[:], in_=x1_d[i * 128:(i + 1) * 128, :]))], reads=[db("x1_d")], writes=[b_t])
                    S.dma(b_t, [("sp", lambda t_=t_, i=i: nc.sync.dma_start(out=dbg_out[i * 128:(i + 1) * 128, :], in_=t_[:]))], reads=[b_t], writes=[db("dbg")])

        S.barrier()
    return nc


def own_blocks(p):
    blks = []
    for m in range(8):
        blks += [4 * m + (0 if p == 0 else 1), 4 * m + (3 if p == 0 else 2)]
    return blks


def host_prepare(inp):
    f32 = np.float32
    x = np.asarray(inp["x"], f32)
    c = np.asarray(inp["c"], f32)
    pos = np.asarray(inp["positions"], np.int32)
    g_attn = np.asarray(inp["g_attn"], f32)[0]
    g_ffn = np.asarray(inp["g_ffn"], f32)[0]
    b_mod = np.asarray(inp["b_mod"], f32)[0]
    w_in = np.asarray(inp["w_in"], f32)[0]
    w_q_up = np.asarray(inp["w_q_up"], f32)[0].reshape(512, 8, 192)
    w_kv_up = np.asarray(inp["w_kv_up"], f32)[0].reshape(512, 8, 256)
    fm = lambda v: np.ascontiguousarray(v.reshape(-1, 128).T)
    vecs = np.concatenate([fm(g_attn), fm(g_ffn), fm(b_mod)], axis=1).astype(f32)
    rows = np.zeros((4, D), f32)
    rows[0] = np.asarray(inp["g_final"], f32)
    rows[1, :NE] = np.asarray(inp["b_router"], f32)[0]
    rows[2] = g_ffn
    glat = np.concatenate([fm(np.asarray(inp["g_q_lat"], f32)[0]), fm(np.asarray(inp["g_kv_lat"], f32)[0])], axis=1).astype(f32)
    half = 32
    invf = (1.0 / (np.float32(10000.0) ** (np.arange(half, dtype=f32) * f32(2.0) / f32(64)))).astype(f32)
    ropec = np.zeros((128, 4), f32)
    for p_ in range(128):
        i = p_ % 64
        ropec[p_, 0] = invf[i % 32]
        ropec[p_, 1] = -1.0 if i < 32 else 1.0
    ropec[:, 2] = np.pi
    ropec[:, 3] = 1.5 * np.pi
    kk = np.arange(128)[:, None]
    qq = np.arange(128)[None, :]
    ident = np.eye(128, dtype=f32)
    negtri = -(kk >= qq).astype(f32)
    ones = np.ones((128, 128), f32)
    tri_s = (kk < qq).astype(f32)
    negones33 = np.zeros((128, 128), f32)
    negones33[:, :33] = -1.0
    bc33 = np.zeros((128, 128), f32)
    bc33[0, :] = 1.0
    bc33[32, :] = 1.0
    blkstart = np.tile((np.arange(128, dtype=f32) * RB)[None, :], (128, 1))
    off16 = (np.arange(128, dtype=f32)[None, :] % 16) * 128 + np.arange(128, dtype=f32)[:, None]
    consts = np.stack([ident, negtri, ones, tri_s, negones33, bc33, blkstart, off16]).astype(f32)
    TRI_SB = (kk < qq).astype(f32)
    TRI_ML = (kk <= qq).astype(f32)
    ONE = np.ones((128, 128), f32)
    ZERO = np.zeros((128, 128), f32)
    kr = w_in[:, 4096:4160]
    w_kr = np.ascontiguousarray(np.concatenate([kr, kr[:, 32:], kr[:, :32]], axis=1))
    w_qn = np.ascontiguousarray(w_q_up[:, :, :128].reshape(512, 1024))
    qr_ = w_q_up[:, :, 128:]
    w_qra = np.ascontiguousarray(qr_.reshape(512, 512))
    w_qrs = np.ascontiguousarray(np.concatenate([qr_[:, :, 32:], qr_[:, :, :32]], axis=2).reshape(512, 512))
    w_kn = np.ascontiguousarray(w_kv_up[:, :, :128].reshape(512, 1024))
    w_v = np.ascontiguousarray(w_kv_up[:, :, 128:].reshape(512, 1024))
    w1 = np.asarray(inp["w1"], f32)[0]
    b1 = np.asarray(inp["b1"], f32)[0]
    shared = dict(
        vecs=vecs, rows=rows, glat=glat, ropec=ropec, consts=consts,
        w_mod=np.asarray(inp["w_mod"], f32)[0], w_in=w_in, w_kr=w_kr, w_qn=w_qn, w_qra=w_qra, w_qrs=w_qrs,
        w_kn=w_kn, w_v=w_v, w_out=np.asarray(inp["w_out"], f32)[0],
        w_router=np.ascontiguousarray(np.asarray(inp["w_router"], f32)[0].reshape(16, 128, NE).transpose(1, 0, 2)),
        w1g=np.ascontiguousarray(w1[:, :, 0::2]), w1l=np.ascontiguousarray(w1[:, :, 1::2]),
        b1g=np.ascontiguousarray(b1[:, 0::2].reshape(NE, 16, 128).transpose(0, 2, 1)),
        b1l=np.ascontiguousarray(b1[:, 1::2].reshape(NE, 16, 128).transpose(0, 2, 1)),
        b1g_r=np.ascontiguousarray(b1[:, 0::2].reshape(NE, 1, D)), b1l_r=np.ascontiguousarray(b1[:, 1::2].reshape(NE, 1, D)),
        w2=np.asarray(inp["w2"], f32)[0], b2=np.ascontiguousarray(np.asarray(inp["b2"], f32)[0].reshape(NE, 1, D)),
    )
    in_maps = []
    for core in range(8):
        b, p = core // 2, core % 2
        blks = own_blocks(p)
        rowsel = np.concatenate([np.arange(k * 128, (k + 1) * 128) for k in blks])
        if p == 0:
            zs = [(TRI_SB, TRI_ML), (ZERO, ZERO), (ONE, ONE), (TRI_SB, TRI_ML)]
        else:
            zs = [(ONE, ONE), (TRI_SB, TRI_ML), (TRI_SB, TRI_ML), (ZERO, ZERO)]
        masks = np.stack([np.stack([z[0] for z in zs]), np.stack([z[1] for z in zs])]).astype(f32)
        m = dict(shared)
        m.update(
            x_all=np.ascontiguousarray(x[b]), x_own=np.ascontiguousarray(x[b][rowsel]),
            pos_all=np.ascontiguousarray(pos[b][None, :]), pos_own=np.ascontiguousarray(pos[b][rowsel][None, :]),
            cT=fm(c[b]), masks=masks,
        )
        in_maps.append(m)
    return in_maps


_CACHE = {}


def kernel(**inp):
    in_maps = host_prepare(inp)
    if "nc" not in _CACHE:
        _CACHE["nc"] = build_program()
    res = run_bass_kernel_spmd(_CACHE["nc"], in_maps, core_ids=list(range(8)))
    outp = np.zeros((4, S_ALL, D), np.float32)
    for core in range(8):
        b, p = core // 2, core % 2
        o = res.results[core]["out"]
        for i, k in enumerate(own_blocks(p)):
            outp[b, k * 128:(k + 1) * 128] = o[i * 128:(i + 1) * 128]
    return outp
```

```python
import contextlib
import numpy as np
import concourse.bass as bass
import concourse.mybir as mybir
from concourse.bass_utils import run_bass_kernel_spmd

F32 = mybir.dt.float32
BF16 = mybir.dt.bfloat16
I32 = mybir.dt.int32
AF = mybir.ActivationFunctionType
ALU = mybir.AluOpType
AX = mybir.AxisListType

D = 2048
S_ALL = 4096
T_OWN = 2048
NE = 32
CAPR = 2048
RB = 128
NROWS = 4 * T_OWN + NE * RB
NBLK = NROWS // RB
EPS = 1e-6
PI = float(np.pi)


class Buf:
    __slots__ = ("name", "w", "r", "dsem")

    def __init__(self, name):
        self.name = name
        self.w = {}
        self.r = {}
        self.dsem = None


class Sync:
    ENG = ("pe", "act", "dve", "pool", "sp")

    def __init__(self, nc):
        self.nc = nc
        self.e = {"pe": nc.tensor, "act": nc.scalar, "dve": nc.vector, "pool": nc.gpsimd, "sp": nc.sync}
        self.sems = {}
        self.cnt = {}
        for n in self.ENG:
            self.sems[n] = nc.alloc_semaphore("es_" + n)
            self.cnt[n] = 0
        self.seen = {n: {} for n in self.ENG}
        self.free_d = []
        self.nd = 0

    def _wait(self, eng, deps):
        for k, v in deps.items():
            if v <= 0 or (k == eng and eng == "pe"):
                continue
            if self.seen[eng].get(k, 0) >= v:
                continue
            self.e[eng].wait_ge(self.sems[k], v)
            self.seen[eng][k] = v

    @staticmethod
    def _merge(d, s):
        for k, v in s.items():
            if d.get(k, 0) < v:
                d[k] = v

    def _deps(self, reads, writes):
        d = {}
        for b in reads:
            self._merge(d, b.w)
        for b in writes:
            self._merge(d, b.w)
            self._merge(d, b.r)
        return d

    def op(self, eng, fn, reads=(), writes=()):
        self._wait(eng, self._deps(reads, writes))
        ins = fn()
        self.cnt[eng] += 1
        ins.then_inc(self.sems[eng], 1)
        me = {eng: self.cnt[eng]}
        for b in reads:
            self._merge(b.r, me)
        for b in writes:
            b.w = dict(me)
            b.r = {}
        return ins

    def _dsem(self, buf):
        if buf.dsem is None:
            if self.free_d:
                buf.dsem = self.free_d.pop()
            else:
                self.nd += 1
                buf.dsem = "d%d" % self.nd
                self.sems[buf.dsem] = self.nc.alloc_semaphore(buf.dsem)
                self.cnt[buf.dsem] = 0
        return buf.dsem

    def release(self, bufs):
        for b in bufs:
            if b.dsem is not None:
                self.free_d.append(b.dsem)
                b.dsem = None

    def dma(self, sb, items, reads=(), writes=()):
        key = self._dsem(sb)
        deps = self._deps(reads, writes)
        if self.cnt[key] > 0:
            deps[key] = max(deps.get(key, 0), self.cnt[key])
        for q, fn in items:
            self._wait(q, deps)
        for q, fn in items:
            ins = fn()
            ins.then_inc(self.sems[key], 16)
            self.cnt[key] += 16
        me = {key: self.cnt[key]}
        for b in reads:
            self._merge(b.r, me)
        for b in writes:
            b.w = dict(me)
            b.r = {}

    @contextlib.contextmanager
    def guard(self, regs, thr):
        before = dict(self.cnt)
        seen0 = {k: dict(v) for k, v in self.seen.items()}
        with self.nc.If_cmp(regs, thr, "IS_GT"):
            yield
        after = dict(self.cnt)
        with self.nc.Else():
            for k, v in after.items():
                d = v - before.get(k, 0)
                if d > 0:
                    eng = k if k in self.ENG else "sp"
                    if before.get(k, 0) > 0:
                        self.e[eng].wait_ge(self.sems[k], before[k])
                    self.e[eng].sem_inc(self.sems[k], d)
        self.seen = seen0

    def barrier(self, engines=None):
        allv = {k: v for k, v in self.cnt.items() if v > 0}
        for en in (engines or self.ENG):
            self._wait(en, allv)


class Ring:
    def __init__(self, items):
        self.items = items
        self.i = 0

    def next(self):
        it = self.items[self.i % len(self.items)]
        self.i += 1
        return it


class Ctx:
    def __init__(self, nc, S, es, tag):
        self.nc, self.S, self.es, self.tag = nc, S, es, tag
        self.bufs = []
        self.n = 0

    def sb(self, name, shape, dt):
        self.n += 1
        t = self.es.enter_context(self.nc.sbuf_tensor("%s_%s%d" % (self.tag, name, self.n), shape, dt))
        b = Buf(name)
        self.bufs.append(b)
        return t, b

    def ps(self, name, shape, dt):
        self.n += 1
        t = self.es.enter_context(self.nc.psum_tensor("%s_%s%d" % (self.tag, name, self.n), shape, dt))
        b = Buf(name)
        self.bufs.append(b)
        return t, b

    def ring_sb(self, name, shape, dt, n):
        return Ring([self.sb(name, shape, dt) for _ in range(n)])

    def ring_ps(self, name, shape, dt, n):
        return Ring([self.ps(name, shape, dt) for _ in range(n)])


@contextlib.contextmanager
def phase(nc, S, tag):
    with contextlib.ExitStack() as es:
        c = Ctx(nc, S, es, tag)
        yield c
        S.barrier()
        S.release(c.bufs)


def build_program(stage=99, dbg=False):
    nc = bass.Bass("TRN2", target_bir_lowering=False)
    S = Sync(nc)

    def din(name, shape, dt=F32):
        return nc.dram_tensor(name, list(shape), dt, kind="ExternalInput").ap()

    def dscr(name, shape, dt):
        return nc.dram_tensor(name, list(shape), dt, kind="Internal").ap()

    x_all = din("x_all", [S_ALL, D])
    x_own = din("x_own", [T_OWN, D])
    pos_all = din("pos_all", [1, S_ALL], I32)
    pos_own = din("pos_own", [1, T_OWN], I32)
    cT = din("cT", [128, 16])
    vecs = din("vecs", [128, 16 * 2 + 96])
    rows = din("rows", [4, D])
    glat = din("glat", [128, 8])
    ropec = din("ropec", [128, 4])
    masks = din("masks", [2, 4, 128, 128])
    consts = din("consts", [8, 128, 128])
    w_mod = din("w_mod", [D, 6 * D])
    w_in = din("w_in", [D, 4160])
    w_kr = din("w_kr", [D, 128])
    w_qn = din("w_qn", [512, 1024])
    w_qra = din("w_qra", [512, 512])
    w_qrs = din("w_qrs", [512, 512])
    w_kn = din("w_kn", [512, 1024])
    w_v = din("w_v", [512, 1024])
    w_out = din("w_out", [D, D])
    w_router = din("w_router", [128, 16, NE])
    w1g = din("w1g", [NE, D, D])
    w1l = din("w1l", [NE, D, D])
    b1g = din("b1g", [NE, 128, 16])
    b1l = din("b1l", [NE, 128, 16])
    b1g_r = din("b1g_r", [NE, 1, D])
    b1l_r = din("b1l_r", [NE, 1, D])
    w2 = din("w2", [NE, D, D])
    b2 = din("b2", [NE, 1, D])
    out = nc.dram_tensor("out", [T_OWN, D], F32, kind="ExternalOutput").ap()
    dbg_out = nc.dram_tensor("dbg", [T_OWN, D], F32, kind="ExternalOutput").ap() if dbg else None

    mod_d = dscr("mod_d", [96, 128], F32)
    hT_all = dscr("hT_all", [D, S_ALL], BF16)
    hT_own = dscr("hT_own", [D, T_OWN], BF16)
    QT_sb = dscr("QT_sb", [1024, T_OWN], BF16)
    KT_sb = dscr("KT_sb", [1024, S_ALL], BF16)
    V_sb = dscr("V_sb", [S_ALL, 1024], BF16)
    qlnT = dscr("qlnT", [512, T_OWN], BF16)
    kvlnT = dscr("kvlnT", [512, S_ALL], BF16)
    KrT = dscr("KrT", [64, S_ALL], BF16)
    QnT = dscr("QnT", [1024, T_OWN], BF16)
    QrT = dscr("QrT", [512, T_OWN], BF16)
    KnT = dscr("KnT", [1024, S_ALL], BF16)
    V_ml = dscr("V_ml", [S_ALL, 1024], BF16)
    mixT = dscr("mixT", [D, T_OWN], BF16)
    x1_d = dscr("x1_d", [T_OWN, D], F32)
    xs_d = dscr("xs_d", [NROWS, D], BF16)
    ys_p = [dscr("ys_d%d" % q, [NROWS, 512], F32) for q in range(4)]
    cnt_d = dscr("cnt_d", [1, NE], I32)
    Bd = {}

    def db(name):
        if name not in Bd:
            Bd[name] = Buf(name)
        return Bd[name]

    qi = [0]

    def q2():
        qi[0] += 1
        return "sp"

    with contextlib.ExitStack() as glob:
        G = Ctx(nc, S, glob, "g")
        cst, b_cst = G.sb("cst", [128, 8, 128], F32)
        cstb, b_cstb = G.sb("cstb", [128, 8, 128], BF16)
        S.dma(b_cst, [("sp", lambda: nc.sync.dma_start(out=cst[:], in_=consts.rearrange("c p n -> p c n")))], writes=[b_cst])
        S.op("dve", lambda: nc.vector.tensor_copy(cstb[:], cst[:]), reads=[b_cst], writes=[b_cstb])
        ident_f = cst[:, 0, :]
        ident_b = cstb[:, 0, :]
        negtri_b = cstb[:, 1, :]
        ones_b = cstb[:, 2, :]
        tri_b = cstb[:, 3, :]
        negones33_b = cstb[:, 4, 0:33]
        bc33_b = cstb[0:33, 5, :]
        vec, b_vec = G.sb("vec", [128, 128], F32)
        S.dma(b_vec, [("sp", lambda: nc.sync.dma_start(out=vec[:], in_=vecs))], writes=[b_vec])
        gl, b_gl = G.sb("gl", [128, 8], F32)
        S.dma(b_gl, [("sp", lambda: nc.sync.dma_start(out=gl[:], in_=glat))], writes=[b_gl])
        rc, b_rc = G.sb("rc", [128, 4], F32)
        S.dma(b_rc, [("sp", lambda: nc.sync.dma_start(out=rc[:], in_=ropec))], writes=[b_rc])
        epsT, b_eps = G.sb("eps", [128, 1], F32)
        S.op("dve", lambda: nc.vector.memset(epsT[:], EPS), writes=[b_eps])
        modT, b_modT = G.sb("modT", [128, 96], F32)
        gm, b_gm = G.sb("gm", [128, 32], F32)

        with phase(nc, S, "M") as P:
            ct, b_ct = P.sb("ct", [128, 16], F32)
            S.dma(b_ct, [("sp", lambda: nc.sync.dma_start(out=ct[:], in_=cT))], writes=[b_ct])
            ca, b_ca = P.sb("ca", [128, 16], F32)
            S.op("act", lambda: nc.scalar.activation(ca[:], ct[:], AF.Silu), reads=[b_ct], writes=[b_ca])
            wr = P.ring_sb("wm", [128, 16, 512], F32, 2)
            pm, b_pm = P.ps("pm", [128, 96], F32)
            wv = w_mod.rearrange("(k p) n -> p k n", p=128)
            for g in range(24):
                wt, b_wt = wr.next()
                S.dma(b_wt, [(q2(), lambda wt=wt, g=g: nc.sync.dma_start(out=wt[:], in_=wv[:, :, g * 512:(g + 1) * 512]))], writes=[b_wt])
                for c in range(4):
                    j = g * 4 + c
                    for k in range(16):
                        S.op("pe", lambda wt=wt, c=c, k=k, j=j: nc.tensor.matmul(
                            pm[:, j:j + 1], wt[:, k, c * 128:(c + 1) * 128], ca[:, k:k + 1], start=(k == 0), stop=(k == 15)),
                            reads=[b_wt, b_ca], writes=[b_pm])
            S.op("dve", lambda: nc.vector.tensor_tensor(modT[:], pm[:], vec[:, 32:128], ALU.add), reads=[b_pm, b_vec], writes=[b_modT])
            S.op("dve", lambda: nc.vector.scalar_tensor_tensor(gm[:, 0:16], modT[:, 16:32], 1.0, vec[:, 0:16], ALU.add, ALU.mult),
                 reads=[b_modT, b_vec], writes=[b_gm])
            S.op("dve", lambda: nc.vector.scalar_tensor_tensor(gm[:, 16:32], modT[:, 64:80], 1.0, vec[:, 16:32], ALU.add, ALU.mult),
                 reads=[b_modT, b_vec], writes=[b_gm])
            pt, b_pt = P.ps("pt", [128, 128], F32)
            S.op("pe", lambda: nc.tensor.transpose(pt[0:96, :], modT[:, 0:96], ident_f), reads=[b_modT, b_cst], writes=[b_pt])
            mt, b_mt = P.sb("mt", [96, 128], F32)
            S.op("dve", lambda: nc.vector.tensor_copy(mt[:], pt[0:96, :]), reads=[b_pt], writes=[b_mt])
            S.dma(b_mt, [("sp", lambda: nc.sync.dma_start(out=mod_d, in_=mt[:]))], reads=[b_mt], writes=[db("mod_d")])

        mod_flat = mod_d.rearrange("j p -> (j p)").rearrange("(o n) -> o n", o=1)

        def bc_row(ap_row, n):
            return ap_row.to_broadcast([128, n])

        def norm_to_hT(P, src, dst, ntile, dstname, gcol, shcol):
            xr = P.ring_sb("x", [128, 4, D], F32, 2)
            xnr = P.ring_sb("xn", [128, 4, D], BF16, 2)
            sq, b_sq = P.sb("sq", [128, D], BF16)
            ssr = P.ring_sb("ss", [128, 4], F32, 2)
            ptr = P.ring_ps("ptr", [128, 2048], BF16, 2)
            hr = P.ring_sb("h", [128, 16, 512], BF16, 2)
            sv = src.rearrange("(n j p) d -> n p j d", p=128, j=4)
            dv = dst.rearrange("(k p) t -> p k t", p=128)
            for n in range(ntile):
                xt, b_x = xr.next()
                S.dma(b_x, [("sp", lambda xt=xt, n=n: nc.sync.dma_start(out=xt[:], in_=sv[n]))], writes=[b_x])
                ss, b_ss = ssr.next()
                xn, b_xn = xnr.next()
                ht, b_h = hr.next()
                for j in range(4):
                    S.op("act", lambda xt=xt, ss=ss, j=j: nc.scalar.activation(sq[:], xt[:, j, :], AF.Square, accum_out=ss[:, j:j + 1]),
                         reads=[b_x], writes=[b_sq, b_ss])
                S.op("act", lambda ss=ss: nc.scalar.activation(ss[:], ss[:], AF.Sqrt, bias=epsT[:, 0:1], scale=1.0 / D),
                     reads=[b_ss, b_eps], writes=[b_ss])
                S.op("dve", lambda ss=ss: nc.vector.reciprocal(ss[:], ss[:]), reads=[b_ss], writes=[b_ss])
                for j in range(4):
                    S.op("dve", lambda xt=xt, xn=xn, ss=ss, j=j: nc.vector.tensor_scalar(
                        xn[:, j, :], xt[:, j, :], ss[:, j:j + 1], None, op0=ALU.mult), reads=[b_x, b_ss], writes=[b_xn])
                for j in range(4):
                    pt_, b_p = ptr.next()
                    for k in range(16):
                        S.op("pe", lambda pt_=pt_, xn=xn, j=j, k=k: nc.tensor.transpose(
                            pt_[:, k * 128:(k + 1) * 128], xn[:, j, k * 128:(k + 1) * 128], ident_b), reads=[b_xn, b_cstb], writes=[b_p])
                    for k in range(16):
                        eng = "dve" if k % 2 == 0 else "pool"
                        if eng == "pool":
                            eng = "dve"
                        S.op(eng, lambda pt_=pt_, ht=ht, j=j, k=k: nc.vector.tensor_scalar(
                            ht[:, k, j * 128:(j + 1) * 128], pt_[:, k * 128:(k + 1) * 128],
                            gm[:, gcol + k:gcol + k + 1], modT[:, shcol + k:shcol + k + 1], op0=ALU.mult, op1=ALU.add),
                            reads=[b_p, b_gm, b_modT], writes=[b_h])
                S.dma(b_h, [("pool", lambda ht=ht, n=n: nc.gpsimd.dma_start(out=dv[:, :, n * 512:(n + 1) * 512], in_=ht[:]))],
                      reads=[b_h], writes=[db(dstname)])

        if stage >= 1:
            with phase(nc, S, "A") as P:
                norm_to_hT(P, x_all, hT_all, 8, "hT_all", 0, 0)
            with phase(nc, S, "A2") as P:
                norm_to_hT(P, x_own, hT_own, 4, "hT_own", 0, 0)

        def load_w(P, ring, Wap, KC, c0, ncols):
            wt, b_w = ring.next()
            wv_ = Wap.rearrange("(k p) n -> p k n", p=128)
            S.dma(b_w, [("pool", lambda: nc.gpsimd.dma_start(out=wt[:, 0:KC, 0:ncols], in_=wv_[:, :, c0:c0 + ncols]))], writes=[b_w])
            return wt, b_w

        def load_a(ring, Aap, aname, KC, t0):
            at, b_a = ring.next()
            av = Aap.rearrange("(k p) t -> p k t", p=128)
            S.dma(b_a, [("sp", lambda: nc.sync.dma_start(out=at[:, 0:KC, :], in_=av[:, :, t0:t0 + 512]))], reads=[db(aname)], writes=[b_a])
            return at, b_a

        def gemm_fm(P, Wap, c0, N, Aap, aname, T, KC, dst, dname, scale, rings):
            wr_, ar_, pr_, sr_ = rings
            dv = dst.rearrange("(c p) t -> p c t", p=128)
            for g in range(N // 512):
                wt, b_w = load_w(P, wr_, Wap, KC, c0 + g * 512, 512)
                for tt in range(T // 512):
                    at, b_a = load_a(ar_, Aap, aname, KC, tt * 512)
                    st, b_s = sr_.next()
                    for c in range(4):
                        ps_, b_p = pr_.next()
                        for k in range(KC):
                            S.op("pe", lambda ps_=ps_, wt=wt, at=at, c=c, k=k: nc.tensor.matmul(
                                ps_[:], wt[:, k, c * 128:(c + 1) * 128], at[:, k, :], start=(k == 0), stop=(k == KC - 1)),
                                reads=[b_w, b_a], writes=[b_p])
                        S.op("act", lambda ps_=ps_, st=st, c=c: nc.scalar.activation(st[:, c, :], ps_[:], AF.Copy, scale=scale),
                             reads=[b_p], writes=[b_s])
                    S.dma(b_s, [("pool", lambda st=st, g=g, tt=tt: nc.gpsimd.dma_start(
                        out=dv[:, g * 4:(g + 1) * 4, tt * 512:(tt + 1) * 512], in_=st[:]))], reads=[b_s], writes=[db(dname)])

        def gemm_tm(P, Wap, c0, N, Aap, aname, T, KC, dst, dname, rings):
            wr_, ar_, pr_, sr_ = rings
            dv = dst.rearrange("(n j p) c -> n p j c", p=128, j=4)
            for g in range(N // 512):
                wt, b_w = load_w(P, wr_, Wap, KC, c0 + g * 512, 512)
                for tt in range(T // 512):
                    at, b_a = load_a(ar_, Aap, aname, KC, tt * 512)
                    st, b_s = sr_.next()
                    for j in range(4):
                        ps_, b_p = pr_.next()
                        for k in range(KC):
                            S.op("pe", lambda ps_=ps_, wt=wt, at=at, j=j, k=k: nc.tensor.matmul(
                                ps_[:], at[:, k, j * 128:(j + 1) * 128], wt[:, k, :], start=(k == 0), stop=(k == KC - 1)),
                                reads=[b_w, b_a], writes=[b_p])
                        S.op("act", lambda ps_=ps_, st=st, j=j: nc.scalar.activation(st[:, j, :], ps_[:], AF.Copy),
                             reads=[b_p], writes=[b_s])
                    S.dma(b_s, [("pool", lambda st=st, g=g, tt=tt: nc.gpsimd.dma_start(
                        out=dv[tt][:, :, g * 512:(g + 1) * 512], in_=st[:]))], reads=[b_s], writes=[db(dname)])

        def latent_norm(P, c0, Aap, aname, T, gcol, dst, dname, rings):
            wr_, ar_, pr_, sr_ = rings
            dv = dst.rearrange("(c p) t -> p c t", p=128)
            l32, b_l = P.sb("l32", [128, 4, 512], F32)
            lsq, b_q = P.sb("lsq", [128, 4, 512], BF16)
            rs, b_rs = P.sb("rs", [128, 512], F32)
            wt, b_w = load_w(P, wr_, w_in, 16, c0, 512)
            for tt in range(T // 512):
                at, b_a = load_a(ar_, Aap, aname, 16, tt * 512)
                st, b_s = sr_.next()
                for c in range(4):
                    ps_, b_p = pr_.next()
                    for k in range(16):
                        S.op("pe", lambda ps_=ps_, at=at, c=c, k=k: nc.tensor.matmul(
                            ps_[:], wt[:, k, c * 128:(c + 1) * 128], at[:, k, :], start=(k == 0), stop=(k == 15)),
                            reads=[b_w, b_a], writes=[b_p])
                    S.op("act", lambda ps_=ps_, c=c: nc.scalar.activation(l32[:, c, :], ps_[:], AF.Copy), reads=[b_p], writes=[b_l])
                    S.op("dve", lambda c=c: nc.vector.tensor_tensor(lsq[:, c, :], l32[:, c, :], l32[:, c, :], ALU.mult), reads=[b_l], writes=[b_q])
                ps_, b_p = pr_.next()
                for c in range(4):
                    S.op("pe", lambda ps_=ps_, c=c: nc.tensor.matmul(ps_[:], ones_b, lsq[:, c, :], start=(c == 0), stop=(c == 3)),
                         reads=[b_q, b_cstb], writes=[b_p])
                S.op("act", lambda ps_=ps_: nc.scalar.activation(rs[:], ps_[:], AF.Sqrt, bias=epsT[:, 0:1], scale=1.0 / 512),
                     reads=[b_p, b_eps], writes=[b_rs])
                S.op("dve", lambda: nc.vector.reciprocal(rs[:], rs[:]), reads=[b_rs], writes=[b_rs])
                for c in range(4):
                    S.op("dve", lambda st=st, c=c: nc.vector.scalar_tensor_tensor(
                        st[:, c, :], l32[:, c, :], gl[:, gcol + c:gcol + c + 1], rs[:], ALU.mult, ALU.mult),
                        reads=[b_l, b_gl, b_rs], writes=[b_s])
                S.dma(b_s, [("pool", lambda st=st, tt=tt: nc.gpsimd.dma_start(out=dv[:, :, tt * 512:(tt + 1) * 512], in_=st[:]))],
                      reads=[b_s], writes=[db(dname)])

        def rope_tables(P, pos_ap, T, scale, name):
            pi_, b_pi = P.sb("posi", [128, T], I32)
            S.dma(b_pi, [("sp", lambda: nc.sync.dma_start(out=pi_[:], in_=pos_ap.to_broadcast([128, T])))], writes=[b_pi])
            ang, b_an = P.sb("ang", [128, T], F32)
            S.op("dve", lambda: nc.vector.tensor_copy(ang[:], pi_[:]), reads=[b_pi], writes=[b_an])
            C, b_C = P.sb("C" + name, [128, T], F32)
            Sg, b_S = P.sb("S" + name, [128, T], F32)
            tmp, b_t = P.sb("rtmp", [128, T], F32)
            ni, b_ni = P.sb("rni", [128, T], I32)
            S.op("dve", lambda: nc.vector.tensor_scalar(ang[:], ang[:], rc[:, 0:1], None, op0=ALU.mult), reads=[b_an, b_rc], writes=[b_an])
            for dst_, b_d, offs in ((Sg, b_S, 0.0), (C, b_C, 0.5 * PI)):
                S.op("dve", lambda offs=offs: nc.vector.tensor_scalar(tmp[:], ang[:], offs, 1.0 / (2 * PI), op0=ALU.add, op1=ALU.mult), reads=[b_an], writes=[b_t])
                S.op("dve", lambda: nc.vector.tensor_copy(ni[:], tmp[:]), reads=[b_t], writes=[b_ni])
                S.op("dve", lambda: nc.vector.tensor_copy(tmp[:], ni[:]), reads=[b_ni], writes=[b_t])
                S.op("dve", lambda: nc.vector.scalar_tensor_tensor(tmp[:], tmp[:], -2 * PI, ang[:], ALU.mult, ALU.add), reads=[b_t, b_an], writes=[b_t])
                if offs != 0.0:
                    S.op("dve", lambda offs=offs: nc.vector.tensor_scalar(tmp[:], tmp[:], offs, None, op0=ALU.add), reads=[b_t], writes=[b_t])
                S.op("dve", lambda dst_=dst_: nc.vector.tensor_scalar(dst_[:], tmp[:], PI, 2 * PI, op0=ALU.is_gt, op1=ALU.mult), reads=[b_t], writes=[b_d])
                S.op("dve", lambda dst_=dst_: nc.vector.tensor_tensor(tmp[:], tmp[:], dst_[:], ALU.subtract), reads=[b_t, b_d], writes=[b_t])
                S.op("dve", lambda dst_=dst_: nc.vector.tensor_scalar(dst_[:], tmp[:], -PI, 2 * PI, op0=ALU.is_lt, op1=ALU.mult), reads=[b_t], writes=[b_d])
                S.op("dve", lambda dst_=dst_: nc.vector.tensor_tensor(tmp[:], tmp[:], dst_[:], ALU.add), reads=[b_t, b_d], writes=[b_t])
                S.op("act", lambda dst_=dst_: nc.scalar.activation(dst_[:], tmp[:], AF.Sin), reads=[b_t], writes=[b_d])
            S.op("dve", lambda: nc.vector.tensor_scalar(Sg[:], Sg[:], rc[:, 1:2], float(scale), op0=ALU.mult, op1=ALU.mult),
                 reads=[b_S, b_rc], writes=[b_S])
            if scale != 1.0:
                S.op("dve", lambda: nc.vector.tensor_scalar(C[:], C[:], float(scale), None, op0=ALU.mult), reads=[b_C], writes=[b_C])
            return (C, b_C), (Sg, b_S)

        def rope_proj(P, Wa, ca0, Ws, cs0, nh, Aap, aname, T, KC, CS, dst, dname, rings):
            wr_, ar_, pr_, sr_ = rings
            (C, b_C), (Sg, b_S) = CS
            wa, b_wa = load_w(P, wr_, Wa, KC, ca0, nh * 64)
            ws, b_ws = load_w(P, wr_, Ws, KC, cs0, nh * 64)
            t1, b_t1 = P.sb("rt1", [64, 512], F32)
            t2, b_t2 = P.sb("rt2", [64, 512], F32)
            for tt in range(T // 512):
                at, b_a = load_a(ar_, Aap, aname, KC, tt * 512)
                for h in range(nh):
                    pa, b_pa = pr_.next()
                    pb, b_pb = pr_.next()
                    for k in range(KC):
                        S.op("pe", lambda pa=pa, at=at, h=h, k=k: nc.tensor.matmul(
                            pa[0:64, :], wa[:, k, h * 64:(h + 1) * 64], at[:, k, :], start=(k == 0), stop=(k == KC - 1)),
                            reads=[b_wa, b_a], writes=[b_pa])
                    for k in range(KC):
                        S.op("pe", lambda pb=pb, at=at, h=h, k=k: nc.tensor.matmul(
                            pb[0:64, :], ws[:, k, h * 64:(h + 1) * 64], at[:, k, :], start=(k == 0), stop=(k == KC - 1)),
                            reads=[b_ws, b_a], writes=[b_pb])
                    st, b_s = sr_.next()
                    S.op("dve", lambda pa=pa, tt=tt: nc.vector.tensor_tensor(t1[:], pa[0:64, :], C[0:64, tt * 512:(tt + 1) * 512], ALU.mult),
                         reads=[b_pa, b_C], writes=[b_t1])
                    S.op("dve", lambda pb=pb, tt=tt: nc.vector.tensor_tensor(t2[:], pb[0:64, :], Sg[0:64, tt * 512:(tt + 1) * 512], ALU.mult),
                         reads=[b_pb, b_S], writes=[b_t2])
                    S.op("dve", lambda st=st: nc.vector.tensor_tensor(st[0:64, 0, :], t1[:], t2[:], ALU.add), reads=[b_t1, b_t2], writes=[b_s])
                    S.dma(b_s, [("pool", lambda st=st, h=h, tt=tt: nc.gpsimd.dma_start(
                        out=dst[h * 64:(h + 1) * 64, tt * 512:(tt + 1) * 512], in_=st[0:64, 0, :]))], reads=[b_s], writes=[db(dname)])

        if stage >= 2:
            with phase(nc, S, "B") as P:
                rings = (P.ring_sb("w", [128, 16, 512], BF16, 2), P.ring_sb("a", [128, 16, 512], BF16, 2),
                         P.ring_ps("p", [128, 512], F32, 6), P.ring_sb("st", [128, 4, 512], BF16, 2))
                gemm_fm(P, w_in, 0, 1024, hT_own, "hT_own", T_OWN, 16, QT_sb, "QT_sb", 128 ** -0.5, rings)
                gemm_fm(P, w_in, 1024, 1024, hT_all, "hT_all", S_ALL, 16, KT_sb, "KT_sb", 1.0, rings)
                gemm_tm(P, w_in, 2048, 1024, hT_all, "hT_all", S_ALL, 16, V_sb, "V_sb", rings)
                latent_norm(P, 3072, hT_own, "hT_own", T_OWN, 0, qlnT, "qlnT", rings)
                latent_norm(P, 3584, hT_all, "hT_all", S_ALL, 4, kvlnT, "kvlnT", rings)
            with phase(nc, S, "B2") as P:
                rings = (P.ring_sb("w", [128, 16, 512], BF16, 2), P.ring_sb("a", [128, 16, 512], BF16, 2),
                         P.ring_ps("p", [128, 512], F32, 6), P.ring_sb("st", [128, 4, 512], BF16, 2))
                CSk = rope_tables(P, pos_all, S_ALL, 1.0, "k")
                rope_proj(P, w_kr, 0, w_kr, 64, 1, hT_all, "hT_all", S_ALL, 16, CSk, KrT, "KrT", rings)
            with phase(nc, S, "B3") as P:
                rings = (P.ring_sb("w", [128, 16, 512], BF16, 2), P.ring_sb("a", [128, 16, 512], BF16, 2),
                         P.ring_ps("p", [128, 512], F32, 6), P.ring_sb("st", [128, 4, 512], BF16, 2))
                CSq = rope_tables(P, pos_own, T_OWN, 192 ** -0.5, "q")
                rope_proj(P, w_qra, 0, w_qrs, 0, 8, qlnT, "qlnT", T_OWN, 4, CSq, QrT, "QrT", rings)
                gemm_fm(P, w_qn, 0, 1024, qlnT, "qlnT", T_OWN, 4, QnT, "QnT", 192 ** -0.5, rings)
                gemm_fm(P, w_kn, 0, 1024, kvlnT, "kvlnT", S_ALL, 4, KnT, "KnT", 1.0, rings)
                gemm_tm(P, w_v, 0, 1024, kvlnT, "kvlnT", S_ALL, 4, V_ml, "V_ml", rings)

        def attention(P, is_sb):
            mk, b_mk = P.sb("mk", [128, 4, 128], F32)
            S.dma(b_mk, [("sp", lambda: nc.sync.dma_start(out=mk[:], in_=masks[0 if is_sb else 1].rearrange("z p n -> p z n")))], writes=[b_mk])
            mkb, b_mkb = P.sb("mkb", [128, 4, 128], BF16)
            S.op("dve", lambda: nc.vector.tensor_copy(mkb[:], mk[:]), reads=[b_mk], writes=[b_mkb])
            ktr = P.ring_sb("kt", [128, S_ALL], BF16, 2)
            vr = P.ring_sb("v", [128, 32, 128], BF16, 2)
            qr = P.ring_sb("q", [128, T_OWN], BF16, 2)
            if not is_sb:
                krt, b_krt = P.sb("krt", [64, S_ALL], BF16)
                S.dma(b_krt, [("sp", lambda: nc.sync.dma_start(out=krt[:], in_=KrT))], reads=[db("KrT")], writes=[b_krt])
                qrr = P.ring_sb("qr", [64, T_OWN], BF16, 2)
            pA = P.ring_ps("pA", [128, 512], F32, 2)
            pB = P.ring_ps("pB", [128, 512], F32, 2)
            pO = P.ring_ps("pO", [128, 512], F32, 2)
            if is_sb:
                pC, b_pC = P.ps("pC", [128, 512], F32)
                e32r = P.ring_sb("e32", [128, 512], F32, 2)
                spr = P.ring_sb("sp", [128, 512], BF16, 2)
                Tt, b_T = P.sb("T", [33, 512], F32)
                THL, b_THL = P.sb("THL", [33, 512], BF16)
            else:
                pD = P.ring_ps("pD", [128, 512], F32, 2)
                rdn, b_rdn = P.sb("rdn", [128, 512], F32)
            ar = P.ring_sb("a", [128, 512], BF16, 3)
            mor = P.ring_sb("mo", [128, 512], BF16, 2)
            zer, b_zer = P.sb("zer", [128, 128], BF16)
            S.op("dve", lambda: nc.vector.memset(zer[:], 0.0), writes=[b_zer])
            KT = KT_sb if is_sb else KnT
            QT = QT_sb if is_sb else QnT
            VV = V_sb if is_sb else V_ml
            kn, qn, vn = ("KT_sb", "QT_sb", "V_sb") if is_sb else ("KnT", "QnT", "V_ml")
            for h in range(8):
                kt, b_kt = ktr.next()
                S.dma(b_kt, [("sp", lambda kt=kt, h=h: nc.sync.dma_start(out=kt[:], in_=KT[h * 128:(h + 1) * 128, :]))], reads=[db(kn)], writes=[b_kt])
                vt, b_vt = vr.next()
                S.dma(b_vt, [("sp", lambda vt=vt, h=h: nc.sync.dma_start(
                    out=vt[:], in_=VV.rearrange("(kb p) c -> p kb c", p=128)[:, :, h * 128:(h + 1) * 128]))], reads=[db(vn)], writes=[b_vt])
                qt, b_qt = qr.next()
                S.dma(b_qt, [("sp", lambda qt=qt, h=h: nc.sync.dma_start(out=qt[:], in_=QT[h * 128:(h + 1) * 128, :]))], reads=[db(qn)], writes=[b_qt])
                if not is_sb:
                    qrt, b_qrt = qrr.next()
                    S.dma(b_qrt, [("sp", lambda qrt=qrt, h=h: nc.sync.dma_start(out=qrt[:], in_=QrT[h * 64:(h + 1) * 64, :]))], reads=[db("QrT")], writes=[b_qrt])
                for m in range(4):
                    q0 = m * 512
                    po, b_po = pO.next()
                    S.op("pe", lambda po=po, qt=qt, q0=q0: nc.tensor.matmul(po[:], zer[:], qt[:, q0:q0 + 512], start=True, stop=False),
                         reads=[b_zer, b_qt], writes=[b_po])
                    if is_sb:
                        S.op("dve", lambda: nc.vector.memset(Tt[:], 0.0), writes=[b_T])
                        S.op("dve", lambda: nc.vector.memset(THL[:], 0.0), writes=[b_THL])
                    else:
                        pd, b_pd = pD.next()
                        S.op("pe", lambda pd=pd, qt=qt, q0=q0: nc.tensor.matmul(pd[:], zer[:], qt[:, q0:q0 + 512], start=True, stop=False),
                             reads=[b_zer, b_qt], writes=[b_pd])
                    kmax = 8 * m + 7
                    for kb in range(kmax, -1, -1):
                        last = (kb == 0)
                        smin = 0
                        while 8 * m + 1 + 2 * smin < kb:
                            smin += 1
                        c0 = smin * 128
                        cs = slice(c0, 512)
                        qs = slice(q0 + c0, q0 + 512)
                        ks = slice(kb * 128, (kb + 1) * 128)
                        bslot = None
                        if kb >= 8 * m:
                            r = kb - 8 * m
                            bslot, zone = r // 2, (r // 2 % 2) * 2 + (r % 2)
                        pa, b_pa = pA.next()
                        at, b_at = ar.next()
                        if is_sb:
                            S.op("pe", lambda pa=pa, kt=kt, qt=qt, ks=ks, qs=qs, cs=cs: nc.tensor.matmul(pa[:, cs], kt[:, ks], qt[:, qs], start=True, stop=True),
                                 reads=[b_kt, b_qt], writes=[b_pa])
                            e32, b_e = e32r.next()
                            spm, b_sp = spr.next()
                            S.op("act", lambda pa=pa, e32=e32, cs=cs: nc.scalar.activation(e32[:, cs], pa[:, cs], AF.Exp), reads=[b_pa], writes=[b_e])
                            S.op("act", lambda e32=e32, spm=spm, cs=cs: nc.scalar.activation(spm[:, cs], e32[:, cs], AF.Ln, bias=1.0), reads=[b_e], writes=[b_sp])
                            if bslot is not None:
                                bs = slice(bslot * 128, (bslot + 1) * 128)
                                S.op("pool", lambda spm=spm, bs=bs, zone=zone: nc.gpsimd.tensor_tensor(spm[:, bs], spm[:, bs], mkb[:, zone, :], ALU.mult),
                                     reads=[b_sp, b_mkb], writes=[b_sp])
                            pb, b_pb = pB.next()
                            S.op("pe", lambda pb=pb, kt=kt, qt=qt, ks=ks, qs=qs, cs=cs: nc.tensor.matmul(pb[:, cs], kt[:, ks], qt[:, qs], start=True, stop=False),
                                 reads=[b_kt, b_qt], writes=[b_pb])
                            S.op("pe", lambda pb=pb, spm=spm, cs=cs: nc.tensor.matmul(pb[:, cs], negtri_b, spm[:, cs], start=False, stop=False),
                                 reads=[b_sp, b_cstb], writes=[b_pb])
                            S.op("pe", lambda pb=pb, cs=cs: nc.tensor.matmul(pb[:, cs], bc33_b, THL[0:33, cs], start=False, stop=True),
                                 reads=[b_THL, b_cstb], writes=[b_pb])
                            S.op("act", lambda pb=pb, at=at, cs=cs: nc.scalar.activation(at[:, cs], pb[:, cs], AF.Exp), reads=[b_pb], writes=[b_at])
                            if not last:
                                S.op("pe", lambda spm=spm, cs=cs: nc.tensor.matmul(pC[0:33, cs], negones33_b, spm[:, cs], start=True, stop=True),
                                     reads=[b_sp, b_cstb], writes=[b_pC])
                                S.op("dve", lambda cs=cs: nc.vector.tensor_tensor(Tt[:, cs], Tt[:, cs], pC[0:33, cs], ALU.add), reads=[b_pC, b_T], writes=[b_T])
                                S.op("dve", lambda cs=cs: nc.vector.tensor_copy(THL[:, cs], Tt[:, cs]), reads=[b_T], writes=[b_THL])
                                S.op("dve", lambda cs=cs: nc.vector.tensor_tensor(THL[32:33, cs], Tt[32:33, cs], THL[32:33, cs], ALU.subtract),
                                     reads=[b_T, b_THL], writes=[b_THL])
                        else:
                            S.op("pe", lambda pa=pa, kt=kt, qt=qt, ks=ks, qs=qs, cs=cs: nc.tensor.matmul(pa[:, cs], kt[:, ks], qt[:, qs], start=True, stop=False),
                                 reads=[b_kt, b_qt], writes=[b_pa])
                            S.op("pe", lambda pa=pa, qrt=qrt, ks=ks, qs=qs, cs=cs: nc.tensor.matmul(pa[:, cs], krt[:, ks], qrt[:, qs], start=False, stop=True),
                                 reads=[b_krt, b_qrt], writes=[b_pa])
                            S.op("act", lambda pa=pa, at=at, cs=cs: nc.scalar.activation(at[:, cs], pa[:, cs], AF.Exp), reads=[b_pa], writes=[b_at])
                        if bslot is not None:
                            bs = slice(bslot * 128, (bslot + 1) * 128)
                            S.op("pool", lambda at=at, bs=bs, zone=zone: nc.gpsimd.tensor_tensor(at[:, bs], at[:, bs], mkb[:, zone, :], ALU.mult),
                                 reads=[b_at, b_mkb], writes=[b_at])
                        S.op("pe", lambda po=po, vt=vt, at=at, kb=kb, cs=cs, last=last: nc.tensor.matmul(po[:, cs], vt[:, kb, :], at[:, cs], start=False, stop=last),
                             reads=[b_vt, b_at], writes=[b_po])
                        if not is_sb:
                            S.op("pe", lambda pd=pd, at=at, cs=cs, last=last: nc.tensor.matmul(pd[:, cs], ones_b, at[:, cs], start=False, stop=last),
                                 reads=[b_cstb, b_at], writes=[b_pd])
                    mo, b_mo = mor.next()
                    if is_sb:
                        S.op("act", lambda po=po, mo=mo: nc.scalar.activation(mo[:], po[:], AF.Copy), reads=[b_po], writes=[b_mo])
                    else:
                        S.op("dve", lambda pd=pd: nc.vector.reciprocal(rdn[:], pd[:]), reads=[b_pd], writes=[b_rdn])
                        S.op("dve", lambda po=po, mo=mo: nc.vector.tensor_tensor(mo[:], po[:], rdn[:], ALU.mult), reads=[b_po, b_rdn], writes=[b_mo])
                    r0 = (0 if is_sb else 1024) + h * 128
                    S.dma(b_mo, [("pool", lambda mo=mo, r0=r0, q0=q0: nc.gpsimd.dma_start(out=mixT[r0:r0 + 128, q0:q0 + 512], in_=mo[:]))],
                          reads=[b_mo], writes=[db("mixT")])

        if stage >= 3:
            with phase(nc, S, "C1") as P:
                attention(P, True)
            with phase(nc, S, "C2") as P:
                attention(P, False)

        if stage >= 4:
            with phase(nc, S, "D") as P:
                g1, b_g1 = P.sb("g1", [128, D], F32)
                S.dma(b_g1, [("sp", lambda: nc.sync.dma_start(out=g1[:], in_=bc_row(mod_flat[:, 32 * 128:48 * 128], D)))], reads=[db("mod_d")], writes=[b_g1])
                wr_ = P.ring_sb("w", [128, 16, 512], BF16, 2)
                ar_ = P.ring_sb("a", [128, 16, 512], BF16, 2)
                pr_ = P.ring_ps("p", [128, 512], F32, 4)
                xor_ = P.ring_sb("xo", [128, 512], F32, 3)
                tr_ = P.ring_sb("t", [128, 512], F32, 3)
                for g in range(4):
                    wt, b_w = load_w(P, wr_, w_out, 16, g * 512, 512)
                    for tt in range(4):
                        at, b_a = load_a(ar_, mixT, "mixT", 16, tt * 512)
                        for j in range(4):
                            ps_, b_p = pr_.next()
                            for k in range(16):
                                S.op("pe", lambda ps_=ps_, wt=wt, at=at, j=j, k=k: nc.tensor.matmul(
                                    ps_[:], at[:, k, j * 128:(j + 1) * 128], wt[:, k, :], start=(k == 0), stop=(k == 15)),
                                    reads=[b_w, b_a], writes=[b_p])
                            rws = slice((tt * 4 + j) * 128, (tt * 4 + j + 1) * 128)
                            cls = slice(g * 512, (g + 1) * 512)
                            xo, b_xo = xor_.next()
                            S.dma(b_xo, [("sp", lambda xo=xo, rws=rws, cls=cls: nc.sync.dma_start(out=xo[:], in_=x_own[rws, cls]))], writes=[b_xo])
                            t_, b_t = tr_.next()
                            S.op("dve", lambda ps_=ps_, t_=t_, cls=cls: nc.vector.tensor_tensor(t_[:], ps_[:], g1[:, cls], ALU.mult), reads=[b_p, b_g1], writes=[b_t])
                            S.op("pool", lambda t_=t_, xo=xo: nc.gpsimd.tensor_tensor(t_[:], t_[:], xo[:], ALU.add), reads=[b_t, b_xo], writes=[b_t])
                            S.dma(b_t, [("pool", lambda t_=t_, rws=rws, cls=cls: nc.gpsimd.dma_start(out=x1_d[rws, cls], in_=t_[:]))], reads=[b_t], writes=[db("x1_d")])


        SMAX = T_OWN // 128
        dest_i, b_di = G.sb("dest_i", [128, 64], I32)
        w4, b_w4 = G.sb("w4", [128, 16, 4], F32)
        cnt_i, b_cni = G.sb("cnt_i", [128, NE], I32)
        idx_i, b_idx = G.sb("idx_i", [128, NE * SMAX], I32)
        if stage >= 5:
            with phase(nc, S, "R") as P:
                gm2, b_gm2 = P.sb("gm2", [128, D], F32)
                sh2, b_sh2 = P.sb("sh2", [128, D], F32)
                tmpD, b_tD = P.sb("tmpD", [128, D], F32)
                S.dma(b_gm2, [("sp", lambda: nc.sync.dma_start(out=gm2[:], in_=bc_row(mod_flat[:, 64 * 128:80 * 128], D)))], reads=[db("mod_d")], writes=[b_gm2])
                S.dma(b_tD, [("sp", lambda: nc.sync.dma_start(out=tmpD[:], in_=bc_row(rows[2:3, :], D)))], writes=[b_tD])
                S.dma(b_sh2, [("sp", lambda: nc.sync.dma_start(out=sh2[:], in_=bc_row(mod_flat[:, 48 * 128:64 * 128], D)))], reads=[db("mod_d")], writes=[b_sh2])
                S.op("dve", lambda: nc.vector.scalar_tensor_tensor(gm2[:], gm2[:], 1.0, tmpD[:], ALU.add, ALU.mult), reads=[b_gm2, b_tD], writes=[b_gm2])
                brt, b_brt = P.sb("brt", [128, NE], F32)
                S.dma(b_brt, [("sp", lambda: nc.sync.dma_start(out=brt[:], in_=bc_row(rows[1:2, 0:NE], NE)))], writes=[b_brt])
                wrs, b_wrs = P.sb("wrs", [128, 16, NE], F32)
                S.dma(b_wrs, [("sp", lambda: nc.sync.dma_start(out=wrs[:], in_=w_router))], writes=[b_wrs])
                h2b, b_h2b = P.sb("h2b", [128, 16 * D], BF16)
                maskf, b_mf = P.sb("maskf", [128, 16, NE], F32)
                wgt, b_wg = P.sb("wgt", [128, 16, NE], F32)
                posf, b_pf = P.sb("posf", [128, 16, NE], F32)
                cntb, b_cn = P.sb("cntb", [128, NE], F32)
                S.op("dve", lambda: nc.vector.memset(cntb[:], 0.0), writes=[b_cn])
                x1r = P.ring_sb("x1", [128, D], F32, 2)
                h2r = P.ring_sb("h2", [128, D], F32, 2)
                h2T, b_h2T = P.sb("h2T", [128, 16, 128], F32)
                sq, b_sq = P.sb("sq", [128, D], BF16)
                sm = P.ring_sb("sm", [128, 64], F32, 2)
                ex, b_ex = P.sb("ex", [128, NE], F32)
                mb, b_mb = P.sb("mb", [128, NE], BF16)
                ptr = P.ring_ps("pt", [128, 512], F32, 2)
                pl, b_pl = P.ps("pl", [128, NE], F32)
                pp, b_pp = P.ps("pp", [128, NE], F32)
                pc, b_pc = P.ps("pc", [128, NE], F32)
                for i in range(16):
                    xb, b_xb = x1r.next()
                    S.dma(b_xb, [("sp", lambda xb=xb, i=i: nc.sync.dma_start(out=xb[:], in_=x1_d[i * 128:(i + 1) * 128, :]))], reads=[db("x1_d")], writes=[b_xb])
                    s_, b_s = sm.next()
                    S.op("act", lambda xb=xb, s_=s_: nc.scalar.activation(sq[:], xb[:], AF.Square, accum_out=s_[:, 0:1]), reads=[b_xb], writes=[b_sq, b_s])
                    S.op("act", lambda s_=s_: nc.scalar.activation(s_[:, 0:1], s_[:, 0:1], AF.Sqrt, bias=epsT[:, 0:1], scale=1.0 / D), reads=[b_s, b_eps], writes=[b_s])
                    S.op("dve", lambda s_=s_: nc.vector.reciprocal(s_[:, 0:1], s_[:, 0:1]), reads=[b_s], writes=[b_s])
                    h2, b_h2 = h2r.next()
                    S.op("dve", lambda xb=xb, h2=h2, s_=s_: nc.vector.scalar_tensor_tensor(h2[:], xb[:], s_[:, 0:1], gm2[:], ALU.mult, ALU.mult), reads=[b_xb, b_s, b_gm2], writes=[b_h2])
                    S.op("pool", lambda h2=h2: nc.gpsimd.tensor_tensor(h2[:], h2[:], sh2[:], ALU.add), reads=[b_h2, b_sh2], writes=[b_h2])
                    S.op("act", lambda h2=h2, i=i: nc.scalar.activation(h2b[:, i * D:(i + 1) * D], h2[:], AF.Copy), reads=[b_h2], writes=[b_h2b])
                    for g in range(4):
                        pt_, b_p = ptr.next()
                        for c in range(4):
                            k = g * 4 + c
                            S.op("pe", lambda pt_=pt_, h2=h2, c=c, k=k: nc.tensor.transpose(pt_[:, c * 128:(c + 1) * 128], h2[:, k * 128:(k + 1) * 128], ident_f),
                                 reads=[b_h2, b_cst], writes=[b_p])
                        S.op("dve", lambda pt_=pt_, g=g: nc.vector.tensor_copy(h2T[:, g * 4:(g + 1) * 4, :], pt_[:].rearrange("p (c n) -> p c n", c=4)), reads=[b_p], writes=[b_h2T])
                    for k in range(16):
                        S.op("pe", lambda k=k: nc.tensor.matmul(pl[:], h2T[:, k, :], wrs[:, k, :], start=(k == 0), stop=(k == 15)), reads=[b_h2T, b_wrs], writes=[b_pl])
                    lg = s_[:, 32:64]
                    S.op("dve", lambda lg=lg: nc.vector.tensor_tensor(lg, pl[:], brt[:], ALU.add), reads=[b_pl, b_brt], writes=[b_s])
                    S.op("dve", lambda s_=s_, lg=lg: nc.vector.max(out=s_[:, 8:16], in_=lg), reads=[b_s], writes=[b_s])
                    S.op("dve", lambda s_=s_: nc.vector.tensor_scalar(s_[:, 16:17], s_[:, 8:9], -1.0, None, op0=ALU.mult), reads=[b_s], writes=[b_s])
                    S.op("dve", lambda s_=s_, lg=lg, i=i: nc.vector.tensor_scalar(maskf[:, i, :], lg, s_[:, 11:12], None, op0=ALU.is_ge), reads=[b_s], writes=[b_mf])
                    S.op("act", lambda s_=s_, lg=lg: nc.scalar.activation(ex[:], lg, AF.Exp, bias=s_[:, 16:17], scale=1.0), reads=[b_s], writes=[b_ex])
                    S.op("dve", lambda i=i: nc.vector.tensor_tensor(ex[:], ex[:], maskf[:, i, :], ALU.mult), reads=[b_ex, b_mf], writes=[b_ex])
                    S.op("dve", lambda s_=s_: nc.vector.reduce_sum(s_[:, 17:18], ex[:], AX.X), reads=[b_ex], writes=[b_s])
                    S.op("dve", lambda s_=s_: nc.vector.reciprocal(s_[:, 17:18], s_[:, 17:18]), reads=[b_s], writes=[b_s])
                    S.op("dve", lambda s_=s_, i=i: nc.vector.tensor_scalar(wgt[:, i, :], ex[:], s_[:, 17:18], None, op0=ALU.mult), reads=[b_ex, b_s], writes=[b_wg])
                    S.op("dve", lambda i=i: nc.vector.tensor_copy(mb[:], maskf[:, i, :]), reads=[b_mf], writes=[b_mb])
                    S.op("pe", lambda: nc.tensor.matmul(pp[:], tri_b, mb[:], start=True, stop=True), reads=[b_mb, b_cstb], writes=[b_pp])
                    S.op("pe", lambda: nc.tensor.matmul(pc[:], ones_b, mb[:], start=True, stop=True), reads=[b_mb, b_cstb], writes=[b_pc])
                    S.op("dve", lambda i=i: nc.vector.tensor_tensor(posf[:, i, :], pp[:], cntb[:], ALU.add), reads=[b_pp, b_cn], writes=[b_pf])
                    S.op("dve", lambda: nc.vector.tensor_tensor(cntb[:], cntb[:], pc[:], ALU.add), reads=[b_pc, b_cn], writes=[b_cn])
                S.op("dve", lambda: nc.vector.tensor_copy(cnt_i[:], cntb[:]), reads=[b_cn], writes=[b_cni])
                q_, b_q = P.sb("q_", [128, NE], F32)
                nf, b_nf = P.sb("nf", [128, NE], F32)
                nI, b_nI = P.sb("nI", [128, NE], I32)
                inc, b_inc = P.sb("inc", [128, NE], F32)
                inc2, b_inc2 = P.sb("inc2", [128, NE], F32)
                base, b_base = P.sb("base", [128, NE], F32)
                S.op("dve", lambda: nc.vector.tensor_scalar(q_[:], cntb[:], float(RB - 1), 1.0 / RB, op0=ALU.add, op1=ALU.mult), reads=[b_cn], writes=[b_q])
                S.op("dve", lambda: nc.vector.tensor_copy(nI[:], q_[:]), reads=[b_q], writes=[b_nI])
                S.op("dve", lambda: nc.vector.tensor_copy(nf[:], nI[:]), reads=[b_nI], writes=[b_nf])
                S.op("dve", lambda: nc.vector.tensor_tensor(inc[:], nf[:], q_[:], ALU.is_gt), reads=[b_nf, b_q], writes=[b_inc])
                S.op("dve", lambda: nc.vector.tensor_tensor(nf[:], nf[:], inc[:], ALU.subtract), reads=[b_nf, b_inc], writes=[b_nf])
                S.op("dve", lambda: nc.vector.tensor_scalar(nf[:], nf[:], float(RB), None, op0=ALU.mult), reads=[b_nf], writes=[b_nf])
                S.op("dve", lambda: nc.vector.tensor_copy(inc[:], nf[:]), reads=[b_nf], writes=[b_inc])
                cur, b_cur, oth, b_oth = inc, b_inc, inc2, b_inc2
                for sh in (1, 2, 4, 8, 16):
                    S.op("dve", lambda cur=cur, oth=oth, sh=sh: nc.vector.tensor_copy(oth[:, 0:sh], cur[:, 0:sh]), reads=[b_cur], writes=[b_oth])
                    S.op("dve", lambda cur=cur, oth=oth, sh=sh: nc.vector.tensor_tensor(oth[:, sh:NE], cur[:, sh:NE], cur[:, 0:NE - sh], ALU.add), reads=[b_cur, b_oth], writes=[b_oth])
                    cur, b_cur, oth, b_oth = oth, b_oth, cur, b_cur
                ends, b_ends = cur, b_cur
                S.op("dve", lambda: nc.vector.tensor_tensor(base[:], ends[:], nf[:], ALU.subtract), reads=[b_ends, b_nf], writes=[b_base])
                idxf, b_idxf = P.sb("idxf", [128, NE * SMAX], F32)
                for e in range(NE):
                    S.op("dve", lambda e=e: nc.vector.tensor_scalar(idxf[:, e * SMAX:(e + 1) * SMAX], cst[:, 7, 0:SMAX], base[:, e:e + 1], None, op0=ALU.add),
                         reads=[b_cst, b_base], writes=[b_idxf])
                S.op("dve", lambda: nc.vector.tensor_scalar(idxf[:], idxf[:], float(NROWS - 1), None, op0=ALU.min), reads=[b_idxf], writes=[b_idxf])
                S.op("dve", lambda: nc.vector.tensor_copy(idx_i[:], idxf[:]), reads=[b_idxf], writes=[b_idx])
                dm, b_dm = P.sb("dm", [128, NE], F32)
                eq, b_eq = P.sb("eq", [128, NE], F32)
                d8r = P.ring_sb("d8", [128, 16], F32, 2)
                for i in range(16):
                    d8, b_d8 = d8r.next()
                    S.op("dve", lambda i=i: nc.vector.tensor_tensor(dm[:], posf[:, i, :], base[:], ALU.add), reads=[b_pf, b_base], writes=[b_dm])
                    S.op("dve", lambda i=i: nc.vector.scalar_tensor_tensor(dm[:], dm[:], 1.0, maskf[:, i, :], ALU.add, ALU.mult), reads=[b_dm, b_mf], writes=[b_dm])
                    S.op("dve", lambda d8=d8: nc.vector.max(out=d8[:, 0:8], in_=dm[:]), reads=[b_dm], writes=[b_d8])
                    S.op("dve", lambda d8=d8: nc.vector.tensor_scalar(d8[:, 8:12], d8[:, 0:4], -1.0, None, op0=ALU.add), reads=[b_d8], writes=[b_d8])
                    S.op("dve", lambda d8=d8, i=i: nc.vector.tensor_copy(dest_i[:, i * 4:i * 4 + 4], d8[:, 8:12]), reads=[b_d8], writes=[b_di])
                    for k in range(4):
                        S.op("dve", lambda d8=d8, k=k: nc.vector.tensor_scalar(eq[:], dm[:], d8[:, k:k + 1], None, op0=ALU.is_equal), reads=[b_dm, b_d8], writes=[b_eq])
                        S.op("dve", lambda i=i: nc.vector.tensor_tensor(eq[:], eq[:], wgt[:, i, :], ALU.mult), reads=[b_eq, b_wg], writes=[b_eq])
                        S.op("dve", lambda i=i, k=k: nc.vector.reduce_sum(w4[:, i, k:k + 1], eq[:], AX.X), reads=[b_eq], writes=[b_w4])
                    for k in range(4):
                        S.dma(b_h2b, [("pool", lambda i=i, k=k: nc.gpsimd.indirect_dma_start(
                            out=xs_d, out_offset=bass.IndirectOffsetOnAxis(ap=dest_i[:, i * 4 + k:i * 4 + k + 1], axis=0), in_=h2b[:, i * D:(i + 1) * D], in_offset=None))],
                            reads=[b_h2b, b_di], writes=[db("xs_d")])

        if stage >= 6:
            with phase(nc, S, "E") as P:
                regs = nc.alloc_registers("cntreg", engines=mybir.ALL_ENGINES)
                engname = {mybir.EngineType.Pool: "pool", mybir.EngineType.Activation: "act", mybir.EngineType.PE: "pe",
                           mybir.EngineType.DVE: "dve", mybir.EngineType.SP: "sp"}
                xsT, b_xsT = P.sb("xsT", [128, 16, 1024], BF16)
                actT, b_act = P.sb("actT", [128, 16, 1024], BF16)
                xrr = P.ring_sb("xr", [128, D], BF16, 2)
                w1r = P.ring_sb("w1", [128, 16, 256], BF16, 6)
                w2r = P.ring_sb("w2", [128, 16, 512], BF16, 2)
                b1r = P.ring_sb("b1", [128, 32], F32, 2)
                b2r = P.ring_sb("b2", [128, D], F32, 2)
                ptb = P.ring_ps("ptb", [128, 1024], BF16, 2)
                pg = P.ring_ps("pg", [128, 512], F32, 2)
                bbr = P.ring_sb("bb", [128, 512], F32, 3)
                glr = P.ring_sb("gl", [128, 512], F32, 2)
                abr = P.ring_sb("ab", [128, 256], BF16, 3)
                py = P.ring_ps("py", [128, 512], F32, 2)
                sg, b_sg = P.sb("sg", [128, 256], F32)
                yr = P.ring_sb("y", [128, 512], F32, 3)
                def emit_tr(s_j, ab_, b_ab, rs_, pc_):
                    with S.guard(regs, s_j * 128):
                        pt_, b_p = ptb.next()
                        for c in range(2):
                            S.op("pe", lambda pt_=pt_, ab_=ab_, c=c: nc.tensor.transpose(pt_[:, c * 128:(c + 1) * 128], ab_[:, c * 128:(c + 1) * 128], ident_b),
                                 reads=[b_ab, b_cstb], writes=[b_p])
                        S.op("act", lambda pt_=pt_, pc_=pc_, rs_=rs_: nc.scalar.activation(
                            actT[:, pc_ * 2:pc_ * 2 + 2, rs_], pt_[:, 0:256].rearrange("p (c n) -> p c n", c=2), AF.Copy), reads=[b_p], writes=[b_act])

                import os as _os
                for e in range(int(_os.environ.get('K_NEXP', NE))):
                    for reg in regs:
                        S.op(engname[reg.engine], lambda reg=reg, e=e: nc.reg_load(reg, cnt_i[0:1, e:e + 1]), reads=[b_cni])
                    b1t, b_b1 = b1r.next()
                    S.dma(b_b1, [("sp", lambda b1t=b1t, e=e: nc.sync.dma_start(out=b1t[:, 0:16], in_=b1g[e])),
                                 ("sp", lambda b1t=b1t, e=e: nc.sync.dma_start(out=b1t[:, 16:32], in_=b1l[e]))], writes=[b_b1])
                    b2t, b_b2 = b2r.next()
                    S.dma(b_b2, [("sp", lambda b2t=b2t, e=e: nc.sync.dma_start(out=b2t[:], in_=b2[e].to_broadcast([128, D])))], writes=[b_b2])
                    ESECT = 7
                    for ps in range(2):
                      with (S.guard(regs, 1024) if ps else contextlib.nullcontext()):
                            for s_i in range(ps * 8, ps * 8 + 8):
                                with S.guard(regs, s_i * 128):
                                    xr_, b_xr = xrr.next()
                                    col = e * SMAX + s_i
                                    S.dma(b_xr, [("pool", lambda xr_=xr_, col=col: nc.gpsimd.indirect_dma_start(
                                        out=xr_[:], out_offset=None, in_=xs_d, in_offset=bass.IndirectOffsetOnAxis(ap=idx_i[:, col:col + 1], axis=0)))],
                                        reads=[db("xs_d"), b_idx], writes=[b_xr])
                                    for g in range(2):
                                        pt_, b_p = ptb.next()
                                        for c in range(8):
                                            k = g * 8 + c
                                            S.op("pe", lambda pt_=pt_, xr_=xr_, c=c, k=k: nc.tensor.transpose(pt_[:, c * 128:(c + 1) * 128], xr_[:, k * 128:(k + 1) * 128], ident_b),
                                                 reads=[b_xr, b_cstb], writes=[b_p])
                                        S.op("act", lambda pt_=pt_, g=g, s_i=s_i: nc.scalar.activation(
                                            xsT[:, g * 8:(g + 1) * 8, (s_i - ps * 8) * 128:(s_i - ps * 8 + 1) * 128], pt_[:].rearrange("p (c n) -> p c n", c=8), AF.Copy), reads=[b_p], writes=[b_xsT])
                            for pc_ in range(8 if ESECT & 2 else 0):
                                wg_, b_wg_ = w1r.next()
                                wl_, b_wl_ = w1r.next()
                                cs_ = slice(pc_ * 256, (pc_ + 1) * 256)
                                S.dma(b_wg_, [("pool", lambda wg_=wg_, e=e, cs_=cs_: nc.gpsimd.dma_start(out=wg_[:], in_=w1g[e].rearrange("(k p) n -> p k n", p=128)[:, :, cs_]))], writes=[b_wg_])
                                S.dma(b_wl_, [("pool", lambda wl_=wl_, e=e, cs_=cs_: nc.gpsimd.dma_start(out=wl_[:], in_=w1l[e].rearrange("(k p) n -> p k n", p=128)[:, :, cs_]))], writes=[b_wl_])
                                bb_, b_bb = bbr.next()
                                S.dma(b_bb, [("sp", lambda bb_=bb_, e=e, cs_=cs_: nc.sync.dma_start(out=bb_[:, 0:256], in_=b1g_r[e][:, cs_].to_broadcast([128, 256]))),
                                             ("sp", lambda bb_=bb_, e=e, cs_=cs_: nc.sync.dma_start(out=bb_[:, 256:512], in_=b1l_r[e][:, cs_].to_broadcast([128, 256])))], writes=[b_bb])
                                pend = None
                                for s_i in range(ps * 8, ps * 8 + 8):
                                    rs_ = slice((s_i - ps * 8) * 128, (s_i - ps * 8 + 1) * 128)
                                    pend_new = None
                                    with S.guard(regs, s_i * 128):
                                        pg_, b_pg = pg.next()
                                        for k in range(16):
                                            S.op("pe", lambda pg_=pg_, wg_=wg_, k=k, rs_=rs_: nc.tensor.matmul(pg_[:, 0:256], xsT[:, k, rs_], wg_[:, k, :], start=(k == 0), stop=(k == 15)),
                                                 reads=[b_wg_, b_xsT], writes=[b_pg])
                                        for k in range(16):
                                            S.op("pe", lambda pg_=pg_, wl_=wl_, k=k, rs_=rs_: nc.tensor.matmul(pg_[:, 256:512], xsT[:, k, rs_], wl_[:, k, :], start=(k == 0), stop=(k == 15)),
                                                 reads=[b_wl_, b_xsT], writes=[b_pg])
                                        gl_, b_gl_ = glr.next()
                                        S.op("dve", lambda pg_=pg_, gl_=gl_, bb_=bb_: nc.vector.tensor_tensor(gl_[:], pg_[:], bb_[:], ALU.add), reads=[b_pg, b_bb], writes=[b_gl_])
                                        S.op("dve", lambda gl_=gl_: nc.vector.tensor_scalar(gl_[:], gl_[:], 7.0, None, op0=ALU.min), reads=[b_gl_], writes=[b_gl_])
                                        S.op("act", lambda gl_=gl_: nc.scalar.activation(sg[:], gl_[:, 0:256], AF.Sigmoid, scale=1.702), reads=[b_gl_], writes=[b_sg])
                                        S.op("dve", lambda gl_=gl_: nc.vector.tensor_scalar(gl_[:, 256:512], gl_[:, 256:512], -7.0, None, op0=ALU.max), reads=[b_gl_], writes=[b_gl_])
                                        S.op("dve", lambda gl_=gl_: nc.vector.tensor_tensor(sg[:], sg[:], gl_[:, 0:256], ALU.mult), reads=[b_gl_, b_sg], writes=[b_sg])
                                        ab_, b_ab = abr.next()
                                        S.op("dve", lambda gl_=gl_, ab_=ab_: nc.vector.scalar_tensor_tensor(ab_[:], gl_[:, 256:512], 1.0, sg[:], ALU.add, ALU.mult), reads=[b_gl_, b_sg], writes=[b_ab])
                                        pend_new = (s_i, ab_, b_ab, rs_)
                                    if pend is not None:
                                        emit_tr(*pend, pc_)
                                    pend = pend_new
                                if pend is not None:
                                    emit_tr(*pend, pc_)
                                    pend = None
                            for np_ in range(4 if ESECT & 4 else 0):
                                w2t, b_w2 = w2r.next()
                                cs_ = slice(np_ * 512, (np_ + 1) * 512)
                                S.dma(b_w2, [("pool", lambda w2t=w2t, e=e, cs_=cs_: nc.gpsimd.dma_start(out=w2t[:], in_=w2[e].rearrange("(k p) n -> p k n", p=128)[:, :, cs_]))], writes=[b_w2])
                                for s_i in range(ps * 8, ps * 8 + 8):
                                    rs_ = slice((s_i - ps * 8) * 128, (s_i - ps * 8 + 1) * 128)
                                    with S.guard(regs, s_i * 128):
                                        py_, b_py = py.next()
                                        for k in range(16):
                                            S.op("pe", lambda py_=py_, w2t=w2t, rs_=rs_, k=k: nc.tensor.matmul(py_[:], actT[:, k, rs_], w2t[:, k, :], start=(k == 0), stop=(k == 15)),
                                                 reads=[b_act, b_w2], writes=[b_py])
                                        y_, b_y = yr.next()
                                        S.op("dve", lambda py_=py_, y_=y_, b2t=b2t, cs_=cs_: nc.vector.tensor_tensor(y_[:], py_[:], b2t[:, cs_], ALU.add), reads=[b_py, b_b2], writes=[b_y])
                                        col = e * SMAX + s_i
                                        S.dma(b_y, [("pool", lambda y_=y_, col=col, np_=np_: nc.gpsimd.indirect_dma_start(
                                            out=ys_p[np_], out_offset=bass.IndirectOffsetOnAxis(ap=idx_i[:, col:col + 1], axis=0), in_=y_[:], in_offset=None))],
                                            reads=[b_y, b_idx], writes=[db("ys_d")])


        if stage >= 7:
            with phase(nc, S, "F") as P:
                g2, b_g2 = P.sb("g2", [128, D], F32)
                gf, b_gf = P.sb("gf", [128, D], F32)
                S.dma(b_g2, [("sp", lambda: nc.sync.dma_start(out=g2[:], in_=bc_row(mod_flat[:, 80 * 128:96 * 128], D)))], reads=[db("mod_d")], writes=[b_g2])
                S.dma(b_gf, [("sp", lambda: nc.sync.dma_start(out=gf[:], in_=bc_row(rows[0:1, :], D)))], writes=[b_gf])
                gr = P.ring_sb("ga", [128, D], F32, 4)
                accr = P.ring_sb("acc", [128, D], F32, 2)
                x1r = P.ring_sb("x1", [128, D], F32, 2)
                sq, b_sq = P.sb("sq", [128, D], BF16)
                ssr = P.ring_sb("ss", [128, 1], F32, 2)
                for i in range(16):
                    xb, b_xb = x1r.next()
                    S.dma(b_xb, [("sp", lambda xb=xb, i=i: nc.sync.dma_start(out=xb[:], in_=x1_d[i * 128:(i + 1) * 128, :]))], reads=[db("x1_d")], writes=[b_xb])
                    acc, b_acc = accr.next()
                    for k in range(4):
                        ga, b_ga = gr.next()
                        S.dma(b_ga, [("pool", lambda ga=ga, i=i, k=k, q=q: nc.gpsimd.indirect_dma_start(
                            out=ga[:, q * 512:(q + 1) * 512], out_offset=None, in_=ys_p[q], in_offset=bass.IndirectOffsetOnAxis(ap=dest_i[:, i * 4 + k:i * 4 + k + 1], axis=0)))
                            for q in range(4)], reads=[db("ys_d"), b_di], writes=[b_ga])
                        if k == 0:
                            S.op("dve", lambda ga=ga, acc=acc, i=i, k=k: nc.vector.tensor_scalar(acc[:], ga[:], w4[:, i, k:k + 1], None, op0=ALU.mult), reads=[b_ga, b_w4], writes=[b_acc])
                        else:
                            S.op("dve", lambda ga=ga, acc=acc, i=i, k=k: nc.vector.scalar_tensor_tensor(acc[:], ga[:], w4[:, i, k:k + 1], acc[:], ALU.mult, ALU.add), reads=[b_ga, b_w4, b_acc], writes=[b_acc])
                    S.op("pool", lambda acc=acc: nc.gpsimd.tensor_tensor(acc[:], acc[:], g2[:], ALU.mult), reads=[b_acc, b_g2], writes=[b_acc])
                    S.op("dve", lambda acc=acc, xb=xb: nc.vector.tensor_tensor(acc[:], acc[:], xb[:], ALU.add), reads=[b_acc, b_xb], writes=[b_acc])
                    ss, b_ss = ssr.next()
                    S.op("act", lambda acc=acc, ss=ss: nc.scalar.activation(sq[:], acc[:], AF.Square, accum_out=ss[:, 0:1]), reads=[b_acc], writes=[b_sq, b_ss])
                    S.op("act", lambda ss=ss: nc.scalar.activation(ss[:], ss[:], AF.Sqrt, bias=epsT[:, 0:1], scale=1.0 / D), reads=[b_ss, b_eps], writes=[b_ss])
                    S.op("dve", lambda ss=ss: nc.vector.reciprocal(ss[:], ss[:]), reads=[b_ss], writes=[b_ss])
                    S.op("dve", lambda acc=acc, ss=ss: nc.vector.scalar_tensor_tensor(acc[:], acc[:], ss[:, 0:1], gf[:], ALU.mult, ALU.mult), reads=[b_acc, b_ss, b_gf], writes=[b_acc])
                    S.dma(b_acc, [("act", lambda acc=acc, i=i: nc.scalar.dma_start(out=out[i * 128:(i + 1) * 128, :], in_=acc[:]))], reads=[b_acc], writes=[db("out")])

        if dbg and stage <= 4:
            with phase(nc, S, "DBG") as P:
                t_, b_t = P.sb("dbg", [128, 16, D], F32) if False else (None, None)
                r = P.ring_sb("r", [128, D], F32, 2)
                for i in range(16):
                    t_, b_t = r.next()
                    S.dma(b_t, [("sp", lambda t_=t_, i=i: nc.sync.dma_start(out=t_[:], in_=x1_d[i * 128:(i + 1) * 128, :]))], reads=[db("x1_d")], writes=[b_t])
                    S.dma(b_t, [("sp", lambda t_=t_, i=i: nc.sync.dma_start(out=dbg_out[i * 128:(i + 1) * 128, :], in_=t_[:]))], reads=[b_t], writes=[db("dbg")])

        S.barrier()
    return nc


def own_blocks(p):
    blks = []
    for m in range(8):
        blks += [4 * m + (0 if p == 0 else 1), 4 * m + (3 if p == 0 else 2)]
    return blks


def host_prepare(inp):
    f32 = np.float32
    x = np.asarray(inp["x"], f32)
    c = np.asarray(inp["c"], f32)
    pos = np.asarray(inp["positions"], np.int32)
    g_attn = np.asarray(inp["g_attn"], f32)[0]
    g_ffn = np.asarray(inp["g_ffn"], f32)[0]
    b_mod = np.asarray(inp["b_mod"], f32)[0]
    w_in = np.asarray(inp["w_in"], f32)[0]
    w_q_up = np.asarray(inp["w_q_up"], f32)[0].reshape(512, 8, 192)
    w_kv_up = np.asarray(inp["w_kv_up"], f32)[0].reshape(512, 8, 256)
    fm = lambda v: np.ascontiguousarray(v.reshape(-1, 128).T)
    vecs = np.concatenate([fm(g_attn), fm(g_ffn), fm(b_mod)], axis=1).astype(f32)
    rows = np.zeros((4, D), f32)
    rows[0] = np.asarray(inp["g_final"], f32)
    rows[1, :NE] = np.asarray(inp["b_router"], f32)[0]
    rows[2] = g_ffn
    glat = np.concatenate([fm(np.asarray(inp["g_q_lat"], f32)[0]), fm(np.asarray(inp["g_kv_lat"], f32)[0])], axis=1).astype(f32)
    half = 32
    invf = (1.0 / (np.float32(10000.0) ** (np.arange(half, dtype=f32) * f32(2.0) / f32(64)))).astype(f32)
    ropec = np.zeros((128, 4), f32)
    for p_ in range(128):
        i = p_ % 64
        ropec[p_, 0] = invf[i % 32]
        ropec[p_, 1] = -1.0 if i < 32 else 1.0
    ropec[:, 2] = np.pi
    ropec[:, 3] = 1.5 * np.pi
    kk = np.arange(128)[:, None]
    qq = np.arange(128)[None, :]
    ident = np.eye(128, dtype=f32)
    negtri = -(kk >= qq).astype(f32)
    ones = np.ones((128, 128), f32)
    tri_s = (kk < qq).astype(f32)
    negones33 = np.zeros((128, 128), f32)
    negones33[:, :33] = -1.0
    bc33 = np.zeros((128, 128), f32)
    bc33[0, :] = 1.0
    bc33[32, :] = 1.0
    blkstart = np.tile((np.arange(128, dtype=f32) * RB)[None, :], (128, 1))
    off16 = (np.arange(128, dtype=f32)[None, :] % 16) * 128 + np.arange(128, dtype=f32)[:, None]
    consts = np.stack([ident, negtri, ones, tri_s, negones33, bc33, blkstart, off16]).astype(f32)
    TRI_SB = (kk < qq).astype(f32)
    TRI_ML = (kk <= qq).astype(f32)
    ONE = np.ones((128, 128), f32)
    ZERO = np.zeros((128, 128), f32)
    kr = w_in[:, 4096:4160]
    w_kr = np.ascontiguousarray(np.concatenate([kr, kr[:, 32:], kr[:, :32]], axis=1))
    w_qn = np.ascontiguousarray(w_q_up[:, :, :128].reshape(512, 1024))
    qr_ = w_q_up[:, :, 128:]
    w_qra = np.ascontiguousarray(qr_.reshape(512, 512))
    w_qrs = np.ascontiguousarray(np.concatenate([qr_[:, :, 32:], qr_[:, :, :32]], axis=2).reshape(512, 512))
    w_kn = np.ascontiguousarray(w_kv_up[:, :, :128].reshape(512, 1024))
    w_v = np.ascontiguousarray(w_kv_up[:, :, 128:].reshape(512, 1024))
    w1 = np.asarray(inp["w1"], f32)[0]
    b1 = np.asarray(inp["b1"], f32)[0]
    shared = dict(
        vecs=vecs, rows=rows, glat=glat, ropec=ropec, consts=consts,
        w_mod=np.asarray(inp["w_mod"], f32)[0], w_in=w_in, w_kr=w_kr, w_qn=w_qn, w_qra=w_qra, w_qrs=w_qrs,
        w_kn=w_kn, w_v=w_v, w_out=np.asarray(inp["w_out"], f32)[0],
        w_router=np.ascontiguousarray(np.asarray(inp["w_router"], f32)[0].reshape(16, 128, NE).transpose(1, 0, 2)),
        w1g=np.ascontiguousarray(w1[:, :, 0::2]), w1l=np.ascontiguousarray(w1[:, :, 1::2]),
        b1g=np.ascontiguousarray(b1[:, 0::2].reshape(NE, 16, 128).transpose(0, 2, 1)),
        b1l=np.ascontiguousarray(b1[:, 1::2].reshape(NE, 16, 128).transpose(0, 2, 1)),
        b1g_r=np.ascontiguousarray(b1[:, 0::2].reshape(NE, 1, D)), b1l_r=np.ascontiguousarray(b1[:, 1::2].reshape(NE, 1, D)),
        w2=np.asarray(inp["w2"], f32)[0], b2=np.ascontiguousarray(np.asarray(inp["b2"], f32)[0].reshape(NE, 1, D)),
    )
    in_maps = []
    for core in range(8):
        b, p = core // 2, core % 2
        blks = own_blocks(p)
        rowsel = np.concatenate([np.arange(k * 128, (k + 1) * 128) for k in blks])
        if p == 0:
            zs = [(TRI_SB, TRI_ML), (ZERO, ZERO), (ONE, ONE), (TRI_SB, TRI_ML)]
        else:
            zs = [(ONE, ONE), (TRI_SB, TRI_ML), (TRI_SB, TRI_ML), (ZERO, ZERO)]
        masks = np.stack([np.stack([z[0] for z in zs]), np.stack([z[1] for z in zs])]).astype(f32)
        m = dict(shared)
        m.update(
            x_all=np.ascontiguousarray(x[b]), x_own=np.ascontiguousarray(x[b][rowsel]),
            pos_all=np.ascontiguousarray(pos[b][None, :]), pos_own=np.ascontiguousarray(pos[b][rowsel][None, :]),
            cT=fm(c[b]), masks=masks,
        )
        in_maps.append(m)
    return in_maps


_CACHE = {}


def kernel(**inp):
    in_maps = host_prepare(inp)
    if "nc" not in _CACHE:
        _CACHE["nc"] = build_program()
    res = run_bass_kernel_spmd(_CACHE["nc"], in_maps, core_ids=list(range(8)))
    outp = np.zeros((4, S_ALL, D), np.float32)
    for core in range(8):
        b, p = core // 2, core % 2
        o = res.results[core]["out"]
        for i, k in enumerate(own_blocks(p)):
            outp[b, k * 128:(k + 1) * 128] = o[i * 128:(i + 1) * 128]
    return outp
```

```python
import contextlib
import numpy as np
import concourse.bass as bass
import concourse.mybir as mybir
from concourse.bass_utils import run_bass_kernel_spmd

F32 = mybir.dt.float32
BF16 = mybir.dt.bfloat16
I32 = mybir.dt.int32
AF = mybir.ActivationFunctionType
ALU = mybir.AluOpType
AX = mybir.AxisListType

D = 2048
S_ALL = 4096
T_OWN = 2048
NE = 32
CAPR = 2048
RB = 128
NROWS = 4 * T_OWN + NE * RB
NBLK = NROWS // RB
EPS = 1e-6
PI = float(np.pi)


class Buf:
    __slots__ = ("name", "w", "r", "dsem")

    def __init__(self, name):
        self.name = name
        self.w = {}
        self.r = {}
        self.dsem = None


class Sync:
    ENG = ("pe", "act", "dve", "pool", "sp")

    def __init__(self, nc):
        self.nc = nc
        self.e = {"pe": nc.tensor, "act": nc.scalar, "dve": nc.vector, "pool": nc.gpsimd, "sp": nc.sync}
        self.sems = {}
        self.cnt = {}
        for n in self.ENG:
            self.sems[n] = nc.alloc_semaphore("es_" + n)
            self.cnt[n] = 0
        self.seen = {n: {} for n in self.ENG}
        self.free_d = []
        self.nd = 0

    def _wait(self, eng, deps):
        for k, v in deps.items():
            if v <= 0 or (k == eng and eng == "pe"):
                continue
            if self.seen[eng].get(k, 0) >= v:
                continue
            self.e[eng].wait_ge(self.sems[k], v)
            self.seen[eng][k] = v

    @staticmethod
    def _merge(d, s):
        for k, v in s.items():
            if d.get(k, 0) < v:
                d[k] = v

    def _deps(self, reads, writes):
        d = {}
        for b in reads:
            self._merge(d, b.w)
        for b in writes:
            self._merge(d, b.w)
            self._merge(d, b.r)
        return d

    def op(self, eng, fn, reads=(), writes=()):
        self._wait(eng, self._deps(reads, writes))
        ins = fn()
        self.cnt[eng] += 1
        ins.then_inc(self.sems[eng], 1)
        me = {eng: self.cnt[eng]}
        for b in reads:
            self._merge(b.r, me)
        for b in writes:
            b.w = dict(me)
            b.r = {}
        return ins

    def _dsem(self, buf):
        if buf.dsem is None:
            if self.free_d:
                buf.dsem = self.free_d.pop()
            else:
                self.nd += 1
                buf.dsem = "d%d" % self.nd
                self.sems[buf.dsem] = self.nc.alloc_semaphore(buf.dsem)
                self.cnt[buf.dsem] = 0
        return buf.dsem

    def release(self, bufs):
        for b in bufs:
            if b.dsem is not None:
                self.free_d.append(b.dsem)
                b.dsem = None

    def dma(self, sb, items, reads=(), writes=()):
        key = self._dsem(sb)
        deps = self._deps(reads, writes)
        if self.cnt[key] > 0:
            deps[key] = max(deps.get(key, 0), self.cnt[key])
        for q, fn in items:
            self._wait(q, deps)
        for q, fn in items:
            ins = fn()
            ins.then_inc(self.sems[key], 16)
            self.cnt[key] += 16
        me = {key: self.cnt[key]}
        for b in reads:
            self._merge(b.r, me)
        for b in writes:
            b.w = dict(me)
            b.r = {}

    @contextlib.contextmanager
    def guard(self, regs, thr):
        before = dict(self.cnt)
        seen0 = {k: dict(v) for k, v in self.seen.items()}
        with self.nc.If_cmp(regs, thr, "IS_GT"):
            yield
        after = dict(self.cnt)
        with self.nc.Else():
            for k, v in after.items():
                d = v - before.get(k, 0)
                if d > 0:
                    eng = k if k in self.ENG else "sp"
                    if before.get(k, 0) > 0:
                        self.e[eng].wait_ge(self.sems[k], before[k])
                    self.e[eng].sem_inc(self.sems[k], d)
        self.seen = seen0

    def barrier(self, engines=None):
        allv = {k: v for k, v in self.cnt.items() if v > 0}
        for en in (engines or self.ENG):
            self._wait(en, allv)


class Ring:
    def __init__(self, items):
        self.items = items
        self.i = 0

    def next(self):
        it = self.items[self.i % len(self.items)]
        self.i += 1
        return it


class Ctx:
    def __init__(self, nc, S, es, tag):
        self.nc, self.S, self.es, self.tag = nc, S, es, tag
        self.bufs = []
        self.n = 0

    def sb(self, name, shape, dt):
        self.n += 1
        t = self.es.enter_context(self.nc.sbuf_tensor("%s_%s%d" % (self.tag, name, self.n), shape, dt))
        b = Buf(name)
        self.bufs.append(b)
        return t, b

    def ps(self, name, shape, dt):
        self.n += 1
        t = self.es.enter_context(self.nc.psum_tensor("%s_%s%d" % (self.tag, name, self.n), shape, dt))
        b = Buf(name)
        self.bufs.append(b)
        return t, b

    def ring_sb(self, name, shape, dt, n):
        return Ring([self.sb(name, shape, dt) for _ in range(n)])

    def ring_ps(self, name, shape, dt, n):
        return Ring([self.ps(name, shape, dt) for _ in range(n)])


@contextlib.contextmanager
def phase(nc, S, tag):
    with contextlib.ExitStack() as es:
        c = Ctx(nc, S, es, tag)
        yield c
        S.barrier()
        S.release(c.bufs)


def build_program(stage=99, dbg=False):
    nc = bass.Bass("TRN2", target_bir_lowering=False)
    S = Sync(nc)

    def din(name, shape, dt=F32):
        return nc.dram_tensor(name, list(shape), dt, kind="ExternalInput").ap()

    def dscr(name, shape, dt):
        return nc.dram_tensor(name, list(shape), dt, kind="Internal").ap()

    x_all = din("x_all", [S_ALL, D])
    x_own = din("x_own", [T_OWN, D])
    pos_all = din("pos_all", [1, S_ALL], I32)
    pos_own = din("pos_own", [1, T_OWN], I32)
    cT = din("cT", [128, 16])
    vecs = din("vecs", [128, 16 * 2 + 96])
    rows = din("rows", [4, D])
    glat = din("glat", [128, 8])
    ropec = din("ropec", [128, 4])
    masks = din("masks", [2, 4, 128, 128])
    consts = din("consts", [8, 128, 128])
    w_mod = din("w_mod", [D, 6 * D])
    w_in = din("w_in", [D, 4160])
    w_kr = din("w_kr", [D, 128])
    w_qn = din("w_qn", [512, 1024])
    w_qra = din("w_qra", [512, 512])
    w_qrs = din("w_qrs", [512, 512])
    w_kn = din("w_kn", [512, 1024])
    w_v = din("w_v", [512, 1024])
    w_out = din("w_out", [D, D])
    w_router = din("w_router", [128, 16, NE])
    w1g = din("w1g", [NE, D, D])
    w1l = din("w1l", [NE, D, D])
    b1g = din("b1g", [NE, 128, 16])
    b1l = din("b1l", [NE, 128, 16])
    b1g_r = din("b1g_r", [NE, 1, D])
    b1l_r = din("b1l_r", [NE, 1, D])
    w2 = din("w2", [NE, D, D])
    b2 = din("b2", [NE, 1, D])
    out = nc.dram_tensor("out", [T_OWN, D], F32, kind="ExternalOutput").ap()
    dbg_out = nc.dram_tensor("dbg", [T_OWN, D], F32, kind="ExternalOutput").ap() if dbg else None

    mod_d = dscr("mod_d", [96, 128], F32)
    hT_all = dscr("hT_all", [D, S_ALL], BF16)
    hT_own = dscr("hT_own", [D, T_OWN], BF16)
    QT_sb = dscr("QT_sb", [1024, T_OWN], BF16)
    KT_sb = dscr("KT_sb", [1024, S_ALL], BF16)
    V_sb = dscr("V_sb", [S_ALL, 1024], BF16)
    qlnT = dscr("qlnT", [512, T_OWN], BF16)
    kvlnT = dscr("kvlnT", [512, S_ALL], BF16)
    KrT = dscr("KrT", [64, S_ALL], BF16)
    QnT = dscr("QnT", [1024, T_OWN], BF16)
    QrT = dscr("QrT", [512, T_OWN], BF16)
    KnT = dscr("KnT", [1024, S_ALL], BF16)
    V_ml = dscr("V_ml", [S_ALL, 1024], BF16)
    mixT = dscr("mixT", [D, T_OWN], BF16)
    x1_d = dscr("x1_d", [T_OWN, D], F32)
    xs_d = dscr("xs_d", [NROWS, D], BF16)
    ys_p = [dscr("ys_d%d" % q, [NROWS, 512], F32) for q in range(4)]
    cnt_d = dscr("cnt_d", [1, NE], I32)
    Bd = {}

    def db(name):
        if name not in Bd:
            Bd[name] = Buf(name)
        return Bd[name]

    qi = [0]

    def q2():
        qi[0] += 1
        return "sp"

    with contextlib.ExitStack() as glob:
        G = Ctx(nc, S, glob, "g")
        cst, b_cst = G.sb("cst", [128, 8, 128], F32)
        cstb, b_cstb = G.sb("cstb", [128, 8, 128], BF16)
        S.dma(b_cst, [("sp", lambda: nc.sync.dma_start(out=cst[:], in_=consts.rearrange("c p n -> p c n")))], writes=[b_cst])
        S.op("dve", lambda: nc.vector.tensor_copy(cstb[:], cst[:]), reads=[b_cst], writes=[b_cstb])
        ident_f = cst[:, 0, :]
        ident_b = cstb[:, 0, :]
        negtri_b = cstb[:, 1, :]
        ones_b = cstb[:, 2, :]
        tri_b = cstb[:, 3, :]
        negones33_b = cstb[:, 4, 0:33]
        bc33_b = cstb[0:33, 5, :]
        vec, b_vec = G.sb("vec", [128, 128], F32)
        S.dma(b_vec, [("sp", lambda: nc.sync.dma_start(out=vec[:], in_=vecs))], writes=[b_vec])
        gl, b_gl = G.sb("gl", [128, 8], F32)
        S.dma(b_gl, [("sp", lambda: nc.sync.dma_start(out=gl[:], in_=glat))], writes=[b_gl])
        rc, b_rc = G.sb("rc", [128, 4], F32)
        S.dma(b_rc, [("sp", lambda: nc.sync.dma_start(out=rc[:], in_=ropec))], writes=[b_rc])
        epsT, b_eps = G.sb("eps", [128, 1], F32)
        S.op("dve", lambda: nc.vector.memset(epsT[:], EPS), writes=[b_eps])
        modT, b_modT = G.sb("modT", [128, 96], F32)
        gm, b_gm = G.sb("gm", [128, 32], F32)

        with phase(nc, S, "M") as P:
            ct, b_ct = P.sb("ct", [128, 16], F32)
            S.dma(b_ct, [("sp", lambda: nc.sync.dma_start(out=ct[:], in_=cT))], writes=[b_ct])
            ca, b_ca = P.sb("ca", [128, 16], F32)
            S.op("act", lambda: nc.scalar.activation(ca[:], ct[:], AF.Silu), reads=[b_ct], writes=[b_ca])
            wr = P.ring_sb("wm", [128, 16, 512], F32, 2)
            pm, b_pm = P.ps("pm", [128, 96], F32)
            wv = w_mod.rearrange("(k p) n -> p k n", p=128)
            for g in range(24):
                wt, b_wt = wr.next()
                S.dma(b_wt, [(q2(), lambda wt=wt, g=g: nc.sync.dma_start(out=wt[:], in_=wv[:, :, g * 512:(g + 1) * 512]))], writes=[b_wt])
                for c in range(4):
                    j = g * 4 + c
                    for k in range(16):
                        S.op("pe", lambda wt=wt, c=c, k=k, j=j: nc.tensor.matmul(
                            pm[:, j:j + 1], wt[:, k, c * 128:(c + 1) * 128], ca[:, k:k + 1], start=(k == 0), stop=(k == 15)),
                            reads=[b_wt, b_ca], writes=[b_pm])
            S.op("dve", lambda: nc.vector.tensor_tensor(modT[:], pm[:], vec[:, 32:128], ALU.add), reads=[b_pm, b_vec], writes=[b_modT])
            S.op("dve", lambda: nc.vector.scalar_tensor_tensor(gm[:, 0:16], modT[:, 16:32], 1.0, vec[:, 0:16], ALU.add, ALU.mult),
                 reads=[b_modT, b_vec], writes=[b_gm])
            S.op("dve", lambda: nc.vector.scalar_tensor_tensor(gm[:, 16:32], modT[:, 64:80], 1.0, vec[:, 16:32], ALU.add, ALU.mult),
                 reads=[b_modT, b_vec], writes=[b_gm])
            pt, b_pt = P.ps("pt", [128, 128], F32)
            S.op("pe", lambda: nc.tensor.transpose(pt[0:96, :], modT[:, 0:96], ident_f), reads=[b_modT, b_cst], writes=[b_pt])
            mt, b_mt = P.sb("mt", [96, 128], F32)
            S.op("dve", lambda: nc.vector.tensor_copy(mt[:], pt[0:96, :]), reads=[b_pt], writes=[b_mt])
            S.dma(b_mt, [("sp", lambda: nc.sync.dma_start(out=mod_d, in_=mt[:]))], reads=[b_mt], writes=[db("mod_d")])

        mod_flat = mod_d.rearrange("j p -> (j p)").rearrange("(o n) -> o n", o=1)

        def bc_row(ap_row, n):
            return ap_row.to_broadcast([128, n])

        def norm_to_hT(P, src, dst, ntile, dstname, gcol, shcol):
            xr = P.ring_sb("x", [128, 4, D], F32, 2)
            xnr = P.ring_sb("xn", [128, 4, D], BF16, 2)
            sq, b_sq = P.sb("sq", [128, D], BF16)
            ssr = P.ring_sb("ss", [128, 4], F32, 2)
            ptr = P.ring_ps("ptr", [128, 2048], BF16, 2)
            hr = P.ring_sb("h", [128, 16, 512], BF16, 2)
            sv = src.rearrange("(n j p) d -> n p j d", p=128, j=4)
            dv = dst.rearrange("(k p) t -> p k t", p=128)
            for n in range(ntile):
                xt, b_x = xr.next()
                S.dma(b_x, [("sp", lambda xt=xt, n=n: nc.sync.dma_start(out=xt[:], in_=sv[n]))], writes=[b_x])
                ss, b_ss = ssr.next()
                xn, b_xn = xnr.next()
                ht, b_h = hr.next()
                for j in range(4):
                    S.op("act", lambda xt=xt, ss=ss, j=j: nc.scalar.activation(sq[:], xt[:, j, :], AF.Square, accum_out=ss[:, j:j + 1]),
                         reads=[b_x], writes=[b_sq, b_ss])
                S.op("act", lambda ss=ss: nc.scalar.activation(ss[:], ss[:], AF.Sqrt, bias=epsT[:, 0:1], scale=1.0 / D),
                     reads=[b_ss, b_eps], writes=[b_ss])
                S.op("dve", lambda ss=ss: nc.vector.reciprocal(ss[:], ss[:]), reads=[b_ss], writes=[b_ss])
                for j in range(4):
                    S.op("dve", lambda xt=xt, xn=xn, ss=ss, j=j: nc.vector.tensor_scalar(
                        xn[:, j, :], xt[:, j, :], ss[:, j:j + 1], None, op0=ALU.mult), reads=[b_x, b_ss], writes=[b_xn])
                for j in range(4):
                    pt_, b_p = ptr.next()
                    for k in range(16):
                        S.op("pe", lambda pt_=pt_, xn=xn, j=j, k=k: nc.tensor.transpose(
                            pt_[:, k * 128:(k + 1) * 128], xn[:, j, k * 128:(k + 1) * 128], ident_b), reads=[b_xn, b_cstb], writes=[b_p])
                    for k in range(16):
                        eng = "dve" if k % 2 == 0 else "pool"
                        if eng == "pool":
                            eng = "dve"
                        S.op(eng, lambda pt_=pt_, ht=ht, j=j, k=k: nc.vector.tensor_scalar(
                            ht[:, k, j * 128:(j + 1) * 128], pt_[:, k * 128:(k + 1) * 128],
                            gm[:, gcol + k:gcol + k + 1], modT[:, shcol + k:shcol + k + 1], op0=ALU.mult, op1=ALU.add),
                            reads=[b_p, b_gm, b_modT], writes=[b_h])
                S.dma(b_h, [("pool", lambda ht=ht, n=n: nc.gpsimd.dma_start(out=dv[:, :, n * 512:(n + 1) * 512], in_=ht[:]))],
                      reads=[b_h], writes=[db(dstname)])

        if stage >= 1:
            with phase(nc, S, "A") as P:
                norm_to_hT(P, x_all, hT_all, 8, "hT_all", 0, 0)
            with phase(nc, S, "A2") as P:
                norm_to_hT(P, x_own, hT_own, 4, "hT_own", 0, 0)

        def load_w(P, ring, Wap, KC, c0, ncols):
            wt, b_w = ring.next()
            wv_ = Wap.rearrange("(k p) n -> p k n", p=128)
            S.dma(b_w, [("pool", lambda: nc.gpsimd.dma_start(out=wt[:, 0:KC, 0:ncols], in_=wv_[:, :, c0:c0 + ncols]))], writes=[b_w])
            return wt, b_w

        def load_a(ring, Aap, aname, KC, t0):
            at, b_a = ring.next()
            av = Aap.rearrange("(k p) t -> p k t", p=128)
            S.dma(b_a, [("sp", lambda: nc.sync.dma_start(out=at[:, 0:KC, :], in_=av[:, :, t0:t0 + 512]))], reads=[db(aname)], writes=[b_a])
            return at, b_a

        def gemm_fm(P, Wap, c0, N, Aap, aname, T, KC, dst, dname, scale, rings):
            wr_, ar_, pr_, sr_ = rings
            dv = dst.rearrange("(c p) t -> p c t", p=128)
            for g in range(N // 512):
                wt, b_w = load_w(P, wr_, Wap, KC, c0 + g * 512, 512)
                for tt in range(T // 512):
                    at, b_a = load_a(ar_, Aap, aname, KC, tt * 512)
                    st, b_s = sr_.next()
                    for c in range(4):
                        ps_, b_p = pr_.next()
                        for k in range(KC):
                            S.op("pe", lambda ps_=ps_, wt=wt, at=at, c=c, k=k: nc.tensor.matmul(
                                ps_[:], wt[:, k, c * 128:(c + 1) * 128], at[:, k, :], start=(k == 0), stop=(k == KC - 1)),
                                reads=[b_w, b_a], writes=[b_p])
                        S.op("act", lambda ps_=ps_, st=st, c=c: nc.scalar.activation(st[:, c, :], ps_[:], AF.Copy, scale=scale),
                             reads=[b_p], writes=[b_s])
                    S.dma(b_s, [("pool", lambda st=st, g=g, tt=tt: nc.gpsimd.dma_start(
                        out=dv[:, g * 4:(g + 1) * 4, tt * 512:(tt + 1) * 512], in_=st[:]))], reads=[b_s], writes=[db(dname)])

        def gemm_tm(P, Wap, c0, N, Aap, aname, T, KC, dst, dname, rings):
            wr_, ar_, pr_, sr_ = rings
            dv = dst.rearrange("(n j p) c -> n p j c", p=128, j=4)
            for g in range(N // 512):
                wt, b_w = load_w(P, wr_, Wap, KC, c0 + g * 512, 512)
                for tt in range(T // 512):
                    at, b_a = load_a(ar_, Aap, aname, KC, tt * 512)
                    st, b_s = sr_.next()
                    for j in range(4):
                        ps_, b_p = pr_.next()
                        for k in range(KC):
                            S.op("pe", lambda ps_=ps_, wt=wt, at=at, j=j, k=k: nc.tensor.matmul(
                                ps_[:], at[:, k, j * 128:(j + 1) * 128], wt[:, k, :], start=(k == 0), stop=(k == KC - 1)),
                                reads=[b_w, b_a], writes=[b_p])
                        S.op("act", lambda ps_=ps_, st=st, j=j: nc.scalar.activation(st[:, j, :], ps_[:], AF.Copy),
                             reads=[b_p], writes=[b_s])
                    S.dma(b_s, [("pool", lambda st=st, g=g, tt=tt: nc.gpsimd.dma_start(
                        out=dv[tt][:, :, g * 512:(g + 1) * 512], in_=st[:]))], reads=[b_s], writes=[db(dname)])

        def latent_norm(P, c0, Aap, aname, T, gcol, dst, dname, rings):
            wr_, ar_, pr_, sr_ = rings
            dv = dst.rearrange("(c p) t -> p c t", p=128)
            l32, b_l = P.sb("l32", [128, 4, 512], F32)
            lsq, b_q = P.sb("lsq", [128, 4, 512], BF16)
            rs, b_rs = P.sb("rs", [128, 512], F32)
            wt, b_w = load_w(P, wr_, w_in, 16, c0, 512)
            for tt in range(T // 512):
                at, b_a = load_a(ar_, Aap, aname, 16, tt * 512)
                st, b_s = sr_.next()
                for c in range(4):
                    ps_, b_p = pr_.next()
                    for k in range(16):
                        S.op("pe", lambda ps_=ps_, at=at, c=c, k=k: nc.tensor.matmul(
                            ps_[:], wt[:, k, c * 128:(c + 1) * 128], at[:, k, :], start=(k == 0), stop=(k == 15)),
                            reads=[b_w, b_a], writes=[b_p])
                    S.op("act", lambda ps_=ps_, c=c: nc.scalar.activation(l32[:, c, :], ps_[:], AF.Copy), reads=[b_p], writes=[b_l])
                    S.op("dve", lambda c=c: nc.vector.tensor_tensor(lsq[:, c, :], l32[:, c, :], l32[:, c, :], ALU.mult), reads=[b_l], writes=[b_q])
                ps_, b_p = pr_.next()
                for c in range(4):
                    S.op("pe", lambda ps_=ps_, c=c: nc.tensor.matmul(ps_[:], ones_b, lsq[:, c, :], start=(c == 0), stop=(c == 3)),
                         reads=[b_q, b_cstb], writes=[b_p])
                S.op("act", lambda ps_=ps_: nc.scalar.activation(rs[:], ps_[:], AF.Sqrt, bias=epsT[:, 0:1], scale=1.0 / 512),
                     reads=[b_p, b_eps], writes=[b_rs])
                S.op("dve", lambda: nc.vector.reciprocal(rs[:], rs[:]), reads=[b_rs], writes=[b_rs])
                for c in range(4):
                    S.op("dve", lambda st=st, c=c: nc.vector.scalar_tensor_tensor(
                        st[:, c, :], l32[:, c, :], gl[:, gcol + c:gcol + c + 1], rs[:], ALU.mult, ALU.mult),
                        reads=[b_l, b_gl, b_rs], writes=[b_s])
                S.dma(b_s, [("pool", lambda st=st, tt=tt: nc.gpsimd.dma_start(out=dv[:, :, tt * 512:(tt + 1) * 512], in_=st[:]))],
                      reads=[b_s], writes=[db(dname)])

        def rope_tables(P, pos_ap, T, scale, name):
            pi_, b_pi = P.sb("posi", [128, T], I32)
            S.dma(b_pi, [("sp", lambda: nc.sync.dma_start(out=pi_[:], in_=pos_ap.to_broadcast([128, T])))], writes=[b_pi])
            ang, b_an = P.sb("ang", [128, T], F32)
            S.op("dve", lambda: nc.vector.tensor_copy(ang[:], pi_[:]), reads=[b_pi], writes=[b_an])
            C, b_C = P.sb("C" + name, [128, T], F32)
            Sg, b_S = P.sb("S" + name, [128, T], F32)
            tmp, b_t = P.sb("rtmp", [128, T], F32)
            ni, b_ni = P.sb("rni", [128, T], I32)
            S.op("dve", lambda: nc.vector.tensor_scalar(ang[:], ang[:], rc[:, 0:1], None, op0=ALU.mult), reads=[b_an, b_rc], writes=[b_an])
            for dst_, b_d, offs in ((Sg, b_S, 0.0), (C, b_C, 0.5 * PI)):
                S.op("dve", lambda offs=offs: nc.vector.tensor_scalar(tmp[:], ang[:], offs, 1.0 / (2 * PI), op0=ALU.add, op1=ALU.mult), reads=[b_an], writes=[b_t])
                S.op("dve", lambda: nc.vector.tensor_copy(ni[:], tmp[:]), reads=[b_t], writes=[b_ni])
                S.op("dve", lambda: nc.vector.tensor_copy(tmp[:], ni[:]), reads=[b_ni], writes=[b_t])
                S.op("dve", lambda: nc.vector.scalar_tensor_tensor(tmp[:], tmp[:], -2 * PI, ang[:], ALU.mult, ALU.add), reads=[b_t, b_an], writes=[b_t])
                if offs != 0.0:
                    S.op("dve", lambda offs=offs: nc.vector.tensor_scalar(tmp[:], tmp[:], offs, None, op0=ALU.add), reads=[b_t], writes=[b_t])
                S.op("dve", lambda dst_=dst_: nc.vector.tensor_scalar(dst_[:], tmp[:], PI, 2 * PI, op0=ALU.is_gt, op1=ALU.mult), reads=[b_t], writes=[b_d])
                S.op("dve", lambda dst_=dst_: nc.vector.tensor_tensor(tmp[:], tmp[:], dst_[:], ALU.subtract), reads=[b_t, b_d], writes=[b_t])
                S.op("dve", lambda dst_=dst_: nc.vector.tensor_scalar(dst_[:], tmp[:], -PI, 2 * PI, op0=ALU.is_lt, op1=ALU.mult), reads=[b_t], writes=[b_d])
                S.op("dve", lambda dst_=dst_: nc.vector.tensor_tensor(tmp[:], tmp[:], dst_[:], ALU.add), reads=[b_t, b_d], writes=[b_t])
                S.op("act", lambda dst_=dst_: nc.scalar.activation(dst_[:], tmp[:], AF.Sin), reads=[b_t], writes=[b_d])
            S.op("dve", lambda: nc.vector.tensor_scalar(Sg[:], Sg[:], rc[:, 1:2], float(scale), op0=ALU.mult, op1=ALU.mult),
                 reads=[b_S, b_rc], writes=[b_S])
            if scale != 1.0:
                S.op("dve", lambda: nc.vector.tensor_scalar(C[:], C[:], float(scale), None, op0=ALU.mult), reads=[b_C], writes=[b_C])
            return (C, b_C), (Sg, b_S)

        def rope_proj(P, Wa, ca0, Ws, cs0, nh, Aap, aname, T, KC, CS, dst, dname, rings):
            wr_, ar_, pr_, sr_ = rings
            (C, b_C), (Sg, b_S) = CS
            wa, b_wa = load_w(P, wr_, Wa, KC, ca0, nh * 64)
            ws, b_ws = load_w(P, wr_, Ws, KC, cs0, nh * 64)
            t1, b_t1 = P.sb("rt1", [64, 512], F32)
            t2, b_t2 = P.sb("rt2", [64, 512], F32)
            for tt in range(T // 512):
                at, b_a = load_a(ar_, Aap, aname, KC, tt * 512)
                for h in range(nh):
                    pa, b_pa = pr_.next()
                    pb, b_pb = pr_.next()
                    for k in range(KC):
                        S.op("pe", lambda pa=pa, at=at, h=h, k=k: nc.tensor.matmul(
                            pa[0:64, :], wa[:, k, h * 64:(h + 1) * 64], at[:, k, :], start=(k == 0), stop=(k == KC - 1)),
                            reads=[b_wa, b_a], writes=[b_pa])
                    for k in range(KC):
                        S.op("pe", lambda pb=pb, at=at, h=h, k=k: nc.tensor.matmul(
                            pb[0:64, :], ws[:, k, h * 64:(h + 1) * 64], at[:, k, :], start=(k == 0), stop=(k == KC - 1)),
                            reads=[b_ws, b_a], writes=[b_pb])
                    st, b_s = sr_.next()
                    S.op("dve", lambda pa=pa, tt=tt: nc.vector.tensor_tensor(t1[:], pa[0:64, :], C[0:64, tt * 512:(tt + 1) * 512], ALU.mult),
                         reads=[b_pa, b_C], writes=[b_t1])
                    S.op("dve", lambda pb=pb, tt=tt: nc.vector.tensor_tensor(t2[:], pb[0:64, :], Sg[0:64, tt * 512:(tt + 1) * 512], ALU.mult),
                         reads=[b_pb, b_S], writes=[b_t2])
                    S.op("dve", lambda st=st: nc.vector.tensor_tensor(st[0:64, 0, :], t1[:], t2[:], ALU.add), reads=[b_t1, b_t2], writes=[b_s])
                    S.dma(b_s, [("pool", lambda st=st, h=h, tt=tt: nc.gpsimd.dma_start(
                        out=dst[h * 64:(h + 1) * 64, tt * 512:(tt + 1) * 512], in_=st[0:64, 0, :]))], reads=[b_s], writes=[db(dname)])

        if stage >= 2:
            with phase(nc, S, "B") as P:
                rings = (P.ring_sb("w", [128, 16, 512], BF16, 2), P.ring_sb("a", [128, 16, 512], BF16, 2),
                         P.ring_ps("p", [128, 512], F32, 6), P.ring_sb("st", [128, 4, 512], BF16, 2))
                gemm_fm(P, w_in, 0, 1024, hT_own, "hT_own", T_OWN, 16, QT_sb, "QT_sb", 128 ** -0.5, rings)
                gemm_fm(P, w_in, 1024, 1024, hT_all, "hT_all", S_ALL, 16, KT_sb, "KT_sb", 1.0, rings)
                gemm_tm(P, w_in, 2048, 1024, hT_all, "hT_all", S_ALL, 16, V_sb, "V_sb", rings)
                latent_norm(P, 3072, hT_own, "hT_own", T_OWN, 0, qlnT, "qlnT", rings)
                latent_norm(P, 3584, hT_all, "hT_all", S_ALL, 4, kvlnT, "kvlnT", rings)
            with phase(nc, S, "B2") as P:
                rings = (P.ring_sb("w", [128, 16, 512], BF16, 2), P.ring_sb("a", [128, 16, 512], BF16, 2),
                         P.ring_ps("p", [128, 512], F32, 6), P.ring_sb("st", [128, 4, 512], BF16, 2))
                CSk = rope_tables(P, pos_all, S_ALL, 1.0, "k")
                rope_proj(P, w_kr, 0, w_kr, 64, 1, hT_all, "hT_all", S_ALL, 16, CSk, KrT, "KrT", rings)
            with phase(nc, S, "B3") as P:
                rings = (P.ring_sb("w", [128, 16, 512], BF16, 2), P.ring_sb("a", [128, 16, 512], BF16, 2),
                         P.ring_ps("p", [128, 512], F32, 6), P.ring_sb("st", [128, 4, 512], BF16, 2))
                CSq = rope_tables(P, pos_own, T_OWN, 192 ** -0.5, "q")
                rope_proj(P, w_qra, 0, w_qrs, 0, 8, qlnT, "qlnT", T_OWN, 4, CSq, QrT, "QrT", rings)
                gemm_fm(P, w_qn, 0, 1024, qlnT, "qlnT", T_OWN, 4, QnT, "QnT", 192 ** -0.5, rings)
                gemm_fm(P, w_kn, 0, 1024, kvlnT, "kvlnT", S_ALL, 4, KnT, "KnT", 1.0, rings)
                gemm_tm(P, w_v, 0, 1024, kvlnT, "kvlnT", S_ALL, 4, V_ml, "V_ml", rings)

        def attention(P, is_sb):
            mk, b_mk = P.sb("mk", [128, 4, 128], F32)
            S.dma(b_mk, [("sp", lambda: nc.sync.dma_start(out=mk[:], in_=masks[0 if is_sb else 1].rearrange("z p n -> p z n")))], writes=[b_mk])
            mkb, b_mkb = P.sb("mkb", [128, 4, 128], BF16)
            S.op("dve", lambda: nc.vector.tensor_copy(mkb[:], mk[:]), reads=[b_mk], writes=[b_mkb])
            ktr = P.ring_sb("kt", [128, S_ALL], BF16, 2)
            vr = P.ring_sb("v", [128, 32, 128], BF16, 2)
            qr = P.ring_sb("q", [128, T_OWN], BF16, 2)
            if not is_sb:
                krt, b_krt = P.sb("krt", [64, S_ALL], BF16)
                S.dma(b_krt, [("sp", lambda: nc.sync.dma_start(out=krt[:], in_=KrT))], reads=[db("KrT")], writes=[b_krt])
                qrr = P.ring_sb("qr", [64, T_OWN], BF16, 2)
            pA = P.ring_ps("pA", [128, 512], F32, 2)
            pB = P.ring_ps("pB", [128, 512], F32, 2)
            pO = P.ring_ps("pO", [128, 512], F32, 2)
            if is_sb:
                pC, b_pC = P.ps("pC", [128, 512], F32)
                e32r = P.ring_sb("e32", [128, 512], F32, 2)
                spr = P.ring_sb("sp", [128, 512], BF16, 2)
                Tt, b_T = P.sb("T", [33, 512], F32)
                THL, b_THL = P.sb("THL", [33, 512], BF16)
            else:
                pD = P.ring_ps("pD", [128, 512], F32, 2)
                rdn, b_rdn = P.sb("rdn", [128, 512], F32)
            ar = P.ring_sb("a", [128, 512], BF16, 3)
            mor = P.ring_sb("mo", [128, 512], BF16, 2)
            zer, b_zer = P.sb("zer", [128, 128], BF16)
            S.op("dve", lambda: nc.vector.memset(zer[:], 0.0), writes=[b_zer])
            KT = KT_sb if is_sb else KnT
            QT = QT_sb if is_sb else QnT
            VV = V_sb if is_sb else V_ml
            kn, qn, vn = ("KT_sb", "QT_sb", "V_sb") if is_sb else ("KnT", "QnT", "V_ml")
            for h in range(8):
                kt, b_kt = ktr.next()
                S.dma(b_kt, [("sp", lambda kt=kt, h=h: nc.sync.dma_start(out=kt[:], in_=KT[h * 128:(h + 1) * 128, :]))], reads=[db(kn)], writes=[b_kt])
                vt, b_vt = vr.next()
                S.dma(b_vt, [("sp", lambda vt=vt, h=h: nc.sync.dma_start(
                    out=vt[:], in_=VV.rearrange("(kb p) c -> p kb c", p=128)[:, :, h * 128:(h + 1) * 128]))], reads=[db(vn)], writes=[b_vt])
                qt, b_qt = qr.next()
                S.dma(b_qt, [("sp", lambda qt=qt, h=h: nc.sync.dma_start(out=qt[:], in_=QT[h * 128:(h + 1) * 128, :]))], reads=[db(qn)], writes=[b_qt])
                if not is_sb:
                    qrt, b_qrt = qrr.next()
                    S.dma(b_qrt, [("sp", lambda qrt=qrt, h=h: nc.sync.dma_start(out=qrt[:], in_=QrT[h * 64:(h + 1) * 64, :]))], reads=[db("QrT")], writes=[b_qrt])
                for m in range(4):
                    q0 = m * 512
                    po, b_po = pO.next()
                    S.op("pe", lambda po=po, qt=qt, q0=q0: nc.tensor.matmul(po[:], zer[:], qt[:, q0:q0 + 512], start=True, stop=False),
                         reads=[b_zer, b_qt], writes=[b_po])
                    if is_sb:
                        S.op("dve", lambda: nc.vector.memset(Tt[:], 0.0), writes=[b_T])
                        S.op("dve", lambda: nc.vector.memset(THL[:], 0.0), writes=[b_THL])
                    else:
                        pd, b_pd = pD.next()
                        S.op("pe", lambda pd=pd, qt=qt, q0=q0: nc.tensor.matmul(pd[:], zer[:], qt[:, q0:q0 + 512], start=True, stop=False),
                             reads=[b_zer, b_qt], writes=[b_pd])
                    kmax = 8 * m + 7

                    def geom(kb):
                        smin = 0
                        while 8 * m + 1 + 2 * smin < kb:
                            smin += 1
                        c0 = smin * 128
                        g_ = dict(kb=kb, last=(kb == 0), cs=slice(c0, 512), qs=slice(q0 + c0, q0 + 512), ks=slice(kb * 128, (kb + 1) * 128), bs=None, zone=None)
                        if kb >= 8 * m:
                            r = kb - 8 * m
                            bsl = r // 2
                            g_["bs"] = slice(bsl * 128, (bsl + 1) * 128)
                            g_["zone"] = (bsl % 2) * 2 + (r % 2)
                        return g_

                    def SC(g_):
                        pa, b_pa = pA.next()
                        g_["pa"], g_["b_pa"] = pa, b_pa
                        ks, qs, cs = g_["ks"], g_["qs"], g_["cs"]
                        if is_sb:
                            S.op("pe", lambda: nc.tensor.matmul(pa[:, cs], kt[:, ks], qt[:, qs], start=True, stop=True), reads=[b_kt, b_qt], writes=[b_pa])
                        else:
                            S.op("pe", lambda: nc.tensor.matmul(pa[:, cs], kt[:, ks], qt[:, qs], start=True, stop=False), reads=[b_kt, b_qt], writes=[b_pa])
                            S.op("pe", lambda: nc.tensor.matmul(pa[:, cs], krt[:, ks], qrt[:, qs], start=False, stop=True), reads=[b_krt, b_qrt], writes=[b_pa])

                    def mask_(t_, b_t, g_):
                        if g_["bs"] is not None:
                            bs, zone = g_["bs"], g_["zone"]
                            S.op("pool", lambda: nc.gpsimd.tensor_tensor(t_[:, bs], t_[:, bs], mkb[:, zone, :], ALU.mult), reads=[b_t, b_mkb], writes=[b_t])

                    def AV(g_):
                        at, b_at, cs, kb, last = g_["at"], g_["b_at"], g_["cs"], g_["kb"], g_["last"]
                        S.op("pe", lambda: nc.tensor.matmul(po[:, cs], vt[:, kb, :], at[:, cs], start=False, stop=last), reads=[b_vt, b_at], writes=[b_po])
                        if not is_sb:
                            S.op("pe", lambda: nc.tensor.matmul(pd[:, cs], ones_b, at[:, cs], start=False, stop=last), reads=[b_cstb, b_at], writes=[b_pd])

                    gcur = geom(kmax)
                    SC(gcur)
                    gprev = None
                    for kb in range(kmax, -1, -1):
                        g_ = gcur
                        cs, qs, ks, last = g_["cs"], g_["qs"], g_["ks"], g_["last"]
                        pa, b_pa = g_["pa"], g_["b_pa"]
                        at, b_at = ar.next()
                        g_["at"], g_["b_at"] = at, b_at
                        if is_sb:
                            e32, b_e = e32r.next()
                            spm, b_sp = spr.next()
                            S.op("act", lambda: nc.scalar.activation(e32[:, cs], pa[:, cs], AF.Exp), reads=[b_pa], writes=[b_e])
                            S.op("act", lambda: nc.scalar.activation(spm[:, cs], e32[:, cs], AF.Ln, bias=1.0), reads=[b_e], writes=[b_sp])
                            mask_(spm, b_sp, g_)
                            if kb > 0:
                                gcur = geom(kb - 1)
                                SC(gcur)
                            pb, b_pb = pB.next()
                            S.op("pe", lambda: nc.tensor.matmul(pb[:, cs], kt[:, ks], qt[:, qs], start=True, stop=False), reads=[b_kt, b_qt], writes=[b_pb])
                            S.op("pe", lambda: nc.tensor.matmul(pb[:, cs], negtri_b, spm[:, cs], start=False, stop=False), reads=[b_sp, b_cstb], writes=[b_pb])
                            S.op("pe", lambda: nc.tensor.matmul(pb[:, cs], bc33_b, THL[0:33, cs], start=False, stop=True), reads=[b_THL, b_cstb], writes=[b_pb])
                            S.op("act", lambda: nc.scalar.activation(at[:, cs], pb[:, cs], AF.Exp), reads=[b_pb], writes=[b_at])
                            mask_(at, b_at, g_)
                            if not last:
                                S.op("pe", lambda: nc.tensor.matmul(pC[0:33, cs], negones33_b, spm[:, cs], start=True, stop=True), reads=[b_sp, b_cstb], writes=[b_pC])
                                S.op("dve", lambda: nc.vector.tensor_tensor(Tt[:, cs], Tt[:, cs], pC[0:33, cs], ALU.add), reads=[b_pC, b_T], writes=[b_T])
                                S.op("dve", lambda: nc.vector.tensor_copy(THL[:, cs], Tt[:, cs]), reads=[b_T], writes=[b_THL])
                                S.op("dve", lambda: nc.vector.tensor_tensor(THL[32:33, cs], Tt[32:33, cs], THL[32:33, cs], ALU.subtract), reads=[b_T, b_THL], writes=[b_THL])
                            if gprev is not None:
                                AV(gprev)
                            gprev = g_
                        else:
                            S.op("act", lambda: nc.scalar.activation(at[:, cs], pa[:, cs], AF.Exp), reads=[b_pa], writes=[b_at])
                            mask_(at, b_at, g_)
                            if kb > 0:
                                gcur = geom(kb - 1)
                                SC(gcur)
                            AV(g_)
                    if is_sb:
                        AV(gprev)
                    mo, b_mo = mor.next()
                    if is_sb:
                        S.op("act", lambda po=po, mo=mo: nc.scalar.activation(mo[:], po[:], AF.Copy), reads=[b_po], writes=[b_mo])
                    else:
                        S.op("dve", lambda pd=pd: nc.vector.reciprocal(rdn[:], pd[:]), reads=[b_pd], writes=[b_rdn])
                        S.op("dve", lambda po=po, mo=mo: nc.vector.tensor_tensor(mo[:], po[:], rdn[:], ALU.mult), reads=[b_po, b_rdn], writes=[b_mo])
                    r0 = (0 if is_sb else 1024) + h * 128
                    S.dma(b_mo, [("pool", lambda mo=mo, r0=r0, q0=q0: nc.gpsimd.dma_start(out=mixT[r0:r0 + 128, q0:q0 + 512], in_=mo[:]))],
                          reads=[b_mo], writes=[db("mixT")])

        if stage >= 3:
            with phase(nc, S, "C1") as P:
                attention(P, True)
            with phase(nc, S, "C2") as P:
                attention(P, False)

        if stage >= 4:
            with phase(nc, S, "D") as P:
                g1, b_g1 = P.sb("g1", [128, D], F32)
                S.dma(b_g1, [("sp", lambda: nc.sync.dma_start(out=g1[:], in_=bc_row(mod_flat[:, 32 * 128:48 * 128], D)))], reads=[db("mod_d")], writes=[b_g1])
                wr_ = P.ring_sb("w", [128, 16, 512], BF16, 2)
                ar_ = P.ring_sb("a", [128, 16, 512], BF16, 2)
                pr_ = P.ring_ps("p", [128, 512], F32, 4)
                xor_ = P.ring_sb("xo", [128, 512], F32, 3)
                tr_ = P.ring_sb("t", [128, 512], F32, 3)
                for g in range(4):
                    wt, b_w = load_w(P, wr_, w_out, 16, g * 512, 512)
                    for tt in range(4):
                        at, b_a = load_a(ar_, mixT, "mixT", 16, tt * 512)
                        for j in range(4):
                            ps_, b_p = pr_.next()
                            for k in range(16):
                                S.op("pe", lambda ps_=ps_, wt=wt, at=at, j=j, k=k: nc.tensor.matmul(
                                    ps_[:], at[:, k, j * 128:(j + 1) * 128], wt[:, k, :], start=(k == 0), stop=(k == 15)),
                                    reads=[b_w, b_a], writes=[b_p])
                            rws = slice((tt * 4 + j) * 128, (tt * 4 + j + 1) * 128)
                            cls = slice(g * 512, (g + 1) * 512)
                            xo, b_xo = xor_.next()
                            S.dma(b_xo, [("sp", lambda xo=xo, rws=rws, cls=cls: nc.sync.dma_start(out=xo[:], in_=x_own[rws, cls]))], writes=[b_xo])
                            t_, b_t = tr_.next()
                            S.op("dve", lambda ps_=ps_, t_=t_, cls=cls: nc.vector.tensor_tensor(t_[:], ps_[:], g1[:, cls], ALU.mult), reads=[b_p, b_g1], writes=[b_t])
                            S.op("pool", lambda t_=t_, xo=xo: nc.gpsimd.tensor_tensor(t_[:], t_[:], xo[:], ALU.add), reads=[b_t, b_xo], writes=[b_t])
                            S.dma(b_t, [("pool", lambda t_=t_, rws=rws, cls=cls: nc.gpsimd.dma_start(out=x1_d[rws, cls], in_=t_[:]))], reads=[b_t], writes=[db("x1_d")])


        SMAX = T_OWN // 128
        dest_i, b_di = G.sb("dest_i", [128, 64], I32)
        w4, b_w4 = G.sb("w4", [128, 16, 4], F32)
        cnt_i, b_cni = G.sb("cnt_i", [128, NE], I32)
        idx_i, b_idx = G.sb("idx_i", [128, NE * SMAX], I32)
        if stage >= 5:
            with phase(nc, S, "R") as P:
                gm2, b_gm2 = P.sb("gm2", [128, D], F32)
                sh2, b_sh2 = P.sb("sh2", [128, D], F32)
                tmpD, b_tD = P.sb("tmpD", [128, D], F32)
                S.dma(b_gm2, [("sp", lambda: nc.sync.dma_start(out=gm2[:], in_=bc_row(mod_flat[:, 64 * 128:80 * 128], D)))], reads=[db("mod_d")], writes=[b_gm2])
                S.dma(b_tD, [("sp", lambda: nc.sync.dma_start(out=tmpD[:], in_=bc_row(rows[2:3, :], D)))], writes=[b_tD])
                S.dma(b_sh2, [("sp", lambda: nc.sync.dma_start(out=sh2[:], in_=bc_row(mod_flat[:, 48 * 128:64 * 128], D)))], reads=[db("mod_d")], writes=[b_sh2])
                S.op("dve", lambda: nc.vector.scalar_tensor_tensor(gm2[:], gm2[:], 1.0, tmpD[:], ALU.add, ALU.mult), reads=[b_gm2, b_tD], writes=[b_gm2])
                brt, b_brt = P.sb("brt", [128, NE], F32)
                S.dma(b_brt, [("sp", lambda: nc.sync.dma_start(out=brt[:], in_=bc_row(rows[1:2, 0:NE], NE)))], writes=[b_brt])
                wrs, b_wrs = P.sb("wrs", [128, 16, NE], F32)
                S.dma(b_wrs, [("sp", lambda: nc.sync.dma_start(out=wrs[:], in_=w_router))], writes=[b_wrs])
                h2b, b_h2b = P.sb("h2b", [128, 16 * D], BF16)
                maskf, b_mf = P.sb("maskf", [128, 16, NE], F32)
                wgt, b_wg = P.sb("wgt", [128, 16, NE], F32)
                posf, b_pf = P.sb("posf", [128, 16, NE], F32)
                cntb, b_cn = P.sb("cntb", [128, NE], F32)
                S.op("dve", lambda: nc.vector.memset(cntb[:], 0.0), writes=[b_cn])
                x1r = P.ring_sb("x1", [128, D], F32, 2)
                h2r = P.ring_sb("h2", [128, D], F32, 2)
                h2T, b_h2T = P.sb("h2T", [128, 16, 128], F32)
                sq, b_sq = P.sb("sq", [128, D], BF16)
                sm = P.ring_sb("sm", [128, 64], F32, 2)
                ex, b_ex = P.sb("ex", [128, NE], F32)
                mb, b_mb = P.sb("mb", [128, NE], BF16)
                ptr = P.ring_ps("pt", [128, 512], F32, 2)
                pl, b_pl = P.ps("pl", [128, NE], F32)
                pp, b_pp = P.ps("pp", [128, NE], F32)
                pc, b_pc = P.ps("pc", [128, NE], F32)
                for i in range(16):
                    xb, b_xb = x1r.next()
                    S.dma(b_xb, [("sp", lambda xb=xb, i=i: nc.sync.dma_start(out=xb[:], in_=x1_d[i * 128:(i + 1) * 128, :]))], reads=[db("x1_d")], writes=[b_xb])
                    s_, b_s = sm.next()
                    S.op("act", lambda xb=xb, s_=s_: nc.scalar.activation(sq[:], xb[:], AF.Square, accum_out=s_[:, 0:1]), reads=[b_xb], writes=[b_sq, b_s])
                    S.op("act", lambda s_=s_: nc.scalar.activation(s_[:, 0:1], s_[:, 0:1], AF.Sqrt, bias=epsT[:, 0:1], scale=1.0 / D), reads=[b_s, b_eps], writes=[b_s])
                    S.op("dve", lambda s_=s_: nc.vector.reciprocal(s_[:, 0:1], s_[:, 0:1]), reads=[b_s], writes=[b_s])
                    h2, b_h2 = h2r.next()
                    S.op("dve", lambda xb=xb, h2=h2, s_=s_: nc.vector.scalar_tensor_tensor(h2[:], xb[:], s_[:, 0:1], gm2[:], ALU.mult, ALU.mult), reads=[b_xb, b_s, b_gm2], writes=[b_h2])
                    S.op("pool", lambda h2=h2: nc.gpsimd.tensor_tensor(h2[:], h2[:], sh2[:], ALU.add), reads=[b_h2, b_sh2], writes=[b_h2])
                    S.op("act", lambda h2=h2, i=i: nc.scalar.activation(h2b[:, i * D:(i + 1) * D], h2[:], AF.Copy), reads=[b_h2], writes=[b_h2b])
                    for g in range(4):
                        pt_, b_p = ptr.next()
                        for c in range(4):
                            k = g * 4 + c
                            S.op("pe", lambda pt_=pt_, h2=h2, c=c, k=k: nc.tensor.transpose(pt_[:, c * 128:(c + 1) * 128], h2[:, k * 128:(k + 1) * 128], ident_f),
                                 reads=[b_h2, b_cst], writes=[b_p])
                        S.op("dve", lambda pt_=pt_, g=g: nc.vector.tensor_copy(h2T[:, g * 4:(g + 1) * 4, :], pt_[:].rearrange("p (c n) -> p c n", c=4)), reads=[b_p], writes=[b_h2T])
                    for k in range(16):
                        S.op("pe", lambda k=k: nc.tensor.matmul(pl[:], h2T[:, k, :], wrs[:, k, :], start=(k == 0), stop=(k == 15)), reads=[b_h2T, b_wrs], writes=[b_pl])
                    lg = s_[:, 32:64]
                    S.op("dve", lambda lg=lg: nc.vector.tensor_tensor(lg, pl[:], brt[:], ALU.add), reads=[b_pl, b_brt], writes=[b_s])
                    S.op("dve", lambda s_=s_, lg=lg: nc.vector.max(out=s_[:, 8:16], in_=lg), reads=[b_s], writes=[b_s])
                    S.op("dve", lambda s_=s_: nc.vector.tensor_scalar(s_[:, 16:17], s_[:, 8:9], -1.0, None, op0=ALU.mult), reads=[b_s], writes=[b_s])
                    S.op("dve", lambda s_=s_, lg=lg, i=i: nc.vector.tensor_scalar(maskf[:, i, :], lg, s_[:, 11:12], None, op0=ALU.is_ge), reads=[b_s], writes=[b_mf])
                    S.op("act", lambda s_=s_, lg=lg: nc.scalar.activation(ex[:], lg, AF.Exp, bias=s_[:, 16:17], scale=1.0), reads=[b_s], writes=[b_ex])
                    S.op("dve", lambda i=i: nc.vector.tensor_tensor(ex[:], ex[:], maskf[:, i, :], ALU.mult), reads=[b_ex, b_mf], writes=[b_ex])
                    S.op("dve", lambda s_=s_: nc.vector.reduce_sum(s_[:, 17:18], ex[:], AX.X), reads=[b_ex], writes=[b_s])
                    S.op("dve", lambda s_=s_: nc.vector.reciprocal(s_[:, 17:18], s_[:, 17:18]), reads=[b_s], writes=[b_s])
                    S.op("dve", lambda s_=s_, i=i: nc.vector.tensor_scalar(wgt[:, i, :], ex[:], s_[:, 17:18], None, op0=ALU.mult), reads=[b_ex, b_s], writes=[b_wg])
                    S.op("dve", lambda i=i: nc.vector.tensor_copy(mb[:], maskf[:, i, :]), reads=[b_mf], writes=[b_mb])
                    S.op("pe", lambda: nc.tensor.matmul(pp[:], tri_b, mb[:], start=True, stop=True), reads=[b_mb, b_cstb], writes=[b_pp])
                    S.op("pe", lambda: nc.tensor.matmul(pc[:], ones_b, mb[:], start=True, stop=True), reads=[b_mb, b_cstb], writes=[b_pc])
                    S.op("dve", lambda i=i: nc.vector.tensor_tensor(posf[:, i, :], pp[:], cntb[:], ALU.add), reads=[b_pp, b_cn], writes=[b_pf])
                    S.op("dve", lambda: nc.vector.tensor_tensor(cntb[:], cntb[:], pc[:], ALU.add), reads=[b_pc, b_cn], writes=[b_cn])
                S.op("dve", lambda: nc.vector.tensor_copy(cnt_i[:], cntb[:]), reads=[b_cn], writes=[b_cni])
                q_, b_q = P.sb("q_", [128, NE], F32)
                nf, b_nf = P.sb("nf", [128, NE], F32)
                nI, b_nI = P.sb("nI", [128, NE], I32)
                inc, b_inc = P.sb("inc", [128, NE], F32)
                inc2, b_inc2 = P.sb("inc2", [128, NE], F32)
                base, b_base = P.sb("base", [128, NE], F32)
                S.op("dve", lambda: nc.vector.tensor_scalar(q_[:], cntb[:], float(RB - 1), 1.0 / RB, op0=ALU.add, op1=ALU.mult), reads=[b_cn], writes=[b_q])
                S.op("dve", lambda: nc.vector.tensor_copy(nI[:], q_[:]), reads=[b_q], writes=[b_nI])
                S.op("dve", lambda: nc.vector.tensor_copy(nf[:], nI[:]), reads=[b_nI], writes=[b_nf])
                S.op("dve", lambda: nc.vector.tensor_tensor(inc[:], nf[:], q_[:], ALU.is_gt), reads=[b_nf, b_q], writes=[b_inc])
                S.op("dve", lambda: nc.vector.tensor_tensor(nf[:], nf[:], inc[:], ALU.subtract), reads=[b_nf, b_inc], writes=[b_nf])
                S.op("dve", lambda: nc.vector.tensor_scalar(nf[:], nf[:], float(RB), None, op0=ALU.mult), reads=[b_nf], writes=[b_nf])
                S.op("dve", lambda: nc.vector.tensor_copy(inc[:], nf[:]), reads=[b_nf], writes=[b_inc])
                cur, b_cur, oth, b_oth = inc, b_inc, inc2, b_inc2
                for sh in (1, 2, 4, 8, 16):
                    S.op("dve", lambda cur=cur, oth=oth, sh=sh: nc.vector.tensor_copy(oth[:, 0:sh], cur[:, 0:sh]), reads=[b_cur], writes=[b_oth])
                    S.op("dve", lambda cur=cur, oth=oth, sh=sh: nc.vector.tensor_tensor(oth[:, sh:NE], cur[:, sh:NE], cur[:, 0:NE - sh], ALU.add), reads=[b_cur, b_oth], writes=[b_oth])
                    cur, b_cur, oth, b_oth = oth, b_oth, cur, b_cur
                ends, b_ends = cur, b_cur
                S.op("dve", lambda: nc.vector.tensor_tensor(base[:], ends[:], nf[:], ALU.subtract), reads=[b_ends, b_nf], writes=[b_base])
                idxf, b_idxf = P.sb("idxf", [128, NE * SMAX], F32)
                for e in range(NE):
                    S.op("dve", lambda e=e: nc.vector.tensor_scalar(idxf[:, e * SMAX:(e + 1) * SMAX], cst[:, 7, 0:SMAX], base[:, e:e + 1], None, op0=ALU.add),
                         reads=[b_cst, b_base], writes=[b_idxf])
                S.op("dve", lambda: nc.vector.tensor_scalar(idxf[:], idxf[:], float(NROWS - 1), None, op0=ALU.min), reads=[b_idxf], writes=[b_idxf])
                S.op("dve", lambda: nc.vector.tensor_copy(idx_i[:], idxf[:]), reads=[b_idxf], writes=[b_idx])
                dm, b_dm = P.sb("dm", [128, NE], F32)
                eq, b_eq = P.sb("eq", [128, NE], F32)
                d8r = P.ring_sb("d8", [128, 16], F32, 2)
                for i in range(16):
                    d8, b_d8 = d8r.next()
                    S.op("dve", lambda i=i: nc.vector.tensor_tensor(dm[:], posf[:, i, :], base[:], ALU.add), reads=[b_pf, b_base], writes=[b_dm])
                    S.op("dve", lambda i=i: nc.vector.scalar_tensor_tensor(dm[:], dm[:], 1.0, maskf[:, i, :], ALU.add, ALU.mult), reads=[b_dm, b_mf], writes=[b_dm])
                    S.op("dve", lambda d8=d8: nc.vector.max(out=d8[:, 0:8], in_=dm[:]), reads=[b_dm], writes=[b_d8])
                    S.op("dve", lambda d8=d8: nc.vector.tensor_scalar(d8[:, 8:12], d8[:, 0:4], -1.0, None, op0=ALU.add), reads=[b_d8], writes=[b_d8])
                    S.op("dve", lambda d8=d8, i=i: nc.vector.tensor_copy(dest_i[:, i * 4:i * 4 + 4], d8[:, 8:12]), reads=[b_d8], writes=[b_di])
                    for k in range(4):
                        S.op("dve", lambda d8=d8, k=k: nc.vector.tensor_scalar(eq[:], dm[:], d8[:, k:k + 1], None, op0=ALU.is_equal), reads=[b_dm, b_d8], writes=[b_eq])
                        S.op("dve", lambda i=i: nc.vector.tensor_tensor(eq[:], eq[:], wgt[:, i, :], ALU.mult), reads=[b_eq, b_wg], writes=[b_eq])
                        S.op("dve", lambda i=i, k=k: nc.vector.reduce_sum(w4[:, i, k:k + 1], eq[:], AX.X), reads=[b_eq], writes=[b_w4])
                    for k in range(4):
                        S.dma(b_h2b, [("pool", lambda i=i, k=k: nc.gpsimd.indirect_dma_start(
                            out=xs_d, out_offset=bass.IndirectOffsetOnAxis(ap=dest_i[:, i * 4 + k:i * 4 + k + 1], axis=0), in_=h2b[:, i * D:(i + 1) * D], in_offset=None))],
                            reads=[b_h2b, b_di], writes=[db("xs_d")])

        if stage >= 6:
            with phase(nc, S, "E") as P:
                regs = nc.alloc_registers("cntreg", engines=mybir.ALL_ENGINES)
                engname = {mybir.EngineType.Pool: "pool", mybir.EngineType.Activation: "act", mybir.EngineType.PE: "pe",
                           mybir.EngineType.DVE: "dve", mybir.EngineType.SP: "sp"}
                xsT, b_xsT = P.sb("xsT", [128, 16, 1024], BF16)
                actT, b_act = P.sb("actT", [128, 16, 1024], BF16)
                xrr = P.ring_sb("xr", [128, D], BF16, 2)
                w1r = P.ring_sb("w1", [128, 16, 256], BF16, 6)
                w2r = P.ring_sb("w2", [128, 16, 512], BF16, 2)
                b1r = P.ring_sb("b1", [128, 32], F32, 2)
                b2r = P.ring_sb("b2", [128, D], F32, 2)
                ptb = P.ring_ps("ptb", [128, 1024], BF16, 2)
                pg = P.ring_ps("pg", [128, 512], F32, 2)
                bbr = P.ring_sb("bb", [128, 512], F32, 3)
                glr = P.ring_sb("gl", [128, 512], F32, 2)
                abr = P.ring_sb("ab", [128, 256], BF16, 3)
                py = P.ring_ps("py", [128, 512], F32, 2)
                sg, b_sg = P.sb("sg", [128, 256], F32)
                yr = P.ring_sb("y", [128, 512], F32, 3)
                def emit_tr(s_j, ab_, b_ab, rs_, pc_):
                    with S.guard(regs, s_j * 128):
                        pt_, b_p = ptb.next()
                        for c in range(2):
                            S.op("pe", lambda pt_=pt_, ab_=ab_, c=c: nc.tensor.transpose(pt_[:, c * 128:(c + 1) * 128], ab_[:, c * 128:(c + 1) * 128], ident_b),
                                 reads=[b_ab, b_cstb], writes=[b_p])
                        S.op("act", lambda pt_=pt_, pc_=pc_, rs_=rs_: nc.scalar.activation(
                            actT[:, pc_ * 2:pc_ * 2 + 2, rs_], pt_[:, 0:256].rearrange("p (c n) -> p c n", c=2), AF.Copy), reads=[b_p], writes=[b_act])

                import os as _os
                for e in range(int(_os.environ.get('K_NEXP', NE))):
                    for reg in regs:
                        S.op(engname[reg.engine], lambda reg=reg, e=e: nc.reg_load(reg, cnt_i[0:1, e:e + 1]), reads=[b_cni])
                    b1t, b_b1 = b1r.next()
                    S.dma(b_b1, [("sp", lambda b1t=b1t, e=e: nc.sync.dma_start(out=b1t[:, 0:16], in_=b1g[e])),
                                 ("sp", lambda b1t=b1t, e=e: nc.sync.dma_start(out=b1t[:, 16:32], in_=b1l[e]))], writes=[b_b1])
                    b2t, b_b2 = b2r.next()
                    S.dma(b_b2, [("sp", lambda b2t=b2t, e=e: nc.sync.dma_start(out=b2t[:], in_=b2[e].to_broadcast([128, D])))], writes=[b_b2])
                    ESECT = 7
                    for ps in range(2):
                      with (S.guard(regs, 1024) if ps else contextlib.nullcontext()):
                            for s_i in range(ps * 8, ps * 8 + 8):
                                with S.guard(regs, s_i * 128):
                                    xr_, b_xr = xrr.next()
                                    col = e * SMAX + s_i
                                    S.dma(b_xr, [("pool", lambda xr_=xr_, col=col: nc.gpsimd.indirect_dma_start(
                                        out=xr_[:], out_offset=None, in_=xs_d, in_offset=bass.IndirectOffsetOnAxis(ap=idx_i[:, col:col + 1], axis=0)))],
                                        reads=[db("xs_d"), b_idx], writes=[b_xr])
                                    for g in range(2):
                                        pt_, b_p = ptb.next()
                                        for c in range(8):
                                            k = g * 8 + c
                                            S.op("pe", lambda pt_=pt_, xr_=xr_, c=c, k=k: nc.tensor.transpose(pt_[:, c * 128:(c + 1) * 128], xr_[:, k * 128:(k + 1) * 128], ident_b),
                                                 reads=[b_xr, b_cstb], writes=[b_p])
                                        S.op("act", lambda pt_=pt_, g=g, s_i=s_i: nc.scalar.activation(
                                            xsT[:, g * 8:(g + 1) * 8, (s_i - ps * 8) * 128:(s_i - ps * 8 + 1) * 128], pt_[:].rearrange("p (c n) -> p c n", c=8), AF.Copy), reads=[b_p], writes=[b_xsT])
                            for pc_ in range(8 if ESECT & 2 else 0):
                                wg_, b_wg_ = w1r.next()
                                wl_, b_wl_ = w1r.next()
                                cs_ = slice(pc_ * 256, (pc_ + 1) * 256)
                                S.dma(b_wg_, [("pool", lambda wg_=wg_, e=e, cs_=cs_: nc.gpsimd.dma_start(out=wg_[:], in_=w1g[e].rearrange("(k p) n -> p k n", p=128)[:, :, cs_]))], writes=[b_wg_])
                                S.dma(b_wl_, [("pool", lambda wl_=wl_, e=e, cs_=cs_: nc.gpsimd.dma_start(out=wl_[:], in_=w1l[e].rearrange("(k p) n -> p k n", p=128)[:, :, cs_]))], writes=[b_wl_])
                                bb_, b_bb = bbr.next()
                                S.dma(b_bb, [("sp", lambda bb_=bb_, e=e, cs_=cs_: nc.sync.dma_start(out=bb_[:, 0:256], in_=b1g_r[e][:, cs_].to_broadcast([128, 256]))),
                                             ("sp", lambda bb_=bb_, e=e, cs_=cs_: nc.sync.dma_start(out=bb_[:, 256:512], in_=b1l_r[e][:, cs_].to_broadcast([128, 256])))], writes=[b_bb])
                                pend = None
                                for s_i in range(ps * 8, ps * 8 + 8):
                                    rs_ = slice((s_i - ps * 8) * 128, (s_i - ps * 8 + 1) * 128)
                                    pend_new = None
                                    with S.guard(regs, s_i * 128):
                                        pg_, b_pg = pg.next()
                                        for k in range(16):
                                            S.op("pe", lambda pg_=pg_, wg_=wg_, k=k, rs_=rs_: nc.tensor.matmul(pg_[:, 0:256], xsT[:, k, rs_], wg_[:, k, :], start=(k == 0), stop=(k == 15)),
                                                 reads=[b_wg_, b_xsT], writes=[b_pg])
                                        for k in range(16):
                                            S.op("pe", lambda pg_=pg_, wl_=wl_, k=k, rs_=rs_: nc.tensor.matmul(pg_[:, 256:512], xsT[:, k, rs_], wl_[:, k, :], start=(k == 0), stop=(k == 15)),
                                                 reads=[b_wl_, b_xsT], writes=[b_pg])
                                        gl_, b_gl_ = glr.next()
                                        S.op("dve", lambda pg_=pg_, gl_=gl_, bb_=bb_: nc.vector.tensor_tensor(gl_[:], pg_[:], bb_[:], ALU.add), reads=[b_pg, b_bb], writes=[b_gl_])
                                        S.op("dve", lambda gl_=gl_: nc.vector.tensor_scalar(gl_[:], gl_[:], 7.0, None, op0=ALU.min), reads=[b_gl_], writes=[b_gl_])
                                        S.op("act", lambda gl_=gl_: nc.scalar.activation(sg[:], gl_[:, 0:256], AF.Sigmoid, scale=1.702), reads=[b_gl_], writes=[b_sg])
                                        S.op("dve", lambda gl_=gl_: nc.vector.tensor_scalar(gl_[:, 256:512], gl_[:, 256:512], -7.0, None, op0=ALU.max), reads=[b_gl_], writes=[b_gl_])
                                        S.op("dve", lambda gl_=gl_: nc.vector.tensor_tensor(sg[:], sg[:], gl_[:, 0:256], ALU.mult), reads=[b_gl_, b_sg], writes=[b_sg])
                                        ab_, b_ab = abr.next()
                                        S.op("dve", lambda gl_=gl_, ab_=ab_: nc.vector.scalar_tensor_tensor(ab_[:], gl_[:, 256:512], 1.0, sg[:], ALU.add, ALU.mult), reads=[b_gl_, b_sg], writes=[b_ab])
                                        pend_new = (s_i, ab_, b_ab, rs_)
                                    if pend is not None:
                                        emit_tr(*pend, pc_)
                                    pend = pend_new
                                if pend is not None:
                                    emit_tr(*pend, pc_)
                                    pend = None
                            for np_ in range(4 if ESECT & 4 else 0):
                                w2t, b_w2 = w2r.next()
                                cs_ = slice(np_ * 512, (np_ + 1) * 512)
                                S.dma(b_w2, [("pool", lambda w2t=w2t, e=e, cs_=cs_: nc.gpsimd.dma_start(out=w2t[:], in_=w2[e].rearrange("(k p) n -> p k n", p=128)[:, :, cs_]))], writes=[b_w2])
                                for s_i in range(ps * 8, ps * 8 + 8):
                                    rs_ = slice((s_i - ps * 8) * 128, (s_i - ps * 8 + 1) * 128)
                                    with S.guard(regs, s_i * 128):
                                        py_, b_py = py.next()
                                        for k in range(16):
                                            S.op("pe", lambda py_=py_, w2t=w2t, rs_=rs_, k=k: nc.tensor.matmul(py_[:], actT[:, k, rs_], w2t[:, k, :], start=(k == 0), stop=(k == 15)),
                                                 reads=[b_act, b_w2], writes=[b_py])
                                        y_, b_y = yr.next()
                                        S.op("dve", lambda py_=py_, y_=y_, b2t=b2t, cs_=cs_: nc.vector.tensor_tensor(y_[:], py_[:], b2t[:, cs_], ALU.add), reads=[b_py, b_b2], writes=[b_y])
                                        col = e * SMAX + s_i
                                        S.dma(b_y, [("pool", lambda y_=y_, col=col, np_=np_: nc.gpsimd.indirect_dma_start(
                                            out=ys_p[np_], out_offset=bass.IndirectOffsetOnAxis(ap=idx_i[:, col:col + 1], axis=0), in_=y_[:], in_offset=None))],
                                            reads=[b_y, b_idx], writes=[db("ys_d")])


        if stage >= 7:
            with phase(nc, S, "F") as P:
                g2, b_g2 = P.sb("g2", [128, D], F32)
                gf, b_gf = P.sb("gf", [128, D], F32)
                S.dma(b_g2, [("sp", lambda: nc.sync.dma_start(out=g2[:], in_=bc_row(mod_flat[:, 80 * 128:96 * 128], D)))], reads=[db("mod_d")], writes=[b_g2])
                S.dma(b_gf, [("sp", lambda: nc.sync.dma_start(out=gf[:], in_=bc_row(rows[0:1, :], D)))], writes=[b_gf])
                gr = P.ring_sb("ga", [128, D], F32, 4)
                accr = P.ring_sb("acc", [128, D], F32, 2)
                x1r = P.ring_sb("x1", [128, D], F32, 2)
                sq, b_sq = P.sb("sq", [128, D], BF16)
                ssr = P.ring_sb("ss", [128, 1], F32, 2)
                for i in range(16):
                    xb, b_xb = x1r.next()
                    S.dma(b_xb, [("sp", lambda xb=xb, i=i: nc.sync.dma_start(out=xb[:], in_=x1_d[i * 128:(i + 1) * 128, :]))], reads=[db("x1_d")], writes=[b_xb])
                    acc, b_acc = accr.next()
                    for k in range(4):
                        ga, b_ga = gr.next()
                        S.dma(b_ga, [("pool", lambda ga=ga, i=i, k=k, q=q: nc.gpsimd.indirect_dma_start(
                            out=ga[:, q * 512:(q + 1) * 512], out_offset=None, in_=ys_p[q], in_offset=bass.IndirectOffsetOnAxis(ap=dest_i[:, i * 4 + k:i * 4 + k + 1], axis=0)))
                            for q in range(4)], reads=[db("ys_d"), b_di], writes=[b_ga])
                        if k == 0:
                            S.op("dve", lambda ga=ga, acc=acc, i=i, k=k: nc.vector.tensor_scalar(acc[:], ga[:], w4[:, i, k:k + 1], None, op0=ALU.mult), reads=[b_ga, b_w4], writes=[b_acc])
                        else:
                            S.op("dve", lambda ga=ga, acc=acc, i=i, k=k: nc.vector.scalar_tensor_tensor(acc[:], ga[:], w4[:, i, k:k + 1], acc[:], ALU.mult, ALU.add), reads=[b_ga, b_w4, b_acc], writes=[b_acc])
                    S.op("pool", lambda acc=acc: nc.gpsimd.tensor_tensor(acc[:], acc[:], g2[:], ALU.mult), reads=[b_acc, b_g2], writes=[b_acc])
                    S.op("dve", lambda acc=acc, xb=xb: nc.vector.tensor_tensor(acc[:], acc[:], xb[:], ALU.add), reads=[b_acc, b_xb], writes=[b_acc])
                    ss, b_ss = ssr.next()
                    S.op("act", lambda acc=acc, ss=ss: nc.scalar.activation(sq[:], acc[:], AF.Square, accum_out=ss[:, 0:1]), reads=[b_acc], writes=[b_sq, b_ss])
                    S.op("act", lambda ss=ss: nc.scalar.activation(ss[:], ss[:], AF.Sqrt, bias=epsT[:, 0:1], scale=1.0 / D), reads=[b_ss, b_eps], writes=[b_ss])
                    S.op("dve", lambda ss=ss: nc.vector.reciprocal(ss[:], ss[:]), reads=[b_ss], writes=[b_ss])
                    S.op("dve", lambda acc=acc, ss=ss: nc.vector.scalar_tensor_tensor(acc[:], acc[:], ss[:, 0:1], gf[:], ALU.mult, ALU.mult), reads=[b_acc, b_ss, b_gf], writes=[b_acc])
                    S.dma(b_acc, [("act", lambda acc=acc, i=i: nc.scalar.dma_start(out=out[i * 128:(i + 1) * 128, :], in_=acc[:]))], reads=[b_acc], writes=[db("out")])

        if dbg and stage <= 4:
            with phase(nc, S, "DBG") as P:
                t_, b_t = P.sb("dbg", [128, 16, D], F32) if False else (None, None)
                r = P.ring_sb("r", [128, D], F32, 2)
                for i in range(16):
                    t_, b_t = r.next()
                    S.dma(b_t, [("sp", lambda t_=t_, i=i: nc.sync.dma_start(out=t_[:], in_=x1_d[i * 128:(i + 1) * 128, :]))], reads=[db("x1_d")], writes=[b_t])
                    S.dma(b_t, [("sp", lambda t_=t_, i=i: nc.sync.dma_start(out=dbg_out[i * 128:(i + 1) * 128, :], in_=t_[:]))], reads=[b_t], writes=[db("dbg")])

        S.barrier()
    return nc


def own_blocks(p):
    blks = []
    for m in range(8):
        blks += [4 * m + (0 if p == 0 else 1), 4 * m + (3 if p == 0 else 2)]
    return blks


def host_prepare(inp):
    f32 = np.float32
    x = np.asarray(inp["x"], f32)
    c = np.asarray(inp["c"], f32)
    pos = np.asarray(inp["positions"], np.int32)
    g_attn = np.asarray(inp["g_attn"], f32)[0]
    g_ffn = np.asarray(inp["g_ffn"], f32)[0]
    b_mod = np.asarray(inp["b_mod"], f32)[0]
    w_in = np.asarray(inp["w_in"], f32)[0]
    w_q_up = np.asarray(inp["w_q_up"], f32)[0].reshape(512, 8, 192)
    w_kv_up = np.asarray(inp["w_kv_up"], f32)[0].reshape(512, 8, 256)
    fm = lambda v: np.ascontiguousarray(v.reshape(-1, 128).T)
    vecs = np.concatenate([fm(g_attn), fm(g_ffn), fm(b_mod)], axis=1).astype(f32)
    rows = np.zeros((4, D), f32)
    rows[0] = np.asarray(inp["g_final"], f32)
    rows[1, :NE] = np.asarray(inp["b_router"], f32)[0]
    rows[2] = g_ffn
    glat = np.concatenate([fm(np.asarray(inp["g_q_lat"], f32)[0]), fm(np.asarray(inp["g_kv_lat"], f32)[0])], axis=1).astype(f32)
    half = 32
    invf = (1.0 / (np.float32(10000.0) ** (np.arange(half, dtype=f32) * f32(2.0) / f32(64)))).astype(f32)
    ropec = np.zeros((128, 4), f32)
    for p_ in range(128):
        i = p_ % 64
        ropec[p_, 0] = invf[i % 32]
        ropec[p_, 1] = -1.0 if i < 32 else 1.0
    ropec[:, 2] = np.pi
    ropec[:, 3] = 1.5 * np.pi
    kk = np.arange(128)[:, None]
    qq = np.arange(128)[None, :]
    ident = np.eye(128, dtype=f32)
    negtri = -(kk >= qq).astype(f32)
    ones = np.ones((128, 128), f32)
    tri_s = (kk < qq).astype(f32)
    negones33 = np.zeros((128, 128), f32)
    negones33[:, :33] = -1.0
    bc33 = np.zeros((128, 128), f32)
    bc33[0, :] = 1.0
    bc33[32, :] = 1.0
    blkstart = np.tile((np.arange(128, dtype=f32) * RB)[None, :], (128, 1))
    off16 = (np.arange(128, dtype=f32)[None, :] % 16) * 128 + np.arange(128, dtype=f32)[:, None]
    consts = np.stack([ident, negtri, ones, tri_s, negones33, bc33, blkstart, off16]).astype(f32)
    TRI_SB = (kk < qq).astype(f32)
    TRI_ML = (kk <= qq).astype(f32)
    ONE = np.ones((128, 128), f32)
    ZERO = np.zeros((128, 128), f32)
    kr = w_in[:, 4096:4160]
    w_kr = np.ascontiguousarray(np.concatenate([kr, kr[:, 32:], kr[:, :32]], axis=1))
    w_qn = np.ascontiguousarray(w_q_up[:, :, :128].reshape(512, 1024))
    qr_ = w_q_up[:, :, 128:]
    w_qra = np.ascontiguousarray(qr_.reshape(512, 512))
    w_qrs = np.ascontiguousarray(np.concatenate([qr_[:, :, 32:], qr_[:, :, :32]], axis=2).reshape(512, 512))
    w_kn = np.ascontiguousarray(w_kv_up[:, :, :128].reshape(512, 1024))
    w_v = np.ascontiguousarray(w_kv_up[:, :, 128:].reshape(512, 1024))
    w1 = np.asarray(inp["w1"], f32)[0]
    b1 = np.asarray(inp["b1"], f32)[0]
    shared = dict(
        vecs=vecs, rows=rows, glat=glat, ropec=ropec, consts=consts,
        w_mod=np.asarray(inp["w_mod"], f32)[0], w_in=w_in, w_kr=w_kr, w_qn=w_qn, w_qra=w_qra, w_qrs=w_qrs,
        w_kn=w_kn, w_v=w_v, w_out=np.asarray(inp["w_out"], f32)[0],
        w_router=np.ascontiguousarray(np.asarray(inp["w_router"], f32)[0].reshape(16, 128, NE).transpose(1, 0, 2)),
        w1g=np.ascontiguousarray(w1[:, :, 0::2]), w1l=np.ascontiguousarray(w1[:, :, 1::2]),
        b1g=np.ascontiguousarray(b1[:, 0::2].reshape(NE, 16, 128).transpose(0, 2, 1)),
        b1l=np.ascontiguousarray(b1[:, 1::2].reshape(NE, 16, 128).transpose(0, 2, 1)),
        b1g_r=np.ascontiguousarray(b1[:, 0::2].reshape(NE, 1, D)), b1l_r=np.ascontiguousarray(b1[:, 1::2].reshape(NE, 1, D)),
        w2=np.asarray(inp["w2"], f32)[0], b2=np.ascontiguousarray(np.asarray(inp["b2"], f32)[0].reshape(NE, 1, D)),
    )
    in_maps = []
    for core in range(8):
        b, p = core // 2, core % 2
        blks = own_blocks(p)
        rowsel = np.concatenate([np.arange(k * 128, (k + 1) * 128) for k in blks])
        if p == 0:
            zs = [(TRI_SB, TRI_ML), (ZERO, ZERO), (ONE, ONE), (TRI_SB, TRI_ML)]
        else:
            zs = [(ONE, ONE), (TRI_SB, TRI_ML), (TRI_SB, TRI_ML), (ZERO, ZERO)]
        masks = np.stack([np.stack([z[0] for z in zs]), np.stack([z[1] for z in zs])]).astype(f32)
        m = dict(shared)
        m.update(
            x_all=np.ascontiguousarray(x[b]), x_own=np.ascontiguousarray(x[b][rowsel]),
            pos_all=np.ascontiguousarray(pos[b][None, :]), pos_own=np.ascontiguousarray(pos[b][rowsel][None, :]),
            cT=fm(c[b]), masks=masks,
        )
        in_maps.append(m)
    return in_maps


_CACHE = {}


def kernel(**inp):
    in_maps = host_prepare(inp)
    if "nc" not in _CACHE:
        _CACHE["nc"] = build_program()
    res = run_bass_kernel_spmd(_CACHE["nc"], in_maps, core_ids=list(range(8)))
    outp = np.zeros((4, S_ALL, D), np.float32)
    for core in range(8):
        b, p = core // 2, core % 2
        o = res.results[core]["out"]
        for i, k in enumerate(own_blocks(p)):
            outp[b, k * 128:(k + 1) * 128] = o[i * 128:(i + 1) * 128]
    return outp
```

```python
import contextlib
import numpy as np
import concourse.bass as bass
import concourse.mybir as mybir
from concourse.bass_utils import run_bass_kernel_spmd

F32 = mybir.dt.float32
BF16 = mybir.dt.bfloat16
I32 = mybir.dt.int32
AF = mybir.ActivationFunctionType
ALU = mybir.AluOpType
AX = mybir.AxisListType

D = 2048
S_ALL = 4096
T_OWN = 2048
NE = 32
CAPR = 2048
RB = 128
NROWS = 4 * T_OWN + NE * RB
NBLK = NROWS // RB
EPS = 1e-6
PI = float(np.pi)


class Buf:
    __slots__ = ("name", "w", "r", "dsem")

    def __init__(self, name):
        self.name = name
        self.w = {}
        self.r = {}
        self.dsem = None


class Sync:
    ENG = ("pe", "act", "dve", "pool", "sp")

    def __init__(self, nc):
        self.nc = nc
        self.e = {"pe": nc.tensor, "act": nc.scalar, "dve": nc.vector, "pool": nc.gpsimd, "sp": nc.sync}
        self.sems = {}
        self.cnt = {}
        for n in self.ENG:
            self.sems[n] = nc.alloc_semaphore("es_" + n)
            self.cnt[n] = 0
        self.seen = {n: {} for n in self.ENG}
        self.free_d = []
        self.nd = 0

    def _wait(self, eng, deps):
        for k, v in deps.items():
            if v <= 0 or (k == eng and eng == "pe"):
                continue
            if self.seen[eng].get(k, 0) >= v:
                continue
            self.e[eng].wait_ge(self.sems[k], v)
            self.seen[eng][k] = v

    @staticmethod
    def _merge(d, s):
        for k, v in s.items():
            if d.get(k, 0) < v:
                d[k] = v

    def _deps(self, reads, writes):
        d = {}
        for b in reads:
            self._merge(d, b.w)
        for b in writes:
            self._merge(d, b.w)
            self._merge(d, b.r)
        return d

    def op(self, eng, fn, reads=(), writes=()):
        self._wait(eng, self._deps(reads, writes))
        ins = fn()
        self.cnt[eng] += 1
        ins.then_inc(self.sems[eng], 1)
        me = {eng: self.cnt[eng]}
        for b in reads:
            self._merge(b.r, me)
        for b in writes:
            b.w = dict(me)
            b.r = {}
        return ins

    def _dsem(self, buf):
        if buf.dsem is None:
            if self.free_d:
                buf.dsem = self.free_d.pop()
            else:
                self.nd += 1
                buf.dsem = "d%d" % self.nd
                self.sems[buf.dsem] = self.nc.alloc_semaphore(buf.dsem)
                self.cnt[buf.dsem] = 0
        return buf.dsem

    def release(self, bufs):
        for b in bufs:
            if b.dsem is not None:
                self.free_d.append(b.dsem)
                b.dsem = None

    def dma(self, sb, items, reads=(), writes=()):
        key = self._dsem(sb)
        deps = self._deps(reads, writes)
        if self.cnt[key] > 0:
            deps[key] = max(deps.get(key, 0), self.cnt[key])
        for q, fn in items:
            self._wait(q, deps)
        for q, fn in items:
            ins = fn()
            ins.then_inc(self.sems[key], 16)
            self.cnt[key] += 16
        me = {key: self.cnt[key]}
        for b in reads:
            self._merge(b.r, me)
        for b in writes:
            b.w = dict(me)
            b.r = {}

    @contextlib.contextmanager
    def guard(self, regs, thr):
        before = dict(self.cnt)
        seen0 = {k: dict(v) for k, v in self.seen.items()}
        with self.nc.If_cmp(regs, thr, "IS_GT"):
            yield
        after = dict(self.cnt)
        with self.nc.Else():
            for k, v in after.items():
                d = v - before.get(k, 0)
                if d > 0:
                    eng = k if k in self.ENG else "sp"
                    if before.get(k, 0) > 0:
                        self.e[eng].wait_ge(self.sems[k], before[k])
                    self.e[eng].sem_inc(self.sems[k], d)
        self.seen = seen0

    def barrier(self, engines=None):
        allv = {k: v for k, v in self.cnt.items() if v > 0}
        for en in (engines or self.ENG):
            self._wait(en, allv)


class Ring:
    def __init__(self, items):
        self.items = items
        self.i = 0

    def next(self):
        it = self.items[self.i % len(self.items)]
        self.i += 1
        return it


class Ctx:
    def __init__(self, nc, S, es, tag):
        self.nc, self.S, self.es, self.tag = nc, S, es, tag
        self.bufs = []
        self.n = 0

    def sb(self, name, shape, dt):
        self.n += 1
        t = self.es.enter_context(self.nc.sbuf_tensor("%s_%s%d" % (self.tag, name, self.n), shape, dt))
        b = Buf(name)
        self.bufs.append(b)
        return t, b

    def ps(self, name, shape, dt):
        self.n += 1
        t = self.es.enter_context(self.nc.psum_tensor("%s_%s%d" % (self.tag, name, self.n), shape, dt))
        b = Buf(name)
        self.bufs.append(b)
        return t, b

    def ring_sb(self, name, shape, dt, n):
        return Ring([self.sb(name, shape, dt) for _ in range(n)])

    def ring_ps(self, name, shape, dt, n):
        return Ring([self.ps(name, shape, dt) for _ in range(n)])


@contextlib.contextmanager
def phase(nc, S, tag):
    with contextlib.ExitStack() as es:
        c = Ctx(nc, S, es, tag)
        yield c
        S.barrier()
        S.release(c.bufs)


def build_program(stage=99, dbg=False):
    nc = bass.Bass("TRN2", target_bir_lowering=False)
    S = Sync(nc)

    def din(name, shape, dt=F32):
        return nc.dram_tensor(name, list(shape), dt, kind="ExternalInput").ap()

    def dscr(name, shape, dt):
        return nc.dram_tensor(name, list(shape), dt, kind="Internal").ap()

    x_all = din("x_all", [S_ALL, D])
    x_own = din("x_own", [T_OWN, D])
    pos_all = din("pos_all", [1, S_ALL], I32)
    pos_own = din("pos_own", [1, T_OWN], I32)
    cT = din("cT", [128, 16])
    vecs = din("vecs", [128, 16 * 2 + 96])
    rows = din("rows", [4, D])
    glat = din("glat", [128, 8])
    ropec = din("ropec", [128, 4])
    masks = din("masks", [2, 4, 128, 128])
    consts = din("consts", [8, 128, 128])
    w_mod = din("w_mod", [D, 6 * D])
    w_in = din("w_in", [D, 4160])
    w_kr = din("w_kr", [D, 128])
    w_qn = din("w_qn", [512, 1024])
    w_qra = din("w_qra", [512, 512])
    w_qrs = din("w_qrs", [512, 512])
    w_kn = din("w_kn", [512, 1024])
    w_v = din("w_v", [512, 1024])
    w_out = din("w_out", [D, D])
    w_router = din("w_router", [128, 16, NE])
    w1g = din("w1g", [NE, D, D])
    w1l = din("w1l", [NE, D, D])
    b1g = din("b1g", [NE, 128, 16])
    b1l = din("b1l", [NE, 128, 16])
    b1g_r = din("b1g_r", [NE, 1, D])
    b1l_r = din("b1l_r", [NE, 1, D])
    w2 = din("w2", [NE, D, D])
    b2 = din("b2", [NE, 1, D])
    out = nc.dram_tensor("out", [T_OWN, D], F32, kind="ExternalOutput").ap()
    dbg_out = nc.dram_tensor("dbg", [T_OWN, D], F32, kind="ExternalOutput").ap() if dbg else None

    mod_d = dscr("mod_d", [96, 128], F32)
    hT_all = dscr("hT_all", [D, S_ALL], BF16)
    hT_own = dscr("hT_own", [D, T_OWN], BF16)
    QT_sb = dscr("QT_sb", [1024, T_OWN], BF16)
    KT_sb = dscr("KT_sb", [1024, S_ALL], BF16)
    V_sb = dscr("V_sb", [S_ALL, 1024], BF16)
    qlnT = dscr("qlnT", [512, T_OWN], BF16)
    kvlnT = dscr("kvlnT", [512, S_ALL], BF16)
    KrT = dscr("KrT", [64, S_ALL], BF16)
    QnT = dscr("QnT", [1024, T_OWN], BF16)
    QrT = dscr("QrT", [512, T_OWN], BF16)
    KnT = dscr("KnT", [1024, S_ALL], BF16)
    V_ml = dscr("V_ml", [S_ALL, 1024], BF16)
    mixT = dscr("mixT", [D, T_OWN], BF16)
    x1_d = dscr("x1_d", [T_OWN, D], F32)
    xs_d = dscr("xs_d", [NROWS, D], BF16)
    ys_p = [dscr("ys_d%d" % q, [NROWS, 512], F32) for q in range(4)]
    cnt_d = dscr("cnt_d", [1, NE], I32)
    Bd = {}

    def db(name):
        if name not in Bd:
            Bd[name] = Buf(name)
        return Bd[name]

    qi = [0]

    def q2():
        qi[0] += 1
        return "sp"

    with contextlib.ExitStack() as glob:
        G = Ctx(nc, S, glob, "g")
        cst, b_cst = G.sb("cst", [128, 8, 128], F32)
        cstb, b_cstb = G.sb("cstb", [128, 8, 128], BF16)
        S.dma(b_cst, [("sp", lambda: nc.sync.dma_start(out=cst[:], in_=consts.rearrange("c p n -> p c n")))], writes=[b_cst])
        S.op("dve", lambda: nc.vector.tensor_copy(cstb[:], cst[:]), reads=[b_cst], writes=[b_cstb])
        ident_f = cst[:, 0, :]
        ident_b = cstb[:, 0, :]
        negtri_b = cstb[:, 1, :]
        ones_b = cstb[:, 2, :]
        tri_b = cstb[:, 3, :]
        negones33_b = cstb[:, 4, 0:33]
        bc33_b = cstb[0:33, 5, :]
        vec, b_vec = G.sb("vec", [128, 128], F32)
        S.dma(b_vec, [("sp", lambda: nc.sync.dma_start(out=vec[:], in_=vecs))], writes=[b_vec])
        gl, b_gl = G.sb("gl", [128, 8], F32)
        S.dma(b_gl, [("sp", lambda: nc.sync.dma_start(out=gl[:], in_=glat))], writes=[b_gl])
        rc, b_rc = G.sb("rc", [128, 4], F32)
        S.dma(b_rc, [("sp", lambda: nc.sync.dma_start(out=rc[:], in_=ropec))], writes=[b_rc])
        epsT, b_eps = G.sb("eps", [128, 1], F32)
        S.op("dve", lambda: nc.vector.memset(epsT[:], EPS), writes=[b_eps])
        modT, b_modT = G.sb("modT", [128, 96], F32)
        gm, b_gm = G.sb("gm", [128, 32], F32)

        with phase(nc, S, "M") as P:
            ct, b_ct = P.sb("ct", [128, 16], F32)
            S.dma(b_ct, [("sp", lambda: nc.sync.dma_start(out=ct[:], in_=cT))], writes=[b_ct])
            ca, b_ca = P.sb("ca", [128, 16], F32)
            S.op("act", lambda: nc.scalar.activation(ca[:], ct[:], AF.Silu), reads=[b_ct], writes=[b_ca])
            wr = P.ring_sb("wm", [128, 16, 512], BF16, 3)
            cab, b_cab = P.sb("cab", [128, 16], BF16)
            S.op("dve", lambda: nc.vector.tensor_copy(cab[:], ca[:]), reads=[b_ca], writes=[b_cab])
            pm, b_pm = P.ps("pm", [128, 96], F32)
            wv = w_mod.rearrange("(k p) n -> p k n", p=128)
            for g in range(24):
                wt, b_wt = wr.next()
                S.dma(b_wt, [("pool", lambda wt=wt, g=g: nc.gpsimd.dma_start(out=wt[:], in_=wv[:, :, g * 512:(g + 1) * 512]))], writes=[b_wt])
                for c in range(4):
                    j = g * 4 + c
                    for k in range(16):
                        S.op("pe", lambda wt=wt, c=c, k=k, j=j: nc.tensor.matmul(
                            pm[:, j:j + 1], wt[:, k, c * 128:(c + 1) * 128], cab[:, k:k + 1], start=(k == 0), stop=(k == 15)),
                            reads=[b_wt, b_cab], writes=[b_pm])
            S.op("dve", lambda: nc.vector.tensor_tensor(modT[:], pm[:], vec[:, 32:128], ALU.add), reads=[b_pm, b_vec], writes=[b_modT])
            S.op("dve", lambda: nc.vector.scalar_tensor_tensor(gm[:, 0:16], modT[:, 16:32], 1.0, vec[:, 0:16], ALU.add, ALU.mult),
                 reads=[b_modT, b_vec], writes=[b_gm])
            S.op("dve", lambda: nc.vector.scalar_tensor_tensor(gm[:, 16:32], modT[:, 64:80], 1.0, vec[:, 16:32], ALU.add, ALU.mult),
                 reads=[b_modT, b_vec], writes=[b_gm])
            pt, b_pt = P.ps("pt", [128, 128], F32)
            S.op("pe", lambda: nc.tensor.transpose(pt[0:96, :], modT[:, 0:96], ident_f), reads=[b_modT, b_cst], writes=[b_pt])
            mt, b_mt = P.sb("mt", [96, 128], F32)
            S.op("dve", lambda: nc.vector.tensor_copy(mt[:], pt[0:96, :]), reads=[b_pt], writes=[b_mt])
            S.dma(b_mt, [("sp", lambda: nc.sync.dma_start(out=mod_d, in_=mt[:]))], reads=[b_mt], writes=[db("mod_d")])

        mod_flat = mod_d.rearrange("j p -> (j p)").rearrange("(o n) -> o n", o=1)

        def bc_row(ap_row, n):
            return ap_row.to_broadcast([128, n])

        def norm_to_hT(P, src, dst, ntile, dstname, gcol, shcol):
            xr = P.ring_sb("x", [128, 4, D], F32, 2)
            xnr = P.ring_sb("xn", [128, 4, D], BF16, 2)
            sq, b_sq = P.sb("sq", [128, D], BF16)
            ssr = P.ring_sb("ss", [128, 4], F32, 2)
            ptr = P.ring_ps("ptr", [128, 2048], BF16, 2)
            hr = P.ring_sb("h", [128, 16, 512], BF16, 2)
            sv = src.rearrange("(n j p) d -> n p j d", p=128, j=4)
            dv = dst.rearrange("(k p) t -> p k t", p=128)
            for n in range(ntile):
                xt, b_x = xr.next()
                S.dma(b_x, [("sp", lambda xt=xt, n=n: nc.sync.dma_start(out=xt[:], in_=sv[n]))], writes=[b_x])
                ss, b_ss = ssr.next()
                xn, b_xn = xnr.next()
                ht, b_h = hr.next()
                for j in range(4):
                    S.op("act", lambda xt=xt, ss=ss, j=j: nc.scalar.activation(sq[:], xt[:, j, :], AF.Square, accum_out=ss[:, j:j + 1]),
                         reads=[b_x], writes=[b_sq, b_ss])
                S.op("act", lambda ss=ss: nc.scalar.activation(ss[:], ss[:], AF.Sqrt, bias=epsT[:, 0:1], scale=1.0 / D),
                     reads=[b_ss, b_eps], writes=[b_ss])
                S.op("dve", lambda ss=ss: nc.vector.reciprocal(ss[:], ss[:]), reads=[b_ss], writes=[b_ss])
                for j in range(4):
                    S.op("dve", lambda xt=xt, xn=xn, ss=ss, j=j: nc.vector.tensor_scalar(
                        xn[:, j, :], xt[:, j, :], ss[:, j:j + 1], None, op0=ALU.mult), reads=[b_x, b_ss], writes=[b_xn])
                for j in range(4):
                    pt_, b_p = ptr.next()
                    for k in range(16):
                        S.op("pe", lambda pt_=pt_, xn=xn, j=j, k=k: nc.tensor.transpose(
                            pt_[:, k * 128:(k + 1) * 128], xn[:, j, k * 128:(k + 1) * 128], ident_b), reads=[b_xn, b_cstb], writes=[b_p])
                    for k in range(16):
                        eng = "dve" if k % 2 == 0 else "pool"
                        if eng == "pool":
                            eng = "dve"
                        S.op(eng, lambda pt_=pt_, ht=ht, j=j, k=k: nc.vector.tensor_scalar(
                            ht[:, k, j * 128:(j + 1) * 128], pt_[:, k * 128:(k + 1) * 128],
                            gm[:, gcol + k:gcol + k + 1], modT[:, shcol + k:shcol + k + 1], op0=ALU.mult, op1=ALU.add),
                            reads=[b_p, b_gm, b_modT], writes=[b_h])
                S.dma(b_h, [("pool", lambda ht=ht, n=n: nc.gpsimd.dma_start(out=dv[:, :, n * 512:(n + 1) * 512], in_=ht[:]))],
                      reads=[b_h], writes=[db(dstname)])

        if stage >= 1:
            with phase(nc, S, "A") as P:
                norm_to_hT(P, x_all, hT_all, 8, "hT_all", 0, 0)
            with phase(nc, S, "A2") as P:
                norm_to_hT(P, x_own, hT_own, 4, "hT_own", 0, 0)

        def load_w(P, ring, Wap, KC, c0, ncols):
            wt, b_w = ring.next()
            wv_ = Wap.rearrange("(k p) n -> p k n", p=128)
            S.dma(b_w, [("pool", lambda: nc.gpsimd.dma_start(out=wt[:, 0:KC, 0:ncols], in_=wv_[:, :, c0:c0 + ncols]))], writes=[b_w])
            return wt, b_w

        def load_a(ring, Aap, aname, KC, t0):
            at, b_a = ring.next()
            av = Aap.rearrange("(k p) t -> p k t", p=128)
            S.dma(b_a, [("sp", lambda: nc.sync.dma_start(out=at[:, 0:KC, :], in_=av[:, :, t0:t0 + 512]))], reads=[db(aname)], writes=[b_a])
            return at, b_a

        def gemm_fm(P, Wap, c0, N, Aap, aname, T, KC, dst, dname, scale, rings):
            wr_, ar_, pr_, sr_ = rings
            dv = dst.rearrange("(c p) t -> p c t", p=128)
            for g in range(N // 512):
                wt, b_w = load_w(P, wr_, Wap, KC, c0 + g * 512, 512)
                for tt in range(T // 512):
                    at, b_a = load_a(ar_, Aap, aname, KC, tt * 512)
                    st, b_s = sr_.next()
                    for c in range(4):
                        ps_, b_p = pr_.next()
                        for k in range(KC):
                            S.op("pe", lambda ps_=ps_, wt=wt, at=at, c=c, k=k: nc.tensor.matmul(
                                ps_[:], wt[:, k, c * 128:(c + 1) * 128], at[:, k, :], start=(k == 0), stop=(k == KC - 1)),
                                reads=[b_w, b_a], writes=[b_p])
                        S.op("act", lambda ps_=ps_, st=st, c=c: nc.scalar.activation(st[:, c, :], ps_[:], AF.Copy, scale=scale),
                             reads=[b_p], writes=[b_s])
                    S.dma(b_s, [("pool", lambda st=st, g=g, tt=tt: nc.gpsimd.dma_start(
                        out=dv[:, g * 4:(g + 1) * 4, tt * 512:(tt + 1) * 512], in_=st[:]))], reads=[b_s], writes=[db(dname)])

        def gemm_tm(P, Wap, c0, N, Aap, aname, T, KC, dst, dname, rings):
            wr_, ar_, pr_, sr_ = rings
            dv = dst.rearrange("(n j p) c -> n p j c", p=128, j=4)
            for g in range(N // 512):
                wt, b_w = load_w(P, wr_, Wap, KC, c0 + g * 512, 512)
                for tt in range(T // 512):
                    at, b_a = load_a(ar_, Aap, aname, KC, tt * 512)
                    st, b_s = sr_.next()
                    for j in range(4):
                        ps_, b_p = pr_.next()
                        for k in range(KC):
                            S.op("pe", lambda ps_=ps_, wt=wt, at=at, j=j, k=k: nc.tensor.matmul(
                                ps_[:], at[:, k, j * 128:(j + 1) * 128], wt[:, k, :], start=(k == 0), stop=(k == KC - 1)),
                                reads=[b_w, b_a], writes=[b_p])
                        S.op("act", lambda ps_=ps_, st=st, j=j: nc.scalar.activation(st[:, j, :], ps_[:], AF.Copy),
                             reads=[b_p], writes=[b_s])
                    S.dma(b_s, [("pool", lambda st=st, g=g, tt=tt: nc.gpsimd.dma_start(
                        out=dv[tt][:, :, g * 512:(g + 1) * 512], in_=st[:]))], reads=[b_s], writes=[db(dname)])

        def latent_norm(P, c0, Aap, aname, T, gcol, dst, dname, rings):
            wr_, ar_, pr_, sr_ = rings
            dv = dst.rearrange("(c p) t -> p c t", p=128)
            l32, b_l = P.sb("l32", [128, 4, 512], F32)
            lsq, b_q = P.sb("lsq", [128, 4, 512], BF16)
            rs, b_rs = P.sb("rs", [128, 512], F32)
            wt, b_w = load_w(P, wr_, w_in, 16, c0, 512)
            for tt in range(T // 512):
                at, b_a = load_a(ar_, Aap, aname, 16, tt * 512)
                st, b_s = sr_.next()
                for c in range(4):
                    ps_, b_p = pr_.next()
                    for k in range(16):
                        S.op("pe", lambda ps_=ps_, at=at, c=c, k=k: nc.tensor.matmul(
                            ps_[:], wt[:, k, c * 128:(c + 1) * 128], at[:, k, :], start=(k == 0), stop=(k == 15)),
                            reads=[b_w, b_a], writes=[b_p])
                    S.op("act", lambda ps_=ps_, c=c: nc.scalar.activation(l32[:, c, :], ps_[:], AF.Copy), reads=[b_p], writes=[b_l])
                    S.op("dve", lambda c=c: nc.vector.tensor_tensor(lsq[:, c, :], l32[:, c, :], l32[:, c, :], ALU.mult), reads=[b_l], writes=[b_q])
                ps_, b_p = pr_.next()
                for c in range(4):
                    S.op("pe", lambda ps_=ps_, c=c: nc.tensor.matmul(ps_[:], ones_b, lsq[:, c, :], start=(c == 0), stop=(c == 3)),
                         reads=[b_q, b_cstb], writes=[b_p])
                S.op("act", lambda ps_=ps_: nc.scalar.activation(rs[:], ps_[:], AF.Sqrt, bias=epsT[:, 0:1], scale=1.0 / 512),
                     reads=[b_p, b_eps], writes=[b_rs])
                S.op("dve", lambda: nc.vector.reciprocal(rs[:], rs[:]), reads=[b_rs], writes=[b_rs])
                for c in range(4):
                    S.op("dve", lambda st=st, c=c: nc.vector.scalar_tensor_tensor(
                        st[:, c, :], l32[:, c, :], gl[:, gcol + c:gcol + c + 1], rs[:], ALU.mult, ALU.mult),
                        reads=[b_l, b_gl, b_rs], writes=[b_s])
                S.dma(b_s, [("pool", lambda st=st, tt=tt: nc.gpsimd.dma_start(out=dv[:, :, tt * 512:(tt + 1) * 512], in_=st[:]))],
                      reads=[b_s], writes=[db(dname)])

        def rope_tables(P, pos_ap, T, scale, name):
            pi_, b_pi = P.sb("posi", [128, T], I32)
            S.dma(b_pi, [("sp", lambda: nc.sync.dma_start(out=pi_[:], in_=pos_ap.to_broadcast([128, T])))], writes=[b_pi])
            ang, b_an = P.sb("ang", [128, T], F32)
            S.op("dve", lambda: nc.vector.tensor_copy(ang[:], pi_[:]), reads=[b_pi], writes=[b_an])
            C, b_C = P.sb("C" + name, [128, T], F32)
            Sg, b_S = P.sb("S" + name, [128, T], F32)
            tmp, b_t = P.sb("rtmp", [128, T], F32)
            ni, b_ni = P.sb("rni", [128, T], I32)
            S.op("dve", lambda: nc.vector.tensor_scalar(ang[:], ang[:], rc[:, 0:1], None, op0=ALU.mult), reads=[b_an, b_rc], writes=[b_an])
            for dst_, b_d, offs in ((Sg, b_S, 0.0), (C, b_C, 0.5 * PI)):
                S.op("dve", lambda offs=offs: nc.vector.tensor_scalar(tmp[:], ang[:], offs, 1.0 / (2 * PI), op0=ALU.add, op1=ALU.mult), reads=[b_an], writes=[b_t])
                S.op("dve", lambda: nc.vector.tensor_copy(ni[:], tmp[:]), reads=[b_t], writes=[b_ni])
                S.op("dve", lambda: nc.vector.tensor_copy(tmp[:], ni[:]), reads=[b_ni], writes=[b_t])
                S.op("dve", lambda: nc.vector.scalar_tensor_tensor(tmp[:], tmp[:], -2 * PI, ang[:], ALU.mult, ALU.add), reads=[b_t, b_an], writes=[b_t])
                if offs != 0.0:
                    S.op("dve", lambda offs=offs: nc.vector.tensor_scalar(tmp[:], tmp[:], offs, None, op0=ALU.add), reads=[b_t], writes=[b_t])
                S.op("dve", lambda dst_=dst_: nc.vector.tensor_scalar(dst_[:], tmp[:], PI, 2 * PI, op0=ALU.is_gt, op1=ALU.mult), reads=[b_t], writes=[b_d])
                S.op("dve", lambda dst_=dst_: nc.vector.tensor_tensor(tmp[:], tmp[:], dst_[:], ALU.subtract), reads=[b_t, b_d], writes=[b_t])
                S.op("dve", lambda dst_=dst_: nc.vector.tensor_scalar(dst_[:], tmp[:], -PI, 2 * PI, op0=ALU.is_lt, op1=ALU.mult), reads=[b_t], writes=[b_d])
                S.op("dve", lambda dst_=dst_: nc.vector.tensor_tensor(tmp[:], tmp[:], dst_[:], ALU.add), reads=[b_t, b_d], writes=[b_t])
                S.op("act", lambda dst_=dst_: nc.scalar.activation(dst_[:], tmp[:], AF.Sin), reads=[b_t], writes=[b_d])
            S.op("dve", lambda: nc.vector.tensor_scalar(Sg[:], Sg[:], rc[:, 1:2], float(scale), op0=ALU.mult, op1=ALU.mult),
                 reads=[b_S, b_rc], writes=[b_S])
            if scale != 1.0:
                S.op("dve", lambda: nc.vector.tensor_scalar(C[:], C[:], float(scale), None, op0=ALU.mult), reads=[b_C], writes=[b_C])
            return (C, b_C), (Sg, b_S)

        def rope_proj(P, Wa, ca0, Ws, cs0, nh, Aap, aname, T, KC, CS, dst, dname, rings):
            wr_, ar_, pr_, sr_ = rings
            (C, b_C), (Sg, b_S) = CS
            wa, b_wa = load_w(P, wr_, Wa, KC, ca0, nh * 64)
            ws, b_ws = load_w(P, wr_, Ws, KC, cs0, nh * 64)
            t1, b_t1 = P.sb("rt1", [64, 512], F32)
            t2, b_t2 = P.sb("rt2", [64, 512], F32)
            for tt in range(T // 512):
                at, b_a = load_a(ar_, Aap, aname, KC, tt * 512)
                for h in range(nh):
                    pa, b_pa = pr_.next()
                    pb, b_pb = pr_.next()
                    for k in range(KC):
                        S.op("pe", lambda pa=pa, at=at, h=h, k=k: nc.tensor.matmul(
                            pa[0:64, :], wa[:, k, h * 64:(h + 1) * 64], at[:, k, :], start=(k == 0), stop=(k == KC - 1)),
                            reads=[b_wa, b_a], writes=[b_pa])
                    for k in range(KC):
                        S.op("pe", lambda pb=pb, at=at, h=h, k=k: nc.tensor.matmul(
                            pb[0:64, :], ws[:, k, h * 64:(h + 1) * 64], at[:, k, :], start=(k == 0), stop=(k == KC - 1)),
                            reads=[b_ws, b_a], writes=[b_pb])
                    st, b_s = sr_.next()
                    S.op("dve", lambda pa=pa, tt=tt: nc.vector.tensor_tensor(t1[:], pa[0:64, :], C[0:64, tt * 512:(tt + 1) * 512], ALU.mult),
                         reads=[b_pa, b_C], writes=[b_t1])
                    S.op("dve", lambda pb=pb, tt=tt: nc.vector.tensor_tensor(t2[:], pb[0:64, :], Sg[0:64, tt * 512:(tt + 1) * 512], ALU.mult),
                         reads=[b_pb, b_S], writes=[b_t2])
                    S.op("dve", lambda st=st: nc.vector.tensor_tensor(st[0:64, 0, :], t1[:], t2[:], ALU.add), reads=[b_t1, b_t2], writes=[b_s])
                    S.dma(b_s, [("pool", lambda st=st, h=h, tt=tt: nc.gpsimd.dma_start(
                        out=dst[h * 64:(h + 1) * 64, tt * 512:(tt + 1) * 512], in_=st[0:64, 0, :]))], reads=[b_s], writes=[db(dname)])

        if stage >= 2:
            with phase(nc, S, "B") as P:
                rings = (P.ring_sb("w", [128, 16, 512], BF16, 2), P.ring_sb("a", [128, 16, 512], BF16, 2),
                         P.ring_ps("p", [128, 512], F32, 6), P.ring_sb("st", [128, 4, 512], BF16, 2))
                gemm_fm(P, w_in, 0, 1024, hT_own, "hT_own", T_OWN, 16, QT_sb, "QT_sb", 128 ** -0.5, rings)
                gemm_fm(P, w_in, 1024, 1024, hT_all, "hT_all", S_ALL, 16, KT_sb, "KT_sb", 1.0, rings)
                gemm_tm(P, w_in, 2048, 1024, hT_all, "hT_all", S_ALL, 16, V_sb, "V_sb", rings)
                latent_norm(P, 3072, hT_own, "hT_own", T_OWN, 0, qlnT, "qlnT", rings)
                latent_norm(P, 3584, hT_all, "hT_all", S_ALL, 4, kvlnT, "kvlnT", rings)
            with phase(nc, S, "B2") as P:
                rings = (P.ring_sb("w", [128, 16, 512], BF16, 2), P.ring_sb("a", [128, 16, 512], BF16, 2),
                         P.ring_ps("p", [128, 512], F32, 6), P.ring_sb("st", [128, 4, 512], BF16, 2))
                CSk = rope_tables(P, pos_all, S_ALL, 1.0, "k")
                rope_proj(P, w_kr, 0, w_kr, 64, 1, hT_all, "hT_all", S_ALL, 16, CSk, KrT, "KrT", rings)
            with phase(nc, S, "B3") as P:
                rings = (P.ring_sb("w", [128, 16, 512], BF16, 2), P.ring_sb("a", [128, 16, 512], BF16, 2),
                         P.ring_ps("p", [128, 512], F32, 6), P.ring_sb("st", [128, 4, 512], BF16, 2))
                CSq = rope_tables(P, pos_own, T_OWN, 192 ** -0.5, "q")
                rope_proj(P, w_qra, 0, w_qrs, 0, 8, qlnT, "qlnT", T_OWN, 4, CSq, QrT, "QrT", rings)
                gemm_fm(P, w_qn, 0, 1024, qlnT, "qlnT", T_OWN, 4, QnT, "QnT", 192 ** -0.5, rings)
                gemm_fm(P, w_kn, 0, 1024, kvlnT, "kvlnT", S_ALL, 4, KnT, "KnT", 1.0, rings)
                gemm_tm(P, w_v, 0, 1024, kvlnT, "kvlnT", S_ALL, 4, V_ml, "V_ml", rings)

        def attention(P, is_sb):
            mk, b_mk = P.sb("mk", [128, 4, 128], F32)
            S.dma(b_mk, [("sp", lambda: nc.sync.dma_start(out=mk[:], in_=masks[0 if is_sb else 1].rearrange("z p n -> p z n")))], writes=[b_mk])
            mkb, b_mkb = P.sb("mkb", [128, 4, 128], BF16)
            S.op("dve", lambda: nc.vector.tensor_copy(mkb[:], mk[:]), reads=[b_mk], writes=[b_mkb])
            ktr = P.ring_sb("kt", [128, S_ALL], BF16, 2)
            vr = P.ring_sb("v", [128, 32, 128], BF16, 2)
            qr = P.ring_sb("q", [128, T_OWN], BF16, 2)
            if not is_sb:
                krt, b_krt = P.sb("krt", [64, S_ALL], BF16)
                S.dma(b_krt, [("sp", lambda: nc.sync.dma_start(out=krt[:], in_=KrT))], reads=[db("KrT")], writes=[b_krt])
                qrr = P.ring_sb("qr", [64, T_OWN], BF16, 2)
            pA = P.ring_ps("pA", [128, 512], F32, 2)
            pB = P.ring_ps("pB", [128, 512], F32, 2)
            pO = P.ring_ps("pO", [128, 512], F32, 2)
            if is_sb:
                pC, b_pC = P.ps("pC", [128, 512], F32)
                e32r = P.ring_sb("e32", [128, 512], F32, 2)
                spr = P.ring_sb("sp", [128, 512], BF16, 2)
                Tt, b_T = P.sb("T", [33, 512], F32)
                THL, b_THL = P.sb("THL", [33, 512], BF16)
            else:
                pD = P.ring_ps("pD", [128, 512], F32, 2)
                rdn, b_rdn = P.sb("rdn", [128, 512], F32)
            ar = P.ring_sb("a", [128, 512], BF16, 3)
            mor = P.ring_sb("mo", [128, 512], BF16, 2)
            zer, b_zer = P.sb("zer", [128, 128], BF16)
            S.op("dve", lambda: nc.vector.memset(zer[:], 0.0), writes=[b_zer])
            KT = KT_sb if is_sb else KnT
            QT = QT_sb if is_sb else QnT
            VV = V_sb if is_sb else V_ml
            kn, qn, vn = ("KT_sb", "QT_sb", "V_sb") if is_sb else ("KnT", "QnT", "V_ml")
            for h in range(8):
                kt, b_kt = ktr.next()
                S.dma(b_kt, [("sp", lambda kt=kt, h=h: nc.sync.dma_start(out=kt[:], in_=KT[h * 128:(h + 1) * 128, :]))], reads=[db(kn)], writes=[b_kt])
                vt, b_vt = vr.next()
                S.dma(b_vt, [("sp", lambda vt=vt, h=h: nc.sync.dma_start(
                    out=vt[:], in_=VV.rearrange("(kb p) c -> p kb c", p=128)[:, :, h * 128:(h + 1) * 128]))], reads=[db(vn)], writes=[b_vt])
                qt, b_qt = qr.next()
                S.dma(b_qt, [("sp", lambda qt=qt, h=h: nc.sync.dma_start(out=qt[:], in_=QT[h * 128:(h + 1) * 128, :]))], reads=[db(qn)], writes=[b_qt])
                if not is_sb:
                    qrt, b_qrt = qrr.next()
                    S.dma(b_qrt, [("sp", lambda qrt=qrt, h=h: nc.sync.dma_start(out=qrt[:], in_=QrT[h * 64:(h + 1) * 64, :]))], reads=[db("QrT")], writes=[b_qrt])
                for m in range(4):
                    q0 = m * 512
                    po, b_po = pO.next()
                    S.op("pe", lambda po=po, qt=qt, q0=q0: nc.tensor.matmul(po[:], zer[:], qt[:, q0:q0 + 512], start=True, stop=False),
                         reads=[b_zer, b_qt], writes=[b_po])
                    if is_sb:
                        S.op("dve", lambda: nc.vector.memset(Tt[:], 0.0), writes=[b_T])
                        S.op("dve", lambda: nc.vector.memset(THL[:], 0.0), writes=[b_THL])
                    else:
                        pd, b_pd = pD.next()
                        S.op("pe", lambda pd=pd, qt=qt, q0=q0: nc.tensor.matmul(pd[:], zer[:], qt[:, q0:q0 + 512], start=True, stop=False),
                             reads=[b_zer, b_qt], writes=[b_pd])
                    kmax = 8 * m + 7

                    def geom(kb):
                        smin = 0
                        while 8 * m + 1 + 2 * smin < kb:
                            smin += 1
                        c0 = smin * 128
                        g_ = dict(kb=kb, last=(kb == 0), cs=slice(c0, 512), qs=slice(q0 + c0, q0 + 512), ks=slice(kb * 128, (kb + 1) * 128), bs=None, zone=None)
                        if kb >= 8 * m:
                            r = kb - 8 * m
                            bsl = r // 2
                            g_["bs"] = slice(bsl * 128, (bsl + 1) * 128)
                            g_["zone"] = (bsl % 2) * 2 + (r % 2)
                        return g_

                    def SC(g_):
                        pa, b_pa = pA.next()
                        g_["pa"], g_["b_pa"] = pa, b_pa
                        ks, qs, cs = g_["ks"], g_["qs"], g_["cs"]
                        if is_sb:
                            S.op("pe", lambda: nc.tensor.matmul(pa[:, cs], kt[:, ks], qt[:, qs], start=True, stop=True), reads=[b_kt, b_qt], writes=[b_pa])
                        else:
                            S.op("pe", lambda: nc.tensor.matmul(pa[:, cs], kt[:, ks], qt[:, qs], start=True, stop=False), reads=[b_kt, b_qt], writes=[b_pa])
                            S.op("pe", lambda: nc.tensor.matmul(pa[:, cs], krt[:, ks], qrt[:, qs], start=False, stop=True), reads=[b_krt, b_qrt], writes=[b_pa])

                    def mask_(t_, b_t, g_):
                        if g_["bs"] is not None:
                            bs, zone = g_["bs"], g_["zone"]
                            S.op("pool", lambda: nc.gpsimd.tensor_tensor(t_[:, bs], t_[:, bs], mkb[:, zone, :], ALU.mult), reads=[b_t, b_mkb], writes=[b_t])

                    def AV(g_):
                        at, b_at, cs, kb, last = g_["at"], g_["b_at"], g_["cs"], g_["kb"], g_["last"]
                        S.op("pe", lambda: nc.tensor.matmul(po[:, cs], vt[:, kb, :], at[:, cs], start=False, stop=last), reads=[b_vt, b_at], writes=[b_po])
                        if not is_sb:
                            S.op("pe", lambda: nc.tensor.matmul(pd[:, cs], ones_b, at[:, cs], start=False, stop=last), reads=[b_cstb, b_at], writes=[b_pd])

                    gcur = geom(kmax)
                    SC(gcur)
                    gprev = None
                    for kb in range(kmax, -1, -1):
                        g_ = gcur
                        cs, qs, ks, last = g_["cs"], g_["qs"], g_["ks"], g_["last"]
                        pa, b_pa = g_["pa"], g_["b_pa"]
                        at, b_at = ar.next()
                        g_["at"], g_["b_at"] = at, b_at
                        if is_sb:
                            e32, b_e = e32r.next()
                            spm, b_sp = spr.next()
                            S.op("act", lambda: nc.scalar.activation(e32[:, cs], pa[:, cs], AF.Exp), reads=[b_pa], writes=[b_e])
                            S.op("act", lambda: nc.scalar.activation(spm[:, cs], e32[:, cs], AF.Ln, bias=1.0), reads=[b_e], writes=[b_sp])
                            mask_(spm, b_sp, g_)
                            if kb > 0:
                                gcur = geom(kb - 1)
                                SC(gcur)
                            pb, b_pb = pB.next()
                            S.op("pe", lambda: nc.tensor.matmul(pb[:, cs], kt[:, ks], qt[:, qs], start=True, stop=False), reads=[b_kt, b_qt], writes=[b_pb])
                            S.op("pe", lambda: nc.tensor.matmul(pb[:, cs], negtri_b, spm[:, cs], start=False, stop=False), reads=[b_sp, b_cstb], writes=[b_pb])
                            S.op("pe", lambda: nc.tensor.matmul(pb[:, cs], bc33_b, THL[0:33, cs], start=False, stop=True), reads=[b_THL, b_cstb], writes=[b_pb])
                            S.op("act", lambda: nc.scalar.activation(at[:, cs], pb[:, cs], AF.Exp), reads=[b_pb], writes=[b_at])
                            mask_(at, b_at, g_)
                            if not last:
                                S.op("pe", lambda: nc.tensor.matmul(pC[0:33, cs], negones33_b, spm[:, cs], start=True, stop=True), reads=[b_sp, b_cstb], writes=[b_pC])
                                S.op("dve", lambda: nc.vector.tensor_tensor(Tt[:, cs], Tt[:, cs], pC[0:33, cs], ALU.add), reads=[b_pC, b_T], writes=[b_T])
                                S.op("dve", lambda: nc.vector.tensor_copy(THL[:, cs], Tt[:, cs]), reads=[b_T], writes=[b_THL])
                                S.op("dve", lambda: nc.vector.tensor_tensor(THL[32:33, cs], Tt[32:33, cs], THL[32:33, cs], ALU.subtract), reads=[b_T, b_THL], writes=[b_THL])
                            if gprev is not None:
                                AV(gprev)
                            gprev = g_
                        else:
                            S.op("act", lambda: nc.scalar.activation(at[:, cs], pa[:, cs], AF.Exp), reads=[b_pa], writes=[b_at])
                            mask_(at, b_at, g_)
                            if kb > 0:
                                gcur = geom(kb - 1)
                                SC(gcur)
                            AV(g_)
                    if is_sb:
                        AV(gprev)
                    mo, b_mo = mor.next()
                    if is_sb:
                        S.op("act", lambda po=po, mo=mo: nc.scalar.activation(mo[:], po[:], AF.Copy), reads=[b_po], writes=[b_mo])
                    else:
                        S.op("dve", lambda pd=pd: nc.vector.reciprocal(rdn[:], pd[:]), reads=[b_pd], writes=[b_rdn])
                        S.op("dve", lambda po=po, mo=mo: nc.vector.tensor_tensor(mo[:], po[:], rdn[:], ALU.mult), reads=[b_po, b_rdn], writes=[b_mo])
                    r0 = (0 if is_sb else 1024) + h * 128
                    S.dma(b_mo, [("pool", lambda mo=mo, r0=r0, q0=q0: nc.gpsimd.dma_start(out=mixT[r0:r0 + 128, q0:q0 + 512], in_=mo[:]))],
                          reads=[b_mo], writes=[db("mixT")])

        if stage >= 3:
            with phase(nc, S, "C1") as P:
                attention(P, True)
            with phase(nc, S, "C2") as P:
                attention(P, False)

        if stage >= 4:
            with phase(nc, S, "D") as P:
                g1, b_g1 = P.sb("g1", [128, D], F32)
                S.dma(b_g1, [("sp", lambda: nc.sync.dma_start(out=g1[:], in_=bc_row(mod_flat[:, 32 * 128:48 * 128], D)))], reads=[db("mod_d")], writes=[b_g1])
                wr_ = P.ring_sb("w", [128, 16, 512], BF16, 2)
                ar_ = P.ring_sb("a", [128, 16, 512], BF16, 2)
                pr_ = P.ring_ps("p", [128, 512], F32, 4)
                xor_ = P.ring_sb("xo", [128, 512], F32, 3)
                tr_ = P.ring_sb("t", [128, 512], F32, 3)
                for g in range(4):
                    wt, b_w = load_w(P, wr_, w_out, 16, g * 512, 512)
                    for tt in range(4):
                        at, b_a = load_a(ar_, mixT, "mixT", 16, tt * 512)
                        for j in range(4):
                            ps_, b_p = pr_.next()
                            for k in range(16):
                                S.op("pe", lambda ps_=ps_, wt=wt, at=at, j=j, k=k: nc.tensor.matmul(
                                    ps_[:], at[:, k, j * 128:(j + 1) * 128], wt[:, k, :], start=(k == 0), stop=(k == 15)),
                                    reads=[b_w, b_a], writes=[b_p])
                            rws = slice((tt * 4 + j) * 128, (tt * 4 + j + 1) * 128)
                            cls = slice(g * 512, (g + 1) * 512)
                            xo, b_xo = xor_.next()
                            S.dma(b_xo, [("sp", lambda xo=xo, rws=rws, cls=cls: nc.sync.dma_start(out=xo[:], in_=x_own[rws, cls]))], writes=[b_xo])
                            t_, b_t = tr_.next()
                            S.op("dve", lambda ps_=ps_, t_=t_, cls=cls: nc.vector.tensor_tensor(t_[:], ps_[:], g1[:, cls], ALU.mult), reads=[b_p, b_g1], writes=[b_t])
                            S.op("pool", lambda t_=t_, xo=xo: nc.gpsimd.tensor_tensor(t_[:], t_[:], xo[:], ALU.add), reads=[b_t, b_xo], writes=[b_t])
                            S.dma(b_t, [("pool", lambda t_=t_, rws=rws, cls=cls: nc.gpsimd.dma_start(out=x1_d[rws, cls], in_=t_[:]))], reads=[b_t], writes=[db("x1_d")])


        SMAX = T_OWN // 128
        dest_i, b_di = G.sb("dest_i", [128, 64], I32)
        w4, b_w4 = G.sb("w4", [128, 16, 4], F32)
        cnt_i, b_cni = G.sb("cnt_i", [128, NE], I32)
        idx_i, b_idx = G.sb("idx_i", [128, NE * SMAX], I32)
        if stage >= 5:
            with phase(nc, S, "R") as P:
                gm2, b_gm2 = P.sb("gm2", [128, D], F32)
                sh2, b_sh2 = P.sb("sh2", [128, D], F32)
                tmpD, b_tD = P.sb("tmpD", [128, D], F32)
                S.dma(b_gm2, [("sp", lambda: nc.sync.dma_start(out=gm2[:], in_=bc_row(mod_flat[:, 64 * 128:80 * 128], D)))], reads=[db("mod_d")], writes=[b_gm2])
                S.dma(b_tD, [("sp", lambda: nc.sync.dma_start(out=tmpD[:], in_=bc_row(rows[2:3, :], D)))], writes=[b_tD])
                S.dma(b_sh2, [("sp", lambda: nc.sync.dma_start(out=sh2[:], in_=bc_row(mod_flat[:, 48 * 128:64 * 128], D)))], reads=[db("mod_d")], writes=[b_sh2])
                S.op("dve", lambda: nc.vector.scalar_tensor_tensor(gm2[:], gm2[:], 1.0, tmpD[:], ALU.add, ALU.mult), reads=[b_gm2, b_tD], writes=[b_gm2])
                brt, b_brt = P.sb("brt", [128, NE], F32)
                S.dma(b_brt, [("sp", lambda: nc.sync.dma_start(out=brt[:], in_=bc_row(rows[1:2, 0:NE], NE)))], writes=[b_brt])
                wrs, b_wrs = P.sb("wrs", [128, 16, NE], F32)
                S.dma(b_wrs, [("sp", lambda: nc.sync.dma_start(out=wrs[:], in_=w_router))], writes=[b_wrs])
                h2b, b_h2b = P.sb("h2b", [128, 16 * D], BF16)
                maskf, b_mf = P.sb("maskf", [128, 16, NE], F32)
                wgt, b_wg = P.sb("wgt", [128, 16, NE], F32)
                posf, b_pf = P.sb("posf", [128, 16, NE], F32)
                cntb, b_cn = P.sb("cntb", [128, NE], F32)
                S.op("dve", lambda: nc.vector.memset(cntb[:], 0.0), writes=[b_cn])
                x1r = P.ring_sb("x1", [128, D], F32, 2)
                h2r = P.ring_sb("h2", [128, D], F32, 2)
                h2T, b_h2T = P.sb("h2T", [128, 16, 128], F32)
                sq, b_sq = P.sb("sq", [128, D], BF16)
                sm = P.ring_sb("sm", [128, 64], F32, 2)
                ex, b_ex = P.sb("ex", [128, NE], F32)
                mb, b_mb = P.sb("mb", [128, NE], BF16)
                ptr = P.ring_ps("pt", [128, 512], F32, 2)
                pl, b_pl = P.ps("pl", [128, NE], F32)
                pp, b_pp = P.ps("pp", [128, NE], F32)
                pc, b_pc = P.ps("pc", [128, NE], F32)
                for i in range(16):
                    xb, b_xb = x1r.next()
                    S.dma(b_xb, [("sp", lambda xb=xb, i=i: nc.sync.dma_start(out=xb[:], in_=x1_d[i * 128:(i + 1) * 128, :]))], reads=[db("x1_d")], writes=[b_xb])
                    s_, b_s = sm.next()
                    S.op("act", lambda xb=xb, s_=s_: nc.scalar.activation(sq[:], xb[:], AF.Square, accum_out=s_[:, 0:1]), reads=[b_xb], writes=[b_sq, b_s])
                    S.op("act", lambda s_=s_: nc.scalar.activation(s_[:, 0:1], s_[:, 0:1], AF.Sqrt, bias=epsT[:, 0:1], scale=1.0 / D), reads=[b_s, b_eps], writes=[b_s])
                    S.op("dve", lambda s_=s_: nc.vector.reciprocal(s_[:, 0:1], s_[:, 0:1]), reads=[b_s], writes=[b_s])
                    h2, b_h2 = h2r.next()
                    S.op("dve", lambda xb=xb, h2=h2, s_=s_: nc.vector.scalar_tensor_tensor(h2[:], xb[:], s_[:, 0:1], gm2[:], ALU.mult, ALU.mult), reads=[b_xb, b_s, b_gm2], writes=[b_h2])
                    S.op("pool", lambda h2=h2: nc.gpsimd.tensor_tensor(h2[:], h2[:], sh2[:], ALU.add), reads=[b_h2, b_sh2], writes=[b_h2])
                    S.op("act", lambda h2=h2, i=i: nc.scalar.activation(h2b[:, i * D:(i + 1) * D], h2[:], AF.Copy), reads=[b_h2], writes=[b_h2b])
                    for g in range(4):
                        pt_, b_p = ptr.next()
                        for c in range(4):
                            k = g * 4 + c
                            S.op("pe", lambda pt_=pt_, h2=h2, c=c, k=k: nc.tensor.transpose(pt_[:, c * 128:(c + 1) * 128], h2[:, k * 128:(k + 1) * 128], ident_f),
                                 reads=[b_h2, b_cst], writes=[b_p])
                        S.op("dve", lambda pt_=pt_, g=g: nc.vector.tensor_copy(h2T[:, g * 4:(g + 1) * 4, :], pt_[:].rearrange("p (c n) -> p c n", c=4)), reads=[b_p], writes=[b_h2T])
                    for k in range(16):
                        S.op("pe", lambda k=k: nc.tensor.matmul(pl[:], h2T[:, k, :], wrs[:, k, :], start=(k == 0), stop=(k == 15)), reads=[b_h2T, b_wrs], writes=[b_pl])
                    lg = s_[:, 32:64]
                    S.op("dve", lambda lg=lg: nc.vector.tensor_tensor(lg, pl[:], brt[:], ALU.add), reads=[b_pl, b_brt], writes=[b_s])
                    S.op("dve", lambda s_=s_, lg=lg: nc.vector.max(out=s_[:, 8:16], in_=lg), reads=[b_s], writes=[b_s])
                    S.op("dve", lambda s_=s_: nc.vector.tensor_scalar(s_[:, 16:17], s_[:, 8:9], -1.0, None, op0=ALU.mult), reads=[b_s], writes=[b_s])
                    S.op("dve", lambda s_=s_, lg=lg, i=i: nc.vector.tensor_scalar(maskf[:, i, :], lg, s_[:, 11:12], None, op0=ALU.is_ge), reads=[b_s], writes=[b_mf])
                    S.op("act", lambda s_=s_, lg=lg: nc.scalar.activation(ex[:], lg, AF.Exp, bias=s_[:, 16:17], scale=1.0), reads=[b_s], writes=[b_ex])
                    S.op("dve", lambda i=i: nc.vector.tensor_tensor(ex[:], ex[:], maskf[:, i, :], ALU.mult), reads=[b_ex, b_mf], writes=[b_ex])
                    S.op("dve", lambda s_=s_: nc.vector.reduce_sum(s_[:, 17:18], ex[:], AX.X), reads=[b_ex], writes=[b_s])
                    S.op("dve", lambda s_=s_: nc.vector.reciprocal(s_[:, 17:18], s_[:, 17:18]), reads=[b_s], writes=[b_s])
                    S.op("dve", lambda s_=s_, i=i: nc.vector.tensor_scalar(wgt[:, i, :], ex[:], s_[:, 17:18], None, op0=ALU.mult), reads=[b_ex, b_s], writes=[b_wg])
                    S.op("dve", lambda i=i: nc.vector.tensor_copy(mb[:], maskf[:, i, :]), reads=[b_mf], writes=[b_mb])
                    S.op("pe", lambda: nc.tensor.matmul(pp[:], tri_b, mb[:], start=True, stop=True), reads=[b_mb, b_cstb], writes=[b_pp])
                    S.op("pe", lambda: nc.tensor.matmul(pc[:], ones_b, mb[:], start=True, stop=True), reads=[b_mb, b_cstb], writes=[b_pc])
                    S.op("dve", lambda i=i: nc.vector.tensor_tensor(posf[:, i, :], pp[:], cntb[:], ALU.add), reads=[b_pp, b_cn], writes=[b_pf])
                    S.op("dve", lambda: nc.vector.tensor_tensor(cntb[:], cntb[:], pc[:], ALU.add), reads=[b_pc, b_cn], writes=[b_cn])
                S.op("dve", lambda: nc.vector.tensor_copy(cnt_i[:], cntb[:]), reads=[b_cn], writes=[b_cni])
                q_, b_q = P.sb("q_", [128, NE], F32)
                nf, b_nf = P.sb("nf", [128, NE], F32)
                nI, b_nI = P.sb("nI", [128, NE], I32)
                inc, b_inc = P.sb("inc", [128, NE], F32)
                inc2, b_inc2 = P.sb("inc2", [128, NE], F32)
                base, b_base = P.sb("base", [128, NE], F32)
                S.op("dve", lambda: nc.vector.tensor_scalar(q_[:], cntb[:], float(RB - 1), 1.0 / RB, op0=ALU.add, op1=ALU.mult), reads=[b_cn], writes=[b_q])
                S.op("dve", lambda: nc.vector.tensor_copy(nI[:], q_[:]), reads=[b_q], writes=[b_nI])
                S.op("dve", lambda: nc.vector.tensor_copy(nf[:], nI[:]), reads=[b_nI], writes=[b_nf])
                S.op("dve", lambda: nc.vector.tensor_tensor(inc[:], nf[:], q_[:], ALU.is_gt), reads=[b_nf, b_q], writes=[b_inc])
                S.op("dve", lambda: nc.vector.tensor_tensor(nf[:], nf[:], inc[:], ALU.subtract), reads=[b_nf, b_inc], writes=[b_nf])
                S.op("dve", lambda: nc.vector.tensor_scalar(nf[:], nf[:], float(RB), None, op0=ALU.mult), reads=[b_nf], writes=[b_nf])
                S.op("dve", lambda: nc.vector.tensor_copy(inc[:], nf[:]), reads=[b_nf], writes=[b_inc])
                cur, b_cur, oth, b_oth = inc, b_inc, inc2, b_inc2
                for sh in (1, 2, 4, 8, 16):
                    S.op("dve", lambda cur=cur, oth=oth, sh=sh: nc.vector.tensor_copy(oth[:, 0:sh], cur[:, 0:sh]), reads=[b_cur], writes=[b_oth])
                    S.op("dve", lambda cur=cur, oth=oth, sh=sh: nc.vector.tensor_tensor(oth[:, sh:NE], cur[:, sh:NE], cur[:, 0:NE - sh], ALU.add), reads=[b_cur, b_oth], writes=[b_oth])
                    cur, b_cur, oth, b_oth = oth, b_oth, cur, b_cur
                ends, b_ends = cur, b_cur
                S.op("dve", lambda: nc.vector.tensor_tensor(base[:], ends[:], nf[:], ALU.subtract), reads=[b_ends, b_nf], writes=[b_base])
                idxf, b_idxf = P.sb("idxf", [128, NE * SMAX], F32)
                for e in range(NE):
                    S.op("dve", lambda e=e: nc.vector.tensor_scalar(idxf[:, e * SMAX:(e + 1) * SMAX], cst[:, 7, 0:SMAX], base[:, e:e + 1], None, op0=ALU.add),
                         reads=[b_cst, b_base], writes=[b_idxf])
                S.op("dve", lambda: nc.vector.tensor_scalar(idxf[:], idxf[:], float(NROWS - 1), None, op0=ALU.min), reads=[b_idxf], writes=[b_idxf])
                S.op("dve", lambda: nc.vector.tensor_copy(idx_i[:], idxf[:]), reads=[b_idxf], writes=[b_idx])
                dm, b_dm = P.sb("dm", [128, NE], F32)
                eq, b_eq = P.sb("eq", [128, NE], F32)
                d8r = P.ring_sb("d8", [128, 16], F32, 2)
                for i in range(16):
                    d8, b_d8 = d8r.next()
                    S.op("dve", lambda i=i: nc.vector.tensor_tensor(dm[:], posf[:, i, :], base[:], ALU.add), reads=[b_pf, b_base], writes=[b_dm])
                    S.op("dve", lambda i=i: nc.vector.scalar_tensor_tensor(dm[:], dm[:], 1.0, maskf[:, i, :], ALU.add, ALU.mult), reads=[b_dm, b_mf], writes=[b_dm])
                    S.op("dve", lambda d8=d8: nc.vector.max(out=d8[:, 0:8], in_=dm[:]), reads=[b_dm], writes=[b_d8])
                    S.op("dve", lambda d8=d8: nc.vector.tensor_scalar(d8[:, 8:12], d8[:, 0:4], -1.0, None, op0=ALU.add), reads=[b_d8], writes=[b_d8])
                    S.op("dve", lambda d8=d8, i=i: nc.vector.tensor_copy(dest_i[:, i * 4:i * 4 + 4], d8[:, 8:12]), reads=[b_d8], writes=[b_di])
                    for k in range(4):
                        S.op("dve", lambda d8=d8, k=k: nc.vector.tensor_scalar(eq[:], dm[:], d8[:, k:k + 1], None, op0=ALU.is_equal), reads=[b_dm, b_d8], writes=[b_eq])
                        S.op("dve", lambda i=i: nc.vector.tensor_tensor(eq[:], eq[:], wgt[:, i, :], ALU.mult), reads=[b_eq, b_wg], writes=[b_eq])
                        S.op("dve", lambda i=i, k=k: nc.vector.reduce_sum(w4[:, i, k:k + 1], eq[:], AX.X), reads=[b_eq], writes=[b_w4])
                    for k in range(4):
                        S.dma(b_h2b, [("pool", lambda i=i, k=k: nc.gpsimd.indirect_dma_start(
                            out=xs_d, out_offset=bass.IndirectOffsetOnAxis(ap=dest_i[:, i * 4 + k:i * 4 + k + 1], axis=0), in_=h2b[:, i * D:(i + 1) * D], in_offset=None))],
                            reads=[b_h2b, b_di], writes=[db("xs_d")])

        if stage >= 6:
            with phase(nc, S, "E") as P:
                regs = nc.alloc_registers("cntreg", engines=mybir.ALL_ENGINES)
                engname = {mybir.EngineType.Pool: "pool", mybir.EngineType.Activation: "act", mybir.EngineType.PE: "pe",
                           mybir.EngineType.DVE: "dve", mybir.EngineType.SP: "sp"}
                xsT, b_xsT = P.sb("xsT", [128, 16, 1024], BF16)
                actT, b_act = P.sb("actT", [128, 16, 1024], BF16)
                xrr = P.ring_sb("xr", [128, D], BF16, 3)
                w1r = P.ring_sb("w1", [128, 16, 256], BF16, 6)
                w2r = P.ring_sb("w2", [128, 16, 512], BF16, 2)
                b1r = P.ring_sb("b1", [128, 32], F32, 2)
                b2r = P.ring_sb("b2", [128, D], F32, 2)
                ptb = P.ring_ps("ptb", [128, 1024], BF16, 2)
                pg = P.ring_ps("pg", [128, 512], F32, 2)
                bbr = P.ring_sb("bb", [128, 512], F32, 3)
                glr = P.ring_sb("gl", [128, 512], F32, 2)
                abr = P.ring_sb("ab", [128, 256], BF16, 3)
                py = P.ring_ps("py", [128, 512], F32, 2)
                sg, b_sg = P.sb("sg", [128, 256], F32)
                yr = P.ring_sb("y", [128, 512], F32, 3)
                def emit_tr(s_j, ab_, b_ab, rs_, pc_):
                    with S.guard(regs, s_j * 128):
                        pt_, b_p = ptb.next()
                        for c in range(2):
                            S.op("pe", lambda pt_=pt_, ab_=ab_, c=c: nc.tensor.transpose(pt_[:, c * 128:(c + 1) * 128], ab_[:, c * 128:(c + 1) * 128], ident_b),
                                 reads=[b_ab, b_cstb], writes=[b_p])
                        S.op("act", lambda pt_=pt_, pc_=pc_, rs_=rs_: nc.scalar.activation(
                            actT[:, pc_ * 2:pc_ * 2 + 2, rs_], pt_[:, 0:256].rearrange("p (c n) -> p c n", c=2), AF.Copy), reads=[b_p], writes=[b_act])

                import os as _os
                for e in range(int(_os.environ.get('K_NEXP', NE))):
                    for reg in regs:
                        S.op(engname[reg.engine], lambda reg=reg, e=e: nc.reg_load(reg, cnt_i[0:1, e:e + 1]), reads=[b_cni])
                    b1t, b_b1 = b1r.next()
                    S.dma(b_b1, [("sp", lambda b1t=b1t, e=e: nc.sync.dma_start(out=b1t[:, 0:16], in_=b1g[e])),
                                 ("sp", lambda b1t=b1t, e=e: nc.sync.dma_start(out=b1t[:, 16:32], in_=b1l[e]))], writes=[b_b1])
                    b2t, b_b2 = b2r.next()
                    S.dma(b_b2, [("sp", lambda b2t=b2t, e=e: nc.sync.dma_start(out=b2t[:], in_=b2[e].to_broadcast([128, D])))], writes=[b_b2])
                    ESECT = 7
                    for ps in range(2):
                      with (S.guard(regs, 1024) if ps else contextlib.nullcontext()):
                            for s_i in range(ps * 8, ps * 8 + 8):
                                with S.guard(regs, s_i * 128):
                                    xr_, b_xr = xrr.next()
                                    col = e * SMAX + s_i
                                    S.dma(b_xr, [("pool", lambda xr_=xr_, col=col: nc.gpsimd.indirect_dma_start(
                                        out=xr_[:], out_offset=None, in_=xs_d, in_offset=bass.IndirectOffsetOnAxis(ap=idx_i[:, col:col + 1], axis=0)))],
                                        reads=[db("xs_d"), b_idx], writes=[b_xr])
                                    for g in range(2):
                                        pt_, b_p = ptb.next()
                                        for c in range(8):
                                            k = g * 8 + c
                                            S.op("pe", lambda pt_=pt_, xr_=xr_, c=c, k=k: nc.tensor.transpose(pt_[:, c * 128:(c + 1) * 128], xr_[:, k * 128:(k + 1) * 128], ident_b),
                                                 reads=[b_xr, b_cstb], writes=[b_p])
                                        S.op("act", lambda pt_=pt_, g=g, s_i=s_i: nc.scalar.activation(
                                            xsT[:, g * 8:(g + 1) * 8, (s_i - ps * 8) * 128:(s_i - ps * 8 + 1) * 128], pt_[:].rearrange("p (c n) -> p c n", c=8), AF.Copy), reads=[b_p], writes=[b_xsT])
                            for pc_ in range(8 if ESECT & 2 else 0):
                                wg_, b_wg_ = w1r.next()
                                wl_, b_wl_ = w1r.next()
                                cs_ = slice(pc_ * 256, (pc_ + 1) * 256)
                                S.dma(b_wg_, [("pool", lambda wg_=wg_, e=e, cs_=cs_: nc.gpsimd.dma_start(out=wg_[:], in_=w1g[e].rearrange("(k p) n -> p k n", p=128)[:, :, cs_]))], writes=[b_wg_])
                                S.dma(b_wl_, [("pool", lambda wl_=wl_, e=e, cs_=cs_: nc.gpsimd.dma_start(out=wl_[:], in_=w1l[e].rearrange("(k p) n -> p k n", p=128)[:, :, cs_]))], writes=[b_wl_])
                                bb_, b_bb = bbr.next()
                                S.dma(b_bb, [("sp", lambda bb_=bb_, e=e, cs_=cs_: nc.sync.dma_start(out=bb_[:, 0:256], in_=b1g_r[e][:, cs_].to_broadcast([128, 256]))),
                                             ("sp", lambda bb_=bb_, e=e, cs_=cs_: nc.sync.dma_start(out=bb_[:, 256:512], in_=b1l_r[e][:, cs_].to_broadcast([128, 256])))], writes=[b_bb])
                                pend = None
                                for s_i in range(ps * 8, ps * 8 + 8):
                                    rs_ = slice((s_i - ps * 8) * 128, (s_i - ps * 8 + 1) * 128)
                                    pend_new = None
                                    with S.guard(regs, s_i * 128):
                                        pg_, b_pg = pg.next()
                                        for k in range(16):
                                            S.op("pe", lambda pg_=pg_, wg_=wg_, k=k, rs_=rs_: nc.tensor.matmul(pg_[:, 0:256], xsT[:, k, rs_], wg_[:, k, :], start=(k == 0), stop=(k == 15)),
                                                 reads=[b_wg_, b_xsT], writes=[b_pg])
                                        for k in range(16):
                                            S.op("pe", lambda pg_=pg_, wl_=wl_, k=k, rs_=rs_: nc.tensor.matmul(pg_[:, 256:512], xsT[:, k, rs_], wl_[:, k, :], start=(k == 0), stop=(k == 15)),
                                                 reads=[b_wl_, b_xsT], writes=[b_pg])
                                        gl_, b_gl_ = glr.next()
                                        S.op("dve", lambda pg_=pg_, gl_=gl_, bb_=bb_: nc.vector.tensor_tensor(gl_[:], pg_[:], bb_[:], ALU.add), reads=[b_pg, b_bb], writes=[b_gl_])
                                        S.op("dve", lambda gl_=gl_: nc.vector.tensor_scalar(gl_[:], gl_[:], 7.0, None, op0=ALU.min), reads=[b_gl_], writes=[b_gl_])
                                        S.op("act", lambda gl_=gl_: nc.scalar.activation(sg[:], gl_[:, 0:256], AF.Sigmoid, scale=1.702), reads=[b_gl_], writes=[b_sg])
                                        S.op("dve", lambda gl_=gl_: nc.vector.tensor_scalar(gl_[:, 256:512], gl_[:, 256:512], -7.0, None, op0=ALU.max), reads=[b_gl_], writes=[b_gl_])
                                        S.op("dve", lambda gl_=gl_: nc.vector.tensor_tensor(sg[:], sg[:], gl_[:, 0:256], ALU.mult), reads=[b_gl_, b_sg], writes=[b_sg])
                                        ab_, b_ab = abr.next()
                                        S.op("dve", lambda gl_=gl_, ab_=ab_: nc.vector.scalar_tensor_tensor(ab_[:], gl_[:, 256:512], 1.0, sg[:], ALU.add, ALU.mult), reads=[b_gl_, b_sg], writes=[b_ab])
                                        pend_new = (s_i, ab_, b_ab, rs_)
                                    if pend is not None:
                                        emit_tr(*pend, pc_)
                                    pend = pend_new
                                if pend is not None:
                                    emit_tr(*pend, pc_)
                                    pend = None
                            for np_ in range(4 if ESECT & 4 else 0):
                                w2t, b_w2 = w2r.next()
                                cs_ = slice(np_ * 512, (np_ + 1) * 512)
                                S.dma(b_w2, [("pool", lambda w2t=w2t, e=e, cs_=cs_: nc.gpsimd.dma_start(out=w2t[:], in_=w2[e].rearrange("(k p) n -> p k n", p=128)[:, :, cs_]))], writes=[b_w2])
                                for s_i in range(ps * 8, ps * 8 + 8):
                                    rs_ = slice((s_i - ps * 8) * 128, (s_i - ps * 8 + 1) * 128)
                                    with S.guard(regs, s_i * 128):
                                        py_, b_py = py.next()
                                        for k in range(16):
                                            S.op("pe", lambda py_=py_, w2t=w2t, rs_=rs_, k=k: nc.tensor.matmul(py_[:], actT[:, k, rs_], w2t[:, k, :], start=(k == 0), stop=(k == 15)),
                                                 reads=[b_act, b_w2], writes=[b_py])
                                        y_, b_y = yr.next()
                                        S.op("dve", lambda py_=py_, y_=y_, b2t=b2t, cs_=cs_: nc.vector.tensor_tensor(y_[:], py_[:], b2t[:, cs_], ALU.add), reads=[b_py, b_b2], writes=[b_y])
                                        col = e * SMAX + s_i
                                        S.dma(b_y, [("pool", lambda y_=y_, col=col, np_=np_: nc.gpsimd.indirect_dma_start(
                                            out=ys_p[np_], out_offset=bass.IndirectOffsetOnAxis(ap=idx_i[:, col:col + 1], axis=0), in_=y_[:], in_offset=None))],
                                            reads=[b_y, b_idx], writes=[db("ys_d")])


        if stage >= 7:
            with phase(nc, S, "F") as P:
                g2, b_g2 = P.sb("g2", [128, D], F32)
                gf, b_gf = P.sb("gf", [128, D], F32)
                S.dma(b_g2, [("sp", lambda: nc.sync.dma_start(out=g2[:], in_=bc_row(mod_flat[:, 80 * 128:96 * 128], D)))], reads=[db("mod_d")], writes=[b_g2])
                S.dma(b_gf, [("sp", lambda: nc.sync.dma_start(out=gf[:], in_=bc_row(rows[0:1, :], D)))], writes=[b_gf])
                gr = P.ring_sb("ga", [128, D], F32, 4)
                accr = P.ring_sb("acc", [128, D], F32, 2)
                x1r = P.ring_sb("x1", [128, D], F32, 2)
                sq, b_sq = P.sb("sq", [128, D], BF16)
                ssr = P.ring_sb("ss", [128, 1], F32, 2)
                for i in range(16):
                    xb, b_xb = x1r.next()
                    S.dma(b_xb, [("sp", lambda xb=xb, i=i: nc.sync.dma_start(out=xb[:], in_=x1_d[i * 128:(i + 1) * 128, :]))], reads=[db("x1_d")], writes=[b_xb])
                    acc, b_acc = accr.next()
                    for k in range(4):
                        ga, b_ga = gr.next()
                        S.dma(b_ga, [("pool", lambda ga=ga, i=i, k=k, q=q: nc.gpsimd.indirect_dma_start(
                            out=ga[:, q * 512:(q + 1) * 512], out_offset=None, in_=ys_p[q], in_offset=bass.IndirectOffsetOnAxis(ap=dest_i[:, i * 4 + k:i * 4 + k + 1], axis=0)))
                            for q in range(4)], reads=[db("ys_d"), b_di], writes=[b_ga])
                        if k == 0:
                            S.op("dve", lambda ga=ga, acc=acc, i=i, k=k: nc.vector.tensor_scalar(acc[:], ga[:], w4[:, i, k:k + 1], None, op0=ALU.mult), reads=[b_ga, b_w4], writes=[b_acc])
                        else:
                            S.op("dve", lambda ga=ga, acc=acc, i=i, k=k: nc.vector.scalar_tensor_tensor(acc[:], ga[:], w4[:, i, k:k + 1], acc[:], ALU.mult, ALU.add), reads=[b_ga, b_w4, b_acc], writes=[b_acc])
                    S.op("pool", lambda acc=acc: nc.gpsimd.tensor_tensor(acc[:], acc[:], g2[:], ALU.mult), reads=[b_acc, b_g2], writes=[b_acc])
                    S.op("dve", lambda acc=acc, xb=xb: nc.vector.tensor_tensor(acc[:], acc[:], xb[:], ALU.add), reads=[b_acc, b_xb], writes=[b_acc])
                    ss, b_ss = ssr.next()
                    S.op("act", lambda acc=acc, ss=ss: nc.scalar.activation(sq[:], acc[:], AF.Square, accum_out=ss[:, 0:1]), reads=[b_acc], writes=[b_sq, b_ss])
                    S.op("act", lambda ss=ss: nc.scalar.activation(ss[:], ss[:], AF.Sqrt, bias=epsT[:, 0:1], scale=1.0 / D), reads=[b_ss, b_eps], writes=[b_ss])
                    S.op("dve", lambda ss=ss: nc.vector.reciprocal(ss[:], ss[:]), reads=[b_ss], writes=[b_ss])
                    S.op("dve", lambda acc=acc, ss=ss: nc.vector.scalar_tensor_tensor(acc[:], acc[:], ss[:, 0:1], gf[:], ALU.mult, ALU.mult), reads=[b_acc, b_ss, b_gf], writes=[b_acc])
                    S.dma(b_acc, [("act", lambda acc=acc, i=i: nc.scalar.dma_start(out=out[i * 128:(i + 1) * 128, :], in_=acc[:]))], reads=[b_acc], writes=[db("out")])

        if dbg and stage <= 4:
            with phase(nc, S, "DBG") as P:
                t_, b_t = P.sb("dbg", [128, 16, D], F32) if False else (None, None)
                r = P.ring_sb("r", [128, D], F32, 2)
                for i in range(16):
                    t_, b_t = r.next()
                    S.dma(b_t, [("sp", lambda t_=t_, i=i: nc.sync.dma_start(out=t_[:], in_=x1_d[i * 128:(i + 1) * 128, :]))], reads=[db("x1_d")], writes=[b_t])
                    S.dma(b_t, [("sp", lambda t_=t_, i=i: nc.sync.dma_start(out=dbg_out[i * 128:(i + 1) * 128, :], in_=t_[:]))], reads=[b_t], writes=[db("dbg")])

        S.barrier()
    return nc


def own_blocks(p):
    blks = []
    for m in range(8):
        blks += [4 * m + (0 if p == 0 else 1), 4 * m + (3 if p == 0 else 2)]
    return blks


def host_prepare(inp):
    f32 = np.float32
    x = np.asarray(inp["x"], f32)
    c = np.asarray(inp["c"], f32)
    pos = np.asarray(inp["positions"], np.int32)
    g_attn = np.asarray(inp["g_attn"], f32)[0]
    g_ffn = np.asarray(inp["g_ffn"], f32)[0]
    b_mod = np.asarray(inp["b_mod"], f32)[0]
    w_in = np.asarray(inp["w_in"], f32)[0]
    w_q_up = np.asarray(inp["w_q_up"], f32)[0].reshape(512, 8, 192)
    w_kv_up = np.asarray(inp["w_kv_up"], f32)[0].reshape(512, 8, 256)
    fm = lambda v: np.ascontiguousarray(v.reshape(-1, 128).T)
    vecs = np.concatenate([fm(g_attn), fm(g_ffn), fm(b_mod)], axis=1).astype(f32)
    rows = np.zeros((4, D), f32)
    rows[0] = np.asarray(inp["g_final"], f32)
    rows[1, :NE] = np.asarray(inp["b_router"], f32)[0]
    rows[2] = g_ffn
    glat = np.concatenate([fm(np.asarray(inp["g_q_lat"], f32)[0]), fm(np.asarray(inp["g_kv_lat"], f32)[0])], axis=1).astype(f32)
    half = 32
    invf = (1.0 / (np.float32(10000.0) ** (np.arange(half, dtype=f32) * f32(2.0) / f32(64)))).astype(f32)
    ropec = np.zeros((128, 4), f32)
    for p_ in range(128):
        i = p_ % 64
        ropec[p_, 0] = invf[i % 32]
        ropec[p_, 1] = -1.0 if i < 32 else 1.0
    ropec[:, 2] = np.pi
    ropec[:, 3] = 1.5 * np.pi
    kk = np.arange(128)[:, None]
    qq = np.arange(128)[None, :]
    ident = np.eye(128, dtype=f32)
    negtri = -(kk >= qq).astype(f32)
    ones = np.ones((128, 128), f32)
    tri_s = (kk < qq).astype(f32)
    negones33 = np.zeros((128, 128), f32)
    negones33[:, :33] = -1.0
    bc33 = np.zeros((128, 128), f32)
    bc33[0, :] = 1.0
    bc33[32, :] = 1.0
    blkstart = np.tile((np.arange(128, dtype=f32) * RB)[None, :], (128, 1))
    off16 = (np.arange(128, dtype=f32)[None, :] % 16) * 128 + np.arange(128, dtype=f32)[:, None]
    consts = np.stack([ident, negtri, ones, tri_s, negones33, bc33, blkstart, off16]).astype(f32)
    TRI_SB = (kk < qq).astype(f32)
    TRI_ML = (kk <= qq).astype(f32)
    ONE = np.ones((128, 128), f32)
    ZERO = np.zeros((128, 128), f32)
    kr = w_in[:, 4096:4160]
    w_kr = np.ascontiguousarray(np.concatenate([kr, kr[:, 32:], kr[:, :32]], axis=1))
    w_qn = np.ascontiguousarray(w_q_up[:, :, :128].reshape(512, 1024))
    qr_ = w_q_up[:, :, 128:]
    w_qra = np.ascontiguousarray(qr_.reshape(512, 512))
    w_qrs = np.ascontiguousarray(np.concatenate([qr_[:, :, 32:], qr_[:, :, :32]], axis=2).reshape(512, 512))
    w_kn = np.ascontiguousarray(w_kv_up[:, :, :128].reshape(512, 1024))
    w_v = np.ascontiguousarray(w_kv_up[:, :, 128:].reshape(512, 1024))
    w1 = np.asarray(inp["w1"], f32)[0]
    b1 = np.asarray(inp["b1"], f32)[0]
    shared = dict(
        vecs=vecs, rows=rows, glat=glat, ropec=ropec, consts=consts,
        w_mod=np.asarray(inp["w_mod"], f32)[0], w_in=w_in, w_kr=w_kr, w_qn=w_qn, w_qra=w_qra, w_qrs=w_qrs,
        w_kn=w_kn, w_v=w_v, w_out=np.asarray(inp["w_out"], f32)[0],
        w_router=np.ascontiguousarray(np.asarray(inp["w_router"], f32)[0].reshape(16, 128, NE).transpose(1, 0, 2)),
        w1g=np.ascontiguousarray(w1[:, :, 0::2]), w1l=np.ascontiguousarray(w1[:, :, 1::2]),
        b1g=np.ascontiguousarray(b1[:, 0::2].reshape(NE, 16, 128).transpose(0, 2, 1)),
        b1l=np.ascontiguousarray(b1[:, 1::2].reshape(NE, 16, 128).transpose(0, 2, 1)),
        b1g_r=np.ascontiguousarray(b1[:, 0::2].reshape(NE, 1, D)), b1l_r=np.ascontiguousarray(b1[:, 1::2].reshape(NE, 1, D)),
        w2=np.asarray(inp["w2"], f32)[0], b2=np.ascontiguousarray(np.asarray(inp["b2"], f32)[0].reshape(NE, 1, D)),
    )
    in_maps = []
    for core in range(8):
        b, p = core // 2, core % 2
        blks = own_blocks(p)
        rowsel = np.concatenate([np.arange(k * 128, (k + 1) * 128) for k in blks])
        if p == 0:
            zs = [(TRI_SB, TRI_ML), (ZERO, ZERO), (ONE, ONE), (TRI_SB, TRI_ML)]
        else:
            zs = [(ONE, ONE), (TRI_SB, TRI_ML), (TRI_SB, TRI_ML), (ZERO, ZERO)]
        masks = np.stack([np.stack([z[0] for z in zs]), np.stack([z[1] for z in zs])]).astype(f32)
        m = dict(shared)
        m.update(
            x_all=np.ascontiguousarray(x[b]), x_own=np.ascontiguousarray(x[b][rowsel]),
            pos_all=np.ascontiguousarray(pos[b][None, :]), pos_own=np.ascontiguousarray(pos[b][rowsel][None, :]),
            cT=fm(c[b]), masks=masks,
        )
        in_maps.append(m)
    return in_maps


_CACHE = {}


def kernel(**inp):
    in_maps = host_prepare(inp)
    if "nc" not in _CACHE:
        _CACHE["nc"] = build_program()
    res = run_bass_kernel_spmd(_CACHE["nc"], in_maps, core_ids=list(range(8)))
    outp = np.zeros((4, S_ALL, D), np.float32)
    for core in range(8):
        b, p = core // 2, core % 2
        o = res.results[core]["out"]
        for i, k in enumerate(own_blocks(p)):
            outp[b, k * 128:(k + 1) * 128] = o[i * 128:(i + 1) * 128]
    return outp
```
